# Optimizing a Trainium2 kernel written in Bass

```python
import math
import jax
import jax.numpy as jnp
from jax import lax
import numpy as np

D_MODEL = 1024
BATCH = 32
SEQ = 2048
DEPTH = 2
DEC_BATCH = 32
DEC_SEQ = 16
PAST_LEN = 2048

CHUNK = 64
Q_BLOCK = 128
N_BRANCH = 4
BRANCH_W = D_MODEL // N_BRANCH
HEAD_DIM = 64
RET_HEADS = BRANCH_W // HEAD_DIM
RET_THETA = 10000.0
FOX_HEADS = BRANCH_W // HEAD_DIM
DIFF_HEADS = BRANCH_W // HEAD_DIM
DIFF_SUB = HEAD_DIM // 2
ROPE_THETA = 500000.0
ROT_DIM = DIFF_SUB // 4
CONV_W = 31
D_FF = 2816
N_EXPERTS = 8
TOP_K = 2
D_EXPERT = 3584
ALPHA = (2.0 * DEPTH) ** 0.25
BETA = (8.0 * DEPTH) ** -0.25
N_DENSE = (DEPTH + 1) // 2
N_MOE = DEPTH // 2
EPS = 1e-5
NEG = -1e30
FORGET_BIAS_MEAN = 2.0

OFF_RET_Q = 0
OFF_RET_K = OFF_RET_Q + BRANCH_W
OFF_RET_V = OFF_RET_K + BRANCH_W
OFF_RET_G = OFF_RET_V + BRANCH_W
OFF_FOX_Q = OFF_RET_G + BRANCH_W
OFF_FOX_K = OFF_FOX_Q + BRANCH_W
OFF_FOX_V = OFF_FOX_K + BRANCH_W
OFF_FOX_F = OFF_FOX_V + BRANCH_W
OFF_CONV = OFF_FOX_F + FOX_HEADS
OFF_DIFF_Q = OFF_CONV + 2 * BRANCH_W
OFF_DIFF_K = OFF_DIFF_Q + BRANCH_W
OFF_DIFF_V = OFF_DIFF_K + BRANCH_W
OFF_GATE = OFF_DIFF_V + BRANCH_W
N_IN = OFF_GATE + N_BRANCH * D_MODEL

kernel_name = 'hybrid_chunk_stream_encoder_step'


def layer_norm(x, g, b):
    xf = x.astype(jnp.float32)
    mu = jnp.mean(xf, -1, keepdims=True)
    var = jnp.mean(jnp.square(xf - mu), -1, keepdims=True)
    return ((xf - mu) * lax.rsqrt(var + EPS) * g.astype(jnp.float32) + b.astype(jnp.float32)).astype(x.dtype)


def head_norm(x):
    xf = x.astype(jnp.float32)
    mu = jnp.mean(xf, -1, keepdims=True)
    var = jnp.mean(jnp.square(xf - mu), -1, keepdims=True)
    return ((xf - mu) * lax.rsqrt(var + EPS)).astype(x.dtype)


def rms_norm(x, g):
    xf = x.astype(jnp.float32)
    ms = jnp.mean(jnp.square(xf), -1, keepdims=True)
    return (xf * lax.rsqrt(ms + EPS) * g.astype(jnp.float32)).astype(x.dtype)


def rope(x, pos, rot_dim, theta):
    half = rot_dim // 2
    inv_freq = jnp.exp(-math.log(theta) * jnp.arange(half, dtype=jnp.float32) / half)
    ang = pos.astype(jnp.float32)[:, None] * inv_freq[None, :]
    cos = jnp.cos(ang)[None, :, None, :].astype(x.dtype)
    sin = jnp.sin(ang)[None, :, None, :].astype(x.dtype)
    x1, x2, rest = x[..., :half], x[..., half:rot_dim], x[..., rot_dim:]
    return jnp.concatenate([x1 * cos - x2 * sin, x2 * cos + x1 * sin, rest], axis=-1)


def over_query_blocks(fn, *qs):
    t = qs[0].shape[1]
    if t <= Q_BLOCK:
        return fn(*qs)
    nb = t // Q_BLOCK
    blocks = tuple(jnp.moveaxis(a.reshape(a.shape[0], nb, Q_BLOCK, *a.shape[2:]), 1, 0) for a in qs)
    out = lax.map(lambda args: fn(*args), blocks)
    out = jnp.moveaxis(out, 0, 1)
    return out.reshape(out.shape[0], t, *out.shape[3:])


def retention(q, k, v, s0):
    b, t, h, dk = q.shape
    dv = v.shape[-1]
    L = min(t, CHUNK)
    nc = t // L
    dt = q.dtype
    log_g = jnp.log1p(-jnp.exp2(-5.0 - jnp.arange(h, dtype=jnp.float32)))
    idx = jnp.arange(L, dtype=jnp.float32)
    inner = jnp.exp(log_g[:, None, None] * jnp.abs(idx[:, None] - idx[None, :])).astype(dt)
    q_dec = jnp.exp(log_g[:, None] * (idx[None, :] + 1.0)).astype(dt)
    k_dec = jnp.exp(log_g[:, None] * (L - 1.0 - idx[None, :])).astype(dt)
    chunk_dec = jnp.exp(log_g * L)[:, None, None]
    qc = q.reshape(b, nc, L, h, dk)
    kc = k.reshape(b, nc, L, h, dk)
    vc = v.reshape(b, nc, L, h, dv)
    scores = jnp.einsum('bclhd,bcmhd->bchlm', qc, kc) * inner
    y_in = jnp.einsum('bchlm,bcmhe->bclhe', scores, vc)
    kv = jnp.einsum('bcmhd,hm,bcmhe->cbhde', kc, k_dec, vc).astype(jnp.float32)

    def step(s, kv_c):
        return chunk_dec * s + kv_c, s

    s_last, s_prev = lax.scan(step, s0.astype(jnp.float32), kv)
    y_x = jnp.einsum('bclhd,hl,cbhde->bclhe', qc, q_dec, s_prev.astype(dt))
    return (y_in + y_x).reshape(b, t, h, dv), s_last.astype(s0.dtype)


def fox_attend(q, fq, qpos, k, v, fk, kpos):
    s = jnp.einsum('bqhd,bkhd->bhqk', q, k).astype(jnp.float32) * (q.shape[-1] ** -0.5)
    s = s + jnp.moveaxis(fq, -1, 1)[..., :, None] - jnp.moveaxis(fk, -1, 1)[..., None, :]
    vis = kpos[None, :] <= qpos[:, None]
    p = jax.nn.softmax(jnp.where(vis, s, NEG), axis=-1)
    return jnp.einsum('bhqk,bkhd->bqhd', p.astype(v.dtype), v)


def diff_attend(q, qpos, k, v, lam, kpos):
    b, tq, n, ds = q.shape
    h = v.shape[2]
    s = jnp.einsum('bqnd,bknd->bnqk', q, k).astype(jnp.float32) * (ds ** -0.5)
    vis = (kpos[None, :] // CHUNK) <= (qpos[:, None] // CHUNK)
    p = jax.nn.softmax(jnp.where(vis, s, NEG), axis=-1).reshape(b, h, 2, tq, -1)
    a = p[:, :, 0] - lam * p[:, :, 1]
    return jnp.einsum('bhqk,bkhe->bqhe', a.astype(v.dtype), v)


def token_mixers(u, pos, past, lam_init, w_in, b_fox_f, w_conv, b_conv, conv_ln_g, conv_ln_b,
                 diff_lambda, diff_subln_g, w_branch, w_out):
    b, t, _ = u.shape
    dt = u.dtype
    proj = u @ w_in

    def cols(off, n):
        return proj[..., off:off + n]

    def heads(a, h):
        return a.reshape(b, t, h, -1)

    rq = rope(heads(cols(OFF_RET_Q, BRANCH_W), RET_HEADS), pos, HEAD_DIM, RET_THETA)
    rk = rope(heads(cols(OFF_RET_K, BRANCH_W), RET_HEADS), pos, HEAD_DIM, RET_THETA) * (HEAD_DIM ** -0.5)
    rv = heads(cols(OFF_RET_V, BRANCH_W), RET_HEADS)
    s0 = jnp.zeros((b, RET_HEADS, HEAD_DIM, HEAD_DIM), dt) if past is None else past[5]
    ry, ret_state = retention(rq, rk, rv, s0)
    h_ret = (head_norm(ry) * jax.nn.silu(heads(cols(OFF_RET_G, BRANCH_W), RET_HEADS))).reshape(b, t, BRANCH_W)

    fq = heads(cols(OFF_FOX_Q, BRANCH_W), FOX_HEADS)
    fk = heads(cols(OFF_FOX_K, BRANCH_W), FOX_HEADS)
    fv = heads(cols(OFF_FOX_V, BRANCH_W), FOX_HEADS)
    lf = jax.nn.log_sigmoid(cols(OFF_FOX_F, FOX_HEADS).astype(jnp.float32) + b_fox_f.astype(jnp.float32))
    if past is None:
        fk_all, fv_all, lf_all = fk, fv, lf
    else:
        fk_all = jnp.concatenate([past[0], fk], axis=1)
        fv_all = jnp.concatenate([past[1], fv], axis=1)
        lf_all = jnp.concatenate([past[2].astype(jnp.float32), lf], axis=1)
    kpos = jnp.arange(fk_all.shape[1], dtype=jnp.int32)
    f_cum = jnp.cumsum(lf_all, axis=1)
    h_fox = over_query_blocks(
        lambda qb, fb, pb: fox_attend(qb, fb, pb[0], fk_all, fv_all, f_cum, kpos),
        fq, f_cum[:, -t:], pos[None]).reshape(b, t, BRANCH_W)

    cu = cols(OFF_CONV, 2 * BRANCH_W)
    glu = cu[..., :BRANCH_W] * jax.nn.sigmoid(cu[..., BRANCH_W:])
    buf = jnp.zeros((b, CONV_W - 1, BRANCH_W), dt) if past is None else past[6]
    xpad = jnp.concatenate([buf, glu], axis=1)
    conv = lax.conv_general_dilated(xpad, w_conv[:, None, :], window_strides=(1,), padding='VALID',
                                    dimension_numbers=('NWC', 'WIO', 'NWC'),
                                    feature_group_count=BRANCH_W) + b_conv
    h_conv = jax.nn.silu(layer_norm(conv, conv_ln_g, conv_ln_b))
    conv_buf = xpad[:, -(CONV_W - 1):]

    dq = rope(cols(OFF_DIFF_Q, BRANCH_W).reshape(b, t, 2 * DIFF_HEADS, DIFF_SUB), pos, ROT_DIM, ROPE_THETA)
    dk = rope(cols(OFF_DIFF_K, BRANCH_W).reshape(b, t, 2 * DIFF_HEADS, DIFF_SUB), pos, ROT_DIM, ROPE_THETA)
    dv = heads(cols(OFF_DIFF_V, BRANCH_W), DIFF_HEADS)
    lamf = diff_lambda.astype(jnp.float32)
    lam = jnp.exp(jnp.sum(lamf[0] * lamf[1])) - jnp.exp(jnp.sum(lamf[2] * lamf[3])) + lam_init
    if past is None:
        dk_all, dv_all = dk, dv
    else:
        dk_all = jnp.concatenate([past[3], dk], axis=1)
        dv_all = jnp.concatenate([past[4], dv], axis=1)
    dkpos = jnp.arange(dk_all.shape[1], dtype=jnp.int32)
    dy = over_query_blocks(lambda qb, pb: diff_attend(qb, pb[0], dk_all, dv_all, lam, dkpos), dq, pos[None])
    h_diff = (rms_norm(dy, diff_subln_g) * (1.0 - lam_init)).reshape(b, t, BRANCH_W)

    branches = (h_ret, h_fox, h_conv, h_diff)
    merged = jax.nn.sigmoid(cols(OFF_GATE, D_MODEL)) * (branches[0] @ w_branch[0])
    for n in range(1, N_BRANCH):
        merged = merged + jax.nn.sigmoid(cols(OFF_GATE + n * D_MODEL, D_MODEL)) * (branches[n] @ w_branch[n])
    out = merged @ w_out
    new_state = (fk, fv, lf.astype(dt), dk, dv, ret_state, conv_buf)
    return out, new_state


def swiglu(x, w_up, w_down):
    a, g = jnp.split(x @ w_up, 2, axis=-1)
    return (jax.nn.silu(a) * g) @ w_down


def moe_swiglu(x, w_router, b_router, w_exp_in, w_exp_out):
    logits = (x @ w_router).astype(jnp.float32) + b_router.astype(jnp.float32)
    top_v, top_i = lax.top_k(logits, TOP_K)
    wts = jax.nn.softmax(top_v, axis=-1)
    combine = jnp.sum(jax.nn.one_hot(top_i, N_EXPERTS, dtype=jnp.float32) * wts[..., None], axis=-2)
    y = combine[..., 0:1].astype(x.dtype) * swiglu(x, w_exp_in[0], w_exp_out[0])
    for e in range(1, N_EXPERTS):
        y = y + combine[..., e:e + 1].astype(x.dtype) * swiglu(x, w_exp_in[e], w_exp_out[e])
    return y


def trunk_layer(x, c, pos, past, l, w_in, b_fox_f, w_conv, b_conv, conv_ln_g, conv_ln_b, diff_lambda,
                diff_subln_g, w_branch, w_out, w_ada, b_ada, ln_g, ln_b, w_ffn_in, w_ffn_out,
                w_router, b_router, w_exp_in, w_exp_out):
    mod = jnp.einsum('bd,sde->bse', jax.nn.silu(c), w_ada[l]) + b_ada[l]
    shift, scale, gate = jnp.split(mod[:, :, None, :], 3, axis=-1)
    u = x * (1.0 + scale[:, 0]) + shift[:, 0]
    lam_init = 0.8 - 0.6 * math.exp(-0.3 * l)
    mix, st = token_mixers(u, pos, past, lam_init, w_in[l], b_fox_f[l], w_conv[l], b_conv[l],
                           conv_ln_g[l], conv_ln_b[l], diff_lambda[l], diff_subln_g[l], w_branch[l], w_out[l])
    x = layer_norm(ALPHA * x + gate[:, 0] * mix, ln_g[l, 0], ln_b[l, 0])
    u = x * (1.0 + scale[:, 1]) + shift[:, 1]
    if l % 2 == 0:
        f = swiglu(u, w_ffn_in[l // 2], w_ffn_out[l // 2])
    else:
        f = moe_swiglu(u, w_router[l // 2], b_router[l // 2], w_exp_in[l // 2], w_exp_out[l // 2])
    x = layer_norm(ALPHA * x + gate[:, 1] * f, ln_g[l, 1], ln_b[l, 1])
    return x, st


def setup_inputs(seed: int = 0) -> dict:
    key = jax.random.key(seed)
    ks = iter(jax.random.split(key, 40))

    def nrm(shape, s=1.0):
        return s * jax.random.normal(next(ks), shape, jnp.float32)

    D = D_MODEL
    return {
        'x_prompt': nrm((BATCH, SEQ, D)),
        'x_sample': nrm((DEC_BATCH, DEC_SEQ, D)),
        'c_prompt': nrm((BATCH, D)),
        'c_sample': nrm((DEC_BATCH, D)),
        'cache_fox_k': nrm((DEPTH, DEC_BATCH, PAST_LEN, FOX_HEADS, HEAD_DIM)),
        'cache_fox_v': nrm((DEPTH, DEC_BATCH, PAST_LEN, FOX_HEADS, HEAD_DIM)),
        'cache_fox_logf': jax.nn.log_sigmoid(FORGET_BIAS_MEAN + nrm((DEPTH, DEC_BATCH, PAST_LEN, FOX_HEADS))),
        'cache_diff_k': nrm((DEPTH, DEC_BATCH, PAST_LEN, 2 * DIFF_HEADS, DIFF_SUB)),
        'cache_diff_v': nrm((DEPTH, DEC_BATCH, PAST_LEN, DIFF_HEADS, HEAD_DIM)),
        'state_ret': nrm((DEPTH, DEC_BATCH, RET_HEADS, HEAD_DIM, HEAD_DIM)),
        'state_conv': nrm((DEPTH, DEC_BATCH, CONV_W - 1, BRANCH_W), 0.5),
        'w_in': nrm((DEPTH, D, N_IN), D ** -0.5),
        'b_fox_f': FORGET_BIAS_MEAN + nrm((DEPTH, FOX_HEADS), 0.1),
        'w_conv': nrm((DEPTH, CONV_W, BRANCH_W), CONV_W ** -0.5),
        'b_conv': nrm((DEPTH, BRANCH_W), 0.02),
        'conv_ln_g': 1.0 + nrm((DEPTH, BRANCH_W), 0.02),
        'conv_ln_b': nrm((DEPTH, BRANCH_W), 0.02),
        'diff_lambda': nrm((DEPTH, 4, DIFF_SUB), 0.1),
        'diff_subln_g': 1.0 + nrm((DEPTH, HEAD_DIM), 0.02),
        'w_branch': nrm((DEPTH, N_BRANCH, BRANCH_W, D), BRANCH_W ** -0.5),
        'w_out': nrm((DEPTH, D, D), BETA * D ** -0.5),
        'w_ada': nrm((DEPTH, 2, D, 3 * D), 0.5 * D ** -0.5),
        'b_ada': nrm((DEPTH, 2, 3 * D), 0.02),
        'ln_g': 1.0 + nrm((DEPTH, 2, D), 0.02),
        'ln_b': nrm((DEPTH, 2, D), 0.02),
        'w_ffn_in': nrm((N_DENSE, D, 2 * D_FF), D ** -0.5),
        'w_ffn_out': nrm((N_DENSE, D_FF, D), BETA * D_FF ** -0.5),
        'w_router': nrm((N_MOE, D, N_EXPERTS), D ** -0.5),
        'b_router': nrm((N_MOE, N_EXPERTS), 0.01),
        'w_exp_in': nrm((N_MOE, N_EXPERTS, D, 2 * D_EXPERT), D ** -0.5),
        'w_exp_out': nrm((N_MOE, N_EXPERTS, D_EXPERT, D), BETA * D_EXPERT ** -0.5),
    }


def reference(x_prompt, x_sample, c_prompt, c_sample, cache_fox_k, cache_fox_v, cache_fox_logf,
              cache_diff_k, cache_diff_v, state_ret, state_conv, w_in, b_fox_f, w_conv, b_conv,
              conv_ln_g, conv_ln_b, diff_lambda, diff_subln_g, w_branch, w_out, w_ada, b_ada,
              ln_g, ln_b, w_ffn_in, w_ffn_out, w_router, b_router, w_exp_in, w_exp_out):
    pos_p = jnp.arange(x_prompt.shape[1], dtype=jnp.int32)
    past_len = cache_fox_k.shape[2]
    pos_s = past_len + jnp.arange(x_sample.shape[1], dtype=jnp.int32)
    yp, ys = x_prompt, x_sample
    new_p, new_s = [], []
    for l in range(DEPTH):
        past_l = (cache_fox_k[l], cache_fox_v[l], cache_fox_logf[l], cache_diff_k[l], cache_diff_v[l],
                  state_ret[l], state_conv[l])
        yp, st_p = trunk_layer(yp, c_prompt, pos_p, None, l, w_in, b_fox_f, w_conv, b_conv, conv_ln_g,
                               conv_ln_b, diff_lambda, diff_subln_g, w_branch, w_out, w_ada, b_ada,
                               ln_g, ln_b, w_ffn_in, w_ffn_out, w_router, b_router, w_exp_in, w_exp_out)
        ys, st_s = trunk_layer(ys, c_sample, pos_s, past_l, l, w_in, b_fox_f, w_conv, b_conv, conv_ln_g,
                               conv_ln_b, diff_lambda, diff_subln_g, w_branch, w_out, w_ada, b_ada,
                               ln_g, ln_b, w_ffn_in, w_ffn_out, w_router, b_router, w_exp_in, w_exp_out)
        new_p.append(st_p)
        new_s.append(st_s)
    p_fox_k, p_fox_v, p_fox_logf, p_diff_k, p_diff_v, p_ret, p_conv = (jnp.stack(a) for a in zip(*new_p))
    s_fox_k, s_fox_v, s_fox_logf, s_diff_k, s_diff_v, s_ret, s_conv = (jnp.stack(a) for a in zip(*new_s))
    return (yp, ys, p_fox_k, p_fox_v, p_fox_logf, p_diff_k, p_diff_v, p_ret, p_conv,
            s_fox_k, s_fox_v, s_fox_logf, s_diff_k, s_diff_v, s_ret, s_conv)
```

```python
import math
from contextlib import ExitStack
import numpy as np
import ml_dtypes
import concourse.bass as bass
import concourse.mybir as mybir
from concourse.bass_utils import run_bass_kernel_spmd

F32 = mybir.dt.float32
BF16 = mybir.dt.bfloat16
AF = mybir.ActivationFunctionType
ALU = mybir.AluOpType
AX = mybir.AxisListType

D = 1024
DEPTH = 2
BW = 256
HD = 64
CONV_W = 31
D_FF = 2816
N_EXP = 8
D_EXP = 3584
ALPHA = (2.0 * DEPTH) ** 0.25
EPS = 1e-5
OFF_RET = 0
OFF_FOX = 1024
OFF_FOX_F = 1792
OFF_CONV = 1796
OFF_DIFF = 2308
OFF_GATE = 3076
N_IN = 7172
SEM_CAP = 30000


class Res:
    __slots__ = ("w", "r", "sem", "cnt", "name", "uid", "excl")
    _n = 0

    def __init__(self, name="", excl=False):
        Res._n += 1
        self.uid = Res._n
        self.excl = excl
        self.w = None
        self.r = {}
        self.sem = None
        self.cnt = 0
        self.name = name


class Sched:
    def __init__(self, nc, es):
        self.nc = nc
        self.es = es
        self.names = ["pe", "act", "dve", "pool", "sp"]
        self.ops = {k: [] for k in self.names}
        self.count = {k: 0 for k in self.names}
        self.clock = {k: {} for k in self.names}
        self.esems = {k: [] for k in self.names}
        self.nsem = 0
        self.const_res = Res("const")
        self.out_events = {}

    def new_sem(self, nm):
        self.nsem += 1
        return self.es.enter_context(self.nc.semaphore("s_%s_%d" % (nm, self.nsem)))

    def esem(self, e, idx):
        while len(self.esems[e]) <= idx:
            self.esems[e].append(self.new_sem(e))
        return self.esems[e][idx]

    def _deps(self, e, reads, writes):
        deps = {}

        def add(key, val):
            if deps.get(key, 0) < val:
                deps[key] = val

        for r in reads:
            if r.w is not None:
                add(*r.w)
            if r.excl:
                for k, v in r.r.items():
                    if k != ("e", e):
                        add(k, v)
        for w in writes:
            if w.w is not None:
                add(*w.w)
            for k, v in w.r.items():
                add(k, v)
        clk = self.clock[e]
        waits = []
        for key, val in deps.items():
            if key[0] == "e" and key[1] == e and e == "pe":
                continue
            if clk.get(key, 0) >= val:
                continue
            clk[key] = val
            waits.append((key, val))
        return waits

    def _mark(self, ev, reads, writes):
        key, val = ev
        for r in reads:
            if r.r.get(key, 0) < val:
                r.r[key] = val
        for w in writes:
            w.w = ev
            w.r = {}

    def op(self, e, reads, writes, fn):
        waits = self._deps(e, reads, writes)
        self.count[e] += 1
        ev = (("e", e), self.count[e])
        self.ops[e].append((waits, fn, ("e", e, self.count[e])))
        self._mark(ev, reads, writes)

    def dma(self, e, out, in_, reads, writes, sres, is_out=False, **kw):
        waits = self._deps(e, reads, writes)
        if sres.sem is None:
            sres.sem = self.new_sem("d")
        sres.cnt += 16
        key = ("d", sres.uid)
        self.semmap[key] = sres.sem
        ev = (key, sres.cnt)

        def fn(eng, out=out, in_=in_, kw=kw):
            return eng.dma_start(out=out, in_=in_, **kw)

        self.ops[e].append((waits, fn, ("d", sres.sem, 16)))
        self._mark(ev, reads, writes)
        self.dma_latest[key] = sres.cnt
        if is_out:
            self.out_events[key] = sres.cnt

    semmap = {}
    dma_latest = {}

    def barrier(self):
        for e in self.names:
            waits = []
            for o in ["pe", "act", "dve", "pool"]:
                if o == e or self.count[o] == 0:
                    continue
                key = ("e", o)
                if self.clock[e].get(key, 0) < self.count[o]:
                    self.clock[e][key] = self.count[o]
                    waits.append((key, self.count[o]))
            for key, val in self.dma_latest.items():
                if self.clock[e].get(key, 0) < val:
                    self.clock[e][key] = val
                    waits.append((key, val))
            if waits:
                self.ops[e].append((waits, None, None))

    def finish(self):
        waits = [(k, v) for k, v in self.out_events.items()]
        self.ops["sp"].append((waits, None, None))

    def _emit_wait(self, eng, key, val):
        if key[0] == "e":
            idx = (val - 1) // SEM_CAP
            eng.wait_ge(self.esem(key[1], idx), val - idx * SEM_CAP)
        else:
            eng.wait_ge(self.semmap[key], val)

    def emit(self, block):
        engs = {"pe": block.tensor, "act": block.scalar, "dve": block.vector,
                "pool": block.gpsimd, "sp": block.sync}
        for name in self.names:
            ops = self.ops[name]

            def body(eng, ops=ops, name=name):
                for waits, fn, inc in ops:
                    for key, val in waits:
                        self._emit_wait(eng, key, val)
                    if fn is None:
                        continue
                    ins = fn(eng)
                    if inc[0] == "e":
                        cnt = inc[2]
                        idx = (cnt - 1) // SEM_CAP
                        ins.then_inc(self.esem(name, idx), 1)
                    else:
                        ins.then_inc(inc[1], 16)

            engs[name](body)


class StopBuild(Exception):
    pass


MAX_STAGE = [10 ** 9]
STOP_NAME = [None]


class Cfg:
    def __init__(self, NP, T, NS, TS, PAST):
        self.NP, self.T, self.NS, self.TS, self.PAST = NP, T, NS, TS, PAST
        self.NSEQ = NP + NS


def host_consts(cfg):
    c = {}
    T, TS, PAST = cfg.T, cfg.TS, cfg.PAST
    c["ident_f"] = np.eye(128, dtype=np.float32)
    c["ones_f"] = np.ones((128, 128), np.float32)
    idx = np.arange(128)
    c["utri_f"] = (idx[:, None] <= idx[None, :]).astype(np.float32)
    c["cmask"] = (idx[:, None] <= idx[None, :]).astype(np.float32)
    c["dmask"] = ((idx[:, None] // 64) <= (idx[None, :] // 64)).astype(np.float32)
    half = 32
    inv = np.exp(-math.log(10000.0) * np.arange(half, dtype=np.float32) / half).astype(np.float32)

    def ret_tab(pos):
        ang = pos.astype(np.float32)[:, None] * inv[None, :]
        cos = np.cos(ang).astype(np.float32)
        sin = np.sin(ang).astype(np.float32)
        tab = np.zeros((len(pos), 2, 8, 32), np.float32)
        tab[:, 0, 0:4] = cos[:, None, :]
        tab[:, 1, 0:4] = sin[:, None, :]
        tab[:, 0, 4:8] = cos[:, None, :] * 0.125
        tab[:, 1, 4:8] = sin[:, None, :] * 0.125
        return tab.reshape(len(pos), 2, 256)

    ntp = T // 64
    rt = np.zeros((ntp + 1, 64, 2, 256), np.float32)
    for i in range(ntp):
        rt[i] = ret_tab(np.arange(i * 64, (i + 1) * 64))
    rt[ntp, :TS] = ret_tab(PAST + np.arange(TS))
    c["ret_rope"] = rt
    inv_d = np.exp(-math.log(500000.0) * np.arange(4, dtype=np.float32) / 4).astype(np.float32)

    def diff_tab(pos):
        ang = pos.astype(np.float32)[:, None] * inv_d[None, :]
        cos = np.cos(ang).astype(np.float32)
        sin = np.sin(ang).astype(np.float32)
        tab = np.zeros((len(pos), 2, 16, 4), np.float32)
        tab[:, 0] = cos[:, None, :]
        tab[:, 1] = sin[:, None, :]
        return tab.reshape(len(pos), 2, 64)

    nt = T // 128
    dt_ = np.zeros((nt + 1, 128, 2, 64), np.float32)
    for i in range(nt):
        dt_[i] = diff_tab(np.arange(i * 128, (i + 1) * 128))
    dt_[nt, :TS] = diff_tab(PAST + np.arange(TS))
    c["diff_rope"] = dt_
    log_g = np.log1p(-np.exp2(-5.0 - np.arange(4, dtype=np.float32))).astype(np.float32)
    rd = np.zeros((2, 64, 2, 4, 64), np.float32)
    rs = np.zeros((2, 64, 2, 4), np.float32)
    for kind, L in ((0, min(T, 64)), (1, TS)):
        ii = np.arange(L, dtype=np.float32)
        inner = np.exp(log_g[:, None, None] * np.abs(ii[:, None] - ii[None, :])).astype(np.float32)
        qdec = np.exp(log_g[:, None] * (ii[None, :] + 1.0)).astype(np.float32)
        kdec = np.exp(log_g[:, None] * (L - 1.0 - ii[None, :])).astype(np.float32)
        cdec = np.exp(log_g * L).astype(np.float32)
        for h in range(4):
            rd[kind, :L, 0, h, :L] = inner[h].T
            rd[kind, :, 1, h, :L] = qdec[h][None, :]
            rs[kind, :L, 0, h] = kdec[h]
            rs[kind, :, 1, h] = cdec[h]
    c["ret_dec"] = rd.reshape(2, 64, 2, 256)
    c["ret_decs"] = rs
    return c


CONST_SHAPES = None


class Builder:
    def __init__(self, cfg, dbg=False):
        self.cfg = cfg
        self.dbg = dbg

    def build(self):
        cfg = self.cfg
        NP, T, NS, TS, PAST = cfg.NP, cfg.T, cfg.NS, cfg.TS, cfg.PAST
        NSEQ = cfg.NSEQ
        nc = bass.Bass("TRN2", target_bir_lowering=False)
        self.nc = nc
        self.es = ExitStack()
        es = self.es
        S = Sched(nc, es)
        Sched.semmap = {}
        Sched.dma_latest = {}
        self.S = S

        def din(name, shape, dt=F32):
            return nc.dram_tensor(name, list(shape), dt, kind="ExternalInput").ap()

        def dout(name, shape):
            return nc.dram_tensor(name, list(shape), F32, kind="ExternalOutput").ap()

        I = {}
        I["xp"] = din("xp", [NP, T, D])
        I["xs"] = din("xs", [NS, TS, D])
        I["cT"] = din("cT", [128, 8, NSEQ])
        for nm in ("cfk", "cfv", "cdk", "cdv"):
            I[nm] = din(nm, [2, NS, PAST, 256])
        I["cfl"] = din("cfl", [2, NS, PAST, 4])
        I["sret"] = din("sret", [2, NS, 4, 64, 64])
        I["sconv"] = din("sconv", [2, NS, 30, 256])
        I["w_in"] = din("w_in", [2, D, N_IN])
        I["b_fox_f"] = din("b_fox_f", [2, 4])
        I["wconvT"] = din("wconvT", [2, 128, 2, 31])
        I["convp"] = din("convp", [2, 128, 3, 2])
        I["diff_lambda"] = din("diff_lambda", [2, 128])
        I["diff_subln_g"] = din("diff_subln_g", [2, 64])
        I["w_branch"] = din("w_branch", [2, 4, 256, D])
        I["w_out"] = din("w_out", [2, D, D])
        I["w_ada"] = din("w_ada", [2, 2, D, 3 * D])
        I["b_adaT"] = din("b_adaT", [128, 4, 24])
        I["b_ada"] = din("b_ada", [2, 2, 3 * D])
        I["ln_g"] = din("ln_g", [2, 2, D])
        I["ln_b"] = din("ln_b", [2, 2, D])
        I["w_ffn_in"] = din("w_ffn_in", [1, D, 2 * D_FF])
        I["w_ffn_out"] = din("w_ffn_out", [1, D_FF, D])
        I["w_router"] = din("w_router", [128, 8, 8])
        I["b_router"] = din("b_router", [1, 8])
        I["w_exp_in"] = din("w_exp_in", [1, N_EXP, D, 2 * D_EXP])
        I["w_exp_out"] = din("w_exp_out", [1, N_EXP, D_EXP, D])
        hc = host_consts(cfg)
        for k, v in hc.items():
            I[k] = din(k, list(v.shape))
        self.I = I
        O = {}
        O["yp"] = dout("yp", [NP, T, D])
        O["ys"] = dout("ys", [NS, TS, D])
        for pre, n, t in (("p", NP, T), ("s", NS, TS)):
            O[pre + "_fox_k"] = dout(pre + "_fox_k", [2, n, t, 256])
            O[pre + "_fox_v"] = dout(pre + "_fox_v", [2, n, t, 256])
            O[pre + "_fox_logf"] = dout(pre + "_fox_logf", [2, n, t, 4])
            O[pre + "_diff_k"] = dout(pre + "_diff_k", [2, n, t, 256])
            O[pre + "_diff_v"] = dout(pre + "_diff_v", [2, n, t, 256])
            O[pre + "_ret"] = dout(pre + "_ret", [2, n, 4, 64, 64])
            O[pre + "_conv"] = dout(pre + "_conv", [2, n, 30, 256])
        self.O = O

        NT = T // 128
        self.NT = NT
        TT = max(NT, 2 * NS)
        NCOL = max(T, NS * TS)
        self.NCOL = NCOL
        NKT = max(NT, PAST // 128 + 1)
        self.NKT = NKT
        NK = NKT * 128

        def sb(name, shape, dt=F32):
            return es.enter_context(nc.sbuf_tensor(name, list(shape), dt))

        self.ACC = sb("ACC", [128, TT, D])
        self.ACCR = [Res("acc%d" % i) for i in range(TT)]
        self.UT = sb("UT", [128, 8, NCOL], BF16)
        self.UTR = Res("ut")
        self.HT = sb("HT", [128, 8, NCOL], BF16)
        self.HTR = [Res("ht%d" % i) for i in range(4)]
        self.NWS = 5
        self.WS = sb("WS", [128, self.NWS, 2048], BF16)
        self.WSR = [Res("ws%d" % i) for i in range(self.NWS)]
        self.wsi = 0
        self.SCR = sb("SCR", [128, 5120])
        self.MODT = sb("MODT", [128, 4, 16, NSEQ])
        self.MODR = Res("modt")
        self.BADA = sb("BADA", [128, 4, 24])
        self.SCT = sb("SCT", [128, 8, NSEQ], BF16)
        self.SCTR = Res("sct")
        self.SCREP = sb("SCREP", [128, 8, 128], BF16)
        self.SCREPR = Res("screp")
        self.GB0 = sb("GB0", [128, D])
        self.GBR = [Res("gb%d" % i) for i in range(max(NS, 1))]
        self.ROWA = sb("ROWA", [128, D])
        self.ROWAR = Res("rowa")
        self.ROWB = sb("ROWB", [128, D])
        self.ROWBR = Res("rowb")
        self.IDF = sb("IDF", [128, 128])
        self.IDB = sb("IDB", [128, 128], BF16)
        self.ONESF = sb("ONESF", [128, 128])
        self.UTRI = sb("UTRI", [128, 128])
        self.CMASK = sb("CMASK", [128, 128], BF16)
        self.DMASK = sb("DMASK", [128, 128], BF16)
        self.RDEC = sb("RDEC", [64, 2, 2, 256])
        self.RDECS = sb("RDECS", [64, 2, 2, 4])
        self.CONSTR = Res("consts")
        self.WCV = sb("WCV", [128, 2, 2, 31])
        self.CVP = sb("CVP", [128, 2, 3, 2])
        self.BFF = sb("BFF", [128, 2, 4])
        self.LAM = sb("LAM", [128, 2, 4])
        self.SUBG = sb("SUBG", [128, 2, 64])
        self.WRT = sb("WRT", [128, 8, 8])
        self.BRT = sb("BRT", [128, 8])
        self.STG = sb("STG", [128, 2, 512])
        self.STGR = [Res("stg0"), Res("stg1")]
        self.stgi = 0
        self.TMPA = sb("TMPA", [128, 1024])
        self.TMPAR = Res("tmpa")
        self.TMPB = sb("TMPB", [128, 1024], BF16)
        self.TMPBR = Res("tmpb")
        self.TMPC = sb("TMPC", [128, 512])
        self.TMPCR = Res("tmpc")
        self.EPSC = sb("EPSC", [128, 1])
        self.ONEC = sb("ONEC", [128, 1])
        self.SM = sb("SM", [128, 64])
        self.SMR = Res("sm")
        self.PSB = []
        for i in range(8):
            t = es.enter_context(nc.psum_tensor("PS%d" % i, [128, 512], F32))
            self.PSB.append((t, Res("ps%d" % i, excl=True)))
        self.psi = 0

        self.stage = 0
        try:
            self.load_consts()
            self.stage_end("consts")
            self.ada_precompute()
            self.stage_end("ada")
            units = [("p", i) for i in range(NP)] + ([("s", 0)] if NS > 0 else [])
            for kind, i in units:
                self.run_unit(kind, i)
        except StopBuild:
            pass
        S.finish()
        with nc.Block() as block:
            S.emit(block)
        return nc

    def stage_end(self, name):
        self.stage += 1
        if self.stage >= MAX_STAGE[0] or name == STOP_NAME[0]:
            print("STOP after stage", self.stage, name)
            raise StopBuild()

    def ps(self, hold=False):
        held = self.__dict__.setdefault("ps_held", set())
        while self.psi in held:
            self.psi = (self.psi + 1) % 8
        i = self.psi
        self.psi = (self.psi + 1) % 8
        if hold:
            held.add(i)
        return self.PSB[i]

    def ps_release(self, pr):
        for i, (t, r) in enumerate(self.PSB):
            if r is pr:
                self.ps_held.discard(i)

    def stg(self):
        i = self.stgi
        self.stgi = 1 - i
        return self.STG[:, i, :], self.STGR[i]

    def load_w(self, src2d, nk, ncols, k0=0):
        i = self.wsi
        self.wsi = (self.wsi + 1) % self.NWS
        res = self.WSR[i]
        view = self.WS[:, i, 0:nk * ncols].rearrange("p (k c) -> p k c", k=nk)
        src = src2d[k0 * 128:(k0 + nk) * 128, :].rearrange("(k p) c -> p k c", p=128)
        self.S.dma("pool", view, src, [], [res], res)
        return view, res

    def cdma(self, out, in_, **kw):
        self.S.dma("sp", out, in_, [], [self.CONSTR], self.CONSTR, **kw)

    def load_consts(self):
        I = self.I
        S = self.S
        self.cdma(self.IDF[:, :], I["ident_f"][:, :])
        self.cdma(self.ONESF[:, :], I["ones_f"][:, :])
        self.cdma(self.UTRI[:, :], I["utri_f"][:, :])
        self.cdma(self.RDEC[:, :, :, :], I["ret_dec"].rearrange("k p w c -> p k w c"))
        self.cdma(self.RDECS[:, :, :, :], I["ret_decs"].rearrange("k p w h -> p k w h"))
        self.cdma(self.WCV[:, :, :, :], I["wconvT"].rearrange("l p g w -> p l g w"))
        self.cdma(self.CVP[:, :, :, :], I["convp"].rearrange("l p a g -> p l a g"))
        self.cdma(self.BFF[:, :, :], I["b_fox_f"].rearrange("l h -> (l h)").partition_broadcast(128).rearrange("p (l h) -> p l h", l=2))
        self.DLAM = self.TMPA[:, 0:256].rearrange("p (l c) -> p l c", l=2)
        S.dma("sp", self.DLAM, I["diff_lambda"].rearrange("l c -> (l c)").partition_broadcast(128).rearrange("p (l c) -> p l c", l=2), [], [self.TMPAR], self.TMPAR)
        self.cdma(self.SUBG[:, :, :], I["diff_subln_g"].rearrange("l c -> (l c)").partition_broadcast(128).rearrange("p (l c) -> p l c", l=2))
        self.cdma(self.WRT[:, :, :], I["w_router"][:, :, :])
        self.cdma(self.BRT[:, :], I["b_router"].rearrange("a e -> (a e)").partition_broadcast(128))
        self.cdma(self.BADA[:, :, :], I["b_adaT"][:, :, :])
        self.CONSTP = Res("constp")
        S.dma("pool", self.CMASK[:, :], I["cmask"][:, :], [], [self.CONSTP], self.CONSTP)
        S.dma("pool", self.DMASK[:, :], I["dmask"][:, :], [], [self.CONSTP], self.CONSTP)
        S.dma("pool", self.IDB[:, :], I["ident_f"][:, :], [], [self.CONSTP], self.CONSTP)
        S.op("dve", [self.CONSTP], [self.CONSTR], lambda e: e.memset(self.EPSC[:, :], EPS))
        S.op("dve", [], [self.CONSTR], lambda e: e.memset(self.EPSC[:, :], EPS))
        S.op("dve", [], [self.CONSTR], lambda e: e.memset(self.ONEC[:, :], 1.0))
        C = self.CONSTR
        for l in range(2):
            lam_init = 0.8 - 0.6 * math.exp(-0.3 * l)
            DL, LAM, TM = self.DLAM, self.LAM, self.SM
            S.op("dve", [C, self.TMPAR], [self.SMR], lambda e, l=l: e.tensor_tensor(out=TM[:, 0:32], in0=DL[:, l, 0:32], in1=DL[:, l, 32:64], op=ALU.mult))
            S.op("dve", [self.SMR], [self.SMR], lambda e, l=l: e.tensor_reduce(out=TM[:, 32:33], in_=TM[:, 0:32], axis=AX.X, op=ALU.add))
            S.op("dve", [C, self.TMPAR], [self.SMR], lambda e, l=l: e.tensor_tensor(out=TM[:, 0:32], in0=DL[:, l, 64:96], in1=DL[:, l, 96:128], op=ALU.mult))
            S.op("dve", [self.SMR], [self.SMR], lambda e, l=l: e.tensor_reduce(out=TM[:, 33:34], in_=TM[:, 0:32], axis=AX.X, op=ALU.add))
            S.op("act", [self.SMR], [self.SMR], lambda e, l=l: e.activation(out=TM[:, 34:36], in_=TM[:, 32:34], func=AF.Exp))
            S.op("dve", [self.SMR], [self.SMR], lambda e, l=l: e.tensor_tensor(out=TM[:, 36:37], in0=TM[:, 34:35], in1=TM[:, 35:36], op=ALU.subtract))
            S.op("dve", [self.SMR], [C], lambda e, l=l, li=lam_init: e.tensor_scalar(out=LAM[:, l, 0:1], in0=TM[:, 36:37], scalar1=li, scalar2=None, op0=ALU.add))
            S.op("dve", [C], [C], lambda e, l=l: e.tensor_scalar(out=LAM[:, l, 1:2], in0=LAM[:, l, 0:1], scalar1=-1.0, scalar2=None, op0=ALU.mult))

    def ada_precompute(self):
        S, I = self.S, self.I
        NSEQ = self.cfg.NSEQ
        CT = self.TMPA[:, 0:8 * NSEQ].rearrange("p (k s) -> p k s", k=8)
        S.dma("sp", CT, I["cT"][:, :, :], [], [self.TMPAR], self.TMPAR)
        S.op("act", [self.TMPAR], [self.SCTR], lambda e: e.activation(out=self.SCT[:, :, :], in_=CT, func=AF.Silu))
        BT = self.BADA
        for l in range(2):
            for s in range(2):
                ls = l * 2 + s
                pt, pr = self.ps()
                for j in range(16):
                    if j % 2 == 0:
                        wv, wr = self.load_w(I["w_ada"][l, s][:, (j // 2) * 256:(j // 2 + 1) * 256], 8, 256)

                    def f(e, j=j, wv=wv, pt=pt):
                        ins = None
                        for kc in range(8):
                            ins = e.matmul(pt[:, j * NSEQ:(j + 1) * NSEQ], wv[:, kc, (j % 2) * 128:(j % 2 + 1) * 128],
                                           self.SCT[:, kc, :], start=(kc == 0), stop=(kc == 7))
                        return ins
                    S.op("pe", [wr, self.SCTR], [pr], f)
                    S.op("act", [pr, self.CONSTR], [self.MODR],
                         lambda e, j=j, ls=ls, pt=pt: e.activation(out=self.MODT[:, ls, j, :], in_=pt[:, j * NSEQ:(j + 1) * NSEQ],
                                                                   func=AF.Identity, bias=BT[:, ls, j:j + 1], scale=1.0))
                S.op("dve", [self.MODR], [self.MODR],
                     lambda e, ls=ls: e.tensor_scalar(out=self.MODT[:, ls, 8:16, :], in0=self.MODT[:, ls, 8:16, :], scalar1=1.0, scalar2=None, op0=ALU.add))

    def gate_rows(self, l, s, seqs, gbviews):
        S, I = self.S, self.I
        S.dma("sp", self.ROWA[:, :], I["b_ada"][l, s, 2 * D:3 * D].partition_broadcast(128), [], [self.ROWAR], self.ROWAR)
        for si, seqg in enumerate(seqs):
            gbv, gbr = gbviews[si]
            S.op("dve", [self.SCTR], [self.SCREPR],
                 lambda e, seqg=seqg: e.tensor_copy(out=self.SCREP[:, :, :], in_=self.SCT[:, :, seqg:seqg + 1].to_broadcast([128, 8, 128])))
            for cb in range(4):
                wv, wr = self.load_w(I["w_ada"][l, s][:, 2 * D + cb * 256:2 * D + (cb + 1) * 256], 8, 256)
                if cb % 2 == 0:
                    pt, pr = self.ps()

                def f(e, wv=wv, pt=pt, cb=cb):
                    ins = None
                    for kc in range(8):
                        ins = e.matmul(pt[:, (cb % 2) * 256:(cb % 2 + 1) * 256], self.SCREP[:, kc, :], wv[:, kc, :],
                                       start=(kc == 0), stop=(kc == 7))
                    return ins
                S.op("pe", [wr, self.SCREPR], [pr], f)
                if cb % 2 == 1:
                    c0 = (cb // 2) * 512
                    S.op("dve", [pr, self.ROWAR], [gbr],
                         lambda e, pt=pt, gbv=gbv, c0=c0: e.tensor_tensor(out=gbv[:, c0:c0 + 512], in0=pt[:, :], in1=self.ROWA[:, c0:c0 + 512], op=ALU.add))

    def run_unit(self, kind, ui):
        cfg, S, I, O = self.cfg, self.S, self.I, self.O
        if kind == "p":
            rows, ntl = 128, self.NT
            seqs = [ui]
            tiles = [(0, i) for i in range(ntl)]
            xsrc = lambda k: I["xp"][ui, k * 128:(k + 1) * 128, :]
            ydst = lambda k: O["yp"][ui, k * 128:(k + 1) * 128, :]
            gbviews = [(self.GB0, self.GBR[0])]
        else:
            rows, ntl = cfg.TS, cfg.NS
            seqs = [cfg.NP + j for j in range(cfg.NS)]
            tiles = [(j, 0) for j in range(ntl)]
            xsrc = lambda k: I["xs"][k, :, :]
            ydst = lambda k: O["ys"][k, :, :]
            gbviews = [(self.ACC[:, cfg.NS + j, :], self.ACCR[cfg.NS + j]) for j in range(cfg.NS)]
        u = dict(kind=kind, ui=ui, rows=rows, ntl=ntl, seqs=seqs, tiles=tiles, ncols=rows * ntl, gb=gbviews)
        self.u = u
        for k in range(ntl):
            S.dma("sp", self.ACC[:rows, k, :], xsrc(k), [], [self.ACCR[k]], self.ACCR[k])
        for l in range(2):
            self.make_uT(l, 0)
            self.stage_end("uT")
            self.gate_rows(l, 0, seqs, gbviews)
            self.stage_end("gate")
            self.mixers(l)
            self.merge_out(l)
            self.stage_end("merge")
            self.layer_norm(l, 0)
            self.stage_end("ln")
            self.make_uT(l, 1, router=(l == 1))
            self.gate_rows(l, 1, seqs, gbviews)
            if l == 0:
                self.ffn(I["w_ffn_in"][0], I["w_ffn_out"][0], D_FF, None)
            else:
                for ex in range(N_EXP):
                    self.ffn(I["w_exp_in"][0, ex], I["w_exp_out"][0, ex], D_EXP, ex)
            self.layer_norm(l, 1)
        for k in range(ntl):
            S.dma("sp", ydst(k), self.ACC[:rows, k, :], [self.ACCR[k]], [], self.ACCR[k], is_out=True)

    def make_uT(self, l, s, router=False):
        S, u = self.S, self.u
        rows = u["rows"]
        ls = l * 2 + s
        if router:
            self.COMB = self.SCR[:, 0:u["ntl"] * 8].rearrange("p (t e) -> p t e", e=8)
            self.COMBR = Res("comb")
        for k, (sl, ti) in enumerate(u["tiles"]):
            seqg = u["seqs"][sl]
            c0 = k * rows
            if router:
                lt, lr = self.ps()
            for half in range(2):
                pt, pr = self.ps()

                def f(e, pt=pt, k=k, half=half):
                    ins = None
                    for j in range(4):
                        c = half * 4 + j
                        ins = e.transpose(out=pt[:, j * 128:j * 128 + rows], in_=self.ACC[:rows, k, c * 128:(c + 1) * 128],
                                          identity=self.IDF[:rows, :rows])
                    return ins
                S.op("pe", [self.ACCR[k], self.CONSTR], [pr], f)
                for j in range(4):
                    c = half * 4 + j
                    if not router:
                        S.op("act", [pr, self.MODR], [self.UTR],
                             lambda e, pt=pt, j=j, c=c, c0=c0, seqg=seqg: e.activation(
                                 out=self.UT[:, c, c0:c0 + rows], in_=pt[:, j * 128:j * 128 + rows], func=AF.Identity,
                                 scale=self.MODT[:, ls, 8 + c, seqg:seqg + 1], bias=self.MODT[:, ls, c, seqg:seqg + 1]))
                    else:
                        S.op("act", [pr, self.MODR], [self.TMPAR],
                             lambda e, pt=pt, j=j, c=c, seqg=seqg: e.activation(
                                 out=self.TMPA[:, c * 128:c * 128 + rows], in_=pt[:, j * 128:j * 128 + rows], func=AF.Identity,
                                 scale=self.MODT[:, ls, 8 + c, seqg:seqg + 1], bias=self.MODT[:, ls, c, seqg:seqg + 1]))
                        S.op("dve", [self.TMPAR], [self.UTR],
                             lambda e, c=c, c0=c0: e.tensor_copy(out=self.UT[:, c, c0:c0 + rows], in_=self.TMPA[:, c * 128:c * 128 + rows]))
            if router:
                def fr(e, lt=lt):
                    ins = None
                    for c in range(8):
                        ins = e.matmul(lt[:rows, 0:8], self.TMPA[:, c * 128:c * 128 + rows], self.WRT[:, c, :], start=(c == 0), stop=(c == 7))
                    return ins
                S.op("pe", [self.TMPAR, self.CONSTR], [lr], fr)
                self.route(k, lt, lr)
            S.op("act", [self.ACCR[k]], [self.ACCR[k]], lambda e, k=k: e.mul(out=self.ACC[:rows, k, :], in_=self.ACC[:rows, k, :], mul=ALPHA))

    def route(self, k, lt, lr):
        S, u = self.S, self.u
        rows = u["rows"]
        SM, R = self.SM, self.SMR
        lg = SM[:rows, 0:8]
        S.op("dve", [lr, self.CONSTR], [R], lambda e: e.tensor_tensor(out=lg, in0=lt[:rows, 0:8], in1=self.BRT[:rows, :], op=ALU.add))
        S.op("dve", [R], [R], lambda e: e.tensor_reduce(out=SM[:rows, 8:9], in_=lg, axis=AX.X, op=ALU.max))
        S.op("dve", [R], [R], lambda e: e.tensor_scalar(out=SM[:rows, 16:24], in0=lg, scalar1=SM[:rows, 8:9], scalar2=None, op0=ALU.is_equal))
        S.op("dve", [R], [R], lambda e: e.scalar_tensor_tensor(out=SM[:rows, 24:32], in0=SM[:rows, 16:24], scalar=-1e30, in1=lg, op0=ALU.mult, op1=ALU.add))
        S.op("dve", [R], [R], lambda e: e.tensor_reduce(out=SM[:rows, 9:10], in_=SM[:rows, 24:32], axis=AX.X, op=ALU.max))
        S.op("dve", [R], [R], lambda e: e.tensor_scalar(out=SM[:rows, 32:40], in0=SM[:rows, 24:32], scalar1=SM[:rows, 9:10], scalar2=None, op0=ALU.is_equal))
        S.op("dve", [R], [R], lambda e: e.tensor_tensor(out=SM[:rows, 10:11], in0=SM[:rows, 9:10], in1=SM[:rows, 8:9], op=ALU.subtract))
        S.op("act", [R], [R], lambda e: e.activation(out=SM[:rows, 11:12], in_=SM[:rows, 10:11], func=AF.Exp))
        S.op("dve", [R], [R], lambda e: e.tensor_scalar(out=SM[:rows, 11:12], in0=SM[:rows, 11:12], scalar1=1.0, scalar2=None, op0=ALU.add))
        S.op("dve", [R], [R], lambda e: e.reciprocal(out=SM[:rows, 12:13], in_=SM[:rows, 11:12]))
        S.op("dve", [R], [R], lambda e: e.tensor_scalar(out=SM[:rows, 13:14], in0=SM[:rows, 12:13], scalar1=-1.0, scalar2=1.0, op0=ALU.mult, op1=ALU.add))
        S.op("dve", [R], [R], lambda e: e.tensor_scalar(out=SM[:rows, 16:24], in0=SM[:rows, 16:24], scalar1=SM[:rows, 12:13], scalar2=None, op0=ALU.mult))
        COMB = self.COMB
        S.op("dve", [R], [self.COMBR], lambda e, k=k: e.scalar_tensor_tensor(out=COMB[:rows, k, :], in0=SM[:rows, 32:40], scalar=SM[:rows, 13:14],
                                                                        in1=SM[:rows, 16:24], op0=ALU.mult, op1=ALU.add))

    def layer_norm(self, l, s):
        S, u, I = self.S, self.u, self.I
        rows = u["rows"]
        S.dma("sp", self.ROWA[:, :], I["ln_g"][l, s, :].partition_broadcast(128), [], [self.ROWAR], self.ROWAR)
        S.dma("sp", self.ROWB[:, :], I["ln_b"][l, s, :].partition_broadcast(128), [], [self.ROWBR], self.ROWBR)
        SM, R = self.SM, self.SMR
        for k in range(u["ntl"]):
            A = self.ACC[:rows, k, :]
            AR = self.ACCR[k]
            S.op("dve", [AR], [R], lambda e, A=A: e.tensor_reduce(out=SM[:rows, 0:1], in_=A, axis=AX.X, op=ALU.add))
            S.op("act", [AR], [self.TMPAR, R], lambda e, A=A: e.activation(out=self.TMPA[:rows, :], in_=A, func=AF.Square, accum_out=SM[:rows, 1:2]))
            S.op("dve", [R], [R], lambda e: e.tensor_scalar(out=SM[:rows, 2:3], in0=SM[:rows, 0:1], scalar1=1.0 / D, scalar2=None, op0=ALU.mult))
            S.op("dve", [R], [R], lambda e: e.tensor_tensor(out=SM[:rows, 3:4], in0=SM[:rows, 2:3], in1=SM[:rows, 2:3], op=ALU.mult))
            S.op("dve", [R], [R], lambda e: e.scalar_tensor_tensor(out=SM[:rows, 4:5], in0=SM[:rows, 1:2], scalar=1.0 / D, in1=SM[:rows, 3:4], op0=ALU.mult, op1=ALU.subtract))
            S.op("act", [R], [R], lambda e: e.activation(out=SM[:rows, 5:6], in_=SM[:rows, 4:5], func=AF.Sqrt, bias=self.EPSC[:rows, :], scale=1.0))
            S.op("dve", [R], [R], lambda e: e.reciprocal(out=SM[:rows, 5:6], in_=SM[:rows, 5:6]))
            S.op("dve", [R], [R], lambda e: e.scalar_tensor_tensor(out=SM[:rows, 6:7], in0=SM[:rows, 2:3], scalar=-1.0, in1=SM[:rows, 5:6], op0=ALU.mult, op1=ALU.mult))
            S.op("act", [AR, R], [AR], lambda e, A=A: e.activation(out=A, in_=A, func=AF.Identity, scale=SM[:rows, 5:6], bias=SM[:rows, 6:7]))
            S.op("dve", [AR, self.ROWAR], [AR], lambda e, A=A: e.tensor_tensor(out=A, in0=A, in1=self.ROWA[:rows, :], op=ALU.mult))
            S.op("dve", [AR, self.ROWBR], [AR], lambda e, A=A: e.tensor_tensor(out=A, in0=A, in1=self.ROWB[:rows, :], op=ALU.add))

    def ffn(self, w_up, w_dn, dff, ex):
        S, u = self.S, self.u
        rows, ntl, ncols = u["rows"], u["ntl"], u["ncols"]
        nch = dff // 128
        HG = self.HT[:, :, :].rearrange("p (b j) c -> p b j c", b=4)
        nblk = (ncols + 511) // 512
        for g in range(nch // 2):
            hb = self.hgi = (getattr(self, "hgi", -1) + 1) % 4
            hres = self.HTR[hb]
            wa, war = self.load_w(w_up[:, g * 256:(g + 1) * 256], 8, 256)
            wg, wgr = self.load_w(w_up[:, dff + g * 256:dff + (g + 1) * 256], 8, 256)
            wd, wdr = self.load_w(w_dn, 2, 1024, k0=2 * g)
            for j in range(2):
                for b in range(nblk):
                    n = min(512, ncols - b * 512)
                    pa, par = self.ps()
                    pg, pgr = self.ps()

                    def f(e, pa=pa, pg=pg, j=j, b=b, n=n, wa=wa, wg=wg):
                        ins = None
                        for kc in range(8):
                            ins = e.matmul(pa[:, 0:n], wa[:, kc, j * 128:(j + 1) * 128], self.UT[:, kc, b * 512:b * 512 + n], start=(kc == 0), stop=(kc == 7))
                        for kc in range(8):
                            ins = e.matmul(pg[:, 0:n], wg[:, kc, j * 128:(j + 1) * 128], self.UT[:, kc, b * 512:b * 512 + n], start=(kc == 0), stop=(kc == 7))
                        return ins
                    S.op("pe", [war, wgr, self.UTR], [par, pgr], f)
                    S.op("act", [par], [self.TMPCR], lambda e, pa=pa, n=n: e.activation(out=self.TMPC[:, 0:n], in_=pa[:, 0:n], func=AF.Silu))
                    S.op("dve", [pgr, self.TMPCR], [hres],
                         lambda e, pg=pg, n=n, hb=hb, j=j, b=b: e.tensor_tensor(out=HG[:, hb, j, b * 512:b * 512 + n], in0=pg[:, 0:n], in1=self.TMPC[:, 0:n], op=ALU.mult))
            for k, (sl, ti) in enumerate(u["tiles"]):
                gbv, gbr = u["gb"][sl]
                for cb in range(2):
                    po, por = self.ps()

                    def f2(e, po=po, k=k, cb=cb, hb=hb, wd=wd):
                        ins = None
                        for j in range(2):
                            ins = e.matmul(po[:rows, :], HG[:, hb, j, k * rows:(k + 1) * rows], wd[:, j, cb * 512:(cb + 1) * 512], start=(j == 0), stop=(j == 1))
                        return ins
                    S.op("pe", [hres, wdr], [por], f2)
                    self.accumulate(k, cb, po, por, gbv, gbr, ex)

    def accumulate(self, k, cb, po, por, gbv, gbr, ex):
        S, u = self.S, self.u
        rows = u["rows"]
        A = self.ACC[:rows, k, cb * 512:(cb + 1) * 512]
        T_ = self.TMPA[:rows, 0:512]
        COMB = getattr(self, "COMB", None)
        if ex is None:
            S.op("dve", [por, gbr], [self.TMPAR], lambda e: e.tensor_tensor(out=T_, in0=po[:rows, :], in1=gbv[:rows, cb * 512:(cb + 1) * 512], op=ALU.mult))
        else:
            S.op("dve", [por, gbr, self.COMBR], [self.TMPAR],
                 lambda e: e.scalar_tensor_tensor(out=T_, in0=po[:rows, :], scalar=COMB[:rows, k, ex:ex + 1], in1=gbv[:rows, cb * 512:(cb + 1) * 512],
                                                  op0=ALU.mult, op1=ALU.mult))
        S.op("dve", [self.TMPAR, self.ACCR[k]], [self.ACCR[k]], lambda e: e.tensor_tensor(out=A, in0=A, in1=T_, op=ALU.add))

    def merge_out(self, l):
        S, u, I = self.S, self.u, self.I
        rows, ntl, ncols = u["rows"], u["ntl"], u["ncols"]
        nblk = (ncols + 511) // 512
        S.barrier()
        NCOL = self.NCOL
        TACC = self.SCR[:, 0:NCOL]
        tar = Res("tacc")
        SG = self.SCR[:, NCOL:NCOL + NCOL // 2].bitcast(BF16)
        sgr = Res("sg")
        MC = self.SCR[:, NCOL + NCOL // 2:NCOL + NCOL // 2 + NCOL].bitcast(BF16).rearrange("p (b c) -> p b c", b=2)
        mcr = [Res("mc0"), Res("mc1")]
        for c in range(8):
            for n in range(4):
                wgv, wgr = self.load_w(I["w_in"][l][:, OFF_GATE + n * D + c * 128:OFF_GATE + n * D + (c + 1) * 128], 8, 128)
                wbv, wbr = self.load_w(I["w_branch"][l, n][:, c * 128:(c + 1) * 128], 2, 128)
                for b in range(nblk):
                    nn = min(512, ncols - b * 512)
                    pg, pgr = self.ps()
                    pb, pbr = self.ps()

                    def f(e, pg=pg, pb=pb, b=b, nn=nn, wgv=wgv, wbv=wbv, n=n):
                        ins = None
                        for kc in range(8):
                            ins = e.matmul(pg[:, 0:nn], wgv[:, kc, :], self.UT[:, kc, b * 512:b * 512 + nn], start=(kc == 0), stop=(kc == 7))
                        for kc in range(2):
                            ins = e.matmul(pb[:, 0:nn], wbv[:, kc, :], self.HT[:, n * 2 + kc, b * 512:b * 512 + nn], start=(kc == 0), stop=(kc == 1))
                        return ins
                    S.op("pe", [wgr, wbr, self.UTR, self.HTR[n]], [pgr, pbr], f)
                    S.op("act", [pgr], [sgr], lambda e, pg=pg, b=b, nn=nn: e.activation(out=SG[:, b * 512:b * 512 + nn], in_=pg[:, 0:nn], func=AF.Sigmoid))
                    sl_ = slice(b * 512, b * 512 + nn)
                    if n == 0:
                        S.op("dve", [pbr, sgr], [tar], lambda e, pb=pb, sl_=sl_, nn=nn: e.tensor_tensor(out=TACC[:, sl_], in0=pb[:, 0:nn], in1=SG[:, sl_], op=ALU.mult))
                    else:
                        S.op("dve", [pbr, sgr], [self.TMPCR], lambda e, pb=pb, sl_=sl_, nn=nn: e.tensor_tensor(out=self.TMPC[:, 0:nn], in0=pb[:, 0:nn], in1=SG[:, sl_], op=ALU.mult))
                        if n < 3:
                            S.op("dve", [self.TMPCR, tar], [tar], lambda e, sl_=sl_, nn=nn: e.tensor_tensor(out=TACC[:, sl_], in0=TACC[:, sl_], in1=self.TMPC[:, 0:nn], op=ALU.add))
                        else:
                            S.op("dve", [self.TMPCR, tar], [mcr[c % 2]],
                                 lambda e, sl_=sl_, nn=nn, c=c: e.tensor_tensor(out=MC[:, c % 2, sl_], in0=TACC[:, sl_], in1=self.TMPC[:, 0:nn], op=ALU.add))
            wo0, wo0r = self.load_w(I["w_out"][l][:, 0:512], 1, 512, k0=c)
            wo1, wo1r = self.load_w(I["w_out"][l][:, 512:1024], 1, 512, k0=c)
            for k, (sl, ti) in enumerate(u["tiles"]):
                gbv, gbr = u["gb"][sl]
                for cb, (wo, wor) in enumerate(((wo0, wo0r), (wo1, wo1r))):
                    po, por = self.ps()
                    S.op("pe", [mcr[c % 2], wor], [por],
                         lambda e, po=po, k=k, wo=wo, c=c: e.matmul(po[:rows, :], MC[:, c % 2, k * rows:(k + 1) * rows], wo[:, 0, :], start=True, stop=True))
                    self.accumulate(k, cb, po, por, gbv, gbr, None)
        S.barrier()

    def proj_tok(self, k0col, nrows, panels):
        S = self.S
        banks = []
        for pi in range(0, len(panels), 2):
            pt, pr = self.ps()
            grp = panels[pi:pi + 2]

            def f(e, pt=pt, grp=grp):
                ins = None
                for gi, (wv, wr, n) in enumerate(grp):
                    for kc in range(8):
                        ins = e.matmul(pt[:nrows, gi * 256:gi * 256 + n], self.UT[:, kc, k0col:k0col + nrows], wv[:, kc, :], start=(kc == 0), stop=(kc == 7))
                return ins
            S.op("pe", [self.UTR] + [g[1] for g in grp], [pr], f)
            banks.append((pt, pr))
        return banks

    def load_panels(self, l, offs):
        I = self.I
        out = []
        for off, n in offs:
            wv, wr = self.load_w(I["w_in"][l][:, off:off + n], 8, n)
            out.append((wv, wr, n))
        return out

    def to_ht(self, mixer, HB, hbr, nrows, col0):
        S = self.S
        pt, pr = self.ps()
        pv = pt[:, :].bitcast(BF16)

        def f(e):
            ins = None
            for g in range(2):
                ins = e.transpose(out=pv[:, g * 128:g * 128 + nrows], in_=HB[:nrows, g * 128:(g + 1) * 128], identity=self.IDB[:nrows, :nrows])
            return ins
        S.op("pe", [hbr, self.CONSTR], [pr], f)
        S.op("act", [pr], [self.HTR[mixer]],
             lambda e: e.copy(out=self.HT[:, mixer * 2:mixer * 2 + 2, col0:col0 + nrows], in_=pv[:, 0:256].rearrange("p (g c) -> p g c", g=2)[:, :, 0:nrows]))

    def mixers(self, l):
        self.mix_ret(l)
        self.stage_end("ret")
        self.mix_fox(l)
        self.stage_end("fox")
        self.mix_conv(l)
        self.stage_end("conv")
        self.mix_diff(l)
        self.stage_end("diff")

    def mix_ret(self, l):
        S, u, I, O, cfg = self.S, self.u, self.I, self.O, self.cfg
        kind = u["kind"]
        pan = self.load_panels(l, [(OFF_RET + i * 256, 256) for i in range(4)])
        rk = 0 if kind == "p" else 1
        RD = self.RDEC
        S32 = self.SCR[0:64, 0:256]
        s32r = Res("s32")
        SBF = self.SCR[0:64, 256:384].bitcast(BF16)
        sbfr = Res("sbf")
        S.barrier()
        if kind == "p":
            seqlist = [(0, [(i, 64) for i in range(cfg.T // 64)])]
        else:
            seqlist = [(j, [(0, cfg.TS)]) for j in range(cfg.NS)]
        for sl, chunks in seqlist:
            seqg = u["seqs"][sl]
            if kind == "p":
                S.op("dve", [], [s32r], lambda e: e.memset(S32, 0.0))
            else:
                S.dma("sp", S32.rearrange("d (h e) -> d h e", h=4), I["sret"][l, sl].rearrange("h d e -> d h e"), [], [s32r], s32r)
            S.op("act", [s32r], [sbfr], lambda e: e.copy(out=SBF, in_=S32))
            for ci, cl in chunks:
                col0 = (ci * 64) if kind == "p" else sl * cfg.TS
                tab = ci if kind == "p" else cfg.T // 64
                (pA, pAr), (pB, pBr) = self.proj_tok(col0, cl, pan)
                RT = self.TMPA[0:64, 0:512].rearrange("p (a c) -> p a c", a=2)
                S.dma("sp", RT, I["ret_rope"][tab], [], [self.TMPAR], self.TMPAR)
                QK = self.TMPB[0:64, 0:512]
                A3 = pA[:cl, :].rearrange("p (h x) -> p h x", h=8)
                C3 = RT[:cl, 0, :].rearrange("p (h x) -> p h x", h=8)
                S3 = RT[:cl, 1, :].rearrange("p (h x) -> p h x", h=8)
                Q3 = QK[:cl, :].rearrange("p (h x) -> p h x", h=8)
                T1 = self.TMPC[:cl, 0:256].rearrange("p (h x) -> p h x", h=8)
                T2 = self.TMPC[:cl, 256:512].rearrange("p (h x) -> p h x", h=8)
                rd1 = [pAr, self.TMPAR]
                S.op("dve", rd1, [self.TMPCR], lambda e, A3=A3, C3=C3, T1=T1: e.tensor_tensor(out=T1, in0=A3[:, :, 0:32], in1=C3, op=ALU.mult))
                S.op("dve", rd1, [self.TMPCR], lambda e, A3=A3, S3=S3, T2=T2: e.tensor_tensor(out=T2, in0=A3[:, :, 32:64], in1=S3, op=ALU.mult))
                S.op("dve", [self.TMPCR], [self.TMPBR], lambda e, Q3=Q3, T1=T1, T2=T2: e.tensor_tensor(out=Q3[:, :, 0:32], in0=T1, in1=T2, op=ALU.subtract))
                S.op("dve", rd1, [self.TMPCR], lambda e, A3=A3, C3=C3, T1=T1: e.tensor_tensor(out=T1, in0=A3[:, :, 32:64], in1=C3, op=ALU.mult))
                S.op("dve", rd1, [self.TMPCR], lambda e, A3=A3, S3=S3, T2=T2: e.tensor_tensor(out=T2, in0=A3[:, :, 0:32], in1=S3, op=ALU.mult))
                S.op("dve", [self.TMPCR], [self.TMPBR], lambda e, Q3=Q3, T1=T1, T2=T2: e.tensor_tensor(out=Q3[:, :, 32:64], in0=T1, in1=T2, op=ALU.add))
                VB = self.TMPB[0:64, 512:768]
                KD = self.TMPB[0:64, 768:1024]
                SGt = self.TMPA[0:64, 512:768]
                S.op("act", [pBr], [self.TMPBR], lambda e, pB=pB, cl=cl: e.copy(out=VB[:cl, :], in_=pB[:cl, 0:256]))
                S.op("act", [pBr], [self.TMPAR], lambda e, pB=pB, cl=cl: e.activation(out=SGt[:cl, :], in_=pB[:cl, 256:512], func=AF.Silu))
                S.op("dve", [self.TMPBR, self.CONSTR], [self.TMPBR], lambda e, cl=cl: e.tensor_tensor(out=KD[:cl, :].rearrange("p (h x) -> p h x", h=4), in0=QK[:cl, 256:512].rearrange("p (h x) -> p h x", h=4),
                                                                                                        in1=self.RDECS[:cl, rk, 0, :].unsqueeze(2).to_broadcast([cl, 4, 64]), op=ALU.mult))
                pT, pTr = self.ps()
                pTv = pT[:, :].bitcast(BF16)

                def ft(e, pTv=pTv, cl=cl):
                    ins = None
                    for j in range(8):
                        ins = e.transpose(out=pTv[0:64, j * 64:j * 64 + cl], in_=QK[:cl, j * 64:(j + 1) * 64], identity=self.IDB[:cl, :cl])
                    return ins
                S.op("pe", [self.TMPBR, self.CONSTR], [pTr], ft)
                QKT = self.SCR[0:64, 384:640].bitcast(BF16).rearrange("p (j c) -> p j c", j=8)
                qktr = getattr(self, "_qktr", None) or Res("qkt")
                self._qktr = qktr
                QDT = self.SCR[0:64, 640:768].bitcast(BF16).rearrange("p (j c) -> p j c", j=4)
                S.op("act", [pTr], [qktr], lambda e, pTv=pTv, cl=cl: e.copy(out=QKT[:, :, 0:cl], in_=pTv[0:64, 0:512].rearrange("p (j c) -> p j c", j=8)[:, :, 0:cl]))
                S.op("dve", [qktr, self.CONSTR], [qktr],
                     lambda e, cl=cl: e.tensor_tensor(out=QDT[:, :, 0:cl], in0=QKT[:, 0:4, 0:cl], in1=RD[:, rk, 1, :].rearrange("p (h c) -> p h c", h=4)[:, :, 0:cl], op=ALU.mult))
                pS, pSr = self.ps()

                def fs(e, pS=pS, cl=cl):
                    ins = None
                    for h in range(4):
                        ins = e.matmul(pS[:cl, h * 64:h * 64 + cl], QKT[:, 4 + h, 0:cl], QKT[:, h, 0:cl], start=True, stop=True)
                    return ins
                S.op("pe", [qktr], [pSr], fs)
                STB = self.SCR[0:64, 768:896].bitcast(BF16).rearrange("p (h c) -> p h c", h=4)
                stbr = getattr(self, "_stbr", None) or Res("stb")
                self._stbr = stbr
                S.op("dve", [pSr, self.CONSTR], [stbr],
                     lambda e, pS=pS, cl=cl: e.tensor_tensor(out=STB[:cl, :, 0:cl], in0=pS[:cl, 0:256].rearrange("p (h c) -> p h c", h=4)[:, :, 0:cl],
                                                             in1=RD[:cl, rk, 0, :].rearrange("p (h c) -> p h c", h=4)[:, :, 0:cl], op=ALU.mult))
                pK, pKr = self.ps()

                def fk(e, pK=pK, cl=cl):
                    ins = None
                    for h in range(4):
                        ins = e.matmul(pK[0:64, h * 64:(h + 1) * 64], KD[:cl, h * 64:(h + 1) * 64], VB[:cl, h * 64:(h + 1) * 64], start=True, stop=True)
                    return ins
                S.op("pe", [self.TMPBR], [pKr], fk)
                pY, pYr = self.ps()

                def fy(e, pY=pY, cl=cl):
                    ins = None
                    for h in range(4):
                        e.matmul(pY[:cl, h * 64:(h + 1) * 64], STB[:cl, h, 0:cl], VB[:cl, h * 64:(h + 1) * 64], start=True, stop=False)
                        ins = e.matmul(pY[:cl, h * 64:(h + 1) * 64], QDT[:, h, 0:cl], SBF[:, h * 64:(h + 1) * 64], start=False, stop=True)
                    return ins
                S.op("pe", [stbr, self.TMPBR, qktr, sbfr], [pYr], fy)
                S.op("dve", [s32r, self.CONSTR], [s32r], lambda e: e.tensor_tensor(out=S32.rearrange("p (h x) -> p h x", h=4), in0=S32.rearrange("p (h x) -> p h x", h=4),
                                                                                      in1=self.RDECS[:, rk, 1, :].unsqueeze(2).to_broadcast([64, 4, 64]), op=ALU.mult))
                S.op("dve", [s32r, pKr], [s32r], lambda e, pK=pK: e.tensor_tensor(out=S32, in0=S32, in1=pK[0:64, 0:256], op=ALU.add))
                S.op("act", [s32r], [sbfr], lambda e: e.copy(out=SBF, in_=S32))
                Y32 = self.TMPA[0:64, 768:1024]
                S.op("act", [pYr], [self.TMPAR], lambda e, pY=pY, cl=cl: e.copy(out=Y32[:cl, :], in_=pY[:cl, 0:256]))
                HB = self.TMPB[0:64, 0:256]
                self.group_norm(Y32, self.TMPAR, cl, 4, 64, True, SGt, HB, self.TMPBR, None, 1.0)
                self.to_ht(0, HB, self.TMPBR, cl, col0)
            dst = O[("p" if kind == "p" else "s") + "_ret"][l, u["ui"] if kind == "p" else sl].rearrange("h d e -> d h e")
            S.dma("sp", dst, S32.rearrange("d (h e) -> d h e", h=4), [s32r], [], s32r, is_out=True)

    def group_norm(self, Y, yr, nrows, G, Dg, center, MUL, OUT, outr, gain_bc, const):
        S = self.S
        SM, R = self.SM, self.SMR
        Y3 = Y[:nrows, 0:G * Dg].rearrange("p (g d) -> p g d", g=G)
        SQ = self.TMPC[:nrows, 0:G * Dg]
        SQ3 = SQ.rearrange("p (g d) -> p g d", g=G)
        s1, s2, mean, var, rstd = (SM[:nrows, 40:40 + G], SM[:nrows, 44:44 + G], SM[:nrows, 48:48 + G], SM[:nrows, 52:52 + G], SM[:nrows, 56:56 + G])
        S.op("dve", [yr], [self.TMPCR], lambda e: e.tensor_tensor(out=SQ, in0=Y[:nrows, 0:G * Dg], in1=Y[:nrows, 0:G * Dg], op=ALU.mult))
        S.op("dve", [self.TMPCR], [R], lambda e: e.tensor_reduce(out=s2, in_=SQ3, axis=AX.X, op=ALU.add))
        if center:
            S.op("dve", [yr], [R], lambda e: e.tensor_reduce(out=s1, in_=Y3, axis=AX.X, op=ALU.add))
            S.op("dve", [R], [R], lambda e: e.tensor_scalar(out=mean, in0=s1, scalar1=1.0 / Dg, scalar2=None, op0=ALU.mult))
            S.op("dve", [R], [R], lambda e: e.tensor_tensor(out=s1, in0=mean, in1=mean, op=ALU.mult))
            S.op("dve", [R], [R], lambda e: e.scalar_tensor_tensor(out=var, in0=s2, scalar=1.0 / Dg, in1=s1, op0=ALU.mult, op1=ALU.subtract))
        else:
            S.op("dve", [R], [R], lambda e: e.tensor_scalar(out=var, in0=s2, scalar1=1.0 / Dg, scalar2=None, op0=ALU.mult))
        S.op("act", [R], [R], lambda e: e.activation(out=rstd, in_=var, func=AF.Sqrt, bias=self.EPSC[:nrows, :], scale=1.0))
        S.op("dve", [R], [R], lambda e: e.reciprocal(out=rstd, in_=rstd))
        if center:
            S.op("dve", [yr, R], [self.TMPCR], lambda e: e.tensor_tensor(out=SQ3, in0=Y3, in1=mean.unsqueeze(2).to_broadcast([nrows, G, Dg]), op=ALU.subtract))
            src, srcr = SQ3, self.TMPCR
        else:
            src, srcr = Y3, yr
        S.op("dve", [srcr, R], [self.TMPCR], lambda e: e.tensor_tensor(out=SQ3, in0=src, in1=rstd.unsqueeze(2).to_broadcast([nrows, G, Dg]), op=ALU.mult))
        O3 = OUT[:nrows, 0:G * Dg].rearrange("p (g d) -> p g d", g=G)
        if gain_bc is not None:
            S.op("dve", [self.TMPCR, self.CONSTR], [self.TMPCR], lambda e: e.tensor_tensor(out=SQ3, in0=SQ3, in1=gain_bc, op=ALU.mult))
        if MUL is not None:
            S.op("dve", [self.TMPCR, yr], [outr], lambda e: e.tensor_tensor(out=OUT[:nrows, 0:G * Dg], in0=SQ, in1=MUL[:nrows, 0:G * Dg], op=ALU.mult))
        else:
            S.op("dve", [self.TMPCR], [outr], lambda e: e.tensor_scalar(out=OUT[:nrows, 0:G * Dg], in0=SQ, scalar1=const, scalar2=None, op0=ALU.mult))

    def attn_buffers(self):
        NK = self.NKT * 128
        KT = self.SCR[:, 0:NK].bitcast(BF16).rearrange("p (g c) -> p g c", g=2)
        VA = self.SCR[:, NK:NK + self.NKT * 130].bitcast(BF16).rearrange("p (t h x) -> p t h x", t=self.NKT, h=4)
        base = NK + self.NKT * 130
        FC = self.SCR[:, base:base + self.NKT * 4].rearrange("p (t h) -> p t h", h=4)
        FT = self.SCR[:, base + self.NKT * 4:base + self.NKT * 8].rearrange("p (t h) -> p t h", h=4)
        LF = self.SCR[:, base + self.NKT * 8:base + self.NKT * 12].rearrange("p (t h) -> p t h", h=4)
        assert base + self.NKT * 12 <= 5120, base + self.NKT * 12
        return KT, VA, FC, FT, LF

    def seq_iter(self):
        u, cfg = self.u, self.cfg
        if u["kind"] == "p":
            return [(0, u["ui"], [(i, i * 128, 128) for i in range(self.NT)], 0)]
        return [(j, j, [(j, 0, cfg.TS)], cfg.PAST // 128) for j in range(cfg.NS)]

    def k_transpose(self, KTOK, ktokr, nrows, KT, ktr, kcol0):
        S = self.S
        pt, pr = self.ps()
        pv = pt[:, :].bitcast(BF16)

        def f(e):
            ins = None
            for g in range(2):
                ins = e.transpose(out=pv[:, g * 128:g * 128 + nrows], in_=KTOK[:nrows, g * 128:(g + 1) * 128], identity=self.IDB[:nrows, :nrows])
            return ins
        S.op("pe", [ktokr, self.CONSTR], [pr], f)
        S.op("act", [pr], [ktr], lambda e: e.copy(out=KT[:, :, kcol0:kcol0 + nrows], in_=pv[:, 0:256].rearrange("p (g c) -> p g c", g=2)[:, :, 0:nrows]))

    def mix_fox(self, l):
        S, u, I, O, cfg = self.S, self.u, self.I, self.O, self.cfg
        kind = u["kind"]
        pre = "p" if kind == "p" else "s"
        pan = self.load_panels(l, [(OFF_FOX, 256), (OFF_FOX + 256, 256), (OFF_FOX + 512, 256), (OFF_FOX_F, 4)])
        S.barrier()
        KT, VA, FC, FT, LF = self.attn_buffers()
        ktr, var, fr = Res("kt"), Res("va"), Res("f")
        SM = self.SM
        for sl, so, newtiles, npast in self.seq_iter():
            S.op("dve", [], [var], lambda e: e.memset(VA[:, :, :, 64:65], 1.0))
            if npast:
                S.dma("sp", LF[:, 0:npast, :], I["cfl"][l, sl].rearrange("(t p) h -> p t h", p=128), [], [fr], fr)
            for kt in range(npast):
                KTOK = self.TMPB[:, 0:256]
                S.dma("pool", KTOK, I["cfk"][l, sl, kt * 128:(kt + 1) * 128, :], [], [self.TMPBR], self.TMPBR)
                self.k_transpose(KTOK, self.TMPBR, 128, KT, ktr, kt * 128)
                S.dma("pool", VA[:, kt, :, 0:64], I["cfv"][l, sl, kt * 128:(kt + 1) * 128, :].rearrange("t (h x) -> t h x", h=4), [], [var], var)
                self.fcum_step(kt, 128, FC, FT, LF, fr)
            for qi, (k, t0, nr) in enumerate(newtiles):
                kt = npast + qi
                col0 = k * u["rows"]
                (pA, pAr), (pB, pBr) = self.proj_tok(col0, nr, pan)
                st, str_ = self.stg()
                S.op("act", [pAr], [str_], lambda e, st=st, pA=pA, nr=nr: e.copy(out=st[:nr, 0:256], in_=pA[:nr, 256:512]))
                S.op("act", [pBr], [str_], lambda e, st=st, pB=pB, nr=nr: e.copy(out=st[:nr, 256:512], in_=pB[:nr, 0:256]))
                S.dma("sp", O[pre + "_fox_k"][l, so, t0:t0 + nr, :], st[:nr, 0:256], [str_], [], str_, is_out=True)
                S.dma("sp", O[pre + "_fox_v"][l, so, t0:t0 + nr, :], st[:nr, 256:512], [str_], [], str_, is_out=True)
                self.stage_end("fox_out")
                QKB = self.TMPB[:, 0:512]
                S.op("dve", [pAr], [self.TMPBR], lambda e, pA=pA, nr=nr: e.tensor_scalar(out=QKB[:nr, :], in0=pA[:nr, :], scalar1=1.0, scalar2=None, op0=ALU.mult))
                self.stage_end("fox_q1")
                S.op("dve", [pBr], [var], lambda e, pB=pB, nr=nr, kt=kt: e.tensor_copy(out=VA[:nr, kt, :, 0:64], in_=pB[:nr, 0:256].rearrange("p (h x) -> p h x", h=4)))
                self.stage_end("fox_q2")
                self.k_transpose(QKB[:, 256:512], self.TMPBR, nr, KT, ktr, kt * 128)
                self.stage_end("fox_q3")
                QT = self.TMPB[:, 512:768].rearrange("p (g c) -> p g c", g=2)
                qtr = self.TMPBR
                pt, pr = self.ps()
                pv = pt[:, :].bitcast(BF16)

                def fq(e, pv=pv, nr=nr):
                    ins = None
                    for g in range(2):
                        ins = e.transpose(out=pv[:, g * 128:g * 128 + nr], in_=QKB[:nr, g * 128:(g + 1) * 128], identity=self.IDB[:nr, :nr])
                    return ins
                S.op("pe", [self.TMPBR, self.CONSTR], [pr], fq)
                S.op("act", [pr], [self.TMPBR], lambda e, pv=pv, nr=nr: e.copy(out=QT[:, :, 0:nr], in_=pv[:, 0:256].rearrange("p (g c) -> p g c", g=2)[:, :, 0:nr]))
                self.stage_end("fox_qk")
                S.op("dve", [pBr, self.CONSTR], [self.SMR], lambda e, pB=pB, nr=nr: e.tensor_tensor(out=SM[:nr, 0:4], in0=pB[:nr, 256:260], in1=self.BFF[:nr, l, :], op=ALU.add))
                S.op("act", [self.SMR], [self.SMR], lambda e, nr=nr: e.activation(out=SM[:nr, 4:8], in_=SM[:nr, 0:4], func=AF.Exp, scale=-1.0))
                S.op("act", [self.SMR], [self.SMR], lambda e, nr=nr: e.activation(out=SM[:nr, 8:12], in_=SM[:nr, 4:8], func=AF.Ln, bias=self.ONEC[:nr, :], scale=1.0))
                S.op("dve", [self.SMR], [fr], lambda e, nr=nr, kt=kt: e.tensor_scalar(out=LF[:nr, kt, :], in0=SM[:nr, 8:12], scalar1=-1.0, scalar2=None, op0=ALU.mult))
                S.dma("sp", O[pre + "_fox_logf"][l, so, t0:t0 + nr, :], LF[:nr, kt, :], [fr], [], fr, is_out=True)
                self.stage_end("fox_lf")
                self.fcum_step(kt, nr, FC, FT, LF, fr)
                self.stage_end("fox_fcum")
                HB = self.TMPB[:, 768:1024]
                for h in range(4):
                    hp, g = (h % 2) * 64, h // 2
                    pY, pYr = self.ps(hold=True)
                    for kk in range(kt + 1):
                        nk = 128 if kk < kt else nr
                        pS, pSr = self.ps()
                        S.op("dve", [fr], [self.SMR], lambda e, nk=nk, kk=kk, kt=kt, h=h: e.tensor_tensor(out=SM[:nk, 16:17], in0=FT[:nk, kt, h:h + 1], in1=FC[:nk, kk, h:h + 1], op=ALU.subtract))
                        S.op("pe", [ktr, self.TMPBR], [pSr],
                             lambda e, pS=pS, nk=nk, kk=kk, hp=hp, g=g, nr=nr: e.matmul(pS[:nk, 0:nr], KT[hp:hp + 64, g, kk * 128:kk * 128 + nk], QT[hp:hp + 64, g, 0:nr], start=True, stop=True))
                        PT = self.TMPC[:, 0:64].bitcast(BF16)
                        S.op("act", [pSr, self.SMR], [self.TMPCR], lambda e, pS=pS, nk=nk, nr=nr, PT=PT: e.activation(out=PT[:nk, 0:nr], in_=pS[:nk, 0:nr], func=AF.Exp, bias=SM[:nk, 16:17], scale=0.125))
                        self.stage_end("fox_exp")
                        if kk == kt:
                            S.op("dve", [self.TMPCR, self.CONSTR], [self.TMPCR], lambda e, nk=nk, nr=nr, PT=PT: e.tensor_tensor(out=PT[:nk, 0:nr], in0=PT[:nk, 0:nr], in1=self.CMASK[:nk, 0:nr], op=ALU.mult))
                        S.op("pe", [self.TMPCR, var], [pYr],
                             lambda e, pY=pY, nk=nk, nr=nr, kk=kk, h=h, kt=kt, PT=PT: e.matmul(pY[:nr, 0:65], PT[:nk, 0:nr], VA[:nk, kk, h, :], start=(kk == 0), stop=(kk == kt)))
                    self.stage_end("fox_pv")
                    S.op("dve", [pYr], [self.SMR], lambda e, pY=pY, nr=nr: e.reciprocal(out=SM[:nr, 20:21], in_=pY[:nr, 64:65]))
                    S.op("act", [pYr, self.SMR], [self.TMPBR], lambda e, pY=pY, nr=nr, h=h: e.activation(out=HB[:nr, h * 64:(h + 1) * 64], in_=pY[:nr, 0:64], func=AF.Copy, scale=SM[:nr, 20:21]))
                    self.ps_release(pYr)
                    self.stage_end("fox_head")
                self.to_ht(1, HB, self.TMPBR, nr, col0)

    def fcum_step(self, kt, nr, FC, FT, LF, fr):
        S = self.S
        pt, pr = self.ps()

        def f(e):
            e.matmul(pt[:, 0:4], self.ONESF[:nr, :], LF[:nr, kt, :], start=True, stop=True)
            return e.matmul(pt[:nr, 4:8], self.UTRI[:nr, :nr], LF[:nr, kt, :], start=True, stop=True)
        S.op("pe", [fr, self.CONSTR], [pr], f)
        if kt == 0:
            S.op("dve", [pr], [fr], lambda e: e.tensor_copy(out=FT[:, kt, :], in_=pt[:, 0:4]))
            S.op("dve", [pr], [fr], lambda e: e.tensor_copy(out=FC[:nr, kt, :], in_=pt[:nr, 4:8]))
        else:
            S.op("dve", [pr, fr], [fr], lambda e: e.tensor_tensor(out=FT[:, kt, :], in0=pt[:, 0:4], in1=FT[:, kt - 1, :], op=ALU.add))
            S.op("dve", [pr, fr], [fr], lambda e: e.tensor_tensor(out=FC[:nr, kt, :], in0=pt[:nr, 4:8], in1=FT[:nr, kt - 1, :], op=ALU.add))

    def mix_conv(self, l):
        S, u, I, O, cfg = self.S, self.u, self.I, self.O, self.cfg
        kind = u["kind"]
        pre = "p" if kind == "p" else "s"
        pan = self.load_panels(l, [(OFF_CONV, 256), (OFF_CONV + 256, 256)])
        S.barrier()
        XP = self.SCR[:, 0:2 * 158].rearrange("p (g c) -> p g c", g=2)
        xpr = Res("xp")
        CO = self.SCR[:, 320:320 + 256].rearrange("p (g c) -> p g c", g=2)
        cor = Res("co")
        CS = self.SCR[:, 576:576 + 256].rearrange("p (g c) -> p g c", g=2)
        csr = Res("cs")
        ST = self.SCR[:, 832:832 + 512]
        str2 = Res("cst")
        for sl, so, newtiles, npast in self.seq_iter():
            if kind == "p":
                S.op("dve", [], [xpr], lambda e: e.memset(XP[:, :, 0:30], 0.0))
            else:
                for g in range(2):
                    S.dma("sp", XP[:, g, 0:30], I["sconv"][l, sl][:, g * 128:(g + 1) * 128].rearrange("t c -> c t"), [], [xpr], xpr, allow_slow_non_contiguous=True)
                S.dma("sp", O["s_conv"][l, so, 0:30 - cfg.TS, :], I["sconv"][l, sl, cfg.TS:30, :], [], [], Res("d2d"), is_out=True)
            for qi, (k, t0, nr) in enumerate(newtiles):
                col0 = k * u["rows"]
                ((pA, pAr),) = self.proj_tok(col0, nr, pan)
                G32 = self.TMPA[:, 0:256]
                S.op("act", [pAr], [self.TMPAR], lambda e, pA=pA, nr=nr: e.activation(out=G32[:nr, :], in_=pA[:nr, 256:512], func=AF.Sigmoid))
                S.op("dve", [pAr, self.TMPAR], [self.TMPAR], lambda e, pA=pA, nr=nr: e.tensor_tensor(out=G32[:nr, :], in0=pA[:nr, 0:256], in1=G32[:nr, :], op=ALU.mult))
                if kind == "p":
                    lo = max(t0, cfg.T - 30)
                    if t0 + nr > lo:
                        S.dma("sp", O["p_conv"][l, so, lo - (cfg.T - 30):30, :], G32[lo - t0:nr, :], [self.TMPAR], [], self.TMPAR, is_out=True)
                else:
                    S.dma("sp", O["s_conv"][l, so, 30 - cfg.TS:30, :], G32[:nr, :], [self.TMPAR], [], self.TMPAR, is_out=True)
                pt, pr = self.ps()

                def f(e, pt=pt, nr=nr):
                    ins = None
                    for g in range(2):
                        ins = e.transpose(out=pt[:, g * 128:g * 128 + nr], in_=G32[:nr, g * 128:(g + 1) * 128], identity=self.IDF[:nr, :nr])
                    return ins
                S.op("pe", [self.TMPAR, self.CONSTR], [pr], f)
                if qi > 0:
                    pnr = newtiles[qi - 1][2]
                    S.op("dve", [xpr], [self.TMPCR], lambda e, pnr=pnr: e.tensor_copy(out=self.TMPC[:, 0:60].rearrange("p (g c) -> p g c", g=2), in_=XP[:, :, pnr:pnr + 30]))
                    S.op("dve", [self.TMPCR], [xpr], lambda e: e.tensor_copy(out=XP[:, :, 0:30], in_=self.TMPC[:, 0:60].rearrange("p (g c) -> p g c", g=2)))
                S.op("act", [pr], [xpr], lambda e, pt=pt, nr=nr: e.copy(out=XP[:, :, 30:30 + nr], in_=pt[:, 0:256].rearrange("p (g c) -> p g c", g=2)[:, :, 0:nr]))
                for g in range(2):
                    S.op("dve", [xpr, self.CONSTR], [cor],
                         lambda e, g=g, nr=nr: e.tensor_scalar(out=CO[:, g, 0:nr], in0=XP[:, g, 0:nr], scalar1=self.WCV[:, l, g, 0:1], scalar2=self.CVP[:, l, 0, g:g + 1], op0=ALU.mult, op1=ALU.add))
                    for w in range(1, CONV_W):
                        S.op("dve", [xpr, self.CONSTR, cor], [cor],
                             lambda e, g=g, nr=nr, w=w: e.scalar_tensor_tensor(out=CO[:, g, 0:nr], in0=XP[:, g, w:w + nr], scalar=self.WCV[:, l, g, w:w + 1], in1=CO[:, g, 0:nr],
                                                                                op0=ALU.mult, op1=ALU.add))
                S.op("dve", [cor], [csr], lambda e, nr=nr: e.tensor_tensor(out=CS[:, :, 0:nr], in0=CO[:, :, 0:nr], in1=CO[:, :, 0:nr], op=ALU.mult))
                ps1, ps1r = self.ps()

                def fs(e, ps1=ps1, nr=nr):
                    e.matmul(ps1[:, 0:nr], self.ONESF[:, :], CO[:, 0, 0:nr], start=True, stop=False)
                    e.matmul(ps1[:, 0:nr], self.ONESF[:, :], CO[:, 1, 0:nr], start=False, stop=True)
                    e.matmul(ps1[:, 128:128 + nr], self.ONESF[:, :], CS[:, 0, 0:nr], start=True, stop=False)
                    return e.matmul(ps1[:, 128:128 + nr], self.ONESF[:, :], CS[:, 1, 0:nr], start=False, stop=True)
                S.op("pe", [cor, csr, self.CONSTR], [ps1r], fs)
                MEAN, MSQ, VAR, RSTD = ST[:, 0:128], ST[:, 128:256], ST[:, 256:384], ST[:, 384:512]
                S.op("dve", [ps1r], [str2], lambda e, ps1=ps1, nr=nr: e.tensor_scalar(out=MEAN[:, 0:nr], in0=ps1[:, 0:nr], scalar1=1.0 / 256, scalar2=None, op0=ALU.mult))
                S.op("dve", [str2], [str2], lambda e, nr=nr: e.tensor_tensor(out=MSQ[:, 0:nr], in0=MEAN[:, 0:nr], in1=MEAN[:, 0:nr], op=ALU.mult))
                S.op("dve", [ps1r, str2], [str2], lambda e, ps1=ps1, nr=nr: e.scalar_tensor_tensor(out=VAR[:, 0:nr], in0=ps1[:, 128:128 + nr], scalar=1.0 / 256, in1=MSQ[:, 0:nr], op0=ALU.mult, op1=ALU.subtract))
                S.op("act", [str2], [str2], lambda e, nr=nr: e.activation(out=RSTD[:, 0:nr], in_=VAR[:, 0:nr], func=AF.Sqrt, bias=self.EPSC[:, :], scale=1.0))
                S.op("dve", [str2], [str2], lambda e, nr=nr: e.reciprocal(out=RSTD[:, 0:nr], in_=RSTD[:, 0:nr]))
                for g in range(2):
                    S.op("dve", [cor, str2], [csr], lambda e, g=g, nr=nr: e.tensor_tensor(out=CS[:, g, 0:nr], in0=CO[:, g, 0:nr], in1=MEAN[:, 0:nr], op=ALU.subtract))
                    S.op("dve", [csr, str2], [csr], lambda e, g=g, nr=nr: e.tensor_tensor(out=CS[:, g, 0:nr], in0=CS[:, g, 0:nr], in1=RSTD[:, 0:nr], op=ALU.mult))
                    S.op("act", [csr, self.CONSTR], [self.HTR[2]],
                         lambda e, g=g, nr=nr, col0=col0: e.activation(out=self.HT[:, 4 + g, col0:col0 + nr], in_=CS[:, g, 0:nr], func=AF.Silu,
                                                                      scale=self.CVP[:, l, 1, g:g + 1], bias=self.CVP[:, l, 2, g:g + 1]))

    def mix_diff(self, l):
        S, u, I, O, cfg = self.S, self.u, self.I, self.O, self.cfg
        kind = u["kind"]
        pre = "p" if kind == "p" else "s"
        lam_init = 0.8 - 0.6 * math.exp(-0.3 * l)
        pan = self.load_panels(l, [(OFF_DIFF, 256), (OFF_DIFF + 256, 256), (OFF_DIFF + 512, 256)])
        S.barrier()
        KT, VA, FC, FT, LF = self.attn_buffers()
        ktr, var = Res("kt"), Res("va")
        SM = self.SM
        for sl, so, newtiles, npast in self.seq_iter():
            S.op("dve", [], [var], lambda e: e.memset(VA[:, :, :, 64:65], 1.0))
            for kt in range(npast):
                KTOK = self.TMPB[:, 0:256]
                S.dma("pool", KTOK, I["cdk"][l, sl, kt * 128:(kt + 1) * 128, :], [], [self.TMPBR], self.TMPBR)
                self.k_transpose(KTOK, self.TMPBR, 128, KT, ktr, kt * 128)
                S.dma("pool", VA[:, kt, :, 0:64], I["cdv"][l, sl, kt * 128:(kt + 1) * 128, :].rearrange("t (h x) -> t h x", h=4), [], [var], var)
            for qi, (k, t0, nr) in enumerate(newtiles):
                kt = npast + qi
                col0 = k * u["rows"]
                tab = (t0 // 128) if kind == "p" else self.NT
                (pA, pAr), (pB, pBr) = self.proj_tok(col0, nr, pan)
                RT = self.TMPA[:, 0:128].rearrange("p (a c) -> p a c", a=2)
                S.dma("sp", RT, I["diff_rope"][tab], [], [self.TMPAR], self.TMPAR)
                R32 = self.TMPA[:, 128:640]
                A3 = pA[:nr, :].rearrange("p (n x) -> p n x", n=16)
                R3 = R32[:nr, :].rearrange("p (n x) -> p n x", n=16)
                C3 = RT[:nr, 0, :].rearrange("p (n x) -> p n x", n=16)
                S3 = RT[:nr, 1, :].rearrange("p (n x) -> p n x", n=16)
                T1 = self.TMPC[:nr, 0:64].rearrange("p (n x) -> p n x", n=16)
                T2 = self.TMPC[:nr, 64:128].rearrange("p (n x) -> p n x", n=16)
                rd1 = [pAr, self.TMPAR]
                S.op("act", [pAr], [self.TMPAR], lambda e, pA=pA, nr=nr: e.copy(out=R32[:nr, :], in_=pA[:nr, :]))
                S.op("dve", rd1, [self.TMPCR], lambda e, A3=A3, C3=C3, T1=T1: e.tensor_tensor(out=T1, in0=A3[:, :, 0:4], in1=C3, op=ALU.mult))
                S.op("dve", rd1, [self.TMPCR], lambda e, A3=A3, S3=S3, T2=T2: e.tensor_tensor(out=T2, in0=A3[:, :, 4:8], in1=S3, op=ALU.mult))
                S.op("dve", [self.TMPCR, self.TMPAR], [self.TMPAR], lambda e, R3=R3, T1=T1, T2=T2: e.tensor_tensor(out=R3[:, :, 0:4], in0=T1, in1=T2, op=ALU.subtract))
                S.op("dve", rd1, [self.TMPCR], lambda e, A3=A3, C3=C3, T1=T1: e.tensor_tensor(out=T1, in0=A3[:, :, 4:8], in1=C3, op=ALU.mult))
                S.op("dve", rd1, [self.TMPCR], lambda e, A3=A3, S3=S3, T2=T2: e.tensor_tensor(out=T2, in0=A3[:, :, 0:4], in1=S3, op=ALU.mult))
                S.op("dve", [self.TMPCR, self.TMPAR], [self.TMPAR], lambda e, R3=R3, T1=T1, T2=T2: e.tensor_tensor(out=R3[:, :, 4:8], in0=T1, in1=T2, op=ALU.add))
                st, str_ = self.stg()
                S.op("act", [pBr], [str_], lambda e, st=st, pB=pB, nr=nr: e.copy(out=st[:nr, 0:256], in_=pB[:nr, 0:256]))
                S.dma("sp", O[pre + "_diff_k"][l, so, t0:t0 + nr, :], R32[:nr, 256:512], [self.TMPAR], [], self.TMPAR, is_out=True)
                S.dma("sp", O[pre + "_diff_v"][l, so, t0:t0 + nr, :], st[:nr, 0:256], [str_], [], str_, is_out=True)
                QA = self.TMPB[:, 0:256]
                QB = self.TMPB[:, 256:512]
                KB = self.TMPB[:, 512:768]
                S.op("dve", [], [self.TMPBR], lambda e: e.memset(self.TMPB[:, 0:512], 0.0))
                R8 = R32[:nr, 0:256].rearrange("p (n t x) -> p n t x", n=4, t=2)
                S.op("dve", [self.TMPAR], [self.TMPBR], lambda e, nr=nr, R8=R8: e.tensor_copy(out=QA[:nr, :].rearrange("p (n t x) -> p n t x", n=4, t=2)[:, :, 0, :], in_=R8[:, :, 0, :]))
                S.op("dve", [self.TMPAR], [self.TMPBR], lambda e, nr=nr, R8=R8: e.tensor_copy(out=QB[:nr, :].rearrange("p (n t x) -> p n t x", n=4, t=2)[:, :, 1, :], in_=R8[:, :, 1, :]))
                S.op("dve", [self.TMPAR], [self.TMPBR], lambda e, nr=nr: e.tensor_copy(out=KB[:nr, :], in_=R32[:nr, 256:512]))
                S.op("dve", [pBr], [var], lambda e, pB=pB, nr=nr, kt=kt: e.tensor_copy(out=VA[:nr, kt, :, 0:64], in_=pB[:nr, 0:256].rearrange("p (h x) -> p h x", h=4)))
                self.k_transpose(KB, self.TMPBR, nr, KT, ktr, kt * 128)
                QT = self.TMPB[:, 768:1024].rearrange("p (a c) -> p a c", a=2)
                QTS = self.SCR[:, 4608:4608 + 256].bitcast(BF16).rearrange("p (v g c) -> p v g c", v=2, g=2)
                qtsr = getattr(self, "_qtsr", None) or Res("qts")
                self._qtsr = qtsr
                for v_, QX in enumerate((QA, QB)):
                    pt, pr = self.ps()
                    pv = pt[:, :].bitcast(BF16)

                    def fq(e, pv=pv, nr=nr, QX=QX):
                        ins = None
                        for g in range(2):
                            ins = e.transpose(out=pv[:, g * 128:g * 128 + nr], in_=QX[:nr, g * 128:(g + 1) * 128], identity=self.IDB[:nr, :nr])
                        return ins
                    S.op("pe", [self.TMPBR, self.CONSTR], [pr], fq)
                    S.op("act", [pr], [qtsr], lambda e, pv=pv, nr=nr, v_=v_: e.copy(out=QTS[:, v_, :, 0:nr], in_=pv[:, 0:256].rearrange("p (g c) -> p g c", g=2)[:, :, 0:nr]))
                Y32 = self.TMPA[:, 640:896]
                for h in range(4):
                    ys = []
                    for sub in range(2):
                        n = 2 * h + sub
                        g = n // 4
                        base = ((n % 4) // 2) * 64
                        pY, pYr = self.ps(hold=True)
                        for kk in range(kt + 1):
                            nk = 128 if kk < kt else nr
                            pS, pSr = self.ps()
                            S.op("pe", [ktr, qtsr], [pSr],
                                 lambda e, pS=pS, nk=nk, kk=kk, base=base, g=g, nr=nr, sub=sub: e.matmul(pS[:nk, 0:nr], KT[base:base + 64, g, kk * 128:kk * 128 + nk], QTS[base:base + 64, sub, g, 0:nr], start=True, stop=True))
                            PT = self.TMPC[:, 128:192].bitcast(BF16)
                            S.op("act", [pSr], [self.TMPCR], lambda e, pS=pS, nk=nk, nr=nr, PT=PT: e.activation(out=PT[:nk, 0:nr], in_=pS[:nk, 0:nr], func=AF.Exp, scale=32.0 ** -0.5))
                            if kk == kt:
                                S.op("dve", [self.TMPCR, self.CONSTR], [self.TMPCR], lambda e, nk=nk, nr=nr, PT=PT: e.tensor_tensor(out=PT[:nk, 0:nr], in0=PT[:nk, 0:nr], in1=self.DMASK[:nk, 0:nr], op=ALU.mult))
                            S.op("pe", [self.TMPCR, var], [pYr],
                                 lambda e, pY=pY, nk=nk, nr=nr, kk=kk, h=h, kt=kt, PT=PT: e.matmul(pY[:nr, 0:65], PT[:nk, 0:nr], VA[:nk, kk, h, :], start=(kk == 0), stop=(kk == kt)))
                        ys.append((pY, pYr))
                    (p0, p0r), (p1, p1r) = ys
                    S.op("dve", [p0r], [self.SMR], lambda e, p0=p0, nr=nr: e.reciprocal(out=SM[:nr, 20:21], in_=p0[:nr, 64:65]))
                    S.op("dve", [p1r], [self.SMR], lambda e, p1=p1, nr=nr: e.reciprocal(out=SM[:nr, 21:22], in_=p1[:nr, 64:65]))
                    S.op("dve", [self.SMR, self.CONSTR], [self.SMR], lambda e, nr=nr: e.tensor_tensor(out=SM[:nr, 22:23], in0=SM[:nr, 21:22], in1=self.LAM[:nr, l, 1:2], op=ALU.mult))
                    T64 = self.TMPC[:, 256:320]
                    S.op("act", [p0r, self.SMR], [self.TMPCR], lambda e, p0=p0, nr=nr: e.activation(out=T64[:nr, :], in_=p0[:nr, 0:64], func=AF.Copy, scale=SM[:nr, 20:21]))
                    S.op("dve", [p1r, self.SMR, self.TMPCR], [self.TMPAR],
                         lambda e, p1=p1, nr=nr, h=h: e.scalar_tensor_tensor(out=Y32[:nr, h * 64:(h + 1) * 64], in0=p1[:nr, 0:64], scalar=SM[:nr, 22:23], in1=T64[:nr, :], op0=ALU.mult, op1=ALU.add))
                    self.ps_release(p0r)
                    self.ps_release(p1r)
                HB = self.TMPB[:, 0:256]
                gbc = self.SUBG[:nr, l, :].unsqueeze(1).to_broadcast([nr, 4, 64])
                self.group_norm(Y32, self.TMPAR, nr, 4, 64, False, None, HB, self.TMPBR, gbc, 1.0 - lam_init)
                self.to_ht(3, HB, self.TMPBR, nr, col0)


def _shard_inputs(cfg, ncores, inp):
    NP, NS = cfg.NP, cfg.NS
    hc = host_consts(cfg)
    f32 = lambda a: np.ascontiguousarray(np.asarray(a, dtype=np.float32))
    shared = {}
    shared["w_in"] = f32(inp["w_in"])
    shared["b_fox_f"] = f32(inp["b_fox_f"])
    wc = f32(inp["w_conv"])
    shared["wconvT"] = np.ascontiguousarray(wc.reshape(2, 31, 2, 128).transpose(0, 3, 2, 1))
    cp = np.stack([f32(inp["b_conv"]), f32(inp["conv_ln_g"]), f32(inp["conv_ln_b"])], axis=1)
    shared["convp"] = np.ascontiguousarray(cp.reshape(2, 3, 2, 128).transpose(0, 3, 1, 2))
    shared["diff_lambda"] = f32(inp["diff_lambda"]).reshape(2, 128)
    shared["diff_subln_g"] = f32(inp["diff_subln_g"])
    shared["w_branch"] = f32(inp["w_branch"])
    shared["w_out"] = f32(inp["w_out"])
    shared["w_ada"] = f32(inp["w_ada"])
    ba = f32(inp["b_ada"])
    shared["b_ada"] = ba
    shared["b_adaT"] = np.ascontiguousarray(ba.reshape(4, 24, 128).transpose(2, 0, 1))
    shared["ln_g"] = f32(inp["ln_g"])
    shared["ln_b"] = f32(inp["ln_b"])
    shared["w_ffn_in"] = f32(inp["w_ffn_in"])
    shared["w_ffn_out"] = f32(inp["w_ffn_out"])
    shared["w_router"] = np.ascontiguousarray(f32(inp["w_router"])[0].reshape(8, 128, 8).transpose(1, 0, 2))
    shared["b_router"] = f32(inp["b_router"])
    shared["w_exp_in"] = f32(inp["w_exp_in"])
    shared["w_exp_out"] = f32(inp["w_exp_out"])
    for k, v in hc.items():
        shared[k] = v
    maps = []
    for c in range(ncores):
        m = dict(shared)
        m["xp"] = f32(inp["x_prompt"][c * NP:(c + 1) * NP])
        m["xs"] = f32(inp["x_sample"][c * NS:(c + 1) * NS])
        call = np.concatenate([f32(inp["c_prompt"][c * NP:(c + 1) * NP]), f32(inp["c_sample"][c * NS:(c + 1) * NS])], axis=0)
        m["cT"] = np.ascontiguousarray(call.reshape(cfg.NSEQ, 8, 128).transpose(2, 1, 0))
        sl = slice(c * NS, (c + 1) * NS)
        m["cfk"] = f32(np.asarray(inp["cache_fox_k"])[:, sl]).reshape(2, NS, cfg.PAST, 256)
        m["cfv"] = f32(np.asarray(inp["cache_fox_v"])[:, sl]).reshape(2, NS, cfg.PAST, 256)
        m["cfl"] = f32(np.asarray(inp["cache_fox_logf"])[:, sl])
        m["cdk"] = f32(np.asarray(inp["cache_diff_k"])[:, sl]).reshape(2, NS, cfg.PAST, 256)
        m["cdv"] = f32(np.asarray(inp["cache_diff_v"])[:, sl]).reshape(2, NS, cfg.PAST, 256)
        m["sret"] = f32(np.asarray(inp["state_ret"])[:, sl])
        m["sconv"] = f32(np.asarray(inp["state_conv"])[:, sl])
        maps.append(m)
    return maps


def _gather(cfg, ncores, results):
    def cat(name, axis):
        return np.concatenate([np.asarray(r[name]) for r in results], axis=axis)
    B, T, TS = cfg.NP * ncores, cfg.T, cfg.TS
    BS = cfg.NS * ncores
    outs = [cat("yp", 0), cat("ys", 0)]
    for pre, nb, t in (("p", B, T), ("s", BS, TS)):
        outs += [cat(pre + "_fox_k", 1).reshape(2, nb, t, 4, 64), cat(pre + "_fox_v", 1).reshape(2, nb, t, 4, 64),
                 cat(pre + "_fox_logf", 1), cat(pre + "_diff_k", 1).reshape(2, nb, t, 8, 32),
                 cat(pre + "_diff_v", 1).reshape(2, nb, t, 4, 64), cat(pre + "_ret", 1), cat(pre + "_conv", 1)]
    return tuple(np.ascontiguousarray(o, dtype=np.float32) for o in outs)


def run(cfg, ncores, inp):
    b = Builder(cfg)
    nc = b.build()
    maps = _shard_inputs(cfg, ncores, inp)
    res = run_bass_kernel_spmd(nc, maps, core_ids=list(range(ncores)))
    return _gather(cfg, ncores, res.results)


def kernel(**inputs):
    ncores = 8
    B, T = inputs["x_prompt"].shape[0], inputs["x_prompt"].shape[1]
    BS, TS = inputs["x_sample"].shape[0], inputs["x_sample"].shape[1]
    PAST = inputs["cache_fox_k"].shape[2]
    cfg = Cfg(B // ncores, T, BS // ncores, TS, PAST)
    return run(cfg, ncores, inputs)
```

```python
import math
from contextlib import ExitStack
import numpy as np
import ml_dtypes
import concourse.bass as bass
import concourse.mybir as mybir
from concourse.bass_utils import run_bass_kernel_spmd

F32 = mybir.dt.float32
BF16 = mybir.dt.bfloat16
AF = mybir.ActivationFunctionType
ALU = mybir.AluOpType
AX = mybir.AxisListType

D = 1024
DEPTH = 2
BW = 256
HD = 64
CONV_W = 31
D_FF = 2816
N_EXP = 8
D_EXP = 3584
ALPHA = (2.0 * DEPTH) ** 0.25
EPS = 1e-5
OFF_RET = 0
OFF_FOX = 1024
OFF_FOX_F = 1792
OFF_CONV = 1796
OFF_DIFF = 2308
OFF_GATE = 3076
N_IN = 7172
SEM_CAP = 30000


class Res:
    __slots__ = ("w", "r", "sem", "cnt", "name", "uid", "excl")
    _n = 0

    def __init__(self, name="", excl=False):
        Res._n += 1
        self.uid = Res._n
        self.excl = excl
        self.w = None
        self.r = {}
        self.sem = None
        self.cnt = 0
        self.name = name


class Sched:
    def __init__(self, nc, es):
        self.nc = nc
        self.es = es
        self.names = ["pe", "act", "dve", "pool", "sp"]
        self.ops = {k: [] for k in self.names}
        self.count = {k: 0 for k in self.names}
        self.clock = {k: {} for k in self.names}
        self.esems = {k: [] for k in self.names}
        self.nsem = 0
        self.const_res = Res("const")
        self.out_events = {}

    def new_sem(self, nm):
        self.nsem += 1
        return self.es.enter_context(self.nc.semaphore("s_%s_%d" % (nm, self.nsem)))

    def esem(self, e, idx):
        while len(self.esems[e]) <= idx:
            self.esems[e].append(self.new_sem(e))
        return self.esems[e][idx]

    def _deps(self, e, reads, writes):
        deps = {}

        def add(key, val):
            if deps.get(key, 0) < val:
                deps[key] = val

        for r in reads:
            if r.w is not None:
                add(*r.w)
            if r.excl:
                for k, v in r.r.items():
                    if k != ("e", e):
                        add(k, v)
        for w in writes:
            if w.w is not None:
                add(*w.w)
            for k, v in w.r.items():
                add(k, v)
        clk = self.clock[e]
        waits = []
        for key, val in deps.items():
            if key[0] == "e" and key[1] == e and e == "pe":
                continue
            if clk.get(key, 0) >= val:
                continue
            clk[key] = val
            waits.append((key, val))
        return waits

    def _mark(self, ev, reads, writes):
        key, val = ev
        for r in reads:
            if r.r.get(key, 0) < val:
                r.r[key] = val
        for w in writes:
            w.w = ev
            w.r = {}

    def op(self, e, reads, writes, fn):
        waits = self._deps(e, reads, writes)
        self.count[e] += 1
        ev = (("e", e), self.count[e])
        self.ops[e].append((waits, fn, ("e", e, self.count[e])))
        self._mark(ev, reads, writes)

    def dma(self, e, out, in_, reads, writes, sres, is_out=False, **kw):
        waits = self._deps(e, reads, writes)
        if sres.sem is None:
            sres.sem = self.new_sem("d")
        sres.cnt += 16
        key = ("d", sres.uid)
        self.semmap[key] = sres.sem
        ev = (key, sres.cnt)

        def fn(eng, out=out, in_=in_, kw=kw):
            return eng.dma_start(out=out, in_=in_, **kw)

        self.ops[e].append((waits, fn, ("d", sres.sem, 16)))
        self._mark(ev, reads, writes)
        self.dma_latest[key] = sres.cnt
        if is_out:
            self.out_events[key] = sres.cnt

    semmap = {}
    dma_latest = {}

    def barrier(self):
        for e in self.names:
            waits = []
            for o in ["pe", "act", "dve", "pool"]:
                if o == e or self.count[o] == 0:
                    continue
                key = ("e", o)
                if self.clock[e].get(key, 0) < self.count[o]:
                    self.clock[e][key] = self.count[o]
                    waits.append((key, self.count[o]))
            for key, val in self.dma_latest.items():
                if self.clock[e].get(key, 0) < val:
                    self.clock[e][key] = val
                    waits.append((key, val))
            if waits:
                self.ops[e].append((waits, None, None))

    def finish(self):
        waits = [(k, v) for k, v in self.out_events.items()]
        self.ops["sp"].append((waits, None, None))

    def _emit_wait(self, eng, key, val):
        if key[0] == "e":
            idx = (val - 1) // SEM_CAP
            eng.wait_ge(self.esem(key[1], idx), val - idx * SEM_CAP)
        else:
            eng.wait_ge(self.semmap[key], val)

    def emit(self, block):
        engs = {"pe": block.tensor, "act": block.scalar, "dve": block.vector,
                "pool": block.gpsimd, "sp": block.sync}
        for name in self.names:
            ops = self.ops[name]

            def body(eng, ops=ops, name=name):
                for waits, fn, inc in ops:
                    for key, val in waits:
                        self._emit_wait(eng, key, val)
                    if fn is None:
                        continue
                    ins = fn(eng)
                    if inc[0] == "e":
                        cnt = inc[2]
                        idx = (cnt - 1) // SEM_CAP
                        ins.then_inc(self.esem(name, idx), 1)
                    else:
                        ins.then_inc(inc[1], 16)

            engs[name](body)


class StopBuild(Exception):
    pass


MAX_STAGE = [10 ** 9]
STOP_NAME = [None]


class Cfg:
    def __init__(self, NP, T, NS, TS, PAST):
        self.NP, self.T, self.NS, self.TS, self.PAST = NP, T, NS, TS, PAST
        self.NSEQ = NP + NS


def host_consts(cfg):
    c = {}
    T, TS, PAST = cfg.T, cfg.TS, cfg.PAST
    c["ident_f"] = np.eye(128, dtype=np.float32)
    c["ones_f"] = np.ones((128, 128), np.float32)
    idx = np.arange(128)
    c["utri_f"] = (idx[:, None] <= idx[None, :]).astype(np.float32)
    c["cmask"] = (idx[:, None] <= idx[None, :]).astype(np.float32)
    c["dmask"] = ((idx[:, None] // 64) <= (idx[None, :] // 64)).astype(np.float32)
    half = 32
    inv = np.exp(-math.log(10000.0) * np.arange(half, dtype=np.float32) / half).astype(np.float32)

    def ret_tab(pos):
        ang = pos.astype(np.float32)[:, None] * inv[None, :]
        cos = np.cos(ang).astype(np.float32)
        sin = np.sin(ang).astype(np.float32)
        tab = np.zeros((len(pos), 2, 8, 32), np.float32)
        tab[:, 0, 0:4] = cos[:, None, :]
        tab[:, 1, 0:4] = sin[:, None, :]
        tab[:, 0, 4:8] = cos[:, None, :] * 0.125
        tab[:, 1, 4:8] = sin[:, None, :] * 0.125
        return tab.reshape(len(pos), 2, 256)

    ntp = T // 64
    rt = np.zeros((ntp + 1, 64, 2, 256), np.float32)
    for i in range(ntp):
        rt[i] = ret_tab(np.arange(i * 64, (i + 1) * 64))
    rt[ntp, :TS] = ret_tab(PAST + np.arange(TS))
    c["ret_rope"] = rt
    inv_d = np.exp(-math.log(500000.0) * np.arange(4, dtype=np.float32) / 4).astype(np.float32)

    def diff_tab(pos):
        ang = pos.astype(np.float32)[:, None] * inv_d[None, :]
        cos = np.cos(ang).astype(np.float32)
        sin = np.sin(ang).astype(np.float32)
        tab = np.zeros((len(pos), 2, 16, 4), np.float32)
        tab[:, 0] = cos[:, None, :]
        tab[:, 1] = sin[:, None, :]
        return tab.reshape(len(pos), 2, 64)

    nt = T // 128
    dt_ = np.zeros((nt + 1, 128, 2, 64), np.float32)
    for i in range(nt):
        dt_[i] = diff_tab(np.arange(i * 128, (i + 1) * 128))
    dt_[nt, :TS] = diff_tab(PAST + np.arange(TS))
    c["diff_rope"] = dt_
    log_g = np.log1p(-np.exp2(-5.0 - np.arange(4, dtype=np.float32))).astype(np.float32)
    rd = np.zeros((2, 64, 2, 4, 64), np.float32)
    rs = np.zeros((2, 64, 2, 4), np.float32)
    for kind, L in ((0, min(T, 64)), (1, TS)):
        ii = np.arange(L, dtype=np.float32)
        inner = np.exp(log_g[:, None, None] * np.abs(ii[:, None] - ii[None, :])).astype(np.float32)
        qdec = np.exp(log_g[:, None] * (ii[None, :] + 1.0)).astype(np.float32)
        kdec = np.exp(log_g[:, None] * (L - 1.0 - ii[None, :])).astype(np.float32)
        cdec = np.exp(log_g * L).astype(np.float32)
        for h in range(4):
            rd[kind, :L, 0, h, :L] = inner[h].T
            rd[kind, :, 1, h, :L] = qdec[h][None, :]
            rs[kind, :L, 0, h] = kdec[h]
            rs[kind, :, 1, h] = cdec[h]
    c["ret_dec"] = rd.reshape(2, 64, 2, 256)
    c["ret_decs"] = rs
    return c


CONST_SHAPES = None


class Builder:
    def __init__(self, cfg, dbg=False):
        self.cfg = cfg
        self.dbg = dbg

    def build(self):
        cfg = self.cfg
        NP, T, NS, TS, PAST = cfg.NP, cfg.T, cfg.NS, cfg.TS, cfg.PAST
        NSEQ = cfg.NSEQ
        nc = bass.Bass("TRN2", target_bir_lowering=False)
        self.nc = nc
        self.es = ExitStack()
        es = self.es
        S = Sched(nc, es)
        Sched.semmap = {}
        Sched.dma_latest = {}
        self.S = S

        def din(name, shape, dt=F32):
            return nc.dram_tensor(name, list(shape), dt, kind="ExternalInput").ap()

        def dout(name, shape):
            return nc.dram_tensor(name, list(shape), F32, kind="ExternalOutput").ap()

        I = {}
        I["xp"] = din("xp", [NP, T, D])
        I["xs"] = din("xs", [NS, TS, D])
        I["cT"] = din("cT", [128, 8, NSEQ])
        for nm in ("cfk", "cfv", "cdk", "cdv"):
            I[nm] = din(nm, [2, NS, PAST, 256])
        I["cfl"] = din("cfl", [2, NS, PAST, 4])
        I["sret"] = din("sret", [2, NS, 4, 64, 64])
        I["sconv"] = din("sconv", [2, NS, 30, 256])
        I["w_in"] = din("w_in", [2, D, N_IN])
        I["b_fox_f"] = din("b_fox_f", [2, 4])
        I["wconvT"] = din("wconvT", [2, 128, 2, 31])
        I["convp"] = din("convp", [2, 128, 3, 2])
        I["diff_lambda"] = din("diff_lambda", [2, 128])
        I["diff_subln_g"] = din("diff_subln_g", [2, 64])
        I["w_branch"] = din("w_branch", [2, 4, 256, D])
        I["w_out"] = din("w_out", [2, D, D])
        I["w_ada"] = din("w_ada", [2, 2, D, 3 * D])
        I["b_adaT"] = din("b_adaT", [128, 4, 24])
        I["b_ada"] = din("b_ada", [2, 2, 3 * D])
        I["ln_g"] = din("ln_g", [2, 2, D])
        I["ln_b"] = din("ln_b", [2, 2, D])
        I["w_ffn_in"] = din("w_ffn_in", [1, D, 2 * D_FF])
        I["w_ffn_out"] = din("w_ffn_out", [1, D_FF, D])
        I["w_router"] = din("w_router", [128, 8, 8])
        I["b_router"] = din("b_router", [1, 8])
        I["w_exp_in"] = din("w_exp_in", [1, N_EXP, D, 2 * D_EXP])
        I["w_exp_out"] = din("w_exp_out", [1, N_EXP, D_EXP, D])
        hc = host_consts(cfg)
        for k, v in hc.items():
            I[k] = din(k, list(v.shape))
        self.I = I
        O = {}
        O["yp"] = dout("yp", [NP, T, D])
        O["ys"] = dout("ys", [NS, TS, D])
        for pre, n, t in (("p", NP, T), ("s", NS, TS)):
            O[pre + "_fox_k"] = dout(pre + "_fox_k", [2, n, t, 256])
            O[pre + "_fox_v"] = dout(pre + "_fox_v", [2, n, t, 256])
            O[pre + "_fox_logf"] = dout(pre + "_fox_logf", [2, n, t, 4])
            O[pre + "_diff_k"] = dout(pre + "_diff_k", [2, n, t, 256])
            O[pre + "_diff_v"] = dout(pre + "_diff_v", [2, n, t, 256])
            O[pre + "_ret"] = dout(pre + "_ret", [2, n, 4, 64, 64])
            O[pre + "_conv"] = dout(pre + "_conv", [2, n, 30, 256])
        self.O = O

        NT = T // 128
        self.NT = NT
        TT = max(NT, 2 * NS)
        NCOL = max(T, NS * TS)
        self.NCOL = NCOL
        NKT = max(NT, PAST // 128 + 1)
        self.NKT = NKT
        NK = NKT * 128

        def sb(name, shape, dt=F32):
            return es.enter_context(nc.sbuf_tensor(name, list(shape), dt))

        self.ACC = sb("ACC", [128, TT, D])
        self.ACCR = [Res("acc%d" % i) for i in range(TT)]
        self.UT = sb("UT", [128, 8, NCOL], BF16)
        self.UTR = Res("ut")
        self.HT = sb("HT", [128, 8, NCOL], BF16)
        self.HTR = [Res("ht%d" % i) for i in range(4)]
        self.NWS = 5
        self.WS = sb("WS", [128, self.NWS, 2048], BF16)
        self.WSR = [Res("ws%d" % i) for i in range(self.NWS)]
        self.wsi = 0
        self.SCR = sb("SCR", [128, 5120])
        self.MODT = sb("MODT", [128, 4, 16, NSEQ])
        self.MODR = Res("modt")
        self.BADA = sb("BADA", [128, 4, 24])
        self.SCT = sb("SCT", [128, 8, NSEQ], BF16)
        self.SCTR = Res("sct")
        self.SCREP = sb("SCREP", [128, 8, 128], BF16)
        self.SCREPR = Res("screp")
        self.GB0 = sb("GB0", [128, D])
        self.GBR = [Res("gb%d" % i) for i in range(max(NS, 1))]
        self.ROWA = sb("ROWA", [128, D])
        self.ROWAR = Res("rowa")
        self.ROWB = sb("ROWB", [128, D])
        self.ROWBR = Res("rowb")
        self.IDF = sb("IDF", [128, 128])
        self.IDB = sb("IDB", [128, 128], BF16)
        self.ONESF = sb("ONESF", [128, 128])
        self.UTRI = sb("UTRI", [128, 128])
        self.CMASK = sb("CMASK", [128, 128], BF16)
        self.DMASK = sb("DMASK", [128, 128], BF16)
        self.RDEC = sb("RDEC", [64, 2, 2, 256])
        self.RDECS = sb("RDECS", [64, 2, 2, 4])
        self.CONSTR = Res("consts")
        self.WCV = sb("WCV", [128, 2, 2, 31])
        self.CVP = sb("CVP", [128, 2, 3, 2])
        self.BFF = sb("BFF", [128, 2, 4])
        self.LAM = sb("LAM", [128, 2, 4])
        self.SUBG = sb("SUBG", [128, 2, 64])
        self.WRT = sb("WRT", [128, 8, 8])
        self.BRT = sb("BRT", [128, 8])
        self.STG = sb("STG", [128, 2, 512])
        self.STGR = [Res("stg0"), Res("stg1")]
        self.stgi = 0
        self.TMPA = sb("TMPA", [128, 1024])
        self.TMPAR = Res("tmpa")
        self.TMPB = sb("TMPB", [128, 1024], BF16)
        self.TMPBR = Res("tmpb")
        self.TMPC = sb("TMPC", [128, 512])
        self.TMPCR = Res("tmpc")
        self.EPSC = sb("EPSC", [128, 1])
        self.ONEC = sb("ONEC", [128, 1])
        self.SM = sb("SM", [128, 64])
        self.SMR = Res("sm")
        self.PTB = sb("PTB", [128, 4, 128], BF16)
        self.PTR = [Res("pt%d" % i) for i in range(4)]
        self.pti = 0
        self.PSB = []
        for i in range(8):
            t = es.enter_context(nc.psum_tensor("PS%d" % i, [128, 512], F32))
            self.PSB.append((t, Res("ps%d" % i, excl=True)))
        self.psi = 0

        self.stage = 0
        try:
            self.load_consts()
            self.stage_end("consts")
            self.ada_precompute()
            self.stage_end("ada")
            units = [("p", i) for i in range(NP)] + ([("s", 0)] if NS > 0 else [])
            for kind, i in units:
                self.run_unit(kind, i)
        except StopBuild:
            pass
        S.finish()
        with nc.Block() as block:
            S.emit(block)
        return nc

    def stage_end(self, name):
        self.stage += 1
        if self.stage >= MAX_STAGE[0] or name == STOP_NAME[0]:
            print("STOP after stage", self.stage, name)
            raise StopBuild()

    def ps(self, hold=False):
        held = self.__dict__.setdefault("ps_held", set())
        while self.psi in held:
            self.psi = (self.psi + 1) % 8
        i = self.psi
        self.psi = (self.psi + 1) % 8
        if hold:
            held.add(i)
        return self.PSB[i]

    def pt_next(self):
        i = self.pti
        self.pti = (i + 1) % 4
        return self.PTB[:, i, :], self.PTR[i]

    def ps_release(self, pr):
        for i, (t, r) in enumerate(self.PSB):
            if r is pr:
                self.ps_held.discard(i)

    def stg(self):
        i = self.stgi
        self.stgi = 1 - i
        return self.STG[:, i, :], self.STGR[i]

    def load_w(self, src2d, nk, ncols, k0=0):
        i = self.wsi
        self.wsi = (self.wsi + 1) % self.NWS
        res = self.WSR[i]
        assert res.w is None or res.r, "WS slot %d reloaded before its consumers were emitted" % i
        view = self.WS[:, i, 0:nk * ncols].rearrange("p (k c) -> p k c", k=nk)
        src = src2d[k0 * 128:(k0 + nk) * 128, :].rearrange("(k p) c -> p k c", p=128)
        self.S.dma("pool", view, src, [], [res], res)
        return view, res

    def cdma(self, out, in_, **kw):
        self.S.dma("sp", out, in_, [], [self.CONSTR], self.CONSTR, **kw)

    def load_consts(self):
        I = self.I
        S = self.S
        self.cdma(self.IDF[:, :], I["ident_f"][:, :])
        self.cdma(self.ONESF[:, :], I["ones_f"][:, :])
        self.cdma(self.UTRI[:, :], I["utri_f"][:, :])
        self.cdma(self.RDEC[:, :, :, :], I["ret_dec"].rearrange("k p w c -> p k w c"))
        self.cdma(self.RDECS[:, :, :, :], I["ret_decs"].rearrange("k p w h -> p k w h"))
        self.cdma(self.WCV[:, :, :, :], I["wconvT"].rearrange("l p g w -> p l g w"))
        self.cdma(self.CVP[:, :, :, :], I["convp"].rearrange("l p a g -> p l a g"))
        self.cdma(self.BFF[:, :, :], I["b_fox_f"].rearrange("l h -> (l h)").partition_broadcast(128).rearrange("p (l h) -> p l h", l=2))
        self.DLAM = self.TMPA[:, 0:256].rearrange("p (l c) -> p l c", l=2)
        S.dma("sp", self.DLAM, I["diff_lambda"].rearrange("l c -> (l c)").partition_broadcast(128).rearrange("p (l c) -> p l c", l=2), [], [self.TMPAR], self.TMPAR)
        self.cdma(self.SUBG[:, :, :], I["diff_subln_g"].rearrange("l c -> (l c)").partition_broadcast(128).rearrange("p (l c) -> p l c", l=2))
        self.cdma(self.WRT[:, :, :], I["w_router"][:, :, :])
        self.cdma(self.BRT[:, :], I["b_router"].rearrange("a e -> (a e)").partition_broadcast(128))
        self.cdma(self.BADA[:, :, :], I["b_adaT"][:, :, :])
        self.CONSTP = Res("constp")
        S.dma("pool", self.CMASK[:, :], I["cmask"][:, :], [], [self.CONSTP], self.CONSTP)
        S.dma("pool", self.DMASK[:, :], I["dmask"][:, :], [], [self.CONSTP], self.CONSTP)
        S.dma("pool", self.IDB[:, :], I["ident_f"][:, :], [], [self.CONSTP], self.CONSTP)
        S.op("dve", [self.CONSTP], [self.CONSTR], lambda e: e.memset(self.EPSC[:, :], EPS))
        S.op("dve", [], [self.CONSTR], lambda e: e.memset(self.EPSC[:, :], EPS))
        S.op("dve", [], [self.CONSTR], lambda e: e.memset(self.ONEC[:, :], 1.0))
        C = self.CONSTR
        for l in range(2):
            lam_init = 0.8 - 0.6 * math.exp(-0.3 * l)
            DL, LAM, TM = self.DLAM, self.LAM, self.SM
            S.op("dve", [C, self.TMPAR], [self.SMR], lambda e, l=l: e.tensor_tensor(out=TM[:, 0:32], in0=DL[:, l, 0:32], in1=DL[:, l, 32:64], op=ALU.mult))
            S.op("dve", [self.SMR], [self.SMR], lambda e, l=l: e.tensor_reduce(out=TM[:, 32:33], in_=TM[:, 0:32], axis=AX.X, op=ALU.add))
            S.op("dve", [C, self.TMPAR], [self.SMR], lambda e, l=l: e.tensor_tensor(out=TM[:, 0:32], in0=DL[:, l, 64:96], in1=DL[:, l, 96:128], op=ALU.mult))
            S.op("dve", [self.SMR], [self.SMR], lambda e, l=l: e.tensor_reduce(out=TM[:, 33:34], in_=TM[:, 0:32], axis=AX.X, op=ALU.add))
            S.op("act", [self.SMR], [self.SMR], lambda e, l=l: e.activation(out=TM[:, 34:36], in_=TM[:, 32:34], func=AF.Exp))
            S.op("dve", [self.SMR], [self.SMR], lambda e, l=l: e.tensor_tensor(out=TM[:, 36:37], in0=TM[:, 34:35], in1=TM[:, 35:36], op=ALU.subtract))
            S.op("dve", [self.SMR], [C], lambda e, l=l, li=lam_init: e.tensor_scalar(out=LAM[:, l, 0:1], in0=TM[:, 36:37], scalar1=li, scalar2=None, op0=ALU.add))
            S.op("dve", [C], [C], lambda e, l=l: e.tensor_scalar(out=LAM[:, l, 1:2], in0=LAM[:, l, 0:1], scalar1=-1.0, scalar2=None, op0=ALU.mult))

    def ada_precompute(self):
        S, I = self.S, self.I
        NSEQ = self.cfg.NSEQ
        CT = self.TMPA[:, 0:8 * NSEQ].rearrange("p (k s) -> p k s", k=8)
        S.dma("sp", CT, I["cT"][:, :, :], [], [self.TMPAR], self.TMPAR)
        S.op("act", [self.TMPAR], [self.SCTR], lambda e: e.activation(out=self.SCT[:, :, :], in_=CT, func=AF.Silu))
        BT = self.BADA
        for l in range(2):
            for s in range(2):
                ls = l * 2 + s
                pt, pr = self.ps()
                for j in range(16):
                    if j % 2 == 0:
                        wv, wr = self.load_w(I["w_ada"][l, s][:, (j // 2) * 256:(j // 2 + 1) * 256], 8, 256)

                    def f(e, j=j, wv=wv, pt=pt):
                        ins = None
                        for kc in range(8):
                            ins = e.matmul(pt[:, j * NSEQ:(j + 1) * NSEQ], wv[:, kc, (j % 2) * 128:(j % 2 + 1) * 128],
                                           self.SCT[:, kc, :], start=(kc == 0), stop=(kc == 7))
                        return ins
                    S.op("pe", [wr, self.SCTR], [pr], f)
                    S.op("act", [pr, self.CONSTR], [self.MODR],
                         lambda e, j=j, ls=ls, pt=pt: e.activation(out=self.MODT[:, ls, j, :], in_=pt[:, j * NSEQ:(j + 1) * NSEQ],
                                                                   func=AF.Identity, bias=BT[:, ls, j:j + 1], scale=1.0))
                S.op("dve", [self.MODR], [self.MODR],
                     lambda e, ls=ls: e.tensor_scalar(out=self.MODT[:, ls, 8:16, :], in0=self.MODT[:, ls, 8:16, :], scalar1=1.0, scalar2=None, op0=ALU.add))

    def gate_rows(self, l, s, seqs, gbviews):
        S, I = self.S, self.I
        S.dma("sp", self.ROWA[:, :], I["b_ada"][l, s, 2 * D:3 * D].partition_broadcast(128), [], [self.ROWAR], self.ROWAR)
        for si, seqg in enumerate(seqs):
            gbv, gbr = gbviews[si]
            S.op("dve", [self.SCTR], [self.SCREPR],
                 lambda e, seqg=seqg: e.tensor_copy(out=self.SCREP[:, :, :], in_=self.SCT[:, :, seqg:seqg + 1].to_broadcast([128, 8, 128])))
            for cb in range(4):
                wv, wr = self.load_w(I["w_ada"][l, s][:, 2 * D + cb * 256:2 * D + (cb + 1) * 256], 8, 256)
                if cb % 2 == 0:
                    pt, pr = self.ps()

                def f(e, wv=wv, pt=pt, cb=cb):
                    ins = None
                    for kc in range(8):
                        ins = e.matmul(pt[:, (cb % 2) * 256:(cb % 2 + 1) * 256], self.SCREP[:, kc, :], wv[:, kc, :],
                                       start=(kc == 0), stop=(kc == 7))
                    return ins
                S.op("pe", [wr, self.SCREPR], [pr], f)
                if cb % 2 == 1:
                    c0 = (cb // 2) * 512
                    S.op("dve", [pr, self.ROWAR], [gbr],
                         lambda e, pt=pt, gbv=gbv, c0=c0: e.tensor_tensor(out=gbv[:, c0:c0 + 512], in0=pt[:, :], in1=self.ROWA[:, c0:c0 + 512], op=ALU.add))

    def run_unit(self, kind, ui):
        cfg, S, I, O = self.cfg, self.S, self.I, self.O
        if kind == "p":
            rows, ntl = 128, self.NT
            seqs = [ui]
            tiles = [(0, i) for i in range(ntl)]
            xsrc = lambda k: I["xp"][ui, k * 128:(k + 1) * 128, :]
            ydst = lambda k: O["yp"][ui, k * 128:(k + 1) * 128, :]
            gbviews = [(self.GB0, self.GBR[0])]
        else:
            rows, ntl = cfg.TS, cfg.NS
            seqs = [cfg.NP + j for j in range(cfg.NS)]
            tiles = [(j, 0) for j in range(ntl)]
            xsrc = lambda k: I["xs"][k, :, :]
            ydst = lambda k: O["ys"][k, :, :]
            gbviews = [(self.ACC[:, cfg.NS + j, :], self.ACCR[cfg.NS + j]) for j in range(cfg.NS)]
        u = dict(kind=kind, ui=ui, rows=rows, ntl=ntl, seqs=seqs, tiles=tiles, ncols=rows * ntl, gb=gbviews)
        self.u = u
        for k in range(ntl):
            S.dma("sp", self.ACC[:rows, k, :], xsrc(k), [], [self.ACCR[k]], self.ACCR[k])
        for l in range(2):
            self.make_uT(l, 0)
            self.stage_end("uT")
            self.gate_rows(l, 0, seqs, gbviews)
            self.stage_end("gate")
            self.mixers(l)
            self.merge_out(l)
            self.stage_end("merge")
            self.layer_norm(l, 0)
            self.stage_end("ln")
            self.make_uT(l, 1, router=(l == 1))
            self.gate_rows(l, 1, seqs, gbviews)
            if l == 0:
                self.ffn(I["w_ffn_in"][0], I["w_ffn_out"][0], D_FF, None)
            else:
                for ex in range(N_EXP):
                    self.ffn(I["w_exp_in"][0, ex], I["w_exp_out"][0, ex], D_EXP, ex)
            self.layer_norm(l, 1)
        for k in range(ntl):
            S.dma("sp", ydst(k), self.ACC[:rows, k, :], [self.ACCR[k]], [], self.ACCR[k], is_out=True)

    def make_uT(self, l, s, router=False):
        S, u = self.S, self.u
        rows = u["rows"]
        ls = l * 2 + s
        if router:
            self.COMB = self.SCR[:, 0:u["ntl"] * 8].rearrange("p (t e) -> p t e", e=8)
            self.COMBR = Res("comb")
        for k, (sl, ti) in enumerate(u["tiles"]):
            seqg = u["seqs"][sl]
            c0 = k * rows
            if router:
                lt, lr = self.ps()
            for half in range(2):
                pt, pr = self.ps()

                def f(e, pt=pt, k=k, half=half):
                    ins = None
                    for j in range(4):
                        c = half * 4 + j
                        ins = e.transpose(out=pt[:, j * 128:j * 128 + rows], in_=self.ACC[:rows, k, c * 128:(c + 1) * 128],
                                          identity=self.IDF[:rows, :rows])
                    return ins
                S.op("pe", [self.ACCR[k], self.CONSTR], [pr], f)
                for j in range(4):
                    c = half * 4 + j
                    if not router:
                        S.op("act", [pr, self.MODR], [self.UTR],
                             lambda e, pt=pt, j=j, c=c, c0=c0, seqg=seqg: e.activation(
                                 out=self.UT[:, c, c0:c0 + rows], in_=pt[:, j * 128:j * 128 + rows], func=AF.Identity,
                                 scale=self.MODT[:, ls, 8 + c, seqg:seqg + 1], bias=self.MODT[:, ls, c, seqg:seqg + 1]))
                    else:
                        S.op("act", [pr, self.MODR], [self.TMPAR],
                             lambda e, pt=pt, j=j, c=c, seqg=seqg: e.activation(
                                 out=self.TMPA[:, c * 128:c * 128 + rows], in_=pt[:, j * 128:j * 128 + rows], func=AF.Identity,
                                 scale=self.MODT[:, ls, 8 + c, seqg:seqg + 1], bias=self.MODT[:, ls, c, seqg:seqg + 1]))
                        S.op("dve", [self.TMPAR], [self.UTR],
                             lambda e, c=c, c0=c0: e.tensor_copy(out=self.UT[:, c, c0:c0 + rows], in_=self.TMPA[:, c * 128:c * 128 + rows]))
            if router:
                def fr(e, lt=lt):
                    ins = None
                    for c in range(8):
                        ins = e.matmul(lt[:rows, 0:8], self.TMPA[:, c * 128:c * 128 + rows], self.WRT[:, c, :], start=(c == 0), stop=(c == 7))
                    return ins
                S.op("pe", [self.TMPAR, self.CONSTR], [lr], fr)
                self.route(k, lt, lr)
            S.op("act", [self.ACCR[k]], [self.ACCR[k]], lambda e, k=k: e.mul(out=self.ACC[:rows, k, :], in_=self.ACC[:rows, k, :], mul=ALPHA))

    def route(self, k, lt, lr):
        S, u = self.S, self.u
        rows = u["rows"]
        SM, R = self.SM, self.SMR
        lg = SM[:rows, 0:8]
        S.op("dve", [lr, self.CONSTR], [R], lambda e: e.tensor_tensor(out=lg, in0=lt[:rows, 0:8], in1=self.BRT[:rows, :], op=ALU.add))
        S.op("dve", [R], [R], lambda e: e.tensor_reduce(out=SM[:rows, 8:9], in_=lg, axis=AX.X, op=ALU.max))
        S.op("dve", [R], [R], lambda e: e.tensor_scalar(out=SM[:rows, 16:24], in0=lg, scalar1=SM[:rows, 8:9], scalar2=None, op0=ALU.is_equal))
        S.op("dve", [R], [R], lambda e: e.scalar_tensor_tensor(out=SM[:rows, 24:32], in0=SM[:rows, 16:24], scalar=-1e30, in1=lg, op0=ALU.mult, op1=ALU.add))
        S.op("dve", [R], [R], lambda e: e.tensor_reduce(out=SM[:rows, 9:10], in_=SM[:rows, 24:32], axis=AX.X, op=ALU.max))
        S.op("dve", [R], [R], lambda e: e.tensor_scalar(out=SM[:rows, 32:40], in0=SM[:rows, 24:32], scalar1=SM[:rows, 9:10], scalar2=None, op0=ALU.is_equal))
        S.op("dve", [R], [R], lambda e: e.tensor_tensor(out=SM[:rows, 10:11], in0=SM[:rows, 9:10], in1=SM[:rows, 8:9], op=ALU.subtract))
        S.op("act", [R], [R], lambda e: e.activation(out=SM[:rows, 11:12], in_=SM[:rows, 10:11], func=AF.Exp))
        S.op("dve", [R], [R], lambda e: e.tensor_scalar(out=SM[:rows, 11:12], in0=SM[:rows, 11:12], scalar1=1.0, scalar2=None, op0=ALU.add))
        S.op("dve", [R], [R], lambda e: e.reciprocal(out=SM[:rows, 12:13], in_=SM[:rows, 11:12]))
        S.op("dve", [R], [R], lambda e: e.tensor_scalar(out=SM[:rows, 13:14], in0=SM[:rows, 12:13], scalar1=-1.0, scalar2=1.0, op0=ALU.mult, op1=ALU.add))
        S.op("dve", [R], [R], lambda e: e.tensor_scalar(out=SM[:rows, 16:24], in0=SM[:rows, 16:24], scalar1=SM[:rows, 12:13], scalar2=None, op0=ALU.mult))
        COMB = self.COMB
        S.op("dve", [R], [self.COMBR], lambda e, k=k: e.scalar_tensor_tensor(out=COMB[:rows, k, :], in0=SM[:rows, 32:40], scalar=SM[:rows, 13:14],
                                                                        in1=SM[:rows, 16:24], op0=ALU.mult, op1=ALU.add))

    def layer_norm(self, l, s):
        S, u, I = self.S, self.u, self.I
        rows = u["rows"]
        S.dma("sp", self.ROWA[:, :], I["ln_g"][l, s, :].partition_broadcast(128), [], [self.ROWAR], self.ROWAR)
        S.dma("sp", self.ROWB[:, :], I["ln_b"][l, s, :].partition_broadcast(128), [], [self.ROWBR], self.ROWBR)
        SM, R = self.SM, self.SMR
        for k in range(u["ntl"]):
            A = self.ACC[:rows, k, :]
            AR = self.ACCR[k]
            S.op("dve", [AR], [R], lambda e, A=A: e.tensor_reduce(out=SM[:rows, 0:1], in_=A, axis=AX.X, op=ALU.add))
            S.op("act", [AR], [self.TMPAR, R], lambda e, A=A: e.activation(out=self.TMPA[:rows, :], in_=A, func=AF.Square, accum_out=SM[:rows, 1:2]))
            S.op("dve", [R], [R], lambda e: e.tensor_scalar(out=SM[:rows, 2:3], in0=SM[:rows, 0:1], scalar1=1.0 / D, scalar2=None, op0=ALU.mult))
            S.op("dve", [R], [R], lambda e: e.tensor_tensor(out=SM[:rows, 3:4], in0=SM[:rows, 2:3], in1=SM[:rows, 2:3], op=ALU.mult))
            S.op("dve", [R], [R], lambda e: e.scalar_tensor_tensor(out=SM[:rows, 4:5], in0=SM[:rows, 1:2], scalar=1.0 / D, in1=SM[:rows, 3:4], op0=ALU.mult, op1=ALU.subtract))
            S.op("act", [R], [R], lambda e: e.activation(out=SM[:rows, 5:6], in_=SM[:rows, 4:5], func=AF.Sqrt, bias=self.EPSC[:rows, :], scale=1.0))
            S.op("dve", [R], [R], lambda e: e.reciprocal(out=SM[:rows, 5:6], in_=SM[:rows, 5:6]))
            S.op("dve", [R], [R], lambda e: e.scalar_tensor_tensor(out=SM[:rows, 6:7], in0=SM[:rows, 2:3], scalar=-1.0, in1=SM[:rows, 5:6], op0=ALU.mult, op1=ALU.mult))
            S.op("act", [AR, R], [AR], lambda e, A=A: e.activation(out=A, in_=A, func=AF.Identity, scale=SM[:rows, 5:6], bias=SM[:rows, 6:7]))
            S.op("dve", [AR, self.ROWAR], [AR], lambda e, A=A: e.tensor_tensor(out=A, in0=A, in1=self.ROWA[:rows, :], op=ALU.mult))
            S.op("dve", [AR, self.ROWBR], [AR], lambda e, A=A: e.tensor_tensor(out=A, in0=A, in1=self.ROWB[:rows, :], op=ALU.add))

    def ffn(self, w_up, w_dn, dff, ex):
        S, u = self.S, self.u
        rows, ntl, ncols = u["rows"], u["ntl"], u["ncols"]
        nch = dff // 128
        G = 4
        HG = self.HT[:, :, :].rearrange("p (b j) c -> p b j c", b=2)
        nblk = (ncols + 511) // 512
        fold = (u["kind"] == "p")
        for g0 in range(0, nch, G):
            gn = min(G, nch - g0)
            hb = self.hgi = (getattr(self, "hgi", -1) + 1) % 2
            hres = [self.HTR[2 * hb], self.HTR[2 * hb + 1]]
            ups, wds = [], []
            for jj in range(0, gn, 2):
                nj = min(2, gn - jj)
                c0 = (g0 + jj) * 128
                wa, war = self.load_w(w_up[:, c0:c0 + nj * 128], 8, nj * 128)
                wg, wgr = self.load_w(w_up[:, dff + c0:dff + c0 + nj * 128], 8, nj * 128)
                ups.append((wa, war, wg, wgr, nj, jj))
            for (wa, war, wg, wgr, nj, jj) in ups:
                for j in range(nj):
                    for b in range(nblk):
                        n = min(512, ncols - b * 512)
                        pa, par = self.ps()
                        pg, pgr = self.ps()

                        def f(e, pa=pa, pg=pg, j=j, b=b, n=n, wa=wa, wg=wg):
                            ins = None
                            for kc in range(8):
                                ins = e.matmul(pa[:, 0:n], wa[:, kc, j * 128:(j + 1) * 128], self.UT[:, kc, b * 512:b * 512 + n], start=(kc == 0), stop=(kc == 7))
                            for kc in range(8):
                                ins = e.matmul(pg[:, 0:n], wg[:, kc, j * 128:(j + 1) * 128], self.UT[:, kc, b * 512:b * 512 + n], start=(kc == 0), stop=(kc == 7))
                            return ins
                        S.op("pe", [war, wgr, self.UTR], [par, pgr], f)
                        S.op("act", [par], [self.TMPCR], lambda e, pa=pa, n=n: e.activation(out=self.TMPC[:, 0:n], in_=pa[:, 0:n], func=AF.Silu))
                        S.op("dve", [pgr, self.TMPCR], hres,
                             lambda e, pg=pg, n=n, hb=hb, jx=jj + j, b=b: e.tensor_tensor(out=HG[:, hb, jx, b * 512:b * 512 + n], in0=pg[:, 0:n], in1=self.TMPC[:, 0:n], op=ALU.mult))
            for jj in range(0, gn, 2):
                nj = min(2, gn - jj)
                wd, wdr = self.load_w(w_dn, nj, 1024, k0=g0 + jj)
                wds.append((wd, wdr, nj))
            if fold:
                gbv0, gbr0 = u["gb"][0]
                for (wd, wdr, nj) in wds:
                    S.op("dve", [wdr, gbr0], [wdr],
                         lambda e, wd=wd, nj=nj, gbv0=gbv0: e.tensor_tensor(out=wd, in0=wd, in1=gbv0[:, :].unsqueeze(1).to_broadcast([128, nj, 1024]), op=ALU.mult))
            for k, (sl, ti) in enumerate(u["tiles"]):
                gbv, gbr = u["gb"][sl]
                for cb in range(2):
                    po, por = self.ps()

                    def f2(e, po=po, k=k, cb=cb, hb=hb, wds=wds, gn=gn):
                        ins = None
                        idx = 0
                        for (wd, wdr, nj) in wds:
                            for j in range(nj):
                                ins = e.matmul(po[:rows, :], HG[:, hb, idx, k * rows:(k + 1) * rows], wd[:, j, cb * 512:(cb + 1) * 512], start=(idx == 0), stop=(idx == gn - 1))
                                idx += 1
                        return ins
                    S.op("pe", hres + [w[1] for w in wds], [por], f2)
                    self.accumulate(k, cb, po, por, gbv, gbr, ex, folded=fold)

    def accumulate(self, k, cb, po, por, gbv, gbr, ex, folded=False):
        S, u = self.S, self.u
        rows = u["rows"]
        A = self.ACC[:rows, k, cb * 512:(cb + 1) * 512]
        T_ = self.TMPA[:rows, 0:512]
        COMB = getattr(self, "COMB", None)
        if folded:
            if ex is None:
                S.op("dve", [por, self.ACCR[k]], [self.ACCR[k]], lambda e: e.tensor_tensor(out=A, in0=po[:rows, :], in1=A, op=ALU.add))
            else:
                S.op("dve", [por, self.COMBR, self.ACCR[k]], [self.ACCR[k]],
                     lambda e: e.scalar_tensor_tensor(out=A, in0=po[:rows, :], scalar=COMB[:rows, k, ex:ex + 1], in1=A, op0=ALU.mult, op1=ALU.add))
            return
        if ex is None:
            S.op("dve", [por, gbr], [self.TMPAR], lambda e: e.tensor_tensor(out=T_, in0=po[:rows, :], in1=gbv[:rows, cb * 512:(cb + 1) * 512], op=ALU.mult))
        else:
            S.op("dve", [por, gbr, self.COMBR], [self.TMPAR],
                 lambda e: e.scalar_tensor_tensor(out=T_, in0=po[:rows, :], scalar=COMB[:rows, k, ex:ex + 1], in1=gbv[:rows, cb * 512:(cb + 1) * 512],
                                                  op0=ALU.mult, op1=ALU.mult))
        S.op("dve", [self.TMPAR, self.ACCR[k]], [self.ACCR[k]], lambda e: e.tensor_tensor(out=A, in0=A, in1=T_, op=ALU.add))

    def merge_out(self, l):
        S, u, I = self.S, self.u, self.I
        rows, ntl, ncols = u["rows"], u["ntl"], u["ncols"]
        nblk = (ncols + 511) // 512
        S.barrier()
        NCOL = self.NCOL
        TACC = self.SCR[:, 0:NCOL]
        tar = Res("tacc")
        SG = self.SCR[:, NCOL:NCOL + NCOL // 2].bitcast(BF16)
        sgr = Res("sg")
        MC = self.SCR[:, NCOL + NCOL // 2:NCOL + NCOL // 2 + NCOL].bitcast(BF16).rearrange("p (b c) -> p b c", b=2)
        mcr = [Res("mc0"), Res("mc1")]
        for c in range(8):
            for n in range(4):
                wgv, wgr = self.load_w(I["w_in"][l][:, OFF_GATE + n * D + c * 128:OFF_GATE + n * D + (c + 1) * 128], 8, 128)
                wbv, wbr = self.load_w(I["w_branch"][l, n][:, c * 128:(c + 1) * 128], 2, 128)
                for b in range(nblk):
                    nn = min(512, ncols - b * 512)
                    pg, pgr = self.ps()
                    pb, pbr = self.ps()

                    def f(e, pg=pg, pb=pb, b=b, nn=nn, wgv=wgv, wbv=wbv, n=n):
                        ins = None
                        for kc in range(8):
                            ins = e.matmul(pg[:, 0:nn], wgv[:, kc, :], self.UT[:, kc, b * 512:b * 512 + nn], start=(kc == 0), stop=(kc == 7))
                        for kc in range(2):
                            ins = e.matmul(pb[:, 0:nn], wbv[:, kc, :], self.HT[:, n * 2 + kc, b * 512:b * 512 + nn], start=(kc == 0), stop=(kc == 1))
                        return ins
                    S.op("pe", [wgr, wbr, self.UTR, self.HTR[n]], [pgr, pbr], f)
                    S.op("act", [pgr], [sgr], lambda e, pg=pg, b=b, nn=nn: e.activation(out=SG[:, b * 512:b * 512 + nn], in_=pg[:, 0:nn], func=AF.Sigmoid))
                    sl_ = slice(b * 512, b * 512 + nn)
                    if n == 0:
                        S.op("dve", [pbr, sgr], [tar], lambda e, pb=pb, sl_=sl_, nn=nn: e.tensor_tensor(out=TACC[:, sl_], in0=pb[:, 0:nn], in1=SG[:, sl_], op=ALU.mult))
                    else:
                        S.op("dve", [pbr, sgr], [self.TMPCR], lambda e, pb=pb, sl_=sl_, nn=nn: e.tensor_tensor(out=self.TMPC[:, 0:nn], in0=pb[:, 0:nn], in1=SG[:, sl_], op=ALU.mult))
                        if n < 3:
                            S.op("dve", [self.TMPCR, tar], [tar], lambda e, sl_=sl_, nn=nn: e.tensor_tensor(out=TACC[:, sl_], in0=TACC[:, sl_], in1=self.TMPC[:, 0:nn], op=ALU.add))
                        else:
                            S.op("dve", [self.TMPCR, tar], [mcr[c % 2]],
                                 lambda e, sl_=sl_, nn=nn, c=c: e.tensor_tensor(out=MC[:, c % 2, sl_], in0=TACC[:, sl_], in1=self.TMPC[:, 0:nn], op=ALU.add))
            wo0, wo0r = self.load_w(I["w_out"][l][:, 0:512], 1, 512, k0=c)
            wo1, wo1r = self.load_w(I["w_out"][l][:, 512:1024], 1, 512, k0=c)
            fold = (u["kind"] == "p")
            if fold:
                gbv0, gbr0 = u["gb"][0]
                for cb_, (wo_, wor_) in enumerate(((wo0, wo0r), (wo1, wo1r))):
                    S.op("dve", [wor_, gbr0], [wor_],
                         lambda e, wo_=wo_, cb_=cb_, gbv0=gbv0: e.tensor_tensor(out=wo_[:, 0, :], in0=wo_[:, 0, :], in1=gbv0[:, cb_ * 512:(cb_ + 1) * 512], op=ALU.mult))
            for k, (sl, ti) in enumerate(u["tiles"]):
                gbv, gbr = u["gb"][sl]
                for cb, (wo, wor) in enumerate(((wo0, wo0r), (wo1, wo1r))):
                    po, por = self.ps()
                    S.op("pe", [mcr[c % 2], wor], [por],
                         lambda e, po=po, k=k, wo=wo, c=c: e.matmul(po[:rows, :], MC[:, c % 2, k * rows:(k + 1) * rows], wo[:, 0, :], start=True, stop=True))
                    self.accumulate(k, cb, po, por, gbv, gbr, None, folded=fold)
        S.barrier()

    def proj_tok(self, k0col, nrows, panels):
        S = self.S
        banks = []
        for pi in range(0, len(panels), 2):
            pt, pr = self.ps()
            grp = panels[pi:pi + 2]

            def f(e, pt=pt, grp=grp):
                ins = None
                for gi, (wv, wr, n) in enumerate(grp):
                    for kc in range(8):
                        ins = e.matmul(pt[:nrows, gi * 256:gi * 256 + n], self.UT[:, kc, k0col:k0col + nrows], wv[:, kc, :], start=(kc == 0), stop=(kc == 7))
                return ins
            S.op("pe", [self.UTR] + [g[1] for g in grp], [pr], f)
            banks.append((pt, pr))
        return banks

    def load_panels(self, l, offs):
        I = self.I
        out = []
        for off, n in offs:
            wv, wr = self.load_w(I["w_in"][l][:, off:off + n], 8, n)
            out.append((wv, wr, n))
        return out

    def to_ht(self, mixer, HB, hbr, nrows, col0):
        S = self.S
        pt, pr = self.ps()
        pv = pt[:, :].bitcast(BF16)

        def f(e):
            ins = None
            for g in range(2):
                ins = e.transpose(out=pv[:, g * 128:g * 128 + nrows], in_=HB[:nrows, g * 128:(g + 1) * 128], identity=self.IDB[:nrows, :nrows])
            return ins
        S.op("pe", [hbr, self.CONSTR], [pr], f)
        S.op("act", [pr], [self.HTR[mixer]],
             lambda e: e.copy(out=self.HT[:, mixer * 2:mixer * 2 + 2, col0:col0 + nrows], in_=pv[:, 0:256].rearrange("p (g c) -> p g c", g=2)[:, :, 0:nrows]))

    def mixers(self, l):
        self.mix_ret(l)
        self.stage_end("ret")
        self.mix_fox(l)
        self.stage_end("fox")
        self.mix_conv(l)
        self.stage_end("conv")
        self.mix_diff(l)
        self.stage_end("diff")

    def mix_ret(self, l):
        S, u, I, O, cfg = self.S, self.u, self.I, self.O, self.cfg
        kind = u["kind"]
        pan = self.load_panels(l, [(OFF_RET + i * 256, 256) for i in range(4)])
        rk = 0 if kind == "p" else 1
        RD = self.RDEC
        S32 = self.SCR[0:64, 0:256]
        s32r = Res("s32")
        SBF = self.SCR[0:64, 256:384].bitcast(BF16)
        sbfr = Res("sbf")
        S.barrier()
        if kind == "p":
            seqlist = [(0, [(i, 64) for i in range(cfg.T // 64)])]
        else:
            seqlist = [(j, [(0, cfg.TS)]) for j in range(cfg.NS)]
        for sl, chunks in seqlist:
            seqg = u["seqs"][sl]
            if kind == "p":
                S.op("dve", [], [s32r], lambda e: e.memset(S32, 0.0))
            else:
                S.dma("sp", S32.rearrange("d (h e) -> d h e", h=4), I["sret"][l, sl].rearrange("h d e -> d h e"), [], [s32r], s32r)
            S.op("act", [s32r], [sbfr], lambda e: e.copy(out=SBF, in_=S32))
            for ci, cl in chunks:
                col0 = (ci * 64) if kind == "p" else sl * cfg.TS
                tab = ci if kind == "p" else cfg.T // 64
                (pA, pAr), (pB, pBr) = self.proj_tok(col0, cl, pan)
                RT = self.TMPA[0:64, 0:512].rearrange("p (a c) -> p a c", a=2)
                S.dma("sp", RT, I["ret_rope"][tab], [], [self.TMPAR], self.TMPAR)
                QK = self.TMPB[0:64, 0:512]
                A3 = pA[:cl, :].rearrange("p (h x) -> p h x", h=8)
                C3 = RT[:cl, 0, :].rearrange("p (h x) -> p h x", h=8)
                S3 = RT[:cl, 1, :].rearrange("p (h x) -> p h x", h=8)
                Q3 = QK[:cl, :].rearrange("p (h x) -> p h x", h=8)
                T1 = self.TMPC[:cl, 0:256].rearrange("p (h x) -> p h x", h=8)
                T2 = self.TMPC[:cl, 256:512].rearrange("p (h x) -> p h x", h=8)
                rd1 = [pAr, self.TMPAR]
                S.op("dve", rd1, [self.TMPCR], lambda e, A3=A3, C3=C3, T1=T1: e.tensor_tensor(out=T1, in0=A3[:, :, 0:32], in1=C3, op=ALU.mult))
                S.op("dve", rd1, [self.TMPCR], lambda e, A3=A3, S3=S3, T2=T2: e.tensor_tensor(out=T2, in0=A3[:, :, 32:64], in1=S3, op=ALU.mult))
                S.op("dve", [self.TMPCR], [self.TMPBR], lambda e, Q3=Q3, T1=T1, T2=T2: e.tensor_tensor(out=Q3[:, :, 0:32], in0=T1, in1=T2, op=ALU.subtract))
                S.op("dve", rd1, [self.TMPCR], lambda e, A3=A3, C3=C3, T1=T1: e.tensor_tensor(out=T1, in0=A3[:, :, 32:64], in1=C3, op=ALU.mult))
                S.op("dve", rd1, [self.TMPCR], lambda e, A3=A3, S3=S3, T2=T2: e.tensor_tensor(out=T2, in0=A3[:, :, 0:32], in1=S3, op=ALU.mult))
                S.op("dve", [self.TMPCR], [self.TMPBR], lambda e, Q3=Q3, T1=T1, T2=T2: e.tensor_tensor(out=Q3[:, :, 32:64], in0=T1, in1=T2, op=ALU.add))
                VB = self.TMPB[0:64, 512:768]
                KD = self.TMPB[0:64, 768:1024]
                SGt = self.TMPA[0:64, 512:768]
                S.op("act", [pBr], [self.TMPBR], lambda e, pB=pB, cl=cl: e.copy(out=VB[:cl, :], in_=pB[:cl, 0:256]))
                S.op("act", [pBr], [self.TMPAR], lambda e, pB=pB, cl=cl: e.activation(out=SGt[:cl, :], in_=pB[:cl, 256:512], func=AF.Silu))
                S.op("dve", [self.TMPBR, self.CONSTR], [self.TMPBR], lambda e, cl=cl: e.tensor_tensor(out=KD[:cl, :].rearrange("p (h x) -> p h x", h=4), in0=QK[:cl, 256:512].rearrange("p (h x) -> p h x", h=4),
                                                                                                        in1=self.RDECS[:cl, rk, 0, :].unsqueeze(2).to_broadcast([cl, 4, 64]), op=ALU.mult))
                pT, pTr = self.ps()
                pTv = pT[:, :].bitcast(BF16)

                def ft(e, pTv=pTv, cl=cl):
                    ins = None
                    for j in range(8):
                        ins = e.transpose(out=pTv[0:64, j * 64:j * 64 + cl], in_=QK[:cl, j * 64:(j + 1) * 64], identity=self.IDB[:cl, :cl])
                    return ins
                S.op("pe", [self.TMPBR, self.CONSTR], [pTr], ft)
                QKT = self.SCR[0:64, 384:640].bitcast(BF16).rearrange("p (j c) -> p j c", j=8)
                qktr = getattr(self, "_qktr", None) or Res("qkt")
                self._qktr = qktr
                QDT = self.SCR[0:64, 640:768].bitcast(BF16).rearrange("p (j c) -> p j c", j=4)
                S.op("act", [pTr], [qktr], lambda e, pTv=pTv, cl=cl: e.copy(out=QKT[:, :, 0:cl], in_=pTv[0:64, 0:512].rearrange("p (j c) -> p j c", j=8)[:, :, 0:cl]))
                S.op("dve", [qktr, self.CONSTR], [qktr],
                     lambda e, cl=cl: e.tensor_tensor(out=QDT[:, :, 0:cl], in0=QKT[:, 0:4, 0:cl], in1=RD[:, rk, 1, :].rearrange("p (h c) -> p h c", h=4)[:, :, 0:cl], op=ALU.mult))
                pS, pSr = self.ps()

                def fs(e, pS=pS, cl=cl):
                    ins = None
                    for h in range(4):
                        ins = e.matmul(pS[:cl, h * 64:h * 64 + cl], QKT[:, 4 + h, 0:cl], QKT[:, h, 0:cl], start=True, stop=True)
                    return ins
                S.op("pe", [qktr], [pSr], fs)
                STB = self.SCR[0:64, 768:896].bitcast(BF16).rearrange("p (h c) -> p h c", h=4)
                stbr = getattr(self, "_stbr", None) or Res("stb")
                self._stbr = stbr
                S.op("dve", [pSr, self.CONSTR], [stbr],
                     lambda e, pS=pS, cl=cl: e.tensor_tensor(out=STB[:cl, :, 0:cl], in0=pS[:cl, 0:256].rearrange("p (h c) -> p h c", h=4)[:, :, 0:cl],
                                                             in1=RD[:cl, rk, 0, :].rearrange("p (h c) -> p h c", h=4)[:, :, 0:cl], op=ALU.mult))
                pK, pKr = self.ps()

                def fk(e, pK=pK, cl=cl):
                    ins = None
                    for h in range(4):
                        ins = e.matmul(pK[0:64, h * 64:(h + 1) * 64], KD[:cl, h * 64:(h + 1) * 64], VB[:cl, h * 64:(h + 1) * 64], start=True, stop=True)
                    return ins
                S.op("pe", [self.TMPBR], [pKr], fk)
                pY, pYr = self.ps()

                def fy(e, pY=pY, cl=cl):
                    ins = None
                    for h in range(4):
                        e.matmul(pY[:cl, h * 64:(h + 1) * 64], STB[:cl, h, 0:cl], VB[:cl, h * 64:(h + 1) * 64], start=True, stop=False)
                        ins = e.matmul(pY[:cl, h * 64:(h + 1) * 64], QDT[:, h, 0:cl], SBF[:, h * 64:(h + 1) * 64], start=False, stop=True)
                    return ins
                S.op("pe", [stbr, self.TMPBR, qktr, sbfr], [pYr], fy)
                S.op("dve", [s32r, self.CONSTR], [s32r], lambda e: e.tensor_tensor(out=S32.rearrange("p (h x) -> p h x", h=4), in0=S32.rearrange("p (h x) -> p h x", h=4),
                                                                                      in1=self.RDECS[:, rk, 1, :].unsqueeze(2).to_broadcast([64, 4, 64]), op=ALU.mult))
                S.op("dve", [s32r, pKr], [s32r], lambda e, pK=pK: e.tensor_tensor(out=S32, in0=S32, in1=pK[0:64, 0:256], op=ALU.add))
                S.op("act", [s32r], [sbfr], lambda e: e.copy(out=SBF, in_=S32))
                Y32 = self.TMPA[0:64, 768:1024]
                S.op("act", [pYr], [self.TMPAR], lambda e, pY=pY, cl=cl: e.copy(out=Y32[:cl, :], in_=pY[:cl, 0:256]))
                HB = self.TMPB[0:64, 0:256]
                self.group_norm(Y32, self.TMPAR, cl, 4, 64, True, SGt, HB, self.TMPBR, None, 1.0)
                self.to_ht(0, HB, self.TMPBR, cl, col0)
            dst = O[("p" if kind == "p" else "s") + "_ret"][l, u["ui"] if kind == "p" else sl].rearrange("h d e -> d h e")
            S.dma("sp", dst, S32.rearrange("d (h e) -> d h e", h=4), [s32r], [], s32r, is_out=True)

    def group_norm(self, Y, yr, nrows, G, Dg, center, MUL, OUT, outr, gain_bc, const):
        S = self.S
        SM, R = self.SM, self.SMR
        Y3 = Y[:nrows, 0:G * Dg].rearrange("p (g d) -> p g d", g=G)
        SQ = self.TMPC[:nrows, 0:G * Dg]
        SQ3 = SQ.rearrange("p (g d) -> p g d", g=G)
        s1, s2, mean, var, rstd = (SM[:nrows, 40:40 + G], SM[:nrows, 44:44 + G], SM[:nrows, 48:48 + G], SM[:nrows, 52:52 + G], SM[:nrows, 56:56 + G])
        S.op("dve", [yr], [self.TMPCR], lambda e: e.tensor_tensor(out=SQ, in0=Y[:nrows, 0:G * Dg], in1=Y[:nrows, 0:G * Dg], op=ALU.mult))
        S.op("dve", [self.TMPCR], [R], lambda e: e.tensor_reduce(out=s2, in_=SQ3, axis=AX.X, op=ALU.add))
        if center:
            S.op("dve", [yr], [R], lambda e: e.tensor_reduce(out=s1, in_=Y3, axis=AX.X, op=ALU.add))
            S.op("dve", [R], [R], lambda e: e.tensor_scalar(out=mean, in0=s1, scalar1=1.0 / Dg, scalar2=None, op0=ALU.mult))
            S.op("dve", [R], [R], lambda e: e.tensor_tensor(out=s1, in0=mean, in1=mean, op=ALU.mult))
            S.op("dve", [R], [R], lambda e: e.scalar_tensor_tensor(out=var, in0=s2, scalar=1.0 / Dg, in1=s1, op0=ALU.mult, op1=ALU.subtract))
        else:
            S.op("dve", [R], [R], lambda e: e.tensor_scalar(out=var, in0=s2, scalar1=1.0 / Dg, scalar2=None, op0=ALU.mult))
        S.op("act", [R], [R], lambda e: e.activation(out=rstd, in_=var, func=AF.Sqrt, bias=self.EPSC[:nrows, :], scale=1.0))
        S.op("dve", [R], [R], lambda e: e.reciprocal(out=rstd, in_=rstd))
        if center:
            S.op("dve", [yr, R], [self.TMPCR], lambda e: e.tensor_tensor(out=SQ3, in0=Y3, in1=mean.unsqueeze(2).to_broadcast([nrows, G, Dg]), op=ALU.subtract))
            src, srcr = SQ3, self.TMPCR
        else:
            src, srcr = Y3, yr
        S.op("dve", [srcr, R], [self.TMPCR], lambda e: e.tensor_tensor(out=SQ3, in0=src, in1=rstd.unsqueeze(2).to_broadcast([nrows, G, Dg]), op=ALU.mult))
        O3 = OUT[:nrows, 0:G * Dg].rearrange("p (g d) -> p g d", g=G)
        if gain_bc is not None:
            S.op("dve", [self.TMPCR, self.CONSTR], [self.TMPCR], lambda e: e.tensor_tensor(out=SQ3, in0=SQ3, in1=gain_bc, op=ALU.mult))
        if MUL is not None:
            S.op("dve", [self.TMPCR, yr], [outr], lambda e: e.tensor_tensor(out=OUT[:nrows, 0:G * Dg], in0=SQ, in1=MUL[:nrows, 0:G * Dg], op=ALU.mult))
        else:
            S.op("dve", [self.TMPCR], [outr], lambda e: e.tensor_scalar(out=OUT[:nrows, 0:G * Dg], in0=SQ, scalar1=const, scalar2=None, op0=ALU.mult))

    def attn_buffers(self):
        NK = self.NKT * 128
        KT = self.SCR[:, 0:NK].bitcast(BF16).rearrange("p (g c) -> p g c", g=2)
        VA = self.SCR[:, NK:NK + self.NKT * 130].bitcast(BF16).rearrange("p (t h x) -> p t h x", t=self.NKT, h=4)
        base = NK + self.NKT * 130
        FC = self.SCR[:, base:base + self.NKT * 4].rearrange("p (t h) -> p t h", h=4)
        FT = self.SCR[:, base + self.NKT * 4:base + self.NKT * 8].rearrange("p (t h) -> p t h", h=4)
        LF = self.SCR[:, base + self.NKT * 8:base + self.NKT * 12].rearrange("p (t h) -> p t h", h=4)
        self.BI = self.SCR[:, base + self.NKT * 12:base + self.NKT * 16].rearrange("p (t h) -> p t h", h=4)
        assert base + self.NKT * 16 <= 5120, base + self.NKT * 16
        return KT, VA, FC, FT, LF

    def seq_iter(self):
        u, cfg = self.u, self.cfg
        if u["kind"] == "p":
            return [(0, u["ui"], [(i, i * 128, 128) for i in range(self.NT)], 0)]
        return [(j, j, [(j, 0, cfg.TS)], cfg.PAST // 128) for j in range(cfg.NS)]

    def k_transpose(self, KTOK, ktokr, nrows, KT, ktr, kcol0):
        S = self.S
        pt, pr = self.ps()
        pv = pt[:, :].bitcast(BF16)

        def f(e):
            ins = None
            for g in range(2):
                ins = e.transpose(out=pv[:, g * 128:g * 128 + nrows], in_=KTOK[:nrows, g * 128:(g + 1) * 128], identity=self.IDB[:nrows, :nrows])
            return ins
        S.op("pe", [ktokr, self.CONSTR], [pr], f)
        S.op("act", [pr], [ktr], lambda e: e.copy(out=KT[:, :, kcol0:kcol0 + nrows], in_=pv[:, 0:256].rearrange("p (g c) -> p g c", g=2)[:, :, 0:nrows]))

    def mix_fox(self, l):
        S, u, I, O, cfg = self.S, self.u, self.I, self.O, self.cfg
        kind = u["kind"]
        pre = "p" if kind == "p" else "s"
        pan = self.load_panels(l, [(OFF_FOX, 256), (OFF_FOX + 256, 256), (OFF_FOX + 512, 256), (OFF_FOX_F, 4)])
        S.barrier()
        KT, VA, FC, FT, LF = self.attn_buffers()
        ktr, var, fr = Res("kt"), Res("va"), Res("f")
        SM = self.SM
        for sl, so, newtiles, npast in self.seq_iter():
            S.op("dve", [], [var], lambda e: e.memset(VA[:, :, :, 64:65], 1.0))
            if npast:
                S.dma("sp", LF[:, 0:npast, :], I["cfl"][l, sl].rearrange("(t p) h -> p t h", p=128), [], [fr], fr)
            for kt in range(npast):
                KTOK = self.TMPB[:, 0:256]
                S.dma("pool", KTOK, I["cfk"][l, sl, kt * 128:(kt + 1) * 128, :], [], [self.TMPBR], self.TMPBR)
                self.k_transpose(KTOK, self.TMPBR, 128, KT, ktr, kt * 128)
                S.dma("pool", VA[:, kt, :, 0:64], I["cfv"][l, sl, kt * 128:(kt + 1) * 128, :].rearrange("t (h x) -> t h x", h=4), [], [var], var)
                self.fcum_step(kt, 128, FC, FT, LF, fr)
            for qi, (k, t0, nr) in enumerate(newtiles):
                kt = npast + qi
                col0 = k * u["rows"]
                (pA, pAr), (pB, pBr) = self.proj_tok(col0, nr, pan)
                st, str_ = self.stg()
                S.op("act", [pAr], [str_], lambda e, st=st, pA=pA, nr=nr: e.copy(out=st[:nr, 0:256], in_=pA[:nr, 256:512]))
                S.op("act", [pBr], [str_], lambda e, st=st, pB=pB, nr=nr: e.copy(out=st[:nr, 256:512], in_=pB[:nr, 0:256]))
                S.dma("sp", O[pre + "_fox_k"][l, so, t0:t0 + nr, :], st[:nr, 0:256], [str_], [], str_, is_out=True)
                S.dma("sp", O[pre + "_fox_v"][l, so, t0:t0 + nr, :], st[:nr, 256:512], [str_], [], str_, is_out=True)
                self.stage_end("fox_out")
                QKB = self.TMPB[:, 0:512]
                S.op("dve", [pAr], [self.TMPBR], lambda e, pA=pA, nr=nr: e.tensor_scalar(out=QKB[:nr, :], in0=pA[:nr, :], scalar1=1.0, scalar2=None, op0=ALU.mult))
                self.stage_end("fox_q1")
                S.op("dve", [pBr], [var], lambda e, pB=pB, nr=nr, kt=kt: e.tensor_copy(out=VA[:nr, kt, :, 0:64], in_=pB[:nr, 0:256].rearrange("p (h x) -> p h x", h=4)))
                self.stage_end("fox_q2")
                self.k_transpose(QKB[:, 256:512], self.TMPBR, nr, KT, ktr, kt * 128)
                self.stage_end("fox_q3")
                QT = self.TMPB[:, 512:768].rearrange("p (g c) -> p g c", g=2)
                qtr = self.TMPBR
                pt, pr = self.ps()
                pv = pt[:, :].bitcast(BF16)

                def fq(e, pv=pv, nr=nr):
                    ins = None
                    for g in range(2):
                        ins = e.transpose(out=pv[:, g * 128:g * 128 + nr], in_=QKB[:nr, g * 128:(g + 1) * 128], identity=self.IDB[:nr, :nr])
                    return ins
                S.op("pe", [self.TMPBR, self.CONSTR], [pr], fq)
                S.op("act", [pr], [self.TMPBR], lambda e, pv=pv, nr=nr: e.copy(out=QT[:, :, 0:nr], in_=pv[:, 0:256].rearrange("p (g c) -> p g c", g=2)[:, :, 0:nr]))
                self.stage_end("fox_qk")
                S.op("dve", [pBr, self.CONSTR], [self.SMR], lambda e, pB=pB, nr=nr: e.tensor_tensor(out=SM[:nr, 0:4], in0=pB[:nr, 256:260], in1=self.BFF[:nr, l, :], op=ALU.add))
                S.op("act", [self.SMR], [self.SMR], lambda e, nr=nr: e.activation(out=SM[:nr, 4:8], in_=SM[:nr, 0:4], func=AF.Exp, scale=-1.0))
                S.op("act", [self.SMR], [self.SMR], lambda e, nr=nr: e.activation(out=SM[:nr, 8:12], in_=SM[:nr, 4:8], func=AF.Ln, bias=self.ONEC[:nr, :], scale=1.0))
                S.op("dve", [self.SMR], [fr], lambda e, nr=nr, kt=kt: e.tensor_scalar(out=LF[:nr, kt, :], in0=SM[:nr, 8:12], scalar1=-1.0, scalar2=None, op0=ALU.mult))
                S.dma("sp", O[pre + "_fox_logf"][l, so, t0:t0 + nr, :], LF[:nr, kt, :], [fr], [], fr, is_out=True)
                self.stage_end("fox_lf")
                self.fcum_step(kt, nr, FC, FT, LF, fr)
                self.stage_end("fox_fcum")
                HB = self.TMPB[:, 768:1024]
                hbr = getattr(self, "_hbr", None) or Res("hb")
                self._hbr = hbr
                BI = self.BI
                bir = getattr(self, "_bir", None) or Res("bi")
                self._bir = bir
                S.op("dve", [fr], [bir], lambda e, kt=kt: e.tensor_tensor(out=BI[:, 0:kt + 1, :], in0=FT[:, kt:kt + 1, :].to_broadcast([128, kt + 1, 4]),
                                                                        in1=FC[:, 0:kt + 1, :], op=ALU.subtract))
                items = [(h, kk) for h in range(4) for kk in range(kt + 1)]
                pSs, pYs = {}, {}

                def stageA(it, kt=kt, nr=nr):
                    h, kk = it
                    hp, g = (h % 2) * 64, h // 2
                    nk = 128 if kk < kt else nr
                    pS, pSr = self.ps()
                    pSs[it] = (pS, pSr)
                    S.op("pe", [ktr, self.TMPBR], [pSr],
                         lambda e, pS=pS, nk=nk, kk=kk, hp=hp, g=g, nr=nr: e.matmul(pS[:nk, 0:nr], KT[hp:hp + 64, g, kk * 128:kk * 128 + nk], QT[hp:hp + 64, g, 0:nr], start=True, stop=True))

                def stageB(it, kt=kt, nr=nr):
                    h, kk = it
                    nk = 128 if kk < kt else nr
                    pS, pSr = pSs.pop(it)
                    if kk == 0:
                        pYs[h] = self.ps(hold=True)
                    pY, pYr = pYs[h]
                    PT, ptr = self.pt_next()
                    S.op("act", [pSr, bir], [ptr], lambda e, pS=pS, nk=nk, nr=nr, PT=PT, kk=kk, h=h: e.activation(out=PT[:nk, 0:nr], in_=pS[:nk, 0:nr], func=AF.Exp, bias=BI[:nk, kk, h:h + 1], scale=0.125))
                    if kk == kt:
                        S.op("dve", [ptr, self.CONSTR], [ptr], lambda e, nk=nk, nr=nr, PT=PT: e.tensor_tensor(out=PT[:nk, 0:nr], in0=PT[:nk, 0:nr], in1=self.CMASK[:nk, 0:nr], op=ALU.mult))
                    S.op("pe", [ptr, var], [pYr],
                         lambda e, pY=pY, nk=nk, nr=nr, kk=kk, h=h, kt=kt, PT=PT: e.matmul(pY[:nr, 0:65], PT[:nk, 0:nr], VA[:nk, kk, h, :], start=(kk == 0), stop=(kk == kt)))
                    if kk == kt:
                        S.op("dve", [pYr], [self.SMR], lambda e, pY=pY, nr=nr, h=h: e.reciprocal(out=SM[:nr, 20 + h:21 + h], in_=pY[:nr, 64:65]))
                        S.op("act", [pYr, self.SMR], [hbr], lambda e, pY=pY, nr=nr, h=h: e.activation(out=HB[:nr, h * 64:(h + 1) * 64], in_=pY[:nr, 0:64], func=AF.Copy, scale=SM[:nr, 20 + h:21 + h]))
                        self.ps_release(pYr)

                LOOK = 3
                for it in items[:LOOK]:
                    stageA(it)
                for i, it in enumerate(items):
                    if i + LOOK < len(items):
                        stageA(items[i + LOOK])
                    stageB(it)
                self.to_ht(1, HB, hbr, nr, col0)

    def fcum_step(self, kt, nr, FC, FT, LF, fr):
        S = self.S
        pt, pr = self.ps()

        def f(e):
            e.matmul(pt[:, 0:4], self.ONESF[:nr, :], LF[:nr, kt, :], start=True, stop=True)
            return e.matmul(pt[:nr, 4:8], self.UTRI[:nr, :nr], LF[:nr, kt, :], start=True, stop=True)
        S.op("pe", [fr, self.CONSTR], [pr], f)
        if kt == 0:
            S.op("dve", [pr], [fr], lambda e: e.tensor_copy(out=FT[:, kt, :], in_=pt[:, 0:4]))
            S.op("dve", [pr], [fr], lambda e: e.tensor_copy(out=FC[:nr, kt, :], in_=pt[:nr, 4:8]))
        else:
            S.op("dve", [pr, fr], [fr], lambda e: e.tensor_tensor(out=FT[:, kt, :], in0=pt[:, 0:4], in1=FT[:, kt - 1, :], op=ALU.add))
            S.op("dve", [pr, fr], [fr], lambda e: e.tensor_tensor(out=FC[:nr, kt, :], in0=pt[:nr, 4:8], in1=FT[:nr, kt - 1, :], op=ALU.add))

    def mix_conv(self, l):
        S, u, I, O, cfg = self.S, self.u, self.I, self.O, self.cfg
        kind = u["kind"]
        pre = "p" if kind == "p" else "s"
        pan = self.load_panels(l, [(OFF_CONV, 256), (OFF_CONV + 256, 256)])
        S.barrier()
        XP = self.SCR[:, 0:2 * 158].rearrange("p (g c) -> p g c", g=2)
        xpr = Res("xp")
        CO = self.SCR[:, 320:320 + 256].rearrange("p (g c) -> p g c", g=2)
        cor = Res("co")
        CS = self.SCR[:, 576:576 + 256].rearrange("p (g c) -> p g c", g=2)
        csr = Res("cs")
        ST = self.SCR[:, 832:832 + 512]
        str2 = Res("cst")
        for sl, so, newtiles, npast in self.seq_iter():
            if kind == "p":
                S.op("dve", [], [xpr], lambda e: e.memset(XP[:, :, 0:30], 0.0))
            else:
                for g in range(2):
                    S.dma("sp", XP[:, g, 0:30], I["sconv"][l, sl][:, g * 128:(g + 1) * 128].rearrange("t c -> c t"), [], [xpr], xpr, allow_slow_non_contiguous=True)
                S.dma("sp", O["s_conv"][l, so, 0:30 - cfg.TS, :], I["sconv"][l, sl, cfg.TS:30, :], [], [], Res("d2d"), is_out=True)
            for qi, (k, t0, nr) in enumerate(newtiles):
                col0 = k * u["rows"]
                ((pA, pAr),) = self.proj_tok(col0, nr, pan)
                G32 = self.TMPA[:, 0:256]
                S.op("act", [pAr], [self.TMPAR], lambda e, pA=pA, nr=nr: e.activation(out=G32[:nr, :], in_=pA[:nr, 256:512], func=AF.Sigmoid))
                S.op("dve", [pAr, self.TMPAR], [self.TMPAR], lambda e, pA=pA, nr=nr: e.tensor_tensor(out=G32[:nr, :], in0=pA[:nr, 0:256], in1=G32[:nr, :], op=ALU.mult))
                if kind == "p":
                    lo = max(t0, cfg.T - 30)
                    if t0 + nr > lo:
                        S.dma("sp", O["p_conv"][l, so, lo - (cfg.T - 30):30, :], G32[lo - t0:nr, :], [self.TMPAR], [], self.TMPAR, is_out=True)
                else:
                    S.dma("sp", O["s_conv"][l, so, 30 - cfg.TS:30, :], G32[:nr, :], [self.TMPAR], [], self.TMPAR, is_out=True)
                pt, pr = self.ps()

                def f(e, pt=pt, nr=nr):
                    ins = None
                    for g in range(2):
                        ins = e.transpose(out=pt[:, g * 128:g * 128 + nr], in_=G32[:nr, g * 128:(g + 1) * 128], identity=self.IDF[:nr, :nr])
                    return ins
                S.op("pe", [self.TMPAR, self.CONSTR], [pr], f)
                if qi > 0:
                    pnr = newtiles[qi - 1][2]
                    S.op("dve", [xpr], [self.TMPCR], lambda e, pnr=pnr: e.tensor_copy(out=self.TMPC[:, 0:60].rearrange("p (g c) -> p g c", g=2), in_=XP[:, :, pnr:pnr + 30]))
                    S.op("dve", [self.TMPCR], [xpr], lambda e: e.tensor_copy(out=XP[:, :, 0:30], in_=self.TMPC[:, 0:60].rearrange("p (g c) -> p g c", g=2)))
                S.op("act", [pr], [xpr], lambda e, pt=pt, nr=nr: e.copy(out=XP[:, :, 30:30 + nr], in_=pt[:, 0:256].rearrange("p (g c) -> p g c", g=2)[:, :, 0:nr]))
                for g in range(2):
                    S.op("dve", [xpr, self.CONSTR], [cor],
                         lambda e, g=g, nr=nr: e.tensor_scalar(out=CO[:, g, 0:nr], in0=XP[:, g, 0:nr], scalar1=self.WCV[:, l, g, 0:1], scalar2=self.CVP[:, l, 0, g:g + 1], op0=ALU.mult, op1=ALU.add))
                    for w in range(1, CONV_W):
                        S.op("dve", [xpr, self.CONSTR, cor], [cor],
                             lambda e, g=g, nr=nr, w=w: e.scalar_tensor_tensor(out=CO[:, g, 0:nr], in0=XP[:, g, w:w + nr], scalar=self.WCV[:, l, g, w:w + 1], in1=CO[:, g, 0:nr],
                                                                                op0=ALU.mult, op1=ALU.add))
                S.op("dve", [cor], [csr], lambda e, nr=nr: e.tensor_tensor(out=CS[:, :, 0:nr], in0=CO[:, :, 0:nr], in1=CO[:, :, 0:nr], op=ALU.mult))
                ps1, ps1r = self.ps()

                def fs(e, ps1=ps1, nr=nr):
                    e.matmul(ps1[:, 0:nr], self.ONESF[:, :], CO[:, 0, 0:nr], start=True, stop=False)
                    e.matmul(ps1[:, 0:nr], self.ONESF[:, :], CO[:, 1, 0:nr], start=False, stop=True)
                    e.matmul(ps1[:, 128:128 + nr], self.ONESF[:, :], CS[:, 0, 0:nr], start=True, stop=False)
                    return e.matmul(ps1[:, 128:128 + nr], self.ONESF[:, :], CS[:, 1, 0:nr], start=False, stop=True)
                S.op("pe", [cor, csr, self.CONSTR], [ps1r], fs)
                MEAN, MSQ, VAR, RSTD = ST[:, 0:128], ST[:, 128:256], ST[:, 256:384], ST[:, 384:512]
                S.op("dve", [ps1r], [str2], lambda e, ps1=ps1, nr=nr: e.tensor_scalar(out=MEAN[:, 0:nr], in0=ps1[:, 0:nr], scalar1=1.0 / 256, scalar2=None, op0=ALU.mult))
                S.op("dve", [str2], [str2], lambda e, nr=nr: e.tensor_tensor(out=MSQ[:, 0:nr], in0=MEAN[:, 0:nr], in1=MEAN[:, 0:nr], op=ALU.mult))
                S.op("dve", [ps1r, str2], [str2], lambda e, ps1=ps1, nr=nr: e.scalar_tensor_tensor(out=VAR[:, 0:nr], in0=ps1[:, 128:128 + nr], scalar=1.0 / 256, in1=MSQ[:, 0:nr], op0=ALU.mult, op1=ALU.subtract))
                S.op("act", [str2], [str2], lambda e, nr=nr: e.activation(out=RSTD[:, 0:nr], in_=VAR[:, 0:nr], func=AF.Sqrt, bias=self.EPSC[:, :], scale=1.0))
                S.op("dve", [str2], [str2], lambda e, nr=nr: e.reciprocal(out=RSTD[:, 0:nr], in_=RSTD[:, 0:nr]))
                for g in range(2):
                    S.op("dve", [cor, str2], [csr], lambda e, g=g, nr=nr: e.tensor_tensor(out=CS[:, g, 0:nr], in0=CO[:, g, 0:nr], in1=MEAN[:, 0:nr], op=ALU.subtract))
                    S.op("dve", [csr, str2], [csr], lambda e, g=g, nr=nr: e.tensor_tensor(out=CS[:, g, 0:nr], in0=CS[:, g, 0:nr], in1=RSTD[:, 0:nr], op=ALU.mult))
                    S.op("act", [csr, self.CONSTR], [self.HTR[2]],
                         lambda e, g=g, nr=nr, col0=col0: e.activation(out=self.HT[:, 4 + g, col0:col0 + nr], in_=CS[:, g, 0:nr], func=AF.Silu,
                                                                      scale=self.CVP[:, l, 1, g:g + 1], bias=self.CVP[:, l, 2, g:g + 1]))

    def mix_diff(self, l):
        S, u, I, O, cfg = self.S, self.u, self.I, self.O, self.cfg
        kind = u["kind"]
        pre = "p" if kind == "p" else "s"
        lam_init = 0.8 - 0.6 * math.exp(-0.3 * l)
        pan = self.load_panels(l, [(OFF_DIFF, 256), (OFF_DIFF + 256, 256), (OFF_DIFF + 512, 256)])
        S.barrier()
        KT, VA, FC, FT, LF = self.attn_buffers()
        ktr, var = Res("kt"), Res("va")
        SM = self.SM
        for sl, so, newtiles, npast in self.seq_iter():
            S.op("dve", [], [var], lambda e: e.memset(VA[:, :, :, 64:65], 1.0))
            for kt in range(npast):
                KTOK = self.TMPB[:, 0:256]
                S.dma("pool", KTOK, I["cdk"][l, sl, kt * 128:(kt + 1) * 128, :], [], [self.TMPBR], self.TMPBR)
                self.k_transpose(KTOK, self.TMPBR, 128, KT, ktr, kt * 128)
                S.dma("pool", VA[:, kt, :, 0:64], I["cdv"][l, sl, kt * 128:(kt + 1) * 128, :].rearrange("t (h x) -> t h x", h=4), [], [var], var)
            for qi, (k, t0, nr) in enumerate(newtiles):
                kt = npast + qi
                col0 = k * u["rows"]
                tab = (t0 // 128) if kind == "p" else self.NT
                (pA, pAr), (pB, pBr) = self.proj_tok(col0, nr, pan)
                RT = self.TMPA[:, 0:128].rearrange("p (a c) -> p a c", a=2)
                S.dma("sp", RT, I["diff_rope"][tab], [], [self.TMPAR], self.TMPAR)
                R32 = self.TMPA[:, 128:640]
                A3 = pA[:nr, :].rearrange("p (n x) -> p n x", n=16)
                R3 = R32[:nr, :].rearrange("p (n x) -> p n x", n=16)
                C3 = RT[:nr, 0, :].rearrange("p (n x) -> p n x", n=16)
                S3 = RT[:nr, 1, :].rearrange("p (n x) -> p n x", n=16)
                T1 = self.TMPC[:nr, 0:64].rearrange("p (n x) -> p n x", n=16)
                T2 = self.TMPC[:nr, 64:128].rearrange("p (n x) -> p n x", n=16)
                rd1 = [pAr, self.TMPAR]
                S.op("act", [pAr], [self.TMPAR], lambda e, pA=pA, nr=nr: e.copy(out=R32[:nr, :], in_=pA[:nr, :]))
                S.op("dve", rd1, [self.TMPCR], lambda e, A3=A3, C3=C3, T1=T1: e.tensor_tensor(out=T1, in0=A3[:, :, 0:4], in1=C3, op=ALU.mult))
                S.op("dve", rd1, [self.TMPCR], lambda e, A3=A3, S3=S3, T2=T2: e.tensor_tensor(out=T2, in0=A3[:, :, 4:8], in1=S3, op=ALU.mult))
                S.op("dve", [self.TMPCR, self.TMPAR], [self.TMPAR], lambda e, R3=R3, T1=T1, T2=T2: e.tensor_tensor(out=R3[:, :, 0:4], in0=T1, in1=T2, op=ALU.subtract))
                S.op("dve", rd1, [self.TMPCR], lambda e, A3=A3, C3=C3, T1=T1: e.tensor_tensor(out=T1, in0=A3[:, :, 4:8], in1=C3, op=ALU.mult))
                S.op("dve", rd1, [self.TMPCR], lambda e, A3=A3, S3=S3, T2=T2: e.tensor_tensor(out=T2, in0=A3[:, :, 0:4], in1=S3, op=ALU.mult))
                S.op("dve", [self.TMPCR, self.TMPAR], [self.TMPAR], lambda e, R3=R3, T1=T1, T2=T2: e.tensor_tensor(out=R3[:, :, 4:8], in0=T1, in1=T2, op=ALU.add))
                st, str_ = self.stg()
                S.op("act", [pBr], [str_], lambda e, st=st, pB=pB, nr=nr: e.copy(out=st[:nr, 0:256], in_=pB[:nr, 0:256]))
                S.dma("sp", O[pre + "_diff_k"][l, so, t0:t0 + nr, :], R32[:nr, 256:512], [self.TMPAR], [], self.TMPAR, is_out=True)
                S.dma("sp", O[pre + "_diff_v"][l, so, t0:t0 + nr, :], st[:nr, 0:256], [str_], [], str_, is_out=True)
                QA = self.TMPB[:, 0:256]
                QB = self.TMPB[:, 256:512]
                KB = self.TMPB[:, 512:768]
                S.op("dve", [], [self.TMPBR], lambda e: e.memset(self.TMPB[:, 0:512], 0.0))
                R8 = R32[:nr, 0:256].rearrange("p (n t x) -> p n t x", n=4, t=2)
                S.op("dve", [self.TMPAR], [self.TMPBR], lambda e, nr=nr, R8=R8: e.tensor_copy(out=QA[:nr, :].rearrange("p (n t x) -> p n t x", n=4, t=2)[:, :, 0, :], in_=R8[:, :, 0, :]))
                S.op("dve", [self.TMPAR], [self.TMPBR], lambda e, nr=nr, R8=R8: e.tensor_copy(out=QB[:nr, :].rearrange("p (n t x) -> p n t x", n=4, t=2)[:, :, 1, :], in_=R8[:, :, 1, :]))
                S.op("dve", [self.TMPAR], [self.TMPBR], lambda e, nr=nr: e.tensor_copy(out=KB[:nr, :], in_=R32[:nr, 256:512]))
                S.op("dve", [pBr], [var], lambda e, pB=pB, nr=nr, kt=kt: e.tensor_copy(out=VA[:nr, kt, :, 0:64], in_=pB[:nr, 0:256].rearrange("p (h x) -> p h x", h=4)))
                self.k_transpose(KB, self.TMPBR, nr, KT, ktr, kt * 128)
                QT = self.TMPB[:, 768:1024].rearrange("p (a c) -> p a c", a=2)
                QTS = self.SCR[:, 4608:4608 + 256].bitcast(BF16).rearrange("p (v g c) -> p v g c", v=2, g=2)
                qtsr = getattr(self, "_qtsr", None) or Res("qts")
                self._qtsr = qtsr
                for v_, QX in enumerate((QA, QB)):
                    pt, pr = self.ps()
                    pv = pt[:, :].bitcast(BF16)

                    def fq(e, pv=pv, nr=nr, QX=QX):
                        ins = None
                        for g in range(2):
                            ins = e.transpose(out=pv[:, g * 128:g * 128 + nr], in_=QX[:nr, g * 128:(g + 1) * 128], identity=self.IDB[:nr, :nr])
                        return ins
                    S.op("pe", [self.TMPBR, self.CONSTR], [pr], fq)
                    S.op("act", [pr], [qtsr], lambda e, pv=pv, nr=nr, v_=v_: e.copy(out=QTS[:, v_, :, 0:nr], in_=pv[:, 0:256].rearrange("p (g c) -> p g c", g=2)[:, :, 0:nr]))
                Y32 = self.TMPA[:, 640:896]
                items = [(h, sub, kk) for h in range(4) for sub in range(2) for kk in range(kt + 1)]
                pSs, pYs = {}, {}

                def stageA(it, kt=kt, nr=nr):
                    h, sub, kk = it
                    n = 2 * h + sub
                    g = n // 4
                    base = ((n % 4) // 2) * 64
                    nk = 128 if kk < kt else nr
                    pS, pSr = self.ps()
                    pSs[it] = (pS, pSr)
                    S.op("pe", [ktr, qtsr], [pSr],
                         lambda e, pS=pS, nk=nk, kk=kk, base=base, g=g, nr=nr, sub=sub: e.matmul(pS[:nk, 0:nr], KT[base:base + 64, g, kk * 128:kk * 128 + nk], QTS[base:base + 64, sub, g, 0:nr], start=True, stop=True))

                def stageB(it, kt=kt, nr=nr):
                    h, sub, kk = it
                    nk = 128 if kk < kt else nr
                    pS, pSr = pSs.pop(it)
                    if kk == 0:
                        pYs[(h, sub)] = self.ps(hold=True)
                    pY, pYr = pYs[(h, sub)]
                    PT, ptr = self.pt_next()
                    S.op("act", [pSr], [ptr], lambda e, pS=pS, nk=nk, nr=nr, PT=PT: e.activation(out=PT[:nk, 0:nr], in_=pS[:nk, 0:nr], func=AF.Exp, scale=32.0 ** -0.5))
                    if kk == kt:
                        S.op("dve", [ptr, self.CONSTR], [ptr], lambda e, nk=nk, nr=nr, PT=PT: e.tensor_tensor(out=PT[:nk, 0:nr], in0=PT[:nk, 0:nr], in1=self.DMASK[:nk, 0:nr], op=ALU.mult))
                    S.op("pe", [ptr, var], [pYr],
                         lambda e, pY=pY, nk=nk, nr=nr, kk=kk, h=h, kt=kt, PT=PT: e.matmul(pY[:nr, 0:65], PT[:nk, 0:nr], VA[:nk, kk, h, :], start=(kk == 0), stop=(kk == kt)))
                    if kk == kt and sub == 1:
                        (p0, p0r), (p1, p1r) = pYs[(h, 0)], pYs[(h, 1)]
                        S.op("dve", [p0r], [self.SMR], lambda e, p0=p0, nr=nr: e.reciprocal(out=SM[:nr, 20:21], in_=p0[:nr, 64:65]))
                        S.op("dve", [p1r], [self.SMR], lambda e, p1=p1, nr=nr: e.reciprocal(out=SM[:nr, 21:22], in_=p1[:nr, 64:65]))
                        S.op("dve", [self.SMR, self.CONSTR], [self.SMR], lambda e, nr=nr: e.tensor_tensor(out=SM[:nr, 22:23], in0=SM[:nr, 21:22], in1=self.LAM[:nr, l, 1:2], op=ALU.mult))
                        T64 = self.TMPC[:, 256:320]
                        S.op("act", [p0r, self.SMR], [self.TMPCR], lambda e, p0=p0, nr=nr, T64=T64: e.activation(out=T64[:nr, :], in_=p0[:nr, 0:64], func=AF.Copy, scale=SM[:nr, 20:21]))
                        S.op("dve", [p1r, self.SMR, self.TMPCR], [self.TMPAR],
                             lambda e, p1=p1, nr=nr, h=h, T64=T64: e.scalar_tensor_tensor(out=Y32[:nr, h * 64:(h + 1) * 64], in0=p1[:nr, 0:64], scalar=SM[:nr, 22:23], in1=T64[:nr, :], op0=ALU.mult, op1=ALU.add))
                        self.ps_release(p0r)
                        self.ps_release(p1r)

                LOOK = 3
                for it in items[:LOOK]:
                    stageA(it)
                for i, it in enumerate(items):
                    if i + LOOK < len(items):
                        stageA(items[i + LOOK])
                    stageB(it)
                HB = self.TMPB[:, 0:256]
                gbc = self.SUBG[:nr, l, :].unsqueeze(1).to_broadcast([nr, 4, 64])
                self.group_norm(Y32, self.TMPAR, nr, 4, 64, False, None, HB, self.TMPBR, gbc, 1.0 - lam_init)
                self.to_ht(3, HB, self.TMPBR, nr, col0)


def _shard_inputs(cfg, ncores, inp):
    NP, NS = cfg.NP, cfg.NS
    hc = host_consts(cfg)
    f32 = lambda a: np.ascontiguousarray(np.asarray(a, dtype=np.float32))
    shared = {}
    shared["w_in"] = f32(inp["w_in"])
    shared["b_fox_f"] = f32(inp["b_fox_f"])
    wc = f32(inp["w_conv"])
    shared["wconvT"] = np.ascontiguousarray(wc.reshape(2, 31, 2, 128).transpose(0, 3, 2, 1))
    cp = np.stack([f32(inp["b_conv"]), f32(inp["conv_ln_g"]), f32(inp["conv_ln_b"])], axis=1)
    shared["convp"] = np.ascontiguousarray(cp.reshape(2, 3, 2, 128).transpose(0, 3, 1, 2))
    shared["diff_lambda"] = f32(inp["diff_lambda"]).reshape(2, 128)
    shared["diff_subln_g"] = f32(inp["diff_subln_g"])
    shared["w_branch"] = f32(inp["w_branch"])
    shared["w_out"] = f32(inp["w_out"])
    shared["w_ada"] = f32(inp["w_ada"])
    ba = f32(inp["b_ada"])
    shared["b_ada"] = ba
    shared["b_adaT"] = np.ascontiguousarray(ba.reshape(4, 24, 128).transpose(2, 0, 1))
    shared["ln_g"] = f32(inp["ln_g"])
    shared["ln_b"] = f32(inp["ln_b"])
    shared["w_ffn_in"] = f32(inp["w_ffn_in"])
    shared["w_ffn_out"] = f32(inp["w_ffn_out"])
    shared["w_router"] = np.ascontiguousarray(f32(inp["w_router"])[0].reshape(8, 128, 8).transpose(1, 0, 2))
    shared["b_router"] = f32(inp["b_router"])
    shared["w_exp_in"] = f32(inp["w_exp_in"])
    shared["w_exp_out"] = f32(inp["w_exp_out"])
    for k, v in hc.items():
        shared[k] = v
    maps = []
    for c in range(ncores):
        m = dict(shared)
        m["xp"] = f32(inp["x_prompt"][c * NP:(c + 1) * NP])
        m["xs"] = f32(inp["x_sample"][c * NS:(c + 1) * NS])
        call = np.concatenate([f32(inp["c_prompt"][c * NP:(c + 1) * NP]), f32(inp["c_sample"][c * NS:(c + 1) * NS])], axis=0)
        m["cT"] = np.ascontiguousarray(call.reshape(cfg.NSEQ, 8, 128).transpose(2, 1, 0))
        sl = slice(c * NS, (c + 1) * NS)
        m["cfk"] = f32(np.asarray(inp["cache_fox_k"])[:, sl]).reshape(2, NS, cfg.PAST, 256)
        m["cfv"] = f32(np.asarray(inp["cache_fox_v"])[:, sl]).reshape(2, NS, cfg.PAST, 256)
        m["cfl"] = f32(np.asarray(inp["cache_fox_logf"])[:, sl])
        m["cdk"] = f32(np.asarray(inp["cache_diff_k"])[:, sl]).reshape(2, NS, cfg.PAST, 256)
        m["cdv"] = f32(np.asarray(inp["cache_diff_v"])[:, sl]).reshape(2, NS, cfg.PAST, 256)
        m["sret"] = f32(np.asarray(inp["state_ret"])[:, sl])
        m["sconv"] = f32(np.asarray(inp["state_conv"])[:, sl])
        maps.append(m)
    return maps


def _gather(cfg, ncores, results):
    def cat(name, axis):
        return np.concatenate([np.asarray(r[name]) for r in results], axis=axis)
    B, T, TS = cfg.NP * ncores, cfg.T, cfg.TS
    BS = cfg.NS * ncores
    outs = [cat("yp", 0), cat("ys", 0)]
    for pre, nb, t in (("p", B, T), ("s", BS, TS)):
        outs += [cat(pre + "_fox_k", 1).reshape(2, nb, t, 4, 64), cat(pre + "_fox_v", 1).reshape(2, nb, t, 4, 64),
                 cat(pre + "_fox_logf", 1), cat(pre + "_diff_k", 1).reshape(2, nb, t, 8, 32),
                 cat(pre + "_diff_v", 1).reshape(2, nb, t, 4, 64), cat(pre + "_ret", 1), cat(pre + "_conv", 1)]
    return tuple(np.ascontiguousarray(o, dtype=np.float32) for o in outs)


def run(cfg, ncores, inp):
    b = Builder(cfg)
    nc = b.build()
    maps = _shard_inputs(cfg, ncores, inp)
    res = run_bass_kernel_spmd(nc, maps, core_ids=list(range(ncores)))
    return _gather(cfg, ncores, res.results)


def kernel(**inputs):
    ncores = 8
    B, T = inputs["x_prompt"].shape[0], inputs["x_prompt"].shape[1]
    BS, TS = inputs["x_sample"].shape[0], inputs["x_sample"].shape[1]
    PAST = inputs["cache_fox_k"].shape[2]
    cfg = Cfg(B // ncores, T, BS // ncores, TS, PAST)
    return run(cfg, ncores, inputs)
```

```python
import math
from contextlib import ExitStack
import numpy as np
import ml_dtypes
import concourse.bass as bass
import concourse.mybir as mybir
from concourse.bass_utils import run_bass_kernel_spmd

F32 = mybir.dt.float32
BF16 = mybir.dt.bfloat16
AF = mybir.ActivationFunctionType
ALU = mybir.AluOpType
AX = mybir.AxisListType

D = 1024
DEPTH = 2
BW = 256
HD = 64
CONV_W = 31
D_FF = 2816
N_EXP = 8
D_EXP = 3584
ALPHA = (2.0 * DEPTH) ** 0.25
EPS = 1e-5
OFF_RET = 0
OFF_FOX = 1024
OFF_FOX_F = 1792
OFF_CONV = 1796
OFF_DIFF = 2308
OFF_GATE = 3076
N_IN = 7172
SEM_CAP = 30000


class Res:
    __slots__ = ("w", "r", "sem", "cnt", "name", "uid", "excl")
    _n = 0

    def __init__(self, name="", excl=False):
        Res._n += 1
        self.uid = Res._n
        self.excl = excl
        self.w = None
        self.r = {}
        self.sem = None
        self.cnt = 0
        self.name = name


class Sched:
    def __init__(self, nc, es):
        self.nc = nc
        self.es = es
        self.names = ["pe", "act", "dve", "pool", "sp"]
        self.ops = {k: [] for k in self.names}
        self.count = {k: 0 for k in self.names}
        self.clock = {k: {} for k in self.names}
        self.esems = {k: [] for k in self.names}
        self.nsem = 0
        self.const_res = Res("const")
        self.out_events = {}

    def new_sem(self, nm):
        self.nsem += 1
        return self.es.enter_context(self.nc.semaphore("s_%s_%d" % (nm, self.nsem)))

    def esem(self, e, idx):
        while len(self.esems[e]) <= idx:
            self.esems[e].append(self.new_sem(e))
        return self.esems[e][idx]

    def _deps(self, e, reads, writes):
        deps = {}

        def add(key, val):
            if deps.get(key, 0) < val:
                deps[key] = val

        for r in reads:
            if r.w is not None:
                add(*r.w)
            if r.excl:
                for k, v in r.r.items():
                    if k != ("e", e):
                        add(k, v)
        for w in writes:
            if w.w is not None:
                add(*w.w)
            for k, v in w.r.items():
                add(k, v)
        clk = self.clock[e]
        waits = []
        for key, val in deps.items():
            if key[0] == "e" and key[1] == e and e == "pe":
                continue
            if clk.get(key, 0) >= val:
                continue
            clk[key] = val
            waits.append((key, val))
        return waits

    def _mark(self, ev, reads, writes):
        key, val = ev
        for r in reads:
            if r.r.get(key, 0) < val:
                r.r[key] = val
        for w in writes:
            w.w = ev
            w.r = {}

    def op(self, e, reads, writes, fn):
        waits = self._deps(e, reads, writes)
        self.count[e] += 1
        ev = (("e", e), self.count[e])
        self.ops[e].append((waits, fn, ("e", e, self.count[e])))
        self._mark(ev, reads, writes)

    def dma(self, e, out, in_, reads, writes, sres, is_out=False, **kw):
        waits = self._deps(e, reads, writes)
        if sres.sem is None:
            sres.sem = self.new_sem("d")
        sres.cnt += 16
        key = ("d", sres.uid)
        self.semmap[key] = sres.sem
        ev = (key, sres.cnt)

        def fn(eng, out=out, in_=in_, kw=kw):
            return eng.dma_start(out=out, in_=in_, **kw)

        self.ops[e].append((waits, fn, ("d", sres.sem, 16)))
        self._mark(ev, reads, writes)
        self.dma_latest[key] = sres.cnt
        if is_out:
            self.out_events[key] = sres.cnt

    semmap = {}
    dma_latest = {}

    def barrier(self):
        for e in self.names:
            waits = []
            for o in ["pe", "act", "dve", "pool"]:
                if o == e or self.count[o] == 0:
                    continue
                key = ("e", o)
                if self.clock[e].get(key, 0) < self.count[o]:
                    self.clock[e][key] = self.count[o]
                    waits.append((key, self.count[o]))
            for key, val in self.dma_latest.items():
                if self.clock[e].get(key, 0) < val:
                    self.clock[e][key] = val
                    waits.append((key, val))
            if waits:
                self.ops[e].append((waits, None, None))

    def finish(self):
        waits = [(k, v) for k, v in self.out_events.items()]
        self.ops["sp"].append((waits, None, None))

    def _emit_wait(self, eng, key, val):
        if key[0] == "e":
            idx = (val - 1) // SEM_CAP
            eng.wait_ge(self.esem(key[1], idx), val - idx * SEM_CAP)
        else:
            eng.wait_ge(self.semmap[key], val)

    def emit(self, block):
        engs = {"pe": block.tensor, "act": block.scalar, "dve": block.vector,
                "pool": block.gpsimd, "sp": block.sync}
        for name in self.names:
            ops = self.ops[name]

            def body(eng, ops=ops, name=name):
                for waits, fn, inc in ops:
                    for key, val in waits:
                        self._emit_wait(eng, key, val)
                    if fn is None:
                        continue
                    ins = fn(eng)
                    if inc[0] == "e":
                        cnt = inc[2]
                        idx = (cnt - 1) // SEM_CAP
                        ins.then_inc(self.esem(name, idx), 1)
                    else:
                        ins.then_inc(inc[1], 16)

            engs[name](body)


class StopBuild(Exception):
    pass


MAX_STAGE = [10 ** 9]
STOP_NAME = [None]


class Cfg:
    def __init__(self, NP, T, NS, TS, PAST):
        self.NP, self.T, self.NS, self.TS, self.PAST = NP, T, NS, TS, PAST
        self.NSEQ = NP + NS


def host_consts(cfg):
    c = {}
    T, TS, PAST = cfg.T, cfg.TS, cfg.PAST
    c["ident_f"] = np.eye(128, dtype=np.float32)
    c["ones_f"] = np.ones((128, 128), np.float32)
    idx = np.arange(128)
    c["utri_f"] = (idx[:, None] <= idx[None, :]).astype(np.float32)
    c["cmask"] = (idx[:, None] <= idx[None, :]).astype(np.float32)
    c["dmask"] = ((idx[:, None] // 64) <= (idx[None, :] // 64)).astype(np.float32)
    half = 32
    inv = np.exp(-math.log(10000.0) * np.arange(half, dtype=np.float32) / half).astype(np.float32)

    def ret_tab(pos):
        ang = pos.astype(np.float32)[:, None] * inv[None, :]
        cos = np.cos(ang).astype(np.float32)
        sin = np.sin(ang).astype(np.float32)
        tab = np.zeros((len(pos), 2, 8, 32), np.float32)
        tab[:, 0, 0:4] = cos[:, None, :]
        tab[:, 1, 0:4] = sin[:, None, :]
        tab[:, 0, 4:8] = cos[:, None, :] * 0.125
        tab[:, 1, 4:8] = sin[:, None, :] * 0.125
        return tab.reshape(len(pos), 2, 256)

    ntp = T // 64
    rt = np.zeros((ntp + 1, 64, 2, 256), np.float32)
    for i in range(ntp):
        rt[i] = ret_tab(np.arange(i * 64, (i + 1) * 64))
    rt[ntp, :TS] = ret_tab(PAST + np.arange(TS))
    c["ret_rope"] = rt
    inv_d = np.exp(-math.log(500000.0) * np.arange(4, dtype=np.float32) / 4).astype(np.float32)

    def diff_tab(pos):
        ang = pos.astype(np.float32)[:, None] * inv_d[None, :]
        cos = np.cos(ang).astype(np.float32)
        sin = np.sin(ang).astype(np.float32)
        tab = np.zeros((len(pos), 2, 16, 4), np.float32)
        tab[:, 0] = cos[:, None, :]
        tab[:, 1] = sin[:, None, :]
        return tab.reshape(len(pos), 2, 64)

    nt = T // 128
    dt_ = np.zeros((nt + 1, 128, 2, 64), np.float32)
    for i in range(nt):
        dt_[i] = diff_tab(np.arange(i * 128, (i + 1) * 128))
    dt_[nt, :TS] = diff_tab(PAST + np.arange(TS))
    c["diff_rope"] = dt_
    log_g = np.log1p(-np.exp2(-5.0 - np.arange(4, dtype=np.float32))).astype(np.float32)
    rd = np.zeros((2, 64, 2, 4, 64), np.float32)
    rs = np.zeros((2, 64, 2, 4), np.float32)
    for kind, L in ((0, min(T, 64)), (1, TS)):
        ii = np.arange(L, dtype=np.float32)
        inner = np.exp(log_g[:, None, None] * np.abs(ii[:, None] - ii[None, :])).astype(np.float32)
        qdec = np.exp(log_g[:, None] * (ii[None, :] + 1.0)).astype(np.float32)
        kdec = np.exp(log_g[:, None] * (L - 1.0 - ii[None, :])).astype(np.float32)
        cdec = np.exp(log_g * L).astype(np.float32)
        for h in range(4):
            rd[kind, :L, 0, h, :L] = inner[h].T
            rd[kind, :, 1, h, :L] = qdec[h][None, :]
            rs[kind, :L, 0, h] = kdec[h]
            rs[kind, :, 1, h] = cdec[h]
    c["ret_dec"] = rd.reshape(2, 64, 2, 256)
    c["ret_decs"] = rs
    return c


CONST_SHAPES = None


class Builder:
    def __init__(self, cfg, dbg=False):
        self.cfg = cfg
        self.dbg = dbg

    def build(self):
        cfg = self.cfg
        NP, T, NS, TS, PAST = cfg.NP, cfg.T, cfg.NS, cfg.TS, cfg.PAST
        NSEQ = cfg.NSEQ
        nc = bass.Bass("TRN2", target_bir_lowering=False)
        self.nc = nc
        self.es = ExitStack()
        es = self.es
        S = Sched(nc, es)
        Sched.semmap = {}
        Sched.dma_latest = {}
        self.S = S

        def din(name, shape, dt=F32):
            return nc.dram_tensor(name, list(shape), dt, kind="ExternalInput").ap()

        def dout(name, shape):
            return nc.dram_tensor(name, list(shape), F32, kind="ExternalOutput").ap()

        I = {}
        I["xp"] = din("xp", [NP, T, D])
        I["xs"] = din("xs", [NS, TS, D])
        I["cT"] = din("cT", [128, 8, NSEQ])
        for nm in ("cfk", "cfv", "cdk", "cdv"):
            I[nm] = din(nm, [2, NS, PAST, 256])
        I["cfl"] = din("cfl", [2, NS, PAST, 4])
        I["sret"] = din("sret", [2, NS, 4, 64, 64])
        I["sconv"] = din("sconv", [2, NS, 30, 256])
        I["w_in"] = din("w_in", [2, D, N_IN])
        I["b_fox_f"] = din("b_fox_f", [2, 4])
        I["wconvT"] = din("wconvT", [2, 128, 2, 31])
        I["convp"] = din("convp", [2, 128, 3, 2])
        I["diff_lambda"] = din("diff_lambda", [2, 128])
        I["diff_subln_g"] = din("diff_subln_g", [2, 64])
        I["w_branch"] = din("w_branch", [2, 4, 256, D])
        I["w_out"] = din("w_out", [2, D, D])
        I["w_ada"] = din("w_ada", [2, 2, D, 3 * D])
        I["b_adaT"] = din("b_adaT", [128, 4, 24])
        I["b_ada"] = din("b_ada", [2, 2, 3 * D])
        I["ln_g"] = din("ln_g", [2, 2, D])
        I["ln_b"] = din("ln_b", [2, 2, D])
        I["w_ffn_in"] = din("w_ffn_in", [1, D, 2 * D_FF])
        I["w_ffn_out"] = din("w_ffn_out", [1, D_FF, D])
        I["w_router"] = din("w_router", [128, 8, 8])
        I["b_router"] = din("b_router", [1, 8])
        I["w_exp_in"] = din("w_exp_in", [1, N_EXP, D, 2 * D_EXP])
        I["w_exp_out"] = din("w_exp_out", [1, N_EXP, D_EXP, D])
        hc = host_consts(cfg)
        for k, v in hc.items():
            I[k] = din(k, list(v.shape))
        self.I = I
        O = {}
        O["yp"] = dout("yp", [NP, T, D])
        O["ys"] = dout("ys", [NS, TS, D])
        for pre, n, t in (("p", NP, T), ("s", NS, TS)):
            O[pre + "_fox_k"] = dout(pre + "_fox_k", [2, n, t, 256])
            O[pre + "_fox_v"] = dout(pre + "_fox_v", [2, n, t, 256])
            O[pre + "_fox_logf"] = dout(pre + "_fox_logf", [2, n, t, 4])
            O[pre + "_diff_k"] = dout(pre + "_diff_k", [2, n, t, 256])
            O[pre + "_diff_v"] = dout(pre + "_diff_v", [2, n, t, 256])
            O[pre + "_ret"] = dout(pre + "_ret", [2, n, 4, 64, 64])
            O[pre + "_conv"] = dout(pre + "_conv", [2, n, 30, 256])
        self.O = O

        NT = T // 128
        self.NT = NT
        TT = max(NT, 2 * NS)
        NCOL = max(T, NS * TS)
        self.NCOL = NCOL
        NKT = max(NT, PAST // 128 + 1)
        self.NKT = NKT
        NK = NKT * 128

        def sb(name, shape, dt=F32):
            return es.enter_context(nc.sbuf_tensor(name, list(shape), dt))

        self.ACC = sb("ACC", [128, TT, D])
        self.ACCR = [Res("acc%d" % i) for i in range(TT)]
        self.UT = sb("UT", [128, 8, NCOL], BF16)
        self.UTR = Res("ut")
        self.HT = sb("HT", [128, 8, NCOL], BF16)
        self.HTR = [Res("ht%d" % i) for i in range(4)]
        self.NWS = 6
        self.WS = sb("WS", [128, self.NWS, 2048], BF16)
        self.WSR = [Res("ws%d" % i) for i in range(self.NWS)]
        self.wsi = 0
        self.SCR = sb("SCR", [128, 5120])
        self.MODT = sb("MODT", [128, 4, 16, NSEQ])
        self.MODR = Res("modt")
        self.BADA = sb("BADA", [128, 4, 24])
        self.SCT = sb("SCT", [128, 8, NSEQ], BF16)
        self.SCTR = Res("sct")
        self.SCREP = sb("SCREP", [128, 8, 128], BF16)
        self.SCREPR = Res("screp")
        self.GB0 = sb("GB0", [128, D])
        self.GBR = [Res("gb%d" % i) for i in range(max(NS, 1))]
        self.ROWA = sb("ROWA", [128, D])
        self.ROWAR = Res("rowa")
        self.ROWB = sb("ROWB", [128, D])
        self.ROWBR = Res("rowb")
        self.IDF = sb("IDF", [128, 128])
        self.IDB = sb("IDB", [128, 128], BF16)
        self.ONESF = sb("ONESF", [128, 128])
        self.UTRI = sb("UTRI", [128, 128])
        self.CMASK = sb("CMASK", [128, 128], BF16)
        self.DMASK = sb("DMASK", [128, 128], BF16)
        self.RDEC = sb("RDEC", [64, 2, 2, 256])
        self.RDECS = sb("RDECS", [64, 2, 2, 4])
        self.CONSTR = Res("consts")
        self.WCV = sb("WCV", [128, 2, 2, 31])
        self.CVP = sb("CVP", [128, 2, 3, 2])
        self.BFF = sb("BFF", [128, 2, 4])
        self.LAM = sb("LAM", [128, 2, 4])
        self.SUBG = sb("SUBG", [128, 2, 64])
        self.WRT = sb("WRT", [128, 8, 8])
        self.BRT = sb("BRT", [128, 8])
        self.STG = sb("STG", [128, 1, 512])
        self.STGR = [Res("stg0")]
        self.stgi = 0
        self.TMPA = sb("TMPA", [128, 1024])
        self.TMPAR = Res("tmpa")
        self.TMPB = sb("TMPB", [128, 1024], BF16)
        self.TMPBR = Res("tmpb")
        self.TMPC = sb("TMPC", [128, 512])
        self.TMPCR = Res("tmpc")
        self.EPSC = sb("EPSC", [128, 1])
        self.ONEC = sb("ONEC", [128, 1])
        self.SM = sb("SM", [128, 64])
        self.SMR = Res("sm")
        self.PTB = sb("PTB", [128, 4, 128], BF16)
        self.PTR = [Res("pt%d" % i) for i in range(4)]
        self.pti = 0
        self.PSB = []
        for i in range(8):
            t = es.enter_context(nc.psum_tensor("PS%d" % i, [128, 512], F32))
            self.PSB.append((t, Res("ps%d" % i, excl=True)))
        self.psi = 0

        self.stage = 0
        try:
            self.load_consts()
            self.stage_end("consts")
            self.ada_precompute()
            self.stage_end("ada")
            units = [("p", i) for i in range(NP)] + ([("s", 0)] if NS > 0 else [])
            for kind, i in units:
                self.run_unit(kind, i)
        except StopBuild:
            pass
        S.finish()
        with nc.Block() as block:
            S.emit(block)
        return nc

    def stage_end(self, name):
        self.stage += 1
        if self.stage >= MAX_STAGE[0] or name == STOP_NAME[0]:
            print("STOP after stage", self.stage, name)
            raise StopBuild()

    def ps(self, hold=False):
        held = self.__dict__.setdefault("ps_held", set())
        while self.psi in held:
            self.psi = (self.psi + 1) % 8
        i = self.psi
        self.psi = (self.psi + 1) % 8
        if hold:
            held.add(i)
        return self.PSB[i]

    def pt_next(self):
        i = self.pti
        self.pti = (i + 1) % 4
        return self.PTB[:, i, :], self.PTR[i]

    def ps_release(self, pr):
        for i, (t, r) in enumerate(self.PSB):
            if r is pr:
                self.ps_held.discard(i)

    def stg(self):
        return self.STG[:, 0, :], self.STGR[0]

    def load_w(self, src2d, nk, ncols, k0=0):
        i = self.wsi
        self.wsi = (self.wsi + 1) % self.NWS
        res = self.WSR[i]
        assert res.w is None or res.r, "WS slot %d reloaded before its consumers were emitted" % i
        view = self.WS[:, i, 0:nk * ncols].rearrange("p (k c) -> p k c", k=nk)
        src = src2d[k0 * 128:(k0 + nk) * 128, :].rearrange("(k p) c -> p k c", p=128)
        self.S.dma("pool", view, src, [], [res], res)
        return view, res

    def cdma(self, out, in_, **kw):
        self.S.dma("sp", out, in_, [], [self.CONSTR], self.CONSTR, **kw)

    def load_consts(self):
        I = self.I
        S = self.S
        self.cdma(self.IDF[:, :], I["ident_f"][:, :])
        self.cdma(self.ONESF[:, :], I["ones_f"][:, :])
        self.cdma(self.UTRI[:, :], I["utri_f"][:, :])
        self.cdma(self.RDEC[:, :, :, :], I["ret_dec"].rearrange("k p w c -> p k w c"))
        self.cdma(self.RDECS[:, :, :, :], I["ret_decs"].rearrange("k p w h -> p k w h"))
        self.cdma(self.WCV[:, :, :, :], I["wconvT"].rearrange("l p g w -> p l g w"))
        self.cdma(self.CVP[:, :, :, :], I["convp"].rearrange("l p a g -> p l a g"))
        self.cdma(self.BFF[:, :, :], I["b_fox_f"].rearrange("l h -> (l h)").partition_broadcast(128).rearrange("p (l h) -> p l h", l=2))
        self.DLAM = self.TMPA[:, 0:256].rearrange("p (l c) -> p l c", l=2)
        S.dma("sp", self.DLAM, I["diff_lambda"].rearrange("l c -> (l c)").partition_broadcast(128).rearrange("p (l c) -> p l c", l=2), [], [self.TMPAR], self.TMPAR)
        self.cdma(self.SUBG[:, :, :], I["diff_subln_g"].rearrange("l c -> (l c)").partition_broadcast(128).rearrange("p (l c) -> p l c", l=2))
        self.cdma(self.WRT[:, :, :], I["w_router"][:, :, :])
        self.cdma(self.BRT[:, :], I["b_router"].rearrange("a e -> (a e)").partition_broadcast(128))
        self.cdma(self.BADA[:, :, :], I["b_adaT"][:, :, :])
        self.CONSTP = Res("constp")
        S.dma("pool", self.CMASK[:, :], I["cmask"][:, :], [], [self.CONSTP], self.CONSTP)
        S.dma("pool", self.DMASK[:, :], I["dmask"][:, :], [], [self.CONSTP], self.CONSTP)
        S.dma("pool", self.IDB[:, :], I["ident_f"][:, :], [], [self.CONSTP], self.CONSTP)
        S.op("dve", [self.CONSTP], [self.CONSTR], lambda e: e.memset(self.EPSC[:, :], EPS))
        S.op("dve", [], [self.CONSTR], lambda e: e.memset(self.EPSC[:, :], EPS))
        S.op("dve", [], [self.CONSTR], lambda e: e.memset(self.ONEC[:, :], 1.0))
        C = self.CONSTR
        for l in range(2):
            lam_init = 0.8 - 0.6 * math.exp(-0.3 * l)
            DL, LAM, TM = self.DLAM, self.LAM, self.SM
            S.op("dve", [C, self.TMPAR], [self.SMR], lambda e, l=l: e.tensor_tensor(out=TM[:, 0:32], in0=DL[:, l, 0:32], in1=DL[:, l, 32:64], op=ALU.mult))
            S.op("dve", [self.SMR], [self.SMR], lambda e, l=l: e.tensor_reduce(out=TM[:, 32:33], in_=TM[:, 0:32], axis=AX.X, op=ALU.add))
            S.op("dve", [C, self.TMPAR], [self.SMR], lambda e, l=l: e.tensor_tensor(out=TM[:, 0:32], in0=DL[:, l, 64:96], in1=DL[:, l, 96:128], op=ALU.mult))
            S.op("dve", [self.SMR], [self.SMR], lambda e, l=l: e.tensor_reduce(out=TM[:, 33:34], in_=TM[:, 0:32], axis=AX.X, op=ALU.add))
            S.op("act", [self.SMR], [self.SMR], lambda e, l=l: e.activation(out=TM[:, 34:36], in_=TM[:, 32:34], func=AF.Exp))
            S.op("dve", [self.SMR], [self.SMR], lambda e, l=l: e.tensor_tensor(out=TM[:, 36:37], in0=TM[:, 34:35], in1=TM[:, 35:36], op=ALU.subtract))
            S.op("dve", [self.SMR], [C], lambda e, l=l, li=lam_init: e.tensor_scalar(out=LAM[:, l, 0:1], in0=TM[:, 36:37], scalar1=li, scalar2=None, op0=ALU.add))
            S.op("dve", [C], [C], lambda e, l=l: e.tensor_scalar(out=LAM[:, l, 1:2], in0=LAM[:, l, 0:1], scalar1=-1.0, scalar2=None, op0=ALU.mult))

    def ada_precompute(self):
        S, I = self.S, self.I
        NSEQ = self.cfg.NSEQ
        CT = self.TMPA[:, 0:8 * NSEQ].rearrange("p (k s) -> p k s", k=8)
        S.dma("sp", CT, I["cT"][:, :, :], [], [self.TMPAR], self.TMPAR)
        S.op("act", [self.TMPAR], [self.SCTR], lambda e: e.activation(out=self.SCT[:, :, :], in_=CT, func=AF.Silu))
        BT = self.BADA
        for l in range(2):
            for s in range(2):
                ls = l * 2 + s
                pt, pr = self.ps()
                for j in range(16):
                    if j % 2 == 0:
                        wv, wr = self.load_w(I["w_ada"][l, s][:, (j // 2) * 256:(j // 2 + 1) * 256], 8, 256)

                    def f(e, j=j, wv=wv, pt=pt):
                        ins = None
                        for kc in range(8):
                            ins = e.matmul(pt[:, j * NSEQ:(j + 1) * NSEQ], wv[:, kc, (j % 2) * 128:(j % 2 + 1) * 128],
                                           self.SCT[:, kc, :], start=(kc == 0), stop=(kc == 7))
                        return ins
                    S.op("pe", [wr, self.SCTR], [pr], f)
                    S.op("act", [pr, self.CONSTR], [self.MODR],
                         lambda e, j=j, ls=ls, pt=pt: e.activation(out=self.MODT[:, ls, j, :], in_=pt[:, j * NSEQ:(j + 1) * NSEQ],
                                                                   func=AF.Identity, bias=BT[:, ls, j:j + 1], scale=1.0))
                S.op("dve", [self.MODR], [self.MODR],
                     lambda e, ls=ls: e.tensor_scalar(out=self.MODT[:, ls, 8:16, :], in0=self.MODT[:, ls, 8:16, :], scalar1=1.0, scalar2=None, op0=ALU.add))

    def gate_rows(self, l, s, seqs, gbviews):
        S, I = self.S, self.I
        S.dma("sp", self.ROWA[:, :], I["b_ada"][l, s, 2 * D:3 * D].partition_broadcast(128), [], [self.ROWAR], self.ROWAR)
        for si, seqg in enumerate(seqs):
            gbv, gbr = gbviews[si]
            S.op("dve", [self.SCTR], [self.SCREPR],
                 lambda e, seqg=seqg: e.tensor_copy(out=self.SCREP[:, :, :], in_=self.SCT[:, :, seqg:seqg + 1].to_broadcast([128, 8, 128])))
            for cb in range(4):
                wv, wr = self.load_w(I["w_ada"][l, s][:, 2 * D + cb * 256:2 * D + (cb + 1) * 256], 8, 256)
                if cb % 2 == 0:
                    pt, pr = self.ps()

                def f(e, wv=wv, pt=pt, cb=cb):
                    ins = None
                    for kc in range(8):
                        ins = e.matmul(pt[:, (cb % 2) * 256:(cb % 2 + 1) * 256], self.SCREP[:, kc, :], wv[:, kc, :],
                                       start=(kc == 0), stop=(kc == 7))
                    return ins
                S.op("pe", [wr, self.SCREPR], [pr], f)
                if cb % 2 == 1:
                    c0 = (cb // 2) * 512
                    S.op("dve", [pr, self.ROWAR], [gbr],
                         lambda e, pt=pt, gbv=gbv, c0=c0: e.tensor_tensor(out=gbv[:, c0:c0 + 512], in0=pt[:, :], in1=self.ROWA[:, c0:c0 + 512], op=ALU.add))

    def run_unit(self, kind, ui):
        cfg, S, I, O = self.cfg, self.S, self.I, self.O
        if kind == "p":
            rows, ntl = 128, self.NT
            seqs = [ui]
            tiles = [(0, i) for i in range(ntl)]
            xsrc = lambda k: I["xp"][ui, k * 128:(k + 1) * 128, :]
            ydst = lambda k: O["yp"][ui, k * 128:(k + 1) * 128, :]
            gbviews = [(self.GB0, self.GBR[0])]
        else:
            rows, ntl = cfg.TS, cfg.NS
            seqs = [cfg.NP + j for j in range(cfg.NS)]
            tiles = [(j, 0) for j in range(ntl)]
            xsrc = lambda k: I["xs"][k, :, :]
            ydst = lambda k: O["ys"][k, :, :]
            gbviews = [(self.ACC[:, cfg.NS + j, :], self.ACCR[cfg.NS + j]) for j in range(cfg.NS)]
        u = dict(kind=kind, ui=ui, rows=rows, ntl=ntl, seqs=seqs, tiles=tiles, ncols=rows * ntl, gb=gbviews)
        self.u = u
        for k in range(ntl):
            S.dma("sp", self.ACC[:rows, k, :], xsrc(k), [], [self.ACCR[k]], self.ACCR[k])
        for l in range(2):
            self.make_uT(l, 0)
            self.stage_end("uT")
            self.gate_rows(l, 0, seqs, gbviews)
            self.stage_end("gate")
            self.mixers(l)
            self.merge_out(l)
            self.stage_end("merge")
            self.layer_norm(l, 0)
            self.stage_end("ln")
            self.make_uT(l, 1, router=(l == 1))
            self.gate_rows(l, 1, seqs, gbviews)
            if l == 0:
                self.ffn(I["w_ffn_in"][0], I["w_ffn_out"][0], D_FF, None)
            else:
                for ex in range(N_EXP):
                    self.ffn(I["w_exp_in"][0, ex], I["w_exp_out"][0, ex], D_EXP, ex)
            self.layer_norm(l, 1)
        for k in range(ntl):
            S.dma("sp", ydst(k), self.ACC[:rows, k, :], [self.ACCR[k]], [], self.ACCR[k], is_out=True)

    def make_uT(self, l, s, router=False):
        S, u = self.S, self.u
        rows = u["rows"]
        ls = l * 2 + s
        if router:
            self.COMB = self.SCR[:, 0:u["ntl"] * 8].rearrange("p (t e) -> p t e", e=8)
            self.COMBR = Res("comb")
        for k, (sl, ti) in enumerate(u["tiles"]):
            seqg = u["seqs"][sl]
            c0 = k * rows
            if router:
                lt, lr = self.ps()
            for half in range(2):
                pt, pr = self.ps()

                def f(e, pt=pt, k=k, half=half):
                    ins = None
                    for j in range(4):
                        c = half * 4 + j
                        ins = e.transpose(out=pt[:, j * 128:j * 128 + rows], in_=self.ACC[:rows, k, c * 128:(c + 1) * 128],
                                          identity=self.IDF[:rows, :rows])
                    return ins
                S.op("pe", [self.ACCR[k], self.CONSTR], [pr], f)
                for j in range(4):
                    c = half * 4 + j
                    if not router:
                        S.op("act", [pr, self.MODR], [self.UTR],
                             lambda e, pt=pt, j=j, c=c, c0=c0, seqg=seqg: e.activation(
                                 out=self.UT[:, c, c0:c0 + rows], in_=pt[:, j * 128:j * 128 + rows], func=AF.Identity,
                                 scale=self.MODT[:, ls, 8 + c, seqg:seqg + 1], bias=self.MODT[:, ls, c, seqg:seqg + 1]))
                    else:
                        S.op("act", [pr, self.MODR], [self.TMPAR],
                             lambda e, pt=pt, j=j, c=c, seqg=seqg: e.activation(
                                 out=self.TMPA[:, c * 128:c * 128 + rows], in_=pt[:, j * 128:j * 128 + rows], func=AF.Identity,
                                 scale=self.MODT[:, ls, 8 + c, seqg:seqg + 1], bias=self.MODT[:, ls, c, seqg:seqg + 1]))
                        S.op("dve", [self.TMPAR], [self.UTR],
                             lambda e, c=c, c0=c0: e.tensor_copy(out=self.UT[:, c, c0:c0 + rows], in_=self.TMPA[:, c * 128:c * 128 + rows]))
            if router:
                def fr(e, lt=lt):
                    ins = None
                    for c in range(8):
                        ins = e.matmul(lt[:rows, 0:8], self.TMPA[:, c * 128:c * 128 + rows], self.WRT[:, c, :], start=(c == 0), stop=(c == 7))
                    return ins
                S.op("pe", [self.TMPAR, self.CONSTR], [lr], fr)
                self.route(k, lt, lr)
            S.op("act", [self.ACCR[k]], [self.ACCR[k]], lambda e, k=k: e.mul(out=self.ACC[:rows, k, :], in_=self.ACC[:rows, k, :], mul=ALPHA))

    def route(self, k, lt, lr):
        S, u = self.S, self.u
        rows = u["rows"]
        SM, R = self.SM, self.SMR
        lg = SM[:rows, 0:8]
        S.op("dve", [lr, self.CONSTR], [R], lambda e: e.tensor_tensor(out=lg, in0=lt[:rows, 0:8], in1=self.BRT[:rows, :], op=ALU.add))
        S.op("dve", [R], [R], lambda e: e.tensor_reduce(out=SM[:rows, 8:9], in_=lg, axis=AX.X, op=ALU.max))
        S.op("dve", [R], [R], lambda e: e.tensor_scalar(out=SM[:rows, 16:24], in0=lg, scalar1=SM[:rows, 8:9], scalar2=None, op0=ALU.is_equal))
        S.op("dve", [R], [R], lambda e: e.scalar_tensor_tensor(out=SM[:rows, 24:32], in0=SM[:rows, 16:24], scalar=-1e30, in1=lg, op0=ALU.mult, op1=ALU.add))
        S.op("dve", [R], [R], lambda e: e.tensor_reduce(out=SM[:rows, 9:10], in_=SM[:rows, 24:32], axis=AX.X, op=ALU.max))
        S.op("dve", [R], [R], lambda e: e.tensor_scalar(out=SM[:rows, 32:40], in0=SM[:rows, 24:32], scalar1=SM[:rows, 9:10], scalar2=None, op0=ALU.is_equal))
        S.op("dve", [R], [R], lambda e: e.tensor_tensor(out=SM[:rows, 10:11], in0=SM[:rows, 9:10], in1=SM[:rows, 8:9], op=ALU.subtract))
        S.op("act", [R], [R], lambda e: e.activation(out=SM[:rows, 11:12], in_=SM[:rows, 10:11], func=AF.Exp))
        S.op("dve", [R], [R], lambda e: e.tensor_scalar(out=SM[:rows, 11:12], in0=SM[:rows, 11:12], scalar1=1.0, scalar2=None, op0=ALU.add))
        S.op("dve", [R], [R], lambda e: e.reciprocal(out=SM[:rows, 12:13], in_=SM[:rows, 11:12]))
        S.op("dve", [R], [R], lambda e: e.tensor_scalar(out=SM[:rows, 13:14], in0=SM[:rows, 12:13], scalar1=-1.0, scalar2=1.0, op0=ALU.mult, op1=ALU.add))
        S.op("dve", [R], [R], lambda e: e.tensor_scalar(out=SM[:rows, 16:24], in0=SM[:rows, 16:24], scalar1=SM[:rows, 12:13], scalar2=None, op0=ALU.mult))
        COMB = self.COMB
        S.op("dve", [R], [self.COMBR], lambda e, k=k: e.scalar_tensor_tensor(out=COMB[:rows, k, :], in0=SM[:rows, 32:40], scalar=SM[:rows, 13:14],
                                                                        in1=SM[:rows, 16:24], op0=ALU.mult, op1=ALU.add))

    def layer_norm(self, l, s):
        S, u, I = self.S, self.u, self.I
        rows = u["rows"]
        S.dma("sp", self.ROWA[:, :], I["ln_g"][l, s, :].partition_broadcast(128), [], [self.ROWAR], self.ROWAR)
        S.dma("sp", self.ROWB[:, :], I["ln_b"][l, s, :].partition_broadcast(128), [], [self.ROWBR], self.ROWBR)
        SM, R = self.SM, self.SMR
        for k in range(u["ntl"]):
            A = self.ACC[:rows, k, :]
            AR = self.ACCR[k]
            S.op("dve", [AR], [R], lambda e, A=A: e.tensor_reduce(out=SM[:rows, 0:1], in_=A, axis=AX.X, op=ALU.add))
            S.op("act", [AR], [self.TMPAR, R], lambda e, A=A: e.activation(out=self.TMPA[:rows, :], in_=A, func=AF.Square, accum_out=SM[:rows, 1:2]))
            S.op("dve", [R], [R], lambda e: e.tensor_scalar(out=SM[:rows, 2:3], in0=SM[:rows, 0:1], scalar1=1.0 / D, scalar2=None, op0=ALU.mult))
            S.op("dve", [R], [R], lambda e: e.tensor_tensor(out=SM[:rows, 3:4], in0=SM[:rows, 2:3], in1=SM[:rows, 2:3], op=ALU.mult))
            S.op("dve", [R], [R], lambda e: e.scalar_tensor_tensor(out=SM[:rows, 4:5], in0=SM[:rows, 1:2], scalar=1.0 / D, in1=SM[:rows, 3:4], op0=ALU.mult, op1=ALU.subtract))
            S.op("act", [R], [R], lambda e: e.activation(out=SM[:rows, 5:6], in_=SM[:rows, 4:5], func=AF.Sqrt, bias=self.EPSC[:rows, :], scale=1.0))
            S.op("dve", [R], [R], lambda e: e.reciprocal(out=SM[:rows, 5:6], in_=SM[:rows, 5:6]))
            S.op("dve", [R], [R], lambda e: e.scalar_tensor_tensor(out=SM[:rows, 6:7], in0=SM[:rows, 2:3], scalar=-1.0, in1=SM[:rows, 5:6], op0=ALU.mult, op1=ALU.mult))
            S.op("act", [AR, R], [AR], lambda e, A=A: e.activation(out=A, in_=A, func=AF.Identity, scale=SM[:rows, 5:6], bias=SM[:rows, 6:7]))
            S.op("dve", [AR, self.ROWAR], [AR], lambda e, A=A: e.tensor_tensor(out=A, in0=A, in1=self.ROWA[:rows, :], op=ALU.mult))
            S.op("dve", [AR, self.ROWBR], [AR], lambda e, A=A: e.tensor_tensor(out=A, in0=A, in1=self.ROWB[:rows, :], op=ALU.add))

    def ffn(self, w_up, w_dn, dff, ex):
        S, u = self.S, self.u
        rows, ntl, ncols = u["rows"], u["ntl"], u["ncols"]
        nch = dff // 128
        G = 4
        HG = self.HT[:, :, :].rearrange("p (b j) c -> p b j c", b=2)
        nblk = (ncols + 511) // 512
        fold = (u["kind"] == "p")
        for g0 in range(0, nch, G):
            gn = min(G, nch - g0)
            hb = self.hgi = (getattr(self, "hgi", -1) + 1) % 2
            hres = [self.HTR[2 * hb], self.HTR[2 * hb + 1]]
            ups, wds = [], []
            for jj in range(0, gn, 2):
                nj = min(2, gn - jj)
                c0 = (g0 + jj) * 128
                wa, war = self.load_w(w_up[:, c0:c0 + nj * 128], 8, nj * 128)
                wg, wgr = self.load_w(w_up[:, dff + c0:dff + c0 + nj * 128], 8, nj * 128)
                ups.append((wa, war, wg, wgr, nj, jj))
            for jj in range(0, gn, 2):
                nj = min(2, gn - jj)
                wd, wdr = self.load_w(w_dn, nj, 1024, k0=g0 + jj)
                wds.append((wd, wdr, nj))
            for (wa, war, wg, wgr, nj, jj) in ups:
                for j in range(nj):
                    for b in range(nblk):
                        n = min(512, ncols - b * 512)
                        pa, par = self.ps()
                        pg, pgr = self.ps()

                        def f(e, pa=pa, pg=pg, j=j, b=b, n=n, wa=wa, wg=wg):
                            ins = None
                            for kc in range(8):
                                ins = e.matmul(pa[:, 0:n], wa[:, kc, j * 128:(j + 1) * 128], self.UT[:, kc, b * 512:b * 512 + n], start=(kc == 0), stop=(kc == 7))
                            for kc in range(8):
                                ins = e.matmul(pg[:, 0:n], wg[:, kc, j * 128:(j + 1) * 128], self.UT[:, kc, b * 512:b * 512 + n], start=(kc == 0), stop=(kc == 7))
                            return ins
                        S.op("pe", [war, wgr, self.UTR], [par, pgr], f)
                        S.op("act", [par], [self.TMPCR], lambda e, pa=pa, n=n: e.activation(out=self.TMPC[:, 0:n], in_=pa[:, 0:n], func=AF.Silu))
                        S.op("dve", [pgr, self.TMPCR], hres,
                             lambda e, pg=pg, n=n, hb=hb, jx=jj + j, b=b: e.tensor_tensor(out=HG[:, hb, jx, b * 512:b * 512 + n], in0=pg[:, 0:n], in1=self.TMPC[:, 0:n], op=ALU.mult))
            if fold:
                gbv0, gbr0 = u["gb"][0]
                for (wd, wdr, nj) in wds:
                    S.op("dve", [wdr, gbr0], [wdr],
                         lambda e, wd=wd, nj=nj, gbv0=gbv0: e.tensor_tensor(out=wd, in0=wd, in1=gbv0[:, :].unsqueeze(1).to_broadcast([128, nj, 1024]), op=ALU.mult))
            for k, (sl, ti) in enumerate(u["tiles"]):
                gbv, gbr = u["gb"][sl]
                for cb in range(2):
                    po, por = self.ps()

                    def f2(e, po=po, k=k, cb=cb, hb=hb, wds=wds, gn=gn):
                        ins = None
                        idx = 0
                        for (wd, wdr, nj) in wds:
                            for j in range(nj):
                                ins = e.matmul(po[:rows, :], HG[:, hb, idx, k * rows:(k + 1) * rows], wd[:, j, cb * 512:(cb + 1) * 512], start=(idx == 0), stop=(idx == gn - 1))
                                idx += 1
                        return ins
                    S.op("pe", hres + [w[1] for w in wds], [por], f2)
                    self.accumulate(k, cb, po, por, gbv, gbr, ex, folded=fold)

    def accumulate(self, k, cb, po, por, gbv, gbr, ex, folded=False):
        S, u = self.S, self.u
        rows = u["rows"]
        A = self.ACC[:rows, k, cb * 512:(cb + 1) * 512]
        T_ = self.TMPA[:rows, 0:512]
        COMB = getattr(self, "COMB", None)
        if folded:
            if ex is None:
                S.op("dve", [por, self.ACCR[k]], [self.ACCR[k]], lambda e: e.tensor_tensor(out=A, in0=po[:rows, :], in1=A, op=ALU.add))
            else:
                S.op("dve", [por, self.COMBR, self.ACCR[k]], [self.ACCR[k]],
                     lambda e: e.scalar_tensor_tensor(out=A, in0=po[:rows, :], scalar=COMB[:rows, k, ex:ex + 1], in1=A, op0=ALU.mult, op1=ALU.add))
            return
        if ex is None:
            S.op("dve", [por, gbr], [self.TMPAR], lambda e: e.tensor_tensor(out=T_, in0=po[:rows, :], in1=gbv[:rows, cb * 512:(cb + 1) * 512], op=ALU.mult))
        else:
            S.op("dve", [por, gbr, self.COMBR], [self.TMPAR],
                 lambda e: e.scalar_tensor_tensor(out=T_, in0=po[:rows, :], scalar=COMB[:rows, k, ex:ex + 1], in1=gbv[:rows, cb * 512:(cb + 1) * 512],
                                                  op0=ALU.mult, op1=ALU.mult))
        S.op("dve", [self.TMPAR, self.ACCR[k]], [self.ACCR[k]], lambda e: e.tensor_tensor(out=A, in0=A, in1=T_, op=ALU.add))

    def merge_out(self, l):
        S, u, I = self.S, self.u, self.I
        rows, ntl, ncols = u["rows"], u["ntl"], u["ncols"]
        nblk = (ncols + 511) // 512
        S.barrier()
        NCOL = self.NCOL
        TACC = self.SCR[:, 0:NCOL]
        tar = Res("tacc")
        SG = self.SCR[:, NCOL:NCOL + NCOL // 2].bitcast(BF16)
        sgr = Res("sg")
        MC = self.SCR[:, NCOL + NCOL // 2:NCOL + NCOL // 2 + NCOL].bitcast(BF16).rearrange("p (b c) -> p b c", b=2)
        mcr = [Res("mc0"), Res("mc1")]
        for c in range(8):
            for n in range(4):
                wgv, wgr = self.load_w(I["w_in"][l][:, OFF_GATE + n * D + c * 128:OFF_GATE + n * D + (c + 1) * 128], 8, 128)
                wbv, wbr = self.load_w(I["w_branch"][l, n][:, c * 128:(c + 1) * 128], 2, 128)
                for b in range(nblk):
                    nn = min(512, ncols - b * 512)
                    pg, pgr = self.ps()
                    pb, pbr = self.ps()

                    def f(e, pg=pg, pb=pb, b=b, nn=nn, wgv=wgv, wbv=wbv, n=n):
                        ins = None
                        for kc in range(8):
                            ins = e.matmul(pg[:, 0:nn], wgv[:, kc, :], self.UT[:, kc, b * 512:b * 512 + nn], start=(kc == 0), stop=(kc == 7))
                        for kc in range(2):
                            ins = e.matmul(pb[:, 0:nn], wbv[:, kc, :], self.HT[:, n * 2 + kc, b * 512:b * 512 + nn], start=(kc == 0), stop=(kc == 1))
                        return ins
                    S.op("pe", [wgr, wbr, self.UTR, self.HTR[n]], [pgr, pbr], f)
                    S.op("act", [pgr], [sgr], lambda e, pg=pg, b=b, nn=nn: e.activation(out=SG[:, b * 512:b * 512 + nn], in_=pg[:, 0:nn], func=AF.Sigmoid))
                    sl_ = slice(b * 512, b * 512 + nn)
                    if n == 0:
                        S.op("dve", [pbr, sgr], [tar], lambda e, pb=pb, sl_=sl_, nn=nn: e.tensor_tensor(out=TACC[:, sl_], in0=pb[:, 0:nn], in1=SG[:, sl_], op=ALU.mult))
                    else:
                        S.op("dve", [pbr, sgr], [self.TMPCR], lambda e, pb=pb, sl_=sl_, nn=nn: e.tensor_tensor(out=self.TMPC[:, 0:nn], in0=pb[:, 0:nn], in1=SG[:, sl_], op=ALU.mult))
                        if n < 3:
                            S.op("dve", [self.TMPCR, tar], [tar], lambda e, sl_=sl_, nn=nn: e.tensor_tensor(out=TACC[:, sl_], in0=TACC[:, sl_], in1=self.TMPC[:, 0:nn], op=ALU.add))
                        else:
                            S.op("dve", [self.TMPCR, tar], [mcr[c % 2]],
                                 lambda e, sl_=sl_, nn=nn, c=c: e.tensor_tensor(out=MC[:, c % 2, sl_], in0=TACC[:, sl_], in1=self.TMPC[:, 0:nn], op=ALU.add))
            wo0, wo0r = self.load_w(I["w_out"][l][:, 0:512], 1, 512, k0=c)
            wo1, wo1r = self.load_w(I["w_out"][l][:, 512:1024], 1, 512, k0=c)
            fold = (u["kind"] == "p")
            if fold:
                gbv0, gbr0 = u["gb"][0]
                for cb_, (wo_, wor_) in enumerate(((wo0, wo0r), (wo1, wo1r))):
                    S.op("dve", [wor_, gbr0], [wor_],
                         lambda e, wo_=wo_, cb_=cb_, gbv0=gbv0: e.tensor_tensor(out=wo_[:, 0, :], in0=wo_[:, 0, :], in1=gbv0[:, cb_ * 512:(cb_ + 1) * 512], op=ALU.mult))
            for k, (sl, ti) in enumerate(u["tiles"]):
                gbv, gbr = u["gb"][sl]
                for cb, (wo, wor) in enumerate(((wo0, wo0r), (wo1, wo1r))):
                    po, por = self.ps()
                    S.op("pe", [mcr[c % 2], wor], [por],
                         lambda e, po=po, k=k, wo=wo, c=c: e.matmul(po[:rows, :], MC[:, c % 2, k * rows:(k + 1) * rows], wo[:, 0, :], start=True, stop=True))
                    self.accumulate(k, cb, po, por, gbv, gbr, None, folded=fold)
        S.barrier()

    def proj_tok(self, k0col, nrows, panels):
        S = self.S
        banks = []
        for pi in range(0, len(panels), 2):
            pt, pr = self.ps()
            grp = panels[pi:pi + 2]

            def f(e, pt=pt, grp=grp):
                ins = None
                for gi, (wv, wr, n) in enumerate(grp):
                    for kc in range(8):
                        ins = e.matmul(pt[:nrows, gi * 256:gi * 256 + n], self.UT[:, kc, k0col:k0col + nrows], wv[:, kc, :], start=(kc == 0), stop=(kc == 7))
                return ins
            S.op("pe", [self.UTR] + [g[1] for g in grp], [pr], f)
            banks.append((pt, pr))
        return banks

    def load_panels(self, l, offs):
        I = self.I
        out = []
        for off, n in offs:
            wv, wr = self.load_w(I["w_in"][l][:, off:off + n], 8, n)
            out.append((wv, wr, n))
        return out

    def to_ht(self, mixer, HB, hbr, nrows, col0):
        S = self.S
        pt, pr = self.ps()
        pv = pt[:, :].bitcast(BF16)

        def f(e):
            ins = None
            for g in range(2):
                ins = e.transpose(out=pv[:, g * 128:g * 128 + nrows], in_=HB[:nrows, g * 128:(g + 1) * 128], identity=self.IDB[:nrows, :nrows])
            return ins
        S.op("pe", [hbr, self.CONSTR], [pr], f)
        S.op("act", [pr], [self.HTR[mixer]],
             lambda e: e.copy(out=self.HT[:, mixer * 2:mixer * 2 + 2, col0:col0 + nrows], in_=pv[:, 0:256].rearrange("p (g c) -> p g c", g=2)[:, :, 0:nrows]))

    def mixers(self, l):
        self.mix_ret(l)
        self.stage_end("ret")
        self.mix_fox(l)
        self.stage_end("fox")
        self.mix_conv(l)
        self.stage_end("conv")
        self.mix_diff(l)
        self.stage_end("diff")

    def mix_ret(self, l):
        S, u, I, O, cfg = self.S, self.u, self.I, self.O, self.cfg
        kind = u["kind"]
        pan = self.load_panels(l, [(OFF_RET + i * 256, 256) for i in range(4)])
        rk = 0 if kind == "p" else 1
        RD = self.RDEC
        S32 = self.SCR[0:64, 0:256]
        s32r = Res("s32")
        SBF = self.SCR[0:64, 256:384].bitcast(BF16)
        sbfr = Res("sbf")
        S.barrier()
        if kind == "p":
            seqlist = [(0, [(i, 64) for i in range(cfg.T // 64)])]
        else:
            seqlist = [(j, [(0, cfg.TS)]) for j in range(cfg.NS)]
        for sl, chunks in seqlist:
            seqg = u["seqs"][sl]
            if kind == "p":
                S.op("dve", [], [s32r], lambda e: e.memset(S32, 0.0))
            else:
                S.dma("sp", S32.rearrange("d (h e) -> d h e", h=4), I["sret"][l, sl].rearrange("h d e -> d h e"), [], [s32r], s32r)
            S.op("act", [s32r], [sbfr], lambda e: e.copy(out=SBF, in_=S32))
            for ci, cl in chunks:
                col0 = (ci * 64) if kind == "p" else sl * cfg.TS
                tab = ci if kind == "p" else cfg.T // 64
                (pA, pAr), (pB, pBr) = self.proj_tok(col0, cl, pan)
                RT = self.TMPA[0:64, 0:512].rearrange("p (a c) -> p a c", a=2)
                S.dma("sp", RT, I["ret_rope"][tab], [], [self.TMPAR], self.TMPAR)
                QK = self.TMPB[0:64, 0:512]
                A3 = pA[:cl, :].rearrange("p (h x) -> p h x", h=8)
                C3 = RT[:cl, 0, :].rearrange("p (h x) -> p h x", h=8)
                S3 = RT[:cl, 1, :].rearrange("p (h x) -> p h x", h=8)
                Q3 = QK[:cl, :].rearrange("p (h x) -> p h x", h=8)
                T1 = self.TMPC[:cl, 0:256].rearrange("p (h x) -> p h x", h=8)
                T2 = self.TMPC[:cl, 256:512].rearrange("p (h x) -> p h x", h=8)
                rd1 = [pAr, self.TMPAR]
                S.op("dve", rd1, [self.TMPCR], lambda e, A3=A3, C3=C3, T1=T1: e.tensor_tensor(out=T1, in0=A3[:, :, 0:32], in1=C3, op=ALU.mult))
                S.op("dve", rd1, [self.TMPCR], lambda e, A3=A3, S3=S3, T2=T2: e.tensor_tensor(out=T2, in0=A3[:, :, 32:64], in1=S3, op=ALU.mult))
                S.op("dve", [self.TMPCR], [self.TMPBR], lambda e, Q3=Q3, T1=T1, T2=T2: e.tensor_tensor(out=Q3[:, :, 0:32], in0=T1, in1=T2, op=ALU.subtract))
                S.op("dve", rd1, [self.TMPCR], lambda e, A3=A3, C3=C3, T1=T1: e.tensor_tensor(out=T1, in0=A3[:, :, 32:64], in1=C3, op=ALU.mult))
                S.op("dve", rd1, [self.TMPCR], lambda e, A3=A3, S3=S3, T2=T2: e.tensor_tensor(out=T2, in0=A3[:, :, 0:32], in1=S3, op=ALU.mult))
                S.op("dve", [self.TMPCR], [self.TMPBR], lambda e, Q3=Q3, T1=T1, T2=T2: e.tensor_tensor(out=Q3[:, :, 32:64], in0=T1, in1=T2, op=ALU.add))
                VB = self.TMPB[0:64, 512:768]
                KD = self.TMPB[0:64, 768:1024]
                SGt = self.TMPA[0:64, 512:768]
                S.op("act", [pBr], [self.TMPBR], lambda e, pB=pB, cl=cl: e.copy(out=VB[:cl, :], in_=pB[:cl, 0:256]))
                S.op("act", [pBr], [self.TMPAR], lambda e, pB=pB, cl=cl: e.activation(out=SGt[:cl, :], in_=pB[:cl, 256:512], func=AF.Silu))
                S.op("dve", [self.TMPBR, self.CONSTR], [self.TMPBR], lambda e, cl=cl: e.tensor_tensor(out=KD[:cl, :].rearrange("p (h x) -> p h x", h=4), in0=QK[:cl, 256:512].rearrange("p (h x) -> p h x", h=4),
                                                                                                        in1=self.RDECS[:cl, rk, 0, :].unsqueeze(2).to_broadcast([cl, 4, 64]), op=ALU.mult))
                pT, pTr = self.ps()
                pTv = pT[:, :].bitcast(BF16)

                def ft(e, pTv=pTv, cl=cl):
                    ins = None
                    for j in range(8):
                        ins = e.transpose(out=pTv[0:64, j * 64:j * 64 + cl], in_=QK[:cl, j * 64:(j + 1) * 64], identity=self.IDB[:cl, :cl])
                    return ins
                S.op("pe", [self.TMPBR, self.CONSTR], [pTr], ft)
                QKT = self.SCR[0:64, 384:640].bitcast(BF16).rearrange("p (j c) -> p j c", j=8)
                qktr = getattr(self, "_qktr", None) or Res("qkt")
                self._qktr = qktr
                QDT = self.SCR[0:64, 640:768].bitcast(BF16).rearrange("p (j c) -> p j c", j=4)
                S.op("act", [pTr], [qktr], lambda e, pTv=pTv, cl=cl: e.copy(out=QKT[:, :, 0:cl], in_=pTv[0:64, 0:512].rearrange("p (j c) -> p j c", j=8)[:, :, 0:cl]))
                S.op("dve", [qktr, self.CONSTR], [qktr],
                     lambda e, cl=cl: e.tensor_tensor(out=QDT[:, :, 0:cl], in0=QKT[:, 0:4, 0:cl], in1=RD[:, rk, 1, :].rearrange("p (h c) -> p h c", h=4)[:, :, 0:cl], op=ALU.mult))
                pS, pSr = self.ps()

                def fs(e, pS=pS, cl=cl):
                    ins = None
                    for h in range(4):
                        ins = e.matmul(pS[:cl, h * 64:h * 64 + cl], QKT[:, 4 + h, 0:cl], QKT[:, h, 0:cl], start=True, stop=True)
                    return ins
                S.op("pe", [qktr], [pSr], fs)
                STB = self.SCR[0:64, 768:896].bitcast(BF16).rearrange("p (h c) -> p h c", h=4)
                stbr = getattr(self, "_stbr", None) or Res("stb")
                self._stbr = stbr
                S.op("dve", [pSr, self.CONSTR], [stbr],
                     lambda e, pS=pS, cl=cl: e.tensor_tensor(out=STB[:cl, :, 0:cl], in0=pS[:cl, 0:256].rearrange("p (h c) -> p h c", h=4)[:, :, 0:cl],
                                                             in1=RD[:cl, rk, 0, :].rearrange("p (h c) -> p h c", h=4)[:, :, 0:cl], op=ALU.mult))
                pK, pKr = self.ps()

                def fk(e, pK=pK, cl=cl):
                    ins = None
                    for h in range(4):
                        ins = e.matmul(pK[0:64, h * 64:(h + 1) * 64], KD[:cl, h * 64:(h + 1) * 64], VB[:cl, h * 64:(h + 1) * 64], start=True, stop=True)
                    return ins
                S.op("pe", [self.TMPBR], [pKr], fk)
                pY, pYr = self.ps()

                def fy(e, pY=pY, cl=cl):
                    ins = None
                    for h in range(4):
                        e.matmul(pY[:cl, h * 64:(h + 1) * 64], STB[:cl, h, 0:cl], VB[:cl, h * 64:(h + 1) * 64], start=True, stop=False)
                        ins = e.matmul(pY[:cl, h * 64:(h + 1) * 64], QDT[:, h, 0:cl], SBF[:, h * 64:(h + 1) * 64], start=False, stop=True)
                    return ins
                S.op("pe", [stbr, self.TMPBR, qktr, sbfr], [pYr], fy)
                S.op("dve", [s32r, self.CONSTR], [s32r], lambda e: e.tensor_tensor(out=S32.rearrange("p (h x) -> p h x", h=4), in0=S32.rearrange("p (h x) -> p h x", h=4),
                                                                                      in1=self.RDECS[:, rk, 1, :].unsqueeze(2).to_broadcast([64, 4, 64]), op=ALU.mult))
                S.op("dve", [s32r, pKr], [s32r], lambda e, pK=pK: e.tensor_tensor(out=S32, in0=S32, in1=pK[0:64, 0:256], op=ALU.add))
                S.op("act", [s32r], [sbfr], lambda e: e.copy(out=SBF, in_=S32))
                Y32 = self.TMPA[0:64, 768:1024]
                S.op("act", [pYr], [self.TMPAR], lambda e, pY=pY, cl=cl: e.copy(out=Y32[:cl, :], in_=pY[:cl, 0:256]))
                HB = self.TMPB[0:64, 0:256]
                self.group_norm(Y32, self.TMPAR, cl, 4, 64, True, SGt, HB, self.TMPBR, None, 1.0)
                self.to_ht(0, HB, self.TMPBR, cl, col0)
            dst = O[("p" if kind == "p" else "s") + "_ret"][l, u["ui"] if kind == "p" else sl].rearrange("h d e -> d h e")
            S.dma("sp", dst, S32.rearrange("d (h e) -> d h e", h=4), [s32r], [], s32r, is_out=True)

    def group_norm(self, Y, yr, nrows, G, Dg, center, MUL, OUT, outr, gain_bc, const):
        S = self.S
        SM, R = self.SM, self.SMR
        Y3 = Y[:nrows, 0:G * Dg].rearrange("p (g d) -> p g d", g=G)
        SQ = self.TMPC[:nrows, 0:G * Dg]
        SQ3 = SQ.rearrange("p (g d) -> p g d", g=G)
        s1, s2, mean, var, rstd = (SM[:nrows, 40:40 + G], SM[:nrows, 44:44 + G], SM[:nrows, 48:48 + G], SM[:nrows, 52:52 + G], SM[:nrows, 56:56 + G])
        S.op("dve", [yr], [self.TMPCR], lambda e: e.tensor_tensor(out=SQ, in0=Y[:nrows, 0:G * Dg], in1=Y[:nrows, 0:G * Dg], op=ALU.mult))
        S.op("dve", [self.TMPCR], [R], lambda e: e.tensor_reduce(out=s2, in_=SQ3, axis=AX.X, op=ALU.add))
        if center:
            S.op("dve", [yr], [R], lambda e: e.tensor_reduce(out=s1, in_=Y3, axis=AX.X, op=ALU.add))
            S.op("dve", [R], [R], lambda e: e.tensor_scalar(out=mean, in0=s1, scalar1=1.0 / Dg, scalar2=None, op0=ALU.mult))
            S.op("dve", [R], [R], lambda e: e.tensor_tensor(out=s1, in0=mean, in1=mean, op=ALU.mult))
            S.op("dve", [R], [R], lambda e: e.scalar_tensor_tensor(out=var, in0=s2, scalar=1.0 / Dg, in1=s1, op0=ALU.mult, op1=ALU.subtract))
        else:
            S.op("dve", [R], [R], lambda e: e.tensor_scalar(out=var, in0=s2, scalar1=1.0 / Dg, scalar2=None, op0=ALU.mult))
        S.op("act", [R], [R], lambda e: e.activation(out=rstd, in_=var, func=AF.Sqrt, bias=self.EPSC[:nrows, :], scale=1.0))
        S.op("dve", [R], [R], lambda e: e.reciprocal(out=rstd, in_=rstd))
        if center:
            S.op("dve", [yr, R], [self.TMPCR], lambda e: e.tensor_tensor(out=SQ3, in0=Y3, in1=mean.unsqueeze(2).to_broadcast([nrows, G, Dg]), op=ALU.subtract))
            src, srcr = SQ3, self.TMPCR
        else:
            src, srcr = Y3, yr
        S.op("dve", [srcr, R], [self.TMPCR], lambda e: e.tensor_tensor(out=SQ3, in0=src, in1=rstd.unsqueeze(2).to_broadcast([nrows, G, Dg]), op=ALU.mult))
        O3 = OUT[:nrows, 0:G * Dg].rearrange("p (g d) -> p g d", g=G)
        if gain_bc is not None:
            S.op("dve", [self.TMPCR, self.CONSTR], [self.TMPCR], lambda e: e.tensor_tensor(out=SQ3, in0=SQ3, in1=gain_bc, op=ALU.mult))
        if MUL is not None:
            S.op("dve", [self.TMPCR, yr], [outr], lambda e: e.tensor_tensor(out=OUT[:nrows, 0:G * Dg], in0=SQ, in1=MUL[:nrows, 0:G * Dg], op=ALU.mult))
        else:
            S.op("dve", [self.TMPCR], [outr], lambda e: e.tensor_scalar(out=OUT[:nrows, 0:G * Dg], in0=SQ, scalar1=const, scalar2=None, op0=ALU.mult))

    def attn_buffers(self):
        NK = self.NKT * 128
        KT = self.SCR[:, 0:NK].bitcast(BF16).rearrange("p (g c) -> p g c", g=2)
        VA = self.SCR[:, NK:NK + self.NKT * 130].bitcast(BF16).rearrange("p (t h x) -> p t h x", t=self.NKT, h=4)
        base = NK + self.NKT * 130
        FC = self.SCR[:, base:base + self.NKT * 4].rearrange("p (t h) -> p t h", h=4)
        FT = self.SCR[:, base + self.NKT * 4:base + self.NKT * 8].rearrange("p (t h) -> p t h", h=4)
        LF = self.SCR[:, base + self.NKT * 8:base + self.NKT * 12].rearrange("p (t h) -> p t h", h=4)
        self.BI = self.SCR[:, base + self.NKT * 12:base + self.NKT * 16].rearrange("p (t h) -> p t h", h=4)
        assert base + self.NKT * 16 <= 5120, base + self.NKT * 16
        return KT, VA, FC, FT, LF

    def seq_iter(self):
        u, cfg = self.u, self.cfg
        if u["kind"] == "p":
            return [(0, u["ui"], [(i, i * 128, 128) for i in range(self.NT)], 0)]
        return [(j, j, [(j, 0, cfg.TS)], cfg.PAST // 128) for j in range(cfg.NS)]

    def k_transpose(self, KTOK, ktokr, nrows, KT, ktr, kcol0):
        S = self.S
        pt, pr = self.ps()
        pv = pt[:, :].bitcast(BF16)

        def f(e):
            ins = None
            for g in range(2):
                ins = e.transpose(out=pv[:, g * 128:g * 128 + nrows], in_=KTOK[:nrows, g * 128:(g + 1) * 128], identity=self.IDB[:nrows, :nrows])
            return ins
        S.op("pe", [ktokr, self.CONSTR], [pr], f)
        S.op("act", [pr], [ktr], lambda e: e.copy(out=KT[:, :, kcol0:kcol0 + nrows], in_=pv[:, 0:256].rearrange("p (g c) -> p g c", g=2)[:, :, 0:nrows]))

    def mix_fox(self, l):
        S, u, I, O, cfg = self.S, self.u, self.I, self.O, self.cfg
        kind = u["kind"]
        pre = "p" if kind == "p" else "s"
        pan = self.load_panels(l, [(OFF_FOX, 256), (OFF_FOX + 256, 256), (OFF_FOX + 512, 256), (OFF_FOX_F, 4)])
        S.barrier()
        KT, VA, FC, FT, LF = self.attn_buffers()
        ktr, var, fr = Res("kt"), Res("va"), Res("f")
        SM = self.SM
        for sl, so, newtiles, npast in self.seq_iter():
            S.op("dve", [], [var], lambda e: e.memset(VA[:, :, :, 64:65], 1.0))
            if npast:
                S.dma("sp", LF[:, 0:npast, :], I["cfl"][l, sl].rearrange("(t p) h -> p t h", p=128), [], [fr], fr)
            for kt in range(npast):
                KTOK = self.TMPB[:, 0:256]
                S.dma("pool", KTOK, I["cfk"][l, sl, kt * 128:(kt + 1) * 128, :], [], [self.TMPBR], self.TMPBR)
                self.k_transpose(KTOK, self.TMPBR, 128, KT, ktr, kt * 128)
                S.dma("pool", VA[:, kt, :, 0:64], I["cfv"][l, sl, kt * 128:(kt + 1) * 128, :].rearrange("t (h x) -> t h x", h=4), [], [var], var)
                self.fcum_step(kt, 128, FC, FT, LF, fr)
            for qi, (k, t0, nr) in enumerate(newtiles):
                kt = npast + qi
                col0 = k * u["rows"]
                (pA, pAr), (pB, pBr) = self.proj_tok(col0, nr, pan)
                st, str_ = self.stg()
                S.op("act", [pAr], [str_], lambda e, st=st, pA=pA, nr=nr: e.copy(out=st[:nr, 0:256], in_=pA[:nr, 256:512]))
                S.op("act", [pBr], [str_], lambda e, st=st, pB=pB, nr=nr: e.copy(out=st[:nr, 256:512], in_=pB[:nr, 0:256]))
                S.dma("sp", O[pre + "_fox_k"][l, so, t0:t0 + nr, :], st[:nr, 0:256], [str_], [], str_, is_out=True)
                S.dma("sp", O[pre + "_fox_v"][l, so, t0:t0 + nr, :], st[:nr, 256:512], [str_], [], str_, is_out=True)
                self.stage_end("fox_out")
                QKB = self.TMPB[:, 0:512]
                S.op("dve", [pAr], [self.TMPBR], lambda e, pA=pA, nr=nr: e.tensor_scalar(out=QKB[:nr, :], in0=pA[:nr, :], scalar1=1.0, scalar2=None, op0=ALU.mult))
                self.stage_end("fox_q1")
                S.op("dve", [pBr], [var], lambda e, pB=pB, nr=nr, kt=kt: e.tensor_copy(out=VA[:nr, kt, :, 0:64], in_=pB[:nr, 0:256].rearrange("p (h x) -> p h x", h=4)))
                self.stage_end("fox_q2")
                self.k_transpose(QKB[:, 256:512], self.TMPBR, nr, KT, ktr, kt * 128)
                self.stage_end("fox_q3")
                QT = self.TMPB[:, 512:768].rearrange("p (g c) -> p g c", g=2)
                qtr = self.TMPBR
                pt, pr = self.ps()
                pv = pt[:, :].bitcast(BF16)

                def fq(e, pv=pv, nr=nr):
                    ins = None
                    for g in range(2):
                        ins = e.transpose(out=pv[:, g * 128:g * 128 + nr], in_=QKB[:nr, g * 128:(g + 1) * 128], identity=self.IDB[:nr, :nr])
                    return ins
                S.op("pe", [self.TMPBR, self.CONSTR], [pr], fq)
                S.op("act", [pr], [self.TMPBR], lambda e, pv=pv, nr=nr: e.copy(out=QT[:, :, 0:nr], in_=pv[:, 0:256].rearrange("p (g c) -> p g c", g=2)[:, :, 0:nr]))
                self.stage_end("fox_qk")
                S.op("dve", [pBr, self.CONSTR], [self.SMR], lambda e, pB=pB, nr=nr: e.tensor_tensor(out=SM[:nr, 0:4], in0=pB[:nr, 256:260], in1=self.BFF[:nr, l, :], op=ALU.add))
                S.op("act", [self.SMR], [self.SMR], lambda e, nr=nr: e.activation(out=SM[:nr, 4:8], in_=SM[:nr, 0:4], func=AF.Exp, scale=-1.0))
                S.op("act", [self.SMR], [self.SMR], lambda e, nr=nr: e.activation(out=SM[:nr, 8:12], in_=SM[:nr, 4:8], func=AF.Ln, bias=self.ONEC[:nr, :], scale=1.0))
                S.op("dve", [self.SMR], [fr], lambda e, nr=nr, kt=kt: e.tensor_scalar(out=LF[:nr, kt, :], in0=SM[:nr, 8:12], scalar1=-1.0, scalar2=None, op0=ALU.mult))
                S.dma("sp", O[pre + "_fox_logf"][l, so, t0:t0 + nr, :], LF[:nr, kt, :], [fr], [], fr, is_out=True)
                self.stage_end("fox_lf")
                self.fcum_step(kt, nr, FC, FT, LF, fr)
                self.stage_end("fox_fcum")
                HB = self.TMPB[:, 768:1024]
                hbr = getattr(self, "_hbr", None) or Res("hb")
                self._hbr = hbr
                BI = self.BI
                bir = getattr(self, "_bir", None) or Res("bi")
                self._bir = bir
                S.op("dve", [fr], [bir], lambda e, kt=kt: e.tensor_tensor(out=BI[:, 0:kt + 1, :], in0=FT[:, kt:kt + 1, :].to_broadcast([128, kt + 1, 4]),
                                                                        in1=FC[:, 0:kt + 1, :], op=ALU.subtract))
                items = [(h, kk) for h in range(4) for kk in range(kt + 1)]
                pSs, pYs = {}, {}

                def stageA(it, kt=kt, nr=nr):
                    h, kk = it
                    hp, g = (h % 2) * 64, h // 2
                    nk = 128 if kk < kt else nr
                    pS, pSr = self.ps()
                    pSs[it] = (pS, pSr)
                    S.op("pe", [ktr, self.TMPBR], [pSr],
                         lambda e, pS=pS, nk=nk, kk=kk, hp=hp, g=g, nr=nr: e.matmul(pS[:nk, 0:nr], KT[hp:hp + 64, g, kk * 128:kk * 128 + nk], QT[hp:hp + 64, g, 0:nr], start=True, stop=True))

                def stageB(it, kt=kt, nr=nr):
                    h, kk = it
                    nk = 128 if kk < kt else nr
                    pS, pSr = pSs.pop(it)
                    if kk == 0:
                        pYs[h] = self.ps(hold=True)
                    pY, pYr = pYs[h]
                    PT, ptr = self.pt_next()
                    S.op("act", [pSr, bir], [ptr], lambda e, pS=pS, nk=nk, nr=nr, PT=PT, kk=kk, h=h: e.activation(out=PT[:nk, 0:nr], in_=pS[:nk, 0:nr], func=AF.Exp, bias=BI[:nk, kk, h:h + 1], scale=0.125))
                    if kk == kt:
                        S.op("dve", [ptr, self.CONSTR], [ptr], lambda e, nk=nk, nr=nr, PT=PT: e.tensor_tensor(out=PT[:nk, 0:nr], in0=PT[:nk, 0:nr], in1=self.CMASK[:nk, 0:nr], op=ALU.mult))
                    S.op("pe", [ptr, var], [pYr],
                         lambda e, pY=pY, nk=nk, nr=nr, kk=kk, h=h, kt=kt, PT=PT: e.matmul(pY[:nr, 0:65], PT[:nk, 0:nr], VA[:nk, kk, h, :], start=(kk == 0), stop=(kk == kt)))
                    if kk == kt:
                        S.op("dve", [pYr], [self.SMR], lambda e, pY=pY, nr=nr, h=h: e.reciprocal(out=SM[:nr, 20 + h:21 + h], in_=pY[:nr, 64:65]))
                        S.op("act", [pYr, self.SMR], [hbr], lambda e, pY=pY, nr=nr, h=h: e.activation(out=HB[:nr, h * 64:(h + 1) * 64], in_=pY[:nr, 0:64], func=AF.Copy, scale=SM[:nr, 20 + h:21 + h]))
                        self.ps_release(pYr)

                LOOK = 3
                for it in items[:LOOK]:
                    stageA(it)
                for i, it in enumerate(items):
                    if i + LOOK < len(items):
                        stageA(items[i + LOOK])
                    stageB(it)
                self.to_ht(1, HB, hbr, nr, col0)

    def fcum_step(self, kt, nr, FC, FT, LF, fr):
        S = self.S
        pt, pr = self.ps()

        def f(e):
            e.matmul(pt[:, 0:4], self.ONESF[:nr, :], LF[:nr, kt, :], start=True, stop=True)
            return e.matmul(pt[:nr, 4:8], self.UTRI[:nr, :nr], LF[:nr, kt, :], start=True, stop=True)
        S.op("pe", [fr, self.CONSTR], [pr], f)
        if kt == 0:
            S.op("dve", [pr], [fr], lambda e: e.tensor_copy(out=FT[:, kt, :], in_=pt[:, 0:4]))
            S.op("dve", [pr], [fr], lambda e: e.tensor_copy(out=FC[:nr, kt, :], in_=pt[:nr, 4:8]))
        else:
            S.op("dve", [pr, fr], [fr], lambda e: e.tensor_tensor(out=FT[:, kt, :], in0=pt[:, 0:4], in1=FT[:, kt - 1, :], op=ALU.add))
            S.op("dve", [pr, fr], [fr], lambda e: e.tensor_tensor(out=FC[:nr, kt, :], in0=pt[:nr, 4:8], in1=FT[:nr, kt - 1, :], op=ALU.add))

    def mix_conv(self, l):
        S, u, I, O, cfg = self.S, self.u, self.I, self.O, self.cfg
        kind = u["kind"]
        pre = "p" if kind == "p" else "s"
        pan = self.load_panels(l, [(OFF_CONV, 256), (OFF_CONV + 256, 256)])
        S.barrier()
        XP = self.SCR[:, 0:2 * 158].rearrange("p (g c) -> p g c", g=2)
        xpr = Res("xp")
        CO = self.SCR[:, 320:320 + 256].rearrange("p (g c) -> p g c", g=2)
        cor = Res("co")
        CS = self.SCR[:, 576:576 + 256].rearrange("p (g c) -> p g c", g=2)
        csr = Res("cs")
        ST = self.SCR[:, 832:832 + 512]
        str2 = Res("cst")
        for sl, so, newtiles, npast in self.seq_iter():
            if kind == "p":
                S.op("dve", [], [xpr], lambda e: e.memset(XP[:, :, 0:30], 0.0))
            else:
                for g in range(2):
                    S.dma("sp", XP[:, g, 0:30], I["sconv"][l, sl][:, g * 128:(g + 1) * 128].rearrange("t c -> c t"), [], [xpr], xpr, allow_slow_non_contiguous=True)
                S.dma("sp", O["s_conv"][l, so, 0:30 - cfg.TS, :], I["sconv"][l, sl, cfg.TS:30, :], [], [], Res("d2d"), is_out=True)
            for qi, (k, t0, nr) in enumerate(newtiles):
                col0 = k * u["rows"]
                ((pA, pAr),) = self.proj_tok(col0, nr, pan)
                G32 = self.TMPA[:, 0:256]
                S.op("act", [pAr], [self.TMPAR], lambda e, pA=pA, nr=nr: e.activation(out=G32[:nr, :], in_=pA[:nr, 256:512], func=AF.Sigmoid))
                S.op("dve", [pAr, self.TMPAR], [self.TMPAR], lambda e, pA=pA, nr=nr: e.tensor_tensor(out=G32[:nr, :], in0=pA[:nr, 0:256], in1=G32[:nr, :], op=ALU.mult))
                if kind == "p":
                    lo = max(t0, cfg.T - 30)
                    if t0 + nr > lo:
                        S.dma("sp", O["p_conv"][l, so, lo - (cfg.T - 30):30, :], G32[lo - t0:nr, :], [self.TMPAR], [], self.TMPAR, is_out=True)
                else:
                    S.dma("sp", O["s_conv"][l, so, 30 - cfg.TS:30, :], G32[:nr, :], [self.TMPAR], [], self.TMPAR, is_out=True)
                pt, pr = self.ps()

                def f(e, pt=pt, nr=nr):
                    ins = None
                    for g in range(2):
                        ins = e.transpose(out=pt[:, g * 128:g * 128 + nr], in_=G32[:nr, g * 128:(g + 1) * 128], identity=self.IDF[:nr, :nr])
                    return ins
                S.op("pe", [self.TMPAR, self.CONSTR], [pr], f)
                if qi > 0:
                    pnr = newtiles[qi - 1][2]
                    S.op("dve", [xpr], [self.TMPCR], lambda e, pnr=pnr: e.tensor_copy(out=self.TMPC[:, 0:60].rearrange("p (g c) -> p g c", g=2), in_=XP[:, :, pnr:pnr + 30]))
                    S.op("dve", [self.TMPCR], [xpr], lambda e: e.tensor_copy(out=XP[:, :, 0:30], in_=self.TMPC[:, 0:60].rearrange("p (g c) -> p g c", g=2)))
                S.op("act", [pr], [xpr], lambda e, pt=pt, nr=nr: e.copy(out=XP[:, :, 30:30 + nr], in_=pt[:, 0:256].rearrange("p (g c) -> p g c", g=2)[:, :, 0:nr]))
                for g in range(2):
                    S.op("dve", [xpr, self.CONSTR], [cor],
                         lambda e, g=g, nr=nr: e.tensor_scalar(out=CO[:, g, 0:nr], in0=XP[:, g, 0:nr], scalar1=self.WCV[:, l, g, 0:1], scalar2=self.CVP[:, l, 0, g:g + 1], op0=ALU.mult, op1=ALU.add))
                    for w in range(1, CONV_W):
                        S.op("dve", [xpr, self.CONSTR, cor], [cor],
                             lambda e, g=g, nr=nr, w=w: e.scalar_tensor_tensor(out=CO[:, g, 0:nr], in0=XP[:, g, w:w + nr], scalar=self.WCV[:, l, g, w:w + 1], in1=CO[:, g, 0:nr],
                                                                                op0=ALU.mult, op1=ALU.add))
                S.op("dve", [cor], [csr], lambda e, nr=nr: e.tensor_tensor(out=CS[:, :, 0:nr], in0=CO[:, :, 0:nr], in1=CO[:, :, 0:nr], op=ALU.mult))
                ps1, ps1r = self.ps()

                def fs(e, ps1=ps1, nr=nr):
                    e.matmul(ps1[:, 0:nr], self.ONESF[:, :], CO[:, 0, 0:nr], start=True, stop=False)
                    e.matmul(ps1[:, 0:nr], self.ONESF[:, :], CO[:, 1, 0:nr], start=False, stop=True)
                    e.matmul(ps1[:, 128:128 + nr], self.ONESF[:, :], CS[:, 0, 0:nr], start=True, stop=False)
                    return e.matmul(ps1[:, 128:128 + nr], self.ONESF[:, :], CS[:, 1, 0:nr], start=False, stop=True)
                S.op("pe", [cor, csr, self.CONSTR], [ps1r], fs)
                MEAN, MSQ, VAR, RSTD = ST[:, 0:128], ST[:, 128:256], ST[:, 256:384], ST[:, 384:512]
                S.op("dve", [ps1r], [str2], lambda e, ps1=ps1, nr=nr: e.tensor_scalar(out=MEAN[:, 0:nr], in0=ps1[:, 0:nr], scalar1=1.0 / 256, scalar2=None, op0=ALU.mult))
                S.op("dve", [str2], [str2], lambda e, nr=nr: e.tensor_tensor(out=MSQ[:, 0:nr], in0=MEAN[:, 0:nr], in1=MEAN[:, 0:nr], op=ALU.mult))
                S.op("dve", [ps1r, str2], [str2], lambda e, ps1=ps1, nr=nr: e.scalar_tensor_tensor(out=VAR[:, 0:nr], in0=ps1[:, 128:128 + nr], scalar=1.0 / 256, in1=MSQ[:, 0:nr], op0=ALU.mult, op1=ALU.subtract))
                S.op("act", [str2], [str2], lambda e, nr=nr: e.activation(out=RSTD[:, 0:nr], in_=VAR[:, 0:nr], func=AF.Sqrt, bias=self.EPSC[:, :], scale=1.0))
                S.op("dve", [str2], [str2], lambda e, nr=nr: e.reciprocal(out=RSTD[:, 0:nr], in_=RSTD[:, 0:nr]))
                for g in range(2):
                    S.op("dve", [cor, str2], [csr], lambda e, g=g, nr=nr: e.tensor_tensor(out=CS[:, g, 0:nr], in0=CO[:, g, 0:nr], in1=MEAN[:, 0:nr], op=ALU.subtract))
                    S.op("dve", [csr, str2], [csr], lambda e, g=g, nr=nr: e.tensor_tensor(out=CS[:, g, 0:nr], in0=CS[:, g, 0:nr], in1=RSTD[:, 0:nr], op=ALU.mult))
                    S.op("act", [csr, self.CONSTR], [self.HTR[2]],
                         lambda e, g=g, nr=nr, col0=col0: e.activation(out=self.HT[:, 4 + g, col0:col0 + nr], in_=CS[:, g, 0:nr], func=AF.Silu,
                                                                      scale=self.CVP[:, l, 1, g:g + 1], bias=self.CVP[:, l, 2, g:g + 1]))

    def mix_diff(self, l):
        S, u, I, O, cfg = self.S, self.u, self.I, self.O, self.cfg
        kind = u["kind"]
        pre = "p" if kind == "p" else "s"
        lam_init = 0.8 - 0.6 * math.exp(-0.3 * l)
        pan = self.load_panels(l, [(OFF_DIFF, 256), (OFF_DIFF + 256, 256), (OFF_DIFF + 512, 256)])
        S.barrier()
        KT, VA, FC, FT, LF = self.attn_buffers()
        ktr, var = Res("kt"), Res("va")
        SM = self.SM
        for sl, so, newtiles, npast in self.seq_iter():
            S.op("dve", [], [var], lambda e: e.memset(VA[:, :, :, 64:65], 1.0))
            for kt in range(npast):
                KTOK = self.TMPB[:, 0:256]
                S.dma("pool", KTOK, I["cdk"][l, sl, kt * 128:(kt + 1) * 128, :], [], [self.TMPBR], self.TMPBR)
                self.k_transpose(KTOK, self.TMPBR, 128, KT, ktr, kt * 128)
                S.dma("pool", VA[:, kt, :, 0:64], I["cdv"][l, sl, kt * 128:(kt + 1) * 128, :].rearrange("t (h x) -> t h x", h=4), [], [var], var)
            for qi, (k, t0, nr) in enumerate(newtiles):
                kt = npast + qi
                col0 = k * u["rows"]
                tab = (t0 // 128) if kind == "p" else self.NT
                (pA, pAr), (pB, pBr) = self.proj_tok(col0, nr, pan)
                RT = self.TMPA[:, 0:128].rearrange("p (a c) -> p a c", a=2)
                S.dma("sp", RT, I["diff_rope"][tab], [], [self.TMPAR], self.TMPAR)
                R32 = self.TMPA[:, 128:640]
                A3 = pA[:nr, :].rearrange("p (n x) -> p n x", n=16)
                R3 = R32[:nr, :].rearrange("p (n x) -> p n x", n=16)
                C3 = RT[:nr, 0, :].rearrange("p (n x) -> p n x", n=16)
                S3 = RT[:nr, 1, :].rearrange("p (n x) -> p n x", n=16)
                T1 = self.TMPC[:nr, 0:64].rearrange("p (n x) -> p n x", n=16)
                T2 = self.TMPC[:nr, 64:128].rearrange("p (n x) -> p n x", n=16)
                rd1 = [pAr, self.TMPAR]
                S.op("act", [pAr], [self.TMPAR], lambda e, pA=pA, nr=nr: e.copy(out=R32[:nr, :], in_=pA[:nr, :]))
                S.op("dve", rd1, [self.TMPCR], lambda e, A3=A3, C3=C3, T1=T1: e.tensor_tensor(out=T1, in0=A3[:, :, 0:4], in1=C3, op=ALU.mult))
                S.op("dve", rd1, [self.TMPCR], lambda e, A3=A3, S3=S3, T2=T2: e.tensor_tensor(out=T2, in0=A3[:, :, 4:8], in1=S3, op=ALU.mult))
                S.op("dve", [self.TMPCR, self.TMPAR], [self.TMPAR], lambda e, R3=R3, T1=T1, T2=T2: e.tensor_tensor(out=R3[:, :, 0:4], in0=T1, in1=T2, op=ALU.subtract))
                S.op("dve", rd1, [self.TMPCR], lambda e, A3=A3, C3=C3, T1=T1: e.tensor_tensor(out=T1, in0=A3[:, :, 4:8], in1=C3, op=ALU.mult))
                S.op("dve", rd1, [self.TMPCR], lambda e, A3=A3, S3=S3, T2=T2: e.tensor_tensor(out=T2, in0=A3[:, :, 0:4], in1=S3, op=ALU.mult))
                S.op("dve", [self.TMPCR, self.TMPAR], [self.TMPAR], lambda e, R3=R3, T1=T1, T2=T2: e.tensor_tensor(out=R3[:, :, 4:8], in0=T1, in1=T2, op=ALU.add))
                st, str_ = self.stg()
                S.op("act", [pBr], [str_], lambda e, st=st, pB=pB, nr=nr: e.copy(out=st[:nr, 0:256], in_=pB[:nr, 0:256]))
                S.dma("sp", O[pre + "_diff_k"][l, so, t0:t0 + nr, :], R32[:nr, 256:512], [self.TMPAR], [], self.TMPAR, is_out=True)
                S.dma("sp", O[pre + "_diff_v"][l, so, t0:t0 + nr, :], st[:nr, 0:256], [str_], [], str_, is_out=True)
                QA = self.TMPB[:, 0:256]
                QB = self.TMPB[:, 256:512]
                KB = self.TMPB[:, 512:768]
                S.op("dve", [], [self.TMPBR], lambda e: e.memset(self.TMPB[:, 0:512], 0.0))
                R8 = R32[:nr, 0:256].rearrange("p (n t x) -> p n t x", n=4, t=2)
                S.op("dve", [self.TMPAR], [self.TMPBR], lambda e, nr=nr, R8=R8: e.tensor_copy(out=QA[:nr, :].rearrange("p (n t x) -> p n t x", n=4, t=2)[:, :, 0, :], in_=R8[:, :, 0, :]))
                S.op("dve", [self.TMPAR], [self.TMPBR], lambda e, nr=nr, R8=R8: e.tensor_copy(out=QB[:nr, :].rearrange("p (n t x) -> p n t x", n=4, t=2)[:, :, 1, :], in_=R8[:, :, 1, :]))
                S.op("dve", [self.TMPAR], [self.TMPBR], lambda e, nr=nr: e.tensor_copy(out=KB[:nr, :], in_=R32[:nr, 256:512]))
                S.op("dve", [pBr], [var], lambda e, pB=pB, nr=nr, kt=kt: e.tensor_copy(out=VA[:nr, kt, :, 0:64], in_=pB[:nr, 0:256].rearrange("p (h x) -> p h x", h=4)))
                self.k_transpose(KB, self.TMPBR, nr, KT, ktr, kt * 128)
                QT = self.TMPB[:, 768:1024].rearrange("p (a c) -> p a c", a=2)
                QTS = self.SCR[:, 4608:4608 + 256].bitcast(BF16).rearrange("p (v g c) -> p v g c", v=2, g=2)
                qtsr = getattr(self, "_qtsr", None) or Res("qts")
                self._qtsr = qtsr
                for v_, QX in enumerate((QA, QB)):
                    pt, pr = self.ps()
                    pv = pt[:, :].bitcast(BF16)

                    def fq(e, pv=pv, nr=nr, QX=QX):
                        ins = None
                        for g in range(2):
                            ins = e.transpose(out=pv[:, g * 128:g * 128 + nr], in_=QX[:nr, g * 128:(g + 1) * 128], identity=self.IDB[:nr, :nr])
                        return ins
                    S.op("pe", [self.TMPBR, self.CONSTR], [pr], fq)
                    S.op("act", [pr], [qtsr], lambda e, pv=pv, nr=nr, v_=v_: e.copy(out=QTS[:, v_, :, 0:nr], in_=pv[:, 0:256].rearrange("p (g c) -> p g c", g=2)[:, :, 0:nr]))
                Y32 = self.TMPA[:, 640:896]
                items = [(h, sub, kk) for h in range(4) for sub in range(2) for kk in range(kt + 1)]
                pSs, pYs = {}, {}

                def stageA(it, kt=kt, nr=nr):
                    h, sub, kk = it
                    n = 2 * h + sub
                    g = n // 4
                    base = ((n % 4) // 2) * 64
                    nk = 128 if kk < kt else nr
                    pS, pSr = self.ps()
                    pSs[it] = (pS, pSr)
                    S.op("pe", [ktr, qtsr], [pSr],
                         lambda e, pS=pS, nk=nk, kk=kk, base=base, g=g, nr=nr, sub=sub: e.matmul(pS[:nk, 0:nr], KT[base:base + 64, g, kk * 128:kk * 128 + nk], QTS[base:base + 64, sub, g, 0:nr], start=True, stop=True))

                def stageB(it, kt=kt, nr=nr):
                    h, sub, kk = it
                    nk = 128 if kk < kt else nr
                    pS, pSr = pSs.pop(it)
                    if kk == 0:
                        pYs[(h, sub)] = self.ps(hold=True)
                    pY, pYr = pYs[(h, sub)]
                    PT, ptr = self.pt_next()
                    S.op("act", [pSr], [ptr], lambda e, pS=pS, nk=nk, nr=nr, PT=PT: e.activation(out=PT[:nk, 0:nr], in_=pS[:nk, 0:nr], func=AF.Exp, scale=32.0 ** -0.5))
                    if kk == kt:
                        S.op("dve", [ptr, self.CONSTR], [ptr], lambda e, nk=nk, nr=nr, PT=PT: e.tensor_tensor(out=PT[:nk, 0:nr], in0=PT[:nk, 0:nr], in1=self.DMASK[:nk, 0:nr], op=ALU.mult))
                    S.op("pe", [ptr, var], [pYr],
                         lambda e, pY=pY, nk=nk, nr=nr, kk=kk, h=h, kt=kt, PT=PT: e.matmul(pY[:nr, 0:65], PT[:nk, 0:nr], VA[:nk, kk, h, :], start=(kk == 0), stop=(kk == kt)))
                    if kk == kt and sub == 1:
                        (p0, p0r), (p1, p1r) = pYs[(h, 0)], pYs[(h, 1)]
                        S.op("dve", [p0r], [self.SMR], lambda e, p0=p0, nr=nr: e.reciprocal(out=SM[:nr, 20:21], in_=p0[:nr, 64:65]))
                        S.op("dve", [p1r], [self.SMR], lambda e, p1=p1, nr=nr: e.reciprocal(out=SM[:nr, 21:22], in_=p1[:nr, 64:65]))
                        S.op("dve", [self.SMR, self.CONSTR], [self.SMR], lambda e, nr=nr: e.tensor_tensor(out=SM[:nr, 22:23], in0=SM[:nr, 21:22], in1=self.LAM[:nr, l, 1:2], op=ALU.mult))
                        T64 = self.TMPC[:, 256:320]
                        S.op("act", [p0r, self.SMR], [self.TMPCR], lambda e, p0=p0, nr=nr, T64=T64: e.activation(out=T64[:nr, :], in_=p0[:nr, 0:64], func=AF.Copy, scale=SM[:nr, 20:21]))
                        S.op("dve", [p1r, self.SMR, self.TMPCR], [self.TMPAR],
                             lambda e, p1=p1, nr=nr, h=h, T64=T64: e.scalar_tensor_tensor(out=Y32[:nr, h * 64:(h + 1) * 64], in0=p1[:nr, 0:64], scalar=SM[:nr, 22:23], in1=T64[:nr, :], op0=ALU.mult, op1=ALU.add))
                        self.ps_release(p0r)
                        self.ps_release(p1r)

                LOOK = 3
                for it in items[:LOOK]:
                    stageA(it)
                for i, it in enumerate(items):
                    if i + LOOK < len(items):
                        stageA(items[i + LOOK])
                    stageB(it)
                HB = self.TMPB[:, 0:256]
                gbc = self.SUBG[:nr, l, :].unsqueeze(1).to_broadcast([nr, 4, 64])
                self.group_norm(Y32, self.TMPAR, nr, 4, 64, False, None, HB, self.TMPBR, gbc, 1.0 - lam_init)
                self.to_ht(3, HB, self.TMPBR, nr, col0)


def _shard_inputs(cfg, ncores, inp):
    NP, NS = cfg.NP, cfg.NS
    hc = host_consts(cfg)
    f32 = lambda a: np.ascontiguousarray(np.asarray(a, dtype=np.float32))
    shared = {}
    shared["w_in"] = f32(inp["w_in"])
    shared["b_fox_f"] = f32(inp["b_fox_f"])
    wc = f32(inp["w_conv"])
    shared["wconvT"] = np.ascontiguousarray(wc.reshape(2, 31, 2, 128).transpose(0, 3, 2, 1))
    cp = np.stack([f32(inp["b_conv"]), f32(inp["conv_ln_g"]), f32(inp["conv_ln_b"])], axis=1)
    shared["convp"] = np.ascontiguousarray(cp.reshape(2, 3, 2, 128).transpose(0, 3, 1, 2))
    shared["diff_lambda"] = f32(inp["diff_lambda"]).reshape(2, 128)
    shared["diff_subln_g"] = f32(inp["diff_subln_g"])
    shared["w_branch"] = f32(inp["w_branch"])
    shared["w_out"] = f32(inp["w_out"])
    shared["w_ada"] = f32(inp["w_ada"])
    ba = f32(inp["b_ada"])
    shared["b_ada"] = ba
    shared["b_adaT"] = np.ascontiguousarray(ba.reshape(4, 24, 128).transpose(2, 0, 1))
    shared["ln_g"] = f32(inp["ln_g"])
    shared["ln_b"] = f32(inp["ln_b"])
    shared["w_ffn_in"] = f32(inp["w_ffn_in"])
    shared["w_ffn_out"] = f32(inp["w_ffn_out"])
    shared["w_router"] = np.ascontiguousarray(f32(inp["w_router"])[0].reshape(8, 128, 8).transpose(1, 0, 2))
    shared["b_router"] = f32(inp["b_router"])
    shared["w_exp_in"] = f32(inp["w_exp_in"])
    shared["w_exp_out"] = f32(inp["w_exp_out"])
    for k, v in hc.items():
        shared[k] = v
    maps = []
    for c in range(ncores):
        m = dict(shared)
        m["xp"] = f32(inp["x_prompt"][c * NP:(c + 1) * NP])
        m["xs"] = f32(inp["x_sample"][c * NS:(c + 1) * NS])
        call = np.concatenate([f32(inp["c_prompt"][c * NP:(c + 1) * NP]), f32(inp["c_sample"][c * NS:(c + 1) * NS])], axis=0)
        m["cT"] = np.ascontiguousarray(call.reshape(cfg.NSEQ, 8, 128).transpose(2, 1, 0))
        sl = slice(c * NS, (c + 1) * NS)
        m["cfk"] = f32(np.asarray(inp["cache_fox_k"])[:, sl]).reshape(2, NS, cfg.PAST, 256)
        m["cfv"] = f32(np.asarray(inp["cache_fox_v"])[:, sl]).reshape(2, NS, cfg.PAST, 256)
        m["cfl"] = f32(np.asarray(inp["cache_fox_logf"])[:, sl])
        m["cdk"] = f32(np.asarray(inp["cache_diff_k"])[:, sl]).reshape(2, NS, cfg.PAST, 256)
        m["cdv"] = f32(np.asarray(inp["cache_diff_v"])[:, sl]).reshape(2, NS, cfg.PAST, 256)
        m["sret"] = f32(np.asarray(inp["state_ret"])[:, sl])
        m["sconv"] = f32(np.asarray(inp["state_conv"])[:, sl])
        maps.append(m)
    return maps


def _gather(cfg, ncores, results):
    def cat(name, axis):
        return np.concatenate([np.asarray(r[name]) for r in results], axis=axis)
    B, T, TS = cfg.NP * ncores, cfg.T, cfg.TS
    BS = cfg.NS * ncores
    outs = [cat("yp", 0), cat("ys", 0)]
    for pre, nb, t in (("p", B, T), ("s", BS, TS)):
        outs += [cat(pre + "_fox_k", 1).reshape(2, nb, t, 4, 64), cat(pre + "_fox_v", 1).reshape(2, nb, t, 4, 64),
                 cat(pre + "_fox_logf", 1), cat(pre + "_diff_k", 1).reshape(2, nb, t, 8, 32),
                 cat(pre + "_diff_v", 1).reshape(2, nb, t, 4, 64), cat(pre + "_ret", 1), cat(pre + "_conv", 1)]
    return tuple(np.ascontiguousarray(o, dtype=np.float32) for o in outs)


def run(cfg, ncores, inp):
    b = Builder(cfg)
    nc = b.build()
    maps = _shard_inputs(cfg, ncores, inp)
    res = run_bass_kernel_spmd(nc, maps, core_ids=list(range(ncores)))
    return _gather(cfg, ncores, res.results)


def kernel(**inputs):
    ncores = 8
    B, T = inputs["x_prompt"].shape[0], inputs["x_prompt"].shape[1]
    BS, TS = inputs["x_sample"].shape[0], inputs["x_sample"].shape[1]
    PAST = inputs["cache_fox_k"].shape[2]
    cfg = Cfg(B // ncores, T, BS // ncores, TS, PAST)
    return run(cfg, ncores, inputs)
```

```python
import math
from contextlib import ExitStack
import numpy as np
import ml_dtypes
import concourse.bass as bass
import concourse.mybir as mybir
from concourse.bass_utils import run_bass_kernel_spmd

F32 = mybir.dt.float32
BF16 = mybir.dt.bfloat16
AF = mybir.ActivationFunctionType
ALU = mybir.AluOpType
AX = mybir.AxisListType

D = 1024
DEPTH = 2
BW = 256
HD = 64
CONV_W = 31
D_FF = 2816
N_EXP = 8
D_EXP = 3584
ALPHA = (2.0 * DEPTH) ** 0.25
EPS = 1e-5
OFF_RET = 0
OFF_FOX = 1024
OFF_FOX_F = 1792
OFF_CONV = 1796
OFF_DIFF = 2308
OFF_GATE = 3076
N_IN = 7172
SEM_CAP = 30000


class Res:
    __slots__ = ("w", "r", "sem", "cnt", "name", "uid", "excl")
    _n = 0

    def __init__(self, name="", excl=False):
        Res._n += 1
        self.uid = Res._n
        self.excl = excl
        self.w = None
        self.r = {}
        self.sem = None
        self.cnt = 0
        self.name = name


class Sched:
    def __init__(self, nc, es):
        self.nc = nc
        self.es = es
        self.names = ["pe", "act", "dve", "pool", "sp"]
        self.ops = {k: [] for k in self.names}
        self.count = {k: 0 for k in self.names}
        self.clock = {k: {} for k in self.names}
        self.esems = {k: [] for k in self.names}
        self.nsem = 0
        self.const_res = Res("const")
        self.out_events = {}

    def new_sem(self, nm):
        self.nsem += 1
        return self.es.enter_context(self.nc.semaphore("s_%s_%d" % (nm, self.nsem)))

    def esem(self, e, idx):
        while len(self.esems[e]) <= idx:
            self.esems[e].append(self.new_sem(e))
        return self.esems[e][idx]

    def _deps(self, e, reads, writes):
        deps = {}

        def add(key, val):
            if deps.get(key, 0) < val:
                deps[key] = val

        for r in reads:
            if r.w is not None:
                add(*r.w)
            if r.excl:
                for k, v in r.r.items():
                    if k != ("e", e):
                        add(k, v)
        for w in writes:
            if w.w is not None:
                add(*w.w)
            for k, v in w.r.items():
                add(k, v)
        clk = self.clock[e]
        waits = []
        for key, val in deps.items():
            if key[0] == "e" and key[1] == e and e == "pe":
                continue
            if clk.get(key, 0) >= val:
                continue
            clk[key] = val
            waits.append((key, val))
        return waits

    def _mark(self, ev, reads, writes):
        key, val = ev
        for r in reads:
            if r.r.get(key, 0) < val:
                r.r[key] = val
        for w in writes:
            w.w = ev
            w.r = {}

    def op(self, e, reads, writes, fn):
        waits = self._deps(e, reads, writes)
        self.count[e] += 1
        ev = (("e", e), self.count[e])
        self.ops[e].append((waits, fn, ("e", e, self.count[e])))
        self._mark(ev, reads, writes)

    def dma(self, e, out, in_, reads, writes, sres, is_out=False, **kw):
        waits = self._deps(e, reads, writes)
        if sres.sem is None:
            sres.sem = self.new_sem("d")
        sres.cnt += 16
        key = ("d", sres.uid)
        self.semmap[key] = sres.sem
        ev = (key, sres.cnt)

        def fn(eng, out=out, in_=in_, kw=kw):
            return eng.dma_start(out=out, in_=in_, **kw)

        self.ops[e].append((waits, fn, ("d", sres.sem, 16)))
        self._mark(ev, reads, writes)
        self.dma_latest[key] = sres.cnt
        if is_out:
            self.out_events[key] = sres.cnt

    semmap = {}
    dma_latest = {}

    def barrier(self):
        for e in self.names:
            waits = []
            for o in ["pe", "act", "dve", "pool"]:
                if o == e or self.count[o] == 0:
                    continue
                key = ("e", o)
                if self.clock[e].get(key, 0) < self.count[o]:
                    self.clock[e][key] = self.count[o]
                    waits.append((key, self.count[o]))
            for key, val in self.dma_latest.items():
                if self.clock[e].get(key, 0) < val:
                    self.clock[e][key] = val
                    waits.append((key, val))
            if waits:
                self.ops[e].append((waits, None, None))

    def finish(self):
        waits = [(k, v) for k, v in self.out_events.items()]
        self.ops["sp"].append((waits, None, None))

    def _emit_wait(self, eng, key, val):
        if key[0] == "e":
            idx = (val - 1) // SEM_CAP
            eng.wait_ge(self.esem(key[1], idx), val - idx * SEM_CAP)
        else:
            eng.wait_ge(self.semmap[key], val)

    def emit(self, block):
        engs = {"pe": block.tensor, "act": block.scalar, "dve": block.vector,
                "pool": block.gpsimd, "sp": block.sync}
        for name in self.names:
            ops = self.ops[name]

            def body(eng, ops=ops, name=name):
                for waits, fn, inc in ops:
                    for key, val in waits:
                        self._emit_wait(eng, key, val)
                    if fn is None:
                        continue
                    ins = fn(eng)
                    if inc[0] == "e":
                        cnt = inc[2]
                        idx = (cnt - 1) // SEM_CAP
                        ins.then_inc(self.esem(name, idx), 1)
                    else:
                        ins.then_inc(inc[1], 16)

            engs[name](body)


class StopBuild(Exception):
    pass


MAX_STAGE = [10 ** 9]
STOP_NAME = [None]


class Cfg:
    def __init__(self, NP, T, NS, TS, PAST):
        self.NP, self.T, self.NS, self.TS, self.PAST = NP, T, NS, TS, PAST
        self.NSEQ = NP + NS


def host_consts(cfg):
    c = {}
    T, TS, PAST = cfg.T, cfg.TS, cfg.PAST
    c["ident_f"] = np.eye(128, dtype=np.float32)
    c["ones_f"] = np.ones((128, 128), np.float32)
    idx = np.arange(128)
    c["utri_f"] = (idx[:, None] <= idx[None, :]).astype(np.float32)
    c["cmask"] = (idx[:, None] <= idx[None, :]).astype(np.float32)
    c["dmask"] = ((idx[:, None] // 64) <= (idx[None, :] // 64)).astype(np.float32)
    half = 32
    inv = np.exp(-math.log(10000.0) * np.arange(half, dtype=np.float32) / half).astype(np.float32)

    def ret_tab(pos):
        ang = pos.astype(np.float32)[:, None] * inv[None, :]
        cos = np.cos(ang).astype(np.float32)
        sin = np.sin(ang).astype(np.float32)
        tab = np.zeros((len(pos), 2, 8, 32), np.float32)
        tab[:, 0, 0:4] = cos[:, None, :]
        tab[:, 1, 0:4] = sin[:, None, :]
        tab[:, 0, 4:8] = cos[:, None, :] * 0.125
        tab[:, 1, 4:8] = sin[:, None, :] * 0.125
        return tab.reshape(len(pos), 2, 256)

    ntp = T // 64
    rt = np.zeros((ntp + 1, 64, 2, 256), np.float32)
    for i in range(ntp):
        rt[i] = ret_tab(np.arange(i * 64, (i + 1) * 64))
    rt[ntp, :TS] = ret_tab(PAST + np.arange(TS))
    c["ret_rope"] = rt
    inv_d = np.exp(-math.log(500000.0) * np.arange(4, dtype=np.float32) / 4).astype(np.float32)

    def diff_tab(pos):
        ang = pos.astype(np.float32)[:, None] * inv_d[None, :]
        cos = np.cos(ang).astype(np.float32)
        sin = np.sin(ang).astype(np.float32)
        tab = np.zeros((len(pos), 2, 16, 4), np.float32)
        tab[:, 0] = cos[:, None, :]
        tab[:, 1] = sin[:, None, :]
        return tab.reshape(len(pos), 2, 64)

    nt = T // 128
    dt_ = np.zeros((nt + 1, 128, 2, 64), np.float32)
    for i in range(nt):
        dt_[i] = diff_tab(np.arange(i * 128, (i + 1) * 128))
    dt_[nt, :TS] = diff_tab(PAST + np.arange(TS))
    c["diff_rope"] = dt_
    log_g = np.log1p(-np.exp2(-5.0 - np.arange(4, dtype=np.float32))).astype(np.float32)
    rd = np.zeros((2, 64, 2, 4, 64), np.float32)
    rs = np.zeros((2, 64, 2, 4), np.float32)
    for kind, L in ((0, min(T, 64)), (1, TS)):
        ii = np.arange(L, dtype=np.float32)
        inner = np.exp(log_g[:, None, None] * np.abs(ii[:, None] - ii[None, :])).astype(np.float32)
        qdec = np.exp(log_g[:, None] * (ii[None, :] + 1.0)).astype(np.float32)
        kdec = np.exp(log_g[:, None] * (L - 1.0 - ii[None, :])).astype(np.float32)
        cdec = np.exp(log_g * L).astype(np.float32)
        for h in range(4):
            rd[kind, :L, 0, h, :L] = inner[h].T
            rd[kind, :, 1, h, :L] = qdec[h][None, :]
            rs[kind, :L, 0, h] = kdec[h]
            rs[kind, :, 1, h] = cdec[h]
    c["ret_dec"] = rd.reshape(2, 64, 2, 256)
    c["ret_decs"] = rs
    return c


CONST_SHAPES = None


class Builder:
    def __init__(self, cfg, dbg=False):
        self.cfg = cfg
        self.dbg = dbg

    def build(self):
        cfg = self.cfg
        NP, T, NS, TS, PAST = cfg.NP, cfg.T, cfg.NS, cfg.TS, cfg.PAST
        NSEQ = cfg.NSEQ
        nc = bass.Bass("TRN2", target_bir_lowering=False)
        self.nc = nc
        self.es = ExitStack()
        es = self.es
        S = Sched(nc, es)
        Sched.semmap = {}
        Sched.dma_latest = {}
        self.S = S

        def din(name, shape, dt=F32):
            return nc.dram_tensor(name, list(shape), dt, kind="ExternalInput").ap()

        def dout(name, shape):
            return nc.dram_tensor(name, list(shape), F32, kind="ExternalOutput").ap()

        I = {}
        I["xp"] = din("xp", [NP, T, D])
        I["xs"] = din("xs", [NS, TS, D])
        I["cT"] = din("cT", [128, 8, NSEQ])
        for nm in ("cfk", "cfv", "cdk", "cdv"):
            I[nm] = din(nm, [2, NS, PAST, 256])
        I["cfl"] = din("cfl", [2, NS, PAST, 4])
        I["sret"] = din("sret", [2, NS, 4, 64, 64])
        I["sconv"] = din("sconv", [2, NS, 30, 256])
        I["w_in"] = din("w_in", [2, D, N_IN])
        I["b_fox_f"] = din("b_fox_f", [2, 4])
        I["wconvT"] = din("wconvT", [2, 128, 2, 31])
        I["convp"] = din("convp", [2, 128, 3, 2])
        I["diff_lambda"] = din("diff_lambda", [2, 128])
        I["diff_subln_g"] = din("diff_subln_g", [2, 64])
        I["w_branch"] = din("w_branch", [2, 4, 256, D])
        I["w_out"] = din("w_out", [2, D, D])
        I["w_ada"] = din("w_ada", [2, 2, D, 3 * D])
        I["b_adaT"] = din("b_adaT", [128, 4, 24])
        I["b_ada"] = din("b_ada", [2, 2, 3 * D])
        I["ln_g"] = din("ln_g", [2, 2, D])
        I["ln_b"] = din("ln_b", [2, 2, D])
        I["w_ffn_in"] = din("w_ffn_in", [1, D, 2 * D_FF])
        I["w_ffn_out"] = din("w_ffn_out", [1, D_FF, D])
        I["w_router"] = din("w_router", [128, 8, 8])
        I["b_router"] = din("b_router", [1, 8])
        I["w_exp_in"] = din("w_exp_in", [1, N_EXP, D, 2 * D_EXP])
        I["w_exp_out"] = din("w_exp_out", [1, N_EXP, D_EXP, D])
        hc = host_consts(cfg)
        for k, v in hc.items():
            I[k] = din(k, list(v.shape))
        self.I = I
        O = {}
        O["yp"] = dout("yp", [NP, T, D])
        O["ys"] = dout("ys", [NS, TS, D])
        for pre, n, t in (("p", NP, T), ("s", NS, TS)):
            O[pre + "_fox_k"] = dout(pre + "_fox_k", [2, n, t, 256])
            O[pre + "_fox_v"] = dout(pre + "_fox_v", [2, n, t, 256])
            O[pre + "_fox_logf"] = dout(pre + "_fox_logf", [2, n, t, 4])
            O[pre + "_diff_k"] = dout(pre + "_diff_k", [2, n, t, 256])
            O[pre + "_diff_v"] = dout(pre + "_diff_v", [2, n, t, 256])
            O[pre + "_ret"] = dout(pre + "_ret", [2, n, 4, 64, 64])
            O[pre + "_conv"] = dout(pre + "_conv", [2, n, 30, 256])
        self.O = O

        NT = T // 128
        self.NT = NT
        TT = max(NT, 2 * NS)
        NCOL = max(T, NS * TS)
        self.NCOL = NCOL
        NKT = max(NT, PAST // 128 + 1)
        self.NKT = NKT
        NK = NKT * 128

        def sb(name, shape, dt=F32):
            return es.enter_context(nc.sbuf_tensor(name, list(shape), dt))

        self.ACC = sb("ACC", [128, TT, D])
        self.ACCR = [Res("acc%d" % i) for i in range(TT)]
        self.UT = sb("UT", [128, 8, NCOL], BF16)
        self.UTR = Res("ut")
        self.HT = sb("HT", [128, 8, NCOL], BF16)
        self.HTR = [Res("ht%d" % i) for i in range(4)]
        self.NWS = 6
        self.WS = sb("WS", [128, self.NWS, 2048], BF16)
        self.WSR = [Res("ws%d" % i) for i in range(self.NWS)]
        self.wsi = 0
        self.SCR = sb("SCR", [128, 5120])
        self.MODT = sb("MODT", [128, 4, 16, NSEQ])
        self.MODR = Res("modt")
        self.BADA = sb("BADA", [128, 4, 24])
        self.SCT = sb("SCT", [128, 8, NSEQ], BF16)
        self.SCTR = Res("sct")
        self.SCREP = sb("SCREP", [128, 8, 128], BF16)
        self.SCREPR = Res("screp")
        self.GB0 = sb("GB0", [128, D])
        self.GBR = [Res("gb%d" % i) for i in range(max(NS, 1))]
        self.ROWA = sb("ROWA", [128, D])
        self.ROWAR = Res("rowa")
        self.ROWB = sb("ROWB", [128, D])
        self.ROWBR = Res("rowb")
        self.IDF = sb("IDF", [128, 128])
        self.IDB = sb("IDB", [128, 128], BF16)
        self.ONESF = sb("ONESF", [128, 128])
        self.UTRI = sb("UTRI", [128, 128])
        self.CMASK = sb("CMASK", [128, 128], BF16)
        self.DMASK = sb("DMASK", [128, 128], BF16)
        self.RDEC = sb("RDEC", [64, 2, 2, 256])
        self.RDECS = sb("RDECS", [64, 2, 2, 4])
        self.CONSTR = Res("consts")
        self.WCV = sb("WCV", [128, 2, 2, 31])
        self.CVP = sb("CVP", [128, 2, 3, 2])
        self.BFF = sb("BFF", [128, 2, 4])
        self.LAM = sb("LAM", [128, 2, 4])
        self.SUBG = sb("SUBG", [128, 2, 64])
        self.WRT = sb("WRT", [128, 8, 8])
        self.BRT = sb("BRT", [128, 8])
        self.STG = sb("STG", [128, 1, 512])
        self.STGR = [Res("stg0")]
        self.stgi = 0
        self.TMPA = sb("TMPA", [128, 1024])
        self.TMPAR = Res("tmpa")
        self.TMPB = sb("TMPB", [128, 1024], BF16)
        self.TMPBR = Res("tmpb")
        self.TMPC = sb("TMPC", [128, 512])
        self.TMPCR = Res("tmpc")
        self.EPSC = sb("EPSC", [128, 1])
        self.ONEC = sb("ONEC", [128, 1])
        self.SM = sb("SM", [128, 64])
        self.SMR = Res("sm")
        self.PTB = sb("PTB", [128, 4, 128], BF16)
        self.PTR = [Res("pt%d" % i) for i in range(4)]
        self.pti = 0
        self.PSB = []
        for i in range(8):
            t = es.enter_context(nc.psum_tensor("PS%d" % i, [128, 512], F32))
            self.PSB.append((t, Res("ps%d" % i, excl=True)))
        self.psi = 0

        self.stage = 0
        try:
            self.load_consts()
            self.stage_end("consts")
            self.ada_precompute()
            self.stage_end("ada")
            units = [("p", i) for i in range(NP)] + ([("s", 0)] if NS > 0 else [])
            for kind, i in units:
                self.run_unit(kind, i)
        except StopBuild:
            pass
        S.finish()
        with nc.Block() as block:
            S.emit(block)
        return nc

    def stage_end(self, name):
        self.stage += 1
        if self.stage >= MAX_STAGE[0] or name == STOP_NAME[0]:
            print("STOP after stage", self.stage, name)
            raise StopBuild()

    def ps(self, hold=False):
        held = self.__dict__.setdefault("ps_held", set())
        while self.psi in held:
            self.psi = (self.psi + 1) % 8
        i = self.psi
        self.psi = (self.psi + 1) % 8
        if hold:
            held.add(i)
        return self.PSB[i]

    def pt_next(self):
        i = self.pti
        self.pti = (i + 1) % 4
        return self.PTB[:, i, :], self.PTR[i]

    def ps_release(self, pr):
        for i, (t, r) in enumerate(self.PSB):
            if r is pr:
                self.ps_held.discard(i)

    def stg(self):
        return self.STG[:, 0, :], self.STGR[0]

    def load_w(self, src2d, nk, ncols, k0=0):
        i = self.wsi
        self.wsi = (self.wsi + 1) % self.NWS
        res = self.WSR[i]
        assert res.w is None or res.r, "WS slot %d reloaded before its consumers were emitted" % i
        view = self.WS[:, i, 0:nk * ncols].rearrange("p (k c) -> p k c", k=nk)
        src = src2d[k0 * 128:(k0 + nk) * 128, :].rearrange("(k p) c -> p k c", p=128)
        self.S.dma("pool", view, src, [], [res], res)
        return view, res

    def cdma(self, out, in_, **kw):
        self.S.dma("sp", out, in_, [], [self.CONSTR], self.CONSTR, **kw)

    def load_consts(self):
        I = self.I
        S = self.S
        self.cdma(self.IDF[:, :], I["ident_f"][:, :])
        self.cdma(self.ONESF[:, :], I["ones_f"][:, :])
        self.cdma(self.UTRI[:, :], I["utri_f"][:, :])
        self.cdma(self.RDEC[:, :, :, :], I["ret_dec"].rearrange("k p w c -> p k w c"))
        self.cdma(self.RDECS[:, :, :, :], I["ret_decs"].rearrange("k p w h -> p k w h"))
        self.cdma(self.WCV[:, :, :, :], I["wconvT"].rearrange("l p g w -> p l g w"))
        self.cdma(self.CVP[:, :, :, :], I["convp"].rearrange("l p a g -> p l a g"))
        self.cdma(self.BFF[:, :, :], I["b_fox_f"].rearrange("l h -> (l h)").partition_broadcast(128).rearrange("p (l h) -> p l h", l=2))
        self.DLAM = self.TMPA[:, 0:256].rearrange("p (l c) -> p l c", l=2)
        S.dma("sp", self.DLAM, I["diff_lambda"].rearrange("l c -> (l c)").partition_broadcast(128).rearrange("p (l c) -> p l c", l=2), [], [self.TMPAR], self.TMPAR)
        self.cdma(self.SUBG[:, :, :], I["diff_subln_g"].rearrange("l c -> (l c)").partition_broadcast(128).rearrange("p (l c) -> p l c", l=2))
        self.cdma(self.WRT[:, :, :], I["w_router"][:, :, :])
        self.cdma(self.BRT[:, :], I["b_router"].rearrange("a e -> (a e)").partition_broadcast(128))
        self.cdma(self.BADA[:, :, :], I["b_adaT"][:, :, :])
        self.CONSTP = Res("constp")
        S.dma("pool", self.CMASK[:, :], I["cmask"][:, :], [], [self.CONSTP], self.CONSTP)
        S.dma("pool", self.DMASK[:, :], I["dmask"][:, :], [], [self.CONSTP], self.CONSTP)
        S.dma("pool", self.IDB[:, :], I["ident_f"][:, :], [], [self.CONSTP], self.CONSTP)
        S.op("dve", [self.CONSTP], [self.CONSTR], lambda e: e.memset(self.EPSC[:, :], EPS))
        S.op("dve", [], [self.CONSTR], lambda e: e.memset(self.EPSC[:, :], EPS))
        S.op("dve", [], [self.CONSTR], lambda e: e.memset(self.ONEC[:, :], 1.0))
        C = self.CONSTR
        for l in range(2):
            lam_init = 0.8 - 0.6 * math.exp(-0.3 * l)
            DL, LAM, TM = self.DLAM, self.LAM, self.SM
            S.op("dve", [C, self.TMPAR], [self.SMR], lambda e, l=l: e.tensor_tensor(out=TM[:, 0:32], in0=DL[:, l, 0:32], in1=DL[:, l, 32:64], op=ALU.mult))
            S.op("dve", [self.SMR], [self.SMR], lambda e, l=l: e.tensor_reduce(out=TM[:, 32:33], in_=TM[:, 0:32], axis=AX.X, op=ALU.add))
            S.op("dve", [C, self.TMPAR], [self.SMR], lambda e, l=l: e.tensor_tensor(out=TM[:, 0:32], in0=DL[:, l, 64:96], in1=DL[:, l, 96:128], op=ALU.mult))
            S.op("dve", [self.SMR], [self.SMR], lambda e, l=l: e.tensor_reduce(out=TM[:, 33:34], in_=TM[:, 0:32], axis=AX.X, op=ALU.add))
            S.op("act", [self.SMR], [self.SMR], lambda e, l=l: e.activation(out=TM[:, 34:36], in_=TM[:, 32:34], func=AF.Exp))
            S.op("dve", [self.SMR], [self.SMR], lambda e, l=l: e.tensor_tensor(out=TM[:, 36:37], in0=TM[:, 34:35], in1=TM[:, 35:36], op=ALU.subtract))
            S.op("dve", [self.SMR], [C], lambda e, l=l, li=lam_init: e.tensor_scalar(out=LAM[:, l, 0:1], in0=TM[:, 36:37], scalar1=li, scalar2=None, op0=ALU.add))
            S.op("dve", [C], [C], lambda e, l=l: e.tensor_scalar(out=LAM[:, l, 1:2], in0=LAM[:, l, 0:1], scalar1=-1.0, scalar2=None, op0=ALU.mult))

    def ada_precompute(self):
        S, I = self.S, self.I
        NSEQ = self.cfg.NSEQ
        CT = self.TMPA[:, 0:8 * NSEQ].rearrange("p (k s) -> p k s", k=8)
        S.dma("sp", CT, I["cT"][:, :, :], [], [self.TMPAR], self.TMPAR)
        S.op("act", [self.TMPAR], [self.SCTR], lambda e: e.activation(out=self.SCT[:, :, :], in_=CT, func=AF.Silu))
        BT = self.BADA
        for l in range(2):
            for s in range(2):
                ls = l * 2 + s
                pt, pr = self.ps()
                for j in range(16):
                    if j % 2 == 0:
                        wv, wr = self.load_w(I["w_ada"][l, s][:, (j // 2) * 256:(j // 2 + 1) * 256], 8, 256)

                    def f(e, j=j, wv=wv, pt=pt):
                        ins = None
                        for kc in range(8):
                            ins = e.matmul(pt[:, j * NSEQ:(j + 1) * NSEQ], wv[:, kc, (j % 2) * 128:(j % 2 + 1) * 128],
                                           self.SCT[:, kc, :], start=(kc == 0), stop=(kc == 7))
                        return ins
                    S.op("pe", [wr, self.SCTR], [pr], f)
                    S.op("act", [pr, self.CONSTR], [self.MODR],
                         lambda e, j=j, ls=ls, pt=pt: e.activation(out=self.MODT[:, ls, j, :], in_=pt[:, j * NSEQ:(j + 1) * NSEQ],
                                                                   func=AF.Identity, bias=BT[:, ls, j:j + 1], scale=1.0))
                S.op("dve", [self.MODR], [self.MODR],
                     lambda e, ls=ls: e.tensor_scalar(out=self.MODT[:, ls, 8:16, :], in0=self.MODT[:, ls, 8:16, :], scalar1=1.0, scalar2=None, op0=ALU.add))

    def gate_rows(self, l, s, seqs, gbviews):
        S, I = self.S, self.I
        S.dma("sp", self.ROWA[:, :], I["b_ada"][l, s, 2 * D:3 * D].partition_broadcast(128), [], [self.ROWAR], self.ROWAR)
        for si, seqg in enumerate(seqs):
            gbv, gbr = gbviews[si]
            S.op("dve", [self.SCTR], [self.SCREPR],
                 lambda e, seqg=seqg: e.tensor_copy(out=self.SCREP[:, :, :], in_=self.SCT[:, :, seqg:seqg + 1].to_broadcast([128, 8, 128])))
            for cb in range(4):
                wv, wr = self.load_w(I["w_ada"][l, s][:, 2 * D + cb * 256:2 * D + (cb + 1) * 256], 8, 256)
                if cb % 2 == 0:
                    pt, pr = self.ps()

                def f(e, wv=wv, pt=pt, cb=cb):
                    ins = None
                    for kc in range(8):
                        ins = e.matmul(pt[:, (cb % 2) * 256:(cb % 2 + 1) * 256], self.SCREP[:, kc, :], wv[:, kc, :],
                                       start=(kc == 0), stop=(kc == 7))
                    return ins
                S.op("pe", [wr, self.SCREPR], [pr], f)
                if cb % 2 == 1:
                    c0 = (cb // 2) * 512
                    S.op("dve", [pr, self.ROWAR], [gbr],
                         lambda e, pt=pt, gbv=gbv, c0=c0: e.tensor_tensor(out=gbv[:, c0:c0 + 512], in0=pt[:, :], in1=self.ROWA[:, c0:c0 + 512], op=ALU.add))

    def run_unit(self, kind, ui):
        cfg, S, I, O = self.cfg, self.S, self.I, self.O
        if kind == "p":
            rows, ntl = 128, self.NT
            seqs = [ui]
            tiles = [(0, i) for i in range(ntl)]
            xsrc = lambda k: I["xp"][ui, k * 128:(k + 1) * 128, :]
            ydst = lambda k: O["yp"][ui, k * 128:(k + 1) * 128, :]
            gbviews = [(self.GB0, self.GBR[0])]
        else:
            rows, ntl = cfg.TS, cfg.NS
            seqs = [cfg.NP + j for j in range(cfg.NS)]
            tiles = [(j, 0) for j in range(ntl)]
            xsrc = lambda k: I["xs"][k, :, :]
            ydst = lambda k: O["ys"][k, :, :]
            gbviews = [(self.ACC[:, cfg.NS + j, :], self.ACCR[cfg.NS + j]) for j in range(cfg.NS)]
        u = dict(kind=kind, ui=ui, rows=rows, ntl=ntl, seqs=seqs, tiles=tiles, ncols=rows * ntl, gb=gbviews)
        self.u = u
        for k in range(ntl):
            S.dma("sp", self.ACC[:rows, k, :], xsrc(k), [], [self.ACCR[k]], self.ACCR[k])
        for l in range(2):
            self.make_uT(l, 0)
            self.stage_end("uT")
            self.gate_rows(l, 0, seqs, gbviews)
            self.stage_end("gate")
            self.mixers(l)
            self.merge_out(l)
            self.stage_end("merge")
            self.layer_norm(l, 0)
            self.stage_end("ln")
            self.make_uT(l, 1, router=(l == 1))
            self.gate_rows(l, 1, seqs, gbviews)
            if l == 0:
                self.ffn(I["w_ffn_in"][0], I["w_ffn_out"][0], D_FF, None)
            else:
                for ex in range(N_EXP):
                    self.ffn(I["w_exp_in"][0, ex], I["w_exp_out"][0, ex], D_EXP, ex)
            self.layer_norm(l, 1)
        for k in range(ntl):
            S.dma("sp", ydst(k), self.ACC[:rows, k, :], [self.ACCR[k]], [], self.ACCR[k], is_out=True)

    def make_uT(self, l, s, router=False):
        S, u = self.S, self.u
        rows = u["rows"]
        ls = l * 2 + s
        if router:
            self.COMB = self.SCR[:, 0:u["ntl"] * 8].rearrange("p (t e) -> p t e", e=8)
            self.COMBR = Res("comb")
        for k, (sl, ti) in enumerate(u["tiles"]):
            seqg = u["seqs"][sl]
            c0 = k * rows
            if router:
                lt, lr = self.ps()
            for half in range(2):
                pt, pr = self.ps()

                def f(e, pt=pt, k=k, half=half):
                    ins = None
                    for j in range(4):
                        c = half * 4 + j
                        ins = e.transpose(out=pt[:, j * 128:j * 128 + rows], in_=self.ACC[:rows, k, c * 128:(c + 1) * 128],
                                          identity=self.IDF[:rows, :rows])
                    return ins
                S.op("pe", [self.ACCR[k], self.CONSTR], [pr], f)
                for j in range(4):
                    c = half * 4 + j
                    if not router:
                        S.op("act", [pr, self.MODR], [self.UTR],
                             lambda e, pt=pt, j=j, c=c, c0=c0, seqg=seqg: e.activation(
                                 out=self.UT[:, c, c0:c0 + rows], in_=pt[:, j * 128:j * 128 + rows], func=AF.Identity,
                                 scale=self.MODT[:, ls, 8 + c, seqg:seqg + 1], bias=self.MODT[:, ls, c, seqg:seqg + 1]))
                    else:
                        S.op("act", [pr, self.MODR], [self.TMPAR],
                             lambda e, pt=pt, j=j, c=c, seqg=seqg: e.activation(
                                 out=self.TMPA[:, c * 128:c * 128 + rows], in_=pt[:, j * 128:j * 128 + rows], func=AF.Identity,
                                 scale=self.MODT[:, ls, 8 + c, seqg:seqg + 1], bias=self.MODT[:, ls, c, seqg:seqg + 1]))
                        S.op("dve", [self.TMPAR], [self.UTR],
                             lambda e, c=c, c0=c0: e.tensor_copy(out=self.UT[:, c, c0:c0 + rows], in_=self.TMPA[:, c * 128:c * 128 + rows]))
            if router:
                def fr(e, lt=lt):
                    ins = None
                    for c in range(8):
                        ins = e.matmul(lt[:rows, 0:8], self.TMPA[:, c * 128:c * 128 + rows], self.WRT[:, c, :], start=(c == 0), stop=(c == 7))
                    return ins
                S.op("pe", [self.TMPAR, self.CONSTR], [lr], fr)
                self.route(k, lt, lr)
            S.op("act", [self.ACCR[k]], [self.ACCR[k]], lambda e, k=k: e.mul(out=self.ACC[:rows, k, :], in_=self.ACC[:rows, k, :], mul=ALPHA))

    def route(self, k, lt, lr):
        S, u = self.S, self.u
        rows = u["rows"]
        SM, R = self.SM, self.SMR
        lg = SM[:rows, 0:8]
        S.op("dve", [lr, self.CONSTR], [R], lambda e: e.tensor_tensor(out=lg, in0=lt[:rows, 0:8], in1=self.BRT[:rows, :], op=ALU.add))
        S.op("dve", [R], [R], lambda e: e.tensor_reduce(out=SM[:rows, 8:9], in_=lg, axis=AX.X, op=ALU.max))
        S.op("dve", [R], [R], lambda e: e.tensor_scalar(out=SM[:rows, 16:24], in0=lg, scalar1=SM[:rows, 8:9], scalar2=None, op0=ALU.is_equal))
        S.op("dve", [R], [R], lambda e: e.scalar_tensor_tensor(out=SM[:rows, 24:32], in0=SM[:rows, 16:24], scalar=-1e30, in1=lg, op0=ALU.mult, op1=ALU.add))
        S.op("dve", [R], [R], lambda e: e.tensor_reduce(out=SM[:rows, 9:10], in_=SM[:rows, 24:32], axis=AX.X, op=ALU.max))
        S.op("dve", [R], [R], lambda e: e.tensor_scalar(out=SM[:rows, 32:40], in0=SM[:rows, 24:32], scalar1=SM[:rows, 9:10], scalar2=None, op0=ALU.is_equal))
        S.op("dve", [R], [R], lambda e: e.tensor_tensor(out=SM[:rows, 10:11], in0=SM[:rows, 9:10], in1=SM[:rows, 8:9], op=ALU.subtract))
        S.op("act", [R], [R], lambda e: e.activation(out=SM[:rows, 11:12], in_=SM[:rows, 10:11], func=AF.Exp))
        S.op("dve", [R], [R], lambda e: e.tensor_scalar(out=SM[:rows, 11:12], in0=SM[:rows, 11:12], scalar1=1.0, scalar2=None, op0=ALU.add))
        S.op("dve", [R], [R], lambda e: e.reciprocal(out=SM[:rows, 12:13], in_=SM[:rows, 11:12]))
        S.op("dve", [R], [R], lambda e: e.tensor_scalar(out=SM[:rows, 13:14], in0=SM[:rows, 12:13], scalar1=-1.0, scalar2=1.0, op0=ALU.mult, op1=ALU.add))
        S.op("dve", [R], [R], lambda e: e.tensor_scalar(out=SM[:rows, 16:24], in0=SM[:rows, 16:24], scalar1=SM[:rows, 12:13], scalar2=None, op0=ALU.mult))
        COMB = self.COMB
        S.op("dve", [R], [self.COMBR], lambda e, k=k: e.scalar_tensor_tensor(out=COMB[:rows, k, :], in0=SM[:rows, 32:40], scalar=SM[:rows, 13:14],
                                                                        in1=SM[:rows, 16:24], op0=ALU.mult, op1=ALU.add))

    def layer_norm(self, l, s):
        S, u, I = self.S, self.u, self.I
        rows = u["rows"]
        S.dma("sp", self.ROWA[:, :], I["ln_g"][l, s, :].partition_broadcast(128), [], [self.ROWAR], self.ROWAR)
        S.dma("sp", self.ROWB[:, :], I["ln_b"][l, s, :].partition_broadcast(128), [], [self.ROWBR], self.ROWBR)
        SM, R = self.SM, self.SMR
        for k in range(u["ntl"]):
            A = self.ACC[:rows, k, :]
            AR = self.ACCR[k]
            S.op("dve", [AR], [R], lambda e, A=A: e.tensor_reduce(out=SM[:rows, 0:1], in_=A, axis=AX.X, op=ALU.add))
            S.op("act", [AR], [self.TMPAR, R], lambda e, A=A: e.activation(out=self.TMPA[:rows, :], in_=A, func=AF.Square, accum_out=SM[:rows, 1:2]))
            S.op("dve", [R], [R], lambda e: e.tensor_scalar(out=SM[:rows, 2:3], in0=SM[:rows, 0:1], scalar1=1.0 / D, scalar2=None, op0=ALU.mult))
            S.op("dve", [R], [R], lambda e: e.tensor_tensor(out=SM[:rows, 3:4], in0=SM[:rows, 2:3], in1=SM[:rows, 2:3], op=ALU.mult))
            S.op("dve", [R], [R], lambda e: e.scalar_tensor_tensor(out=SM[:rows, 4:5], in0=SM[:rows, 1:2], scalar=1.0 / D, in1=SM[:rows, 3:4], op0=ALU.mult, op1=ALU.subtract))
            S.op("act", [R], [R], lambda e: e.activation(out=SM[:rows, 5:6], in_=SM[:rows, 4:5], func=AF.Sqrt, bias=self.EPSC[:rows, :], scale=1.0))
            S.op("dve", [R], [R], lambda e: e.reciprocal(out=SM[:rows, 5:6], in_=SM[:rows, 5:6]))
            S.op("dve", [R], [R], lambda e: e.scalar_tensor_tensor(out=SM[:rows, 6:7], in0=SM[:rows, 2:3], scalar=-1.0, in1=SM[:rows, 5:6], op0=ALU.mult, op1=ALU.mult))
            S.op("act", [AR, R], [AR], lambda e, A=A: e.activation(out=A, in_=A, func=AF.Identity, scale=SM[:rows, 5:6], bias=SM[:rows, 6:7]))
            S.op("dve", [AR, self.ROWAR], [AR], lambda e, A=A: e.tensor_tensor(out=A, in0=A, in1=self.ROWA[:rows, :], op=ALU.mult))
            S.op("dve", [AR, self.ROWBR], [AR], lambda e, A=A: e.tensor_tensor(out=A, in0=A, in1=self.ROWB[:rows, :], op=ALU.add))

    def ffn(self, w_up, w_dn, dff, ex):
        S, u = self.S, self.u
        rows, ntl, ncols = u["rows"], u["ntl"], u["ncols"]
        nch = dff // 128
        G = 4
        HG = self.HT[:, :, :].rearrange("p (b j) c -> p b j c", b=2)
        nblk = (ncols + 511) // 512
        fold = (u["kind"] == "p")
        for g0 in range(0, nch, G):
            gn = min(G, nch - g0)
            hb = self.hgi = (getattr(self, "hgi", -1) + 1) % 2
            hres = [self.HTR[2 * hb], self.HTR[2 * hb + 1]]
            ups, wds = [], []
            for jj in range(0, gn, 2):
                nj = min(2, gn - jj)
                c0 = (g0 + jj) * 128
                wa, war = self.load_w(w_up[:, c0:c0 + nj * 128], 8, nj * 128)
                wg, wgr = self.load_w(w_up[:, dff + c0:dff + c0 + nj * 128], 8, nj * 128)
                ups.append((wa, war, wg, wgr, nj, jj))
            for jj in range(0, gn, 2):
                nj = min(2, gn - jj)
                wd, wdr = self.load_w(w_dn, nj, 1024, k0=g0 + jj)
                wds.append((wd, wdr, nj))
            for (wa, war, wg, wgr, nj, jj) in ups:
                for j in range(nj):
                    for b in range(nblk):
                        n = min(512, ncols - b * 512)
                        pa, par = self.ps()
                        pg, pgr = self.ps()

                        def f(e, pa=pa, pg=pg, j=j, b=b, n=n, wa=wa, wg=wg):
                            ins = None
                            for kc in range(8):
                                ins = e.matmul(pa[:, 0:n], wa[:, kc, j * 128:(j + 1) * 128], self.UT[:, kc, b * 512:b * 512 + n], start=(kc == 0), stop=(kc == 7))
                            for kc in range(8):
                                ins = e.matmul(pg[:, 0:n], wg[:, kc, j * 128:(j + 1) * 128], self.UT[:, kc, b * 512:b * 512 + n], start=(kc == 0), stop=(kc == 7))
                            return ins
                        S.op("pe", [war, wgr, self.UTR], [par, pgr], f)
                        S.op("act", [par], [self.TMPCR], lambda e, pa=pa, n=n: e.activation(out=self.TMPC[:, 0:n], in_=pa[:, 0:n], func=AF.Silu))
                        S.op("dve", [pgr, self.TMPCR], hres,
                             lambda e, pg=pg, n=n, hb=hb, jx=jj + j, b=b: e.tensor_tensor(out=HG[:, hb, jx, b * 512:b * 512 + n], in0=pg[:, 0:n], in1=self.TMPC[:, 0:n], op=ALU.mult))
            if fold:
                gbv0, gbr0 = u["gb"][0]
                for (wd, wdr, nj) in wds:
                    S.op("dve", [wdr, gbr0], [wdr],
                         lambda e, wd=wd, nj=nj, gbv0=gbv0: e.tensor_tensor(out=wd, in0=wd, in1=gbv0[:, :].unsqueeze(1).to_broadcast([128, nj, 1024]), op=ALU.mult))
            for k, (sl, ti) in enumerate(u["tiles"]):
                gbv, gbr = u["gb"][sl]
                for cb in range(2):
                    po, por = self.ps()

                    def f2(e, po=po, k=k, cb=cb, hb=hb, wds=wds, gn=gn):
                        ins = None
                        idx = 0
                        for (wd, wdr, nj) in wds:
                            for j in range(nj):
                                ins = e.matmul(po[:rows, :], HG[:, hb, idx, k * rows:(k + 1) * rows], wd[:, j, cb * 512:(cb + 1) * 512], start=(idx == 0), stop=(idx == gn - 1))
                                idx += 1
                        return ins
                    S.op("pe", hres + [w[1] for w in wds], [por], f2)
                    self.accumulate(k, cb, po, por, gbv, gbr, ex, folded=fold)

    def accumulate(self, k, cb, po, por, gbv, gbr, ex, folded=False):
        S, u = self.S, self.u
        rows = u["rows"]
        A = self.ACC[:rows, k, cb * 512:(cb + 1) * 512]
        T_ = self.TMPA[:rows, 0:512]
        COMB = getattr(self, "COMB", None)
        if folded:
            if ex is None:
                S.op("dve", [por, self.ACCR[k]], [self.ACCR[k]], lambda e: e.tensor_tensor(out=A, in0=po[:rows, :], in1=A, op=ALU.add))
            else:
                S.op("dve", [por, self.COMBR, self.ACCR[k]], [self.ACCR[k]],
                     lambda e: e.scalar_tensor_tensor(out=A, in0=po[:rows, :], scalar=COMB[:rows, k, ex:ex + 1], in1=A, op0=ALU.mult, op1=ALU.add))
            return
        if ex is None:
            S.op("dve", [por, gbr], [self.TMPAR], lambda e: e.tensor_tensor(out=T_, in0=po[:rows, :], in1=gbv[:rows, cb * 512:(cb + 1) * 512], op=ALU.mult))
        else:
            S.op("dve", [por, gbr, self.COMBR], [self.TMPAR],
                 lambda e: e.scalar_tensor_tensor(out=T_, in0=po[:rows, :], scalar=COMB[:rows, k, ex:ex + 1], in1=gbv[:rows, cb * 512:(cb + 1) * 512],
                                                  op0=ALU.mult, op1=ALU.mult))
        S.op("dve", [self.TMPAR, self.ACCR[k]], [self.ACCR[k]], lambda e: e.tensor_tensor(out=A, in0=A, in1=T_, op=ALU.add))

    def merge_out(self, l):
        S, u, I = self.S, self.u, self.I
        rows, ntl, ncols = u["rows"], u["ntl"], u["ncols"]
        nblk = (ncols + 511) // 512
        S.barrier()
        NCOL = self.NCOL
        TACC = self.SCR[:, 0:NCOL]
        tar = Res("tacc")
        SG = self.SCR[:, NCOL:NCOL + NCOL // 2].bitcast(BF16)
        sgr = Res("sg")
        MC = self.SCR[:, NCOL + NCOL // 2:NCOL + NCOL // 2 + NCOL].bitcast(BF16).rearrange("p (b c) -> p b c", b=2)
        mcr = [Res("mc0"), Res("mc1")]
        for c in range(8):
            for n in range(4):
                wgv, wgr = self.load_w(I["w_in"][l][:, OFF_GATE + n * D + c * 128:OFF_GATE + n * D + (c + 1) * 128], 8, 128)
                wbv, wbr = self.load_w(I["w_branch"][l, n][:, c * 128:(c + 1) * 128], 2, 128)
                for b in range(nblk):
                    nn = min(512, ncols - b * 512)
                    pg, pgr = self.ps()
                    pb, pbr = self.ps()

                    def f(e, pg=pg, pb=pb, b=b, nn=nn, wgv=wgv, wbv=wbv, n=n):
                        ins = None
                        for kc in range(8):
                            ins = e.matmul(pg[:, 0:nn], wgv[:, kc, :], self.UT[:, kc, b * 512:b * 512 + nn], start=(kc == 0), stop=(kc == 7))
                        for kc in range(2):
                            ins = e.matmul(pb[:, 0:nn], wbv[:, kc, :], self.HT[:, n * 2 + kc, b * 512:b * 512 + nn], start=(kc == 0), stop=(kc == 1))
                        return ins
                    S.op("pe", [wgr, wbr, self.UTR, self.HTR[n]], [pgr, pbr], f)
                    S.op("act", [pgr], [sgr], lambda e, pg=pg, b=b, nn=nn: e.activation(out=SG[:, b * 512:b * 512 + nn], in_=pg[:, 0:nn], func=AF.Sigmoid))
                    sl_ = slice(b * 512, b * 512 + nn)
                    if n == 0:
                        S.op("dve", [pbr, sgr], [tar], lambda e, pb=pb, sl_=sl_, nn=nn: e.tensor_tensor(out=TACC[:, sl_], in0=pb[:, 0:nn], in1=SG[:, sl_], op=ALU.mult))
                    else:
                        S.op("dve", [pbr, sgr], [self.TMPCR], lambda e, pb=pb, sl_=sl_, nn=nn: e.tensor_tensor(out=self.TMPC[:, 0:nn], in0=pb[:, 0:nn], in1=SG[:, sl_], op=ALU.mult))
                        if n < 3:
                            S.op("dve", [self.TMPCR, tar], [tar], lambda e, sl_=sl_, nn=nn: e.tensor_tensor(out=TACC[:, sl_], in0=TACC[:, sl_], in1=self.TMPC[:, 0:nn], op=ALU.add))
                        else:
                            S.op("dve", [self.TMPCR, tar], [mcr[c % 2]],
                                 lambda e, sl_=sl_, nn=nn, c=c: e.tensor_tensor(out=MC[:, c % 2, sl_], in0=TACC[:, sl_], in1=self.TMPC[:, 0:nn], op=ALU.add))
            wo0, wo0r = self.load_w(I["w_out"][l][:, 0:512], 1, 512, k0=c)
            wo1, wo1r = self.load_w(I["w_out"][l][:, 512:1024], 1, 512, k0=c)
            fold = (u["kind"] == "p")
            if fold:
                gbv0, gbr0 = u["gb"][0]
                for cb_, (wo_, wor_) in enumerate(((wo0, wo0r), (wo1, wo1r))):
                    S.op("dve", [wor_, gbr0], [wor_],
                         lambda e, wo_=wo_, cb_=cb_, gbv0=gbv0: e.tensor_tensor(out=wo_[:, 0, :], in0=wo_[:, 0, :], in1=gbv0[:, cb_ * 512:(cb_ + 1) * 512], op=ALU.mult))
            for k, (sl, ti) in enumerate(u["tiles"]):
                gbv, gbr = u["gb"][sl]
                for cb, (wo, wor) in enumerate(((wo0, wo0r), (wo1, wo1r))):
                    po, por = self.ps()
                    S.op("pe", [mcr[c % 2], wor], [por],
                         lambda e, po=po, k=k, wo=wo, c=c: e.matmul(po[:rows, :], MC[:, c % 2, k * rows:(k + 1) * rows], wo[:, 0, :], start=True, stop=True))
                    self.accumulate(k, cb, po, por, gbv, gbr, None, folded=fold)
        S.barrier()

    def proj_tok(self, k0col, nrows, panels, hold=False):
        S = self.S
        banks = []
        for pi in range(0, len(panels), 2):
            pt, pr = self.ps(hold=hold)
            grp = panels[pi:pi + 2]

            def f(e, pt=pt, grp=grp):
                ins = None
                for gi, (wv, wr, n) in enumerate(grp):
                    for kc in range(8):
                        ins = e.matmul(pt[:nrows, gi * 256:gi * 256 + n], self.UT[:, kc, k0col:k0col + nrows], wv[:, kc, :], start=(kc == 0), stop=(kc == 7))
                return ins
            S.op("pe", [self.UTR] + [g[1] for g in grp], [pr], f)
            banks.append((pt, pr))
        return banks

    def load_panels(self, l, offs):
        I = self.I
        out = []
        for off, n in offs:
            wv, wr = self.load_w(I["w_in"][l][:, off:off + n], 8, n)
            out.append((wv, wr, n))
        return out

    def to_ht(self, mixer, HB, hbr, nrows, col0):
        S = self.S
        pt, pr = self.ps()
        pv = pt[:, :].bitcast(BF16)

        def f(e):
            ins = None
            for g in range(2):
                ins = e.transpose(out=pv[:, g * 128:g * 128 + nrows], in_=HB[:nrows, g * 128:(g + 1) * 128], identity=self.IDB[:nrows, :nrows])
            return ins
        S.op("pe", [hbr, self.CONSTR], [pr], f)
        S.op("act", [pr], [self.HTR[mixer]],
             lambda e: e.copy(out=self.HT[:, mixer * 2:mixer * 2 + 2, col0:col0 + nrows], in_=pv[:, 0:256].rearrange("p (g c) -> p g c", g=2)[:, :, 0:nrows]))

    def mixers(self, l):
        self.mix_ret(l)
        self.stage_end("ret")
        self.mix_fox(l)
        self.stage_end("fox")
        self.mix_conv(l)
        self.stage_end("conv")
        self.mix_diff(l)
        self.stage_end("diff")

    def mix_ret(self, l):
        S, u, I, O, cfg = self.S, self.u, self.I, self.O, self.cfg
        kind = u["kind"]
        pan = self.load_panels(l, [(OFF_RET + i * 256, 256) for i in range(4)])
        rk = 0 if kind == "p" else 1
        RD = self.RDEC
        S32 = self.SCR[0:64, 0:256]
        s32r = Res("s32")
        SBF = self.SCR[0:64, 256:384].bitcast(BF16)
        sbfr = Res("sbf")
        S.barrier()
        if kind == "p":
            seqlist = [(0, [(i, 64) for i in range(cfg.T // 64)])]
        else:
            seqlist = [(j, [(0, cfg.TS)]) for j in range(cfg.NS)]
        for sl, chunks in seqlist:
            seqg = u["seqs"][sl]
            if kind == "p":
                S.op("dve", [], [s32r], lambda e: e.memset(S32, 0.0))
            else:
                S.dma("sp", S32.rearrange("d (h e) -> d h e", h=4), I["sret"][l, sl].rearrange("h d e -> d h e"), [], [s32r], s32r)
            S.op("act", [s32r], [sbfr], lambda e: e.copy(out=SBF, in_=S32))
            for ci, cl in chunks:
                col0 = (ci * 64) if kind == "p" else sl * cfg.TS
                tab = ci if kind == "p" else cfg.T // 64
                (pA, pAr), (pB, pBr) = self.proj_tok(col0, cl, pan)
                RT = self.TMPA[0:64, 0:512].rearrange("p (a c) -> p a c", a=2)
                S.dma("sp", RT, I["ret_rope"][tab], [], [self.TMPAR], self.TMPAR)
                QK = self.TMPB[0:64, 0:512]
                A3 = pA[:cl, :].rearrange("p (h x) -> p h x", h=8)
                C3 = RT[:cl, 0, :].rearrange("p (h x) -> p h x", h=8)
                S3 = RT[:cl, 1, :].rearrange("p (h x) -> p h x", h=8)
                Q3 = QK[:cl, :].rearrange("p (h x) -> p h x", h=8)
                T1 = self.TMPC[:cl, 0:256].rearrange("p (h x) -> p h x", h=8)
                T2 = self.TMPC[:cl, 256:512].rearrange("p (h x) -> p h x", h=8)
                rd1 = [pAr, self.TMPAR]
                S.op("dve", rd1, [self.TMPCR], lambda e, A3=A3, C3=C3, T1=T1: e.tensor_tensor(out=T1, in0=A3[:, :, 0:32], in1=C3, op=ALU.mult))
                S.op("dve", rd1, [self.TMPCR], lambda e, A3=A3, S3=S3, T2=T2: e.tensor_tensor(out=T2, in0=A3[:, :, 32:64], in1=S3, op=ALU.mult))
                S.op("dve", [self.TMPCR], [self.TMPBR], lambda e, Q3=Q3, T1=T1, T2=T2: e.tensor_tensor(out=Q3[:, :, 0:32], in0=T1, in1=T2, op=ALU.subtract))
                S.op("dve", rd1, [self.TMPCR], lambda e, A3=A3, C3=C3, T1=T1: e.tensor_tensor(out=T1, in0=A3[:, :, 32:64], in1=C3, op=ALU.mult))
                S.op("dve", rd1, [self.TMPCR], lambda e, A3=A3, S3=S3, T2=T2: e.tensor_tensor(out=T2, in0=A3[:, :, 0:32], in1=S3, op=ALU.mult))
                S.op("dve", [self.TMPCR], [self.TMPBR], lambda e, Q3=Q3, T1=T1, T2=T2: e.tensor_tensor(out=Q3[:, :, 32:64], in0=T1, in1=T2, op=ALU.add))
                VB = self.TMPB[0:64, 512:768]
                KD = self.TMPB[0:64, 768:1024]
                SGt = self.TMPA[0:64, 512:768]
                S.op("act", [pBr], [self.TMPBR], lambda e, pB=pB, cl=cl: e.copy(out=VB[:cl, :], in_=pB[:cl, 0:256]))
                S.op("act", [pBr], [self.TMPAR], lambda e, pB=pB, cl=cl: e.activation(out=SGt[:cl, :], in_=pB[:cl, 256:512], func=AF.Silu))
                S.op("dve", [self.TMPBR, self.CONSTR], [self.TMPBR], lambda e, cl=cl: e.tensor_tensor(out=KD[:cl, :].rearrange("p (h x) -> p h x", h=4), in0=QK[:cl, 256:512].rearrange("p (h x) -> p h x", h=4),
                                                                                                        in1=self.RDECS[:cl, rk, 0, :].unsqueeze(2).to_broadcast([cl, 4, 64]), op=ALU.mult))
                pT, pTr = self.ps()
                pTv = pT[:, :].bitcast(BF16)

                def ft(e, pTv=pTv, cl=cl):
                    ins = None
                    for j in range(8):
                        ins = e.transpose(out=pTv[0:64, j * 64:j * 64 + cl], in_=QK[:cl, j * 64:(j + 1) * 64], identity=self.IDB[:cl, :cl])
                    return ins
                S.op("pe", [self.TMPBR, self.CONSTR], [pTr], ft)
                QKT = self.SCR[0:64, 384:640].bitcast(BF16).rearrange("p (j c) -> p j c", j=8)
                qktr = getattr(self, "_qktr", None) or Res("qkt")
                self._qktr = qktr
                QDT = self.SCR[0:64, 640:768].bitcast(BF16).rearrange("p (j c) -> p j c", j=4)
                S.op("act", [pTr], [qktr], lambda e, pTv=pTv, cl=cl: e.copy(out=QKT[:, :, 0:cl], in_=pTv[0:64, 0:512].rearrange("p (j c) -> p j c", j=8)[:, :, 0:cl]))
                S.op("dve", [qktr, self.CONSTR], [qktr],
                     lambda e, cl=cl: e.tensor_tensor(out=QDT[:, :, 0:cl], in0=QKT[:, 0:4, 0:cl], in1=RD[:, rk, 1, :].rearrange("p (h c) -> p h c", h=4)[:, :, 0:cl], op=ALU.mult))
                pS, pSr = self.ps()

                def fs(e, pS=pS, cl=cl):
                    ins = None
                    for h in range(4):
                        ins = e.matmul(pS[:cl, h * 64:h * 64 + cl], QKT[:, 4 + h, 0:cl], QKT[:, h, 0:cl], start=True, stop=True)
                    return ins
                S.op("pe", [qktr], [pSr], fs)
                STB = self.SCR[0:64, 768:896].bitcast(BF16).rearrange("p (h c) -> p h c", h=4)
                stbr = getattr(self, "_stbr", None) or Res("stb")
                self._stbr = stbr
                S.op("dve", [pSr, self.CONSTR], [stbr],
                     lambda e, pS=pS, cl=cl: e.tensor_tensor(out=STB[:cl, :, 0:cl], in0=pS[:cl, 0:256].rearrange("p (h c) -> p h c", h=4)[:, :, 0:cl],
                                                             in1=RD[:cl, rk, 0, :].rearrange("p (h c) -> p h c", h=4)[:, :, 0:cl], op=ALU.mult))
                pK, pKr = self.ps()

                def fk(e, pK=pK, cl=cl):
                    ins = None
                    for h in range(4):
                        ins = e.matmul(pK[0:64, h * 64:(h + 1) * 64], KD[:cl, h * 64:(h + 1) * 64], VB[:cl, h * 64:(h + 1) * 64], start=True, stop=True)
                    return ins
                S.op("pe", [self.TMPBR], [pKr], fk)
                pY, pYr = self.ps()

                def fy(e, pY=pY, cl=cl):
                    ins = None
                    for h in range(4):
                        e.matmul(pY[:cl, h * 64:(h + 1) * 64], STB[:cl, h, 0:cl], VB[:cl, h * 64:(h + 1) * 64], start=True, stop=False)
                        ins = e.matmul(pY[:cl, h * 64:(h + 1) * 64], QDT[:, h, 0:cl], SBF[:, h * 64:(h + 1) * 64], start=False, stop=True)
                    return ins
                S.op("pe", [stbr, self.TMPBR, qktr, sbfr], [pYr], fy)
                S.op("dve", [s32r, self.CONSTR], [s32r], lambda e: e.tensor_tensor(out=S32.rearrange("p (h x) -> p h x", h=4), in0=S32.rearrange("p (h x) -> p h x", h=4),
                                                                                      in1=self.RDECS[:, rk, 1, :].unsqueeze(2).to_broadcast([64, 4, 64]), op=ALU.mult))
                S.op("dve", [s32r, pKr], [s32r], lambda e, pK=pK: e.tensor_tensor(out=S32, in0=S32, in1=pK[0:64, 0:256], op=ALU.add))
                S.op("act", [s32r], [sbfr], lambda e: e.copy(out=SBF, in_=S32))
                Y32 = self.TMPA[0:64, 768:1024]
                S.op("act", [pYr], [self.TMPAR], lambda e, pY=pY, cl=cl: e.copy(out=Y32[:cl, :], in_=pY[:cl, 0:256]))
                HB = self.TMPB[0:64, 0:256]
                self.group_norm(Y32, self.TMPAR, cl, 4, 64, True, SGt, HB, self.TMPBR, None, 1.0)
                self.to_ht(0, HB, self.TMPBR, cl, col0)
            dst = O[("p" if kind == "p" else "s") + "_ret"][l, u["ui"] if kind == "p" else sl].rearrange("h d e -> d h e")
            S.dma("sp", dst, S32.rearrange("d (h e) -> d h e", h=4), [s32r], [], s32r, is_out=True)

    def group_norm(self, Y, yr, nrows, G, Dg, center, MUL, OUT, outr, gain_bc, const):
        S = self.S
        SM, R = self.SM, self.SMR
        Y3 = Y[:nrows, 0:G * Dg].rearrange("p (g d) -> p g d", g=G)
        SQ = self.TMPC[:nrows, 0:G * Dg]
        SQ3 = SQ.rearrange("p (g d) -> p g d", g=G)
        s1, s2, mean, var, rstd = (SM[:nrows, 40:40 + G], SM[:nrows, 44:44 + G], SM[:nrows, 48:48 + G], SM[:nrows, 52:52 + G], SM[:nrows, 56:56 + G])
        S.op("dve", [yr], [self.TMPCR], lambda e: e.tensor_tensor(out=SQ, in0=Y[:nrows, 0:G * Dg], in1=Y[:nrows, 0:G * Dg], op=ALU.mult))
        S.op("dve", [self.TMPCR], [R], lambda e: e.tensor_reduce(out=s2, in_=SQ3, axis=AX.X, op=ALU.add))
        if center:
            S.op("dve", [yr], [R], lambda e: e.tensor_reduce(out=s1, in_=Y3, axis=AX.X, op=ALU.add))
            S.op("dve", [R], [R], lambda e: e.tensor_scalar(out=mean, in0=s1, scalar1=1.0 / Dg, scalar2=None, op0=ALU.mult))
            S.op("dve", [R], [R], lambda e: e.tensor_tensor(out=s1, in0=mean, in1=mean, op=ALU.mult))
            S.op("dve", [R], [R], lambda e: e.scalar_tensor_tensor(out=var, in0=s2, scalar=1.0 / Dg, in1=s1, op0=ALU.mult, op1=ALU.subtract))
        else:
            S.op("dve", [R], [R], lambda e: e.tensor_scalar(out=var, in0=s2, scalar1=1.0 / Dg, scalar2=None, op0=ALU.mult))
        S.op("act", [R], [R], lambda e: e.activation(out=rstd, in_=var, func=AF.Sqrt, bias=self.EPSC[:nrows, :], scale=1.0))
        S.op("dve", [R], [R], lambda e: e.reciprocal(out=rstd, in_=rstd))
        if center:
            S.op("dve", [yr, R], [self.TMPCR], lambda e: e.tensor_tensor(out=SQ3, in0=Y3, in1=mean.unsqueeze(2).to_broadcast([nrows, G, Dg]), op=ALU.subtract))
            src, srcr = SQ3, self.TMPCR
        else:
            src, srcr = Y3, yr
        S.op("dve", [srcr, R], [self.TMPCR], lambda e: e.tensor_tensor(out=SQ3, in0=src, in1=rstd.unsqueeze(2).to_broadcast([nrows, G, Dg]), op=ALU.mult))
        O3 = OUT[:nrows, 0:G * Dg].rearrange("p (g d) -> p g d", g=G)
        if gain_bc is not None:
            S.op("dve", [self.TMPCR, self.CONSTR], [self.TMPCR], lambda e: e.tensor_tensor(out=SQ3, in0=SQ3, in1=gain_bc, op=ALU.mult))
        if MUL is not None:
            S.op("dve", [self.TMPCR, yr], [outr], lambda e: e.tensor_tensor(out=OUT[:nrows, 0:G * Dg], in0=SQ, in1=MUL[:nrows, 0:G * Dg], op=ALU.mult))
        else:
            S.op("dve", [self.TMPCR], [outr], lambda e: e.tensor_scalar(out=OUT[:nrows, 0:G * Dg], in0=SQ, scalar1=const, scalar2=None, op0=ALU.mult))

    def attn_buffers(self):
        NK = self.NKT * 128
        KT = self.SCR[:, 0:NK].bitcast(BF16).rearrange("p (g c) -> p g c", g=2)
        VA = self.SCR[:, NK:NK + self.NKT * 130].bitcast(BF16).rearrange("p (t h x) -> p t h x", t=self.NKT, h=4)
        base = NK + self.NKT * 130
        FC = self.SCR[:, base:base + self.NKT * 4].rearrange("p (t h) -> p t h", h=4)
        FT = self.SCR[:, base + self.NKT * 4:base + self.NKT * 8].rearrange("p (t h) -> p t h", h=4)
        LF = self.SCR[:, base + self.NKT * 8:base + self.NKT * 12].rearrange("p (t h) -> p t h", h=4)
        self.BI2 = [self.SCR[:, base + self.NKT * (12 + 4 * i):base + self.NKT * (16 + 4 * i)].rearrange("p (t h) -> p t h", h=4) for i in range(2)]
        self.QT2 = self.SCR[:, base + self.NKT * 20:base + self.NKT * 20 + 128].bitcast(BF16).rearrange("p (g c) -> p g c", g=2)
        assert base + self.NKT * 20 + 128 <= 5120, base + self.NKT * 20 + 128
        return KT, VA, FC, FT, LF

    def seq_iter(self):
        u, cfg = self.u, self.cfg
        if u["kind"] == "p":
            return [(0, u["ui"], [(i, i * 128, 128) for i in range(self.NT)], 0)]
        return [(j, j, [(j, 0, cfg.TS)], cfg.PAST // 128) for j in range(cfg.NS)]

    def k_transpose(self, KTOK, ktokr, nrows, KT, ktr, kcol0):
        S = self.S
        pt, pr = self.ps()
        pv = pt[:, :].bitcast(BF16)

        def f(e):
            ins = None
            for g in range(2):
                ins = e.transpose(out=pv[:, g * 128:g * 128 + nrows], in_=KTOK[:nrows, g * 128:(g + 1) * 128], identity=self.IDB[:nrows, :nrows])
            return ins
        S.op("pe", [ktokr, self.CONSTR], [pr], f)
        S.op("act", [pr], [ktr], lambda e: e.copy(out=KT[:, :, kcol0:kcol0 + nrows], in_=pv[:, 0:256].rearrange("p (g c) -> p g c", g=2)[:, :, 0:nrows]))

    def mix_fox(self, l):
        S, u, I, O, cfg = self.S, self.u, self.I, self.O, self.cfg
        kind = u["kind"]
        pre = "p" if kind == "p" else "s"
        pan = self.load_panels(l, [(OFF_FOX, 256), (OFF_FOX + 256, 256), (OFF_FOX + 512, 256), (OFF_FOX_F, 4)])
        S.barrier()
        KT, VA, FC, FT, LF = self.attn_buffers()
        ktr, var, fr = Res("kt"), Res("va"), Res("f")
        SM = self.SM
        for sl, so, newtiles, npast in self.seq_iter():
            S.op("dve", [], [var], lambda e: e.memset(VA[:, :, :, 64:65], 1.0))
            if npast:
                S.dma("sp", LF[:, 0:npast, :], I["cfl"][l, sl].rearrange("(t p) h -> p t h", p=128), [], [fr], fr)
            qkbr = getattr(self, "_qkbr", None) or Res("qkb")
            self._qkbr = qkbr
            for kt in range(npast):
                KTOK = self.TMPB[:, 0:256]
                S.dma("pool", KTOK, I["cfk"][l, sl, kt * 128:(kt + 1) * 128, :], [], [qkbr], qkbr)
                self.k_transpose(KTOK, qkbr, 128, KT, ktr, kt * 128)
                S.dma("pool", VA[:, kt, :, 0:64], I["cfv"][l, sl, kt * 128:(kt + 1) * 128, :].rearrange("t (h x) -> t h x", h=4), [], [var], var)
                self.fcum_step(kt, 128, FC, FT, LF, fr)
            HB = self.TMPB[:, 768:1024]
            hbr = getattr(self, "_hbr", None) or Res("hb")
            self._hbr = hbr
            qkbr = getattr(self, "_qkbr", None) or Res("qkb")
            self._qkbr = qkbr
            if not hasattr(self, "_qtr"):
                self._qtr = [Res("qt0"), Res("qt1")]
                self._bir = [Res("bi0"), Res("bi1")]
            QTs = [self.TMPB[:, 512:768].rearrange("p (g c) -> p g c", g=2), self.QT2]
            qtrs, birs, BIs = self._qtr, self._bir, self.BI2
            QKB = self.TMPB[:, 0:512]

            def prologue(qi):
                k, t0, nr = newtiles[qi]
                kt = npast + qi
                qb = kt % 2
                col0 = k * u["rows"]
                (pA, pAr), (pB, pBr) = self.proj_tok(col0, nr, pan, hold=True)
                yield
                st, str_ = self.stg()
                S.op("act", [pAr], [str_], lambda e: e.copy(out=st[:nr, 0:256], in_=pA[:nr, 256:512]))
                S.op("act", [pBr], [str_], lambda e: e.copy(out=st[:nr, 256:512], in_=pB[:nr, 0:256]))
                S.dma("sp", O[pre + "_fox_k"][l, so, t0:t0 + nr, :], st[:nr, 0:256], [str_], [], str_, is_out=True)
                S.dma("sp", O[pre + "_fox_v"][l, so, t0:t0 + nr, :], st[:nr, 256:512], [str_], [], str_, is_out=True)
                S.op("dve", [pAr], [qkbr], lambda e: e.tensor_scalar(out=QKB[:nr, :], in0=pA[:nr, :], scalar1=1.0, scalar2=None, op0=ALU.mult))
                S.op("dve", [pBr], [var], lambda e: e.tensor_copy(out=VA[:nr, kt, :, 0:64], in_=pB[:nr, 0:256].rearrange("p (h x) -> p h x", h=4)))
                S.op("dve", [pBr, self.CONSTR], [self.SMR], lambda e: e.tensor_tensor(out=SM[:nr, 0:4], in0=pB[:nr, 256:260], in1=self.BFF[:nr, l, :], op=ALU.add))
                self.ps_release(pAr)
                self.ps_release(pBr)
                yield
                self.k_transpose(QKB[:, 256:512], qkbr, nr, KT, ktr, kt * 128)
                QT = QTs[qb]
                pt, pr = self.ps()
                pv = pt[:, :].bitcast(BF16)

                def fq(e):
                    ins = None
                    for g in range(2):
                        ins = e.transpose(out=pv[:, g * 128:g * 128 + nr], in_=QKB[:nr, g * 128:(g + 1) * 128], identity=self.IDB[:nr, :nr])
                    return ins
                S.op("pe", [qkbr, self.CONSTR], [pr], fq)
                S.op("act", [pr], [qtrs[qb]], lambda e: e.copy(out=QT[:, :, 0:nr], in_=pv[:, 0:256].rearrange("p (g c) -> p g c", g=2)[:, :, 0:nr]))
                S.op("act", [self.SMR], [self.SMR], lambda e: e.activation(out=SM[:nr, 4:8], in_=SM[:nr, 0:4], func=AF.Exp, scale=-1.0))
                yield
                S.op("act", [self.SMR], [self.SMR], lambda e: e.activation(out=SM[:nr, 8:12], in_=SM[:nr, 4:8], func=AF.Ln, bias=self.ONEC[:nr, :], scale=1.0))
                S.op("dve", [self.SMR], [fr], lambda e: e.tensor_scalar(out=LF[:nr, kt, :], in0=SM[:nr, 8:12], scalar1=-1.0, scalar2=None, op0=ALU.mult))
                S.dma("sp", O[pre + "_fox_logf"][l, so, t0:t0 + nr, :], LF[:nr, kt, :], [fr], [], fr, is_out=True)
                yield
                self.fcum_step(kt, nr, FC, FT, LF, fr)
                yield
                BI = BIs[qb]
                S.op("dve", [fr], [birs[qb]], lambda e: e.tensor_tensor(out=BI[:, 0:kt + 1, :], in0=FT[:, kt:kt + 1, :].to_broadcast([128, kt + 1, 4]),
                                                                       in1=FC[:, 0:kt + 1, :], op=ALU.subtract))

            def attend(qi, bg):
                k, t0, nr = newtiles[qi]
                kt = npast + qi
                qb = kt % 2
                col0 = k * u["rows"]
                QT, qtr, BI, bir = QTs[qb], qtrs[qb], BIs[qb], birs[qb]
                items = [(h, kk) for h in range(4) for kk in range(kt + 1)]
                pSs, pYs = {}, {}

                def stageA(it):
                    h, kk = it
                    hp, g = (h % 2) * 64, h // 2
                    nk = 128 if kk < kt else nr
                    pS, pSr = self.ps(hold=True)
                    pSs[it] = (pS, pSr)
                    S.op("pe", [ktr, qtr], [pSr],
                         lambda e: e.matmul(pS[:nk, 0:nr], KT[hp:hp + 64, g, kk * 128:kk * 128 + nk], QT[hp:hp + 64, g, 0:nr], start=True, stop=True))

                def stageB(it):
                    h, kk = it
                    nk = 128 if kk < kt else nr
                    pS, pSr = pSs.pop(it)
                    if kk == 0:
                        pYs[h] = self.ps(hold=True)
                    pY, pYr = pYs[h]
                    PT, ptr = self.pt_next()
                    S.op("act", [pSr, bir], [ptr], lambda e: e.activation(out=PT[:nk, 0:nr], in_=pS[:nk, 0:nr], func=AF.Exp, bias=BI[:nk, kk, h:h + 1], scale=0.125))
                    self.ps_release(pSr)
                    if kk == kt:
                        S.op("dve", [ptr, self.CONSTR], [ptr], lambda e: e.tensor_tensor(out=PT[:nk, 0:nr], in0=PT[:nk, 0:nr], in1=self.CMASK[:nk, 0:nr], op=ALU.mult))
                    S.op("pe", [ptr, var], [pYr],
                         lambda e: e.matmul(pY[:nr, 0:65], PT[:nk, 0:nr], VA[:nk, kk, h, :], start=(kk == 0), stop=(kk == kt)))
                    if kk == kt:
                        S.op("dve", [pYr], [self.SMR], lambda e: e.reciprocal(out=SM[:nr, 20 + h:21 + h], in_=pY[:nr, 64:65]))
                        S.op("act", [pYr, self.SMR], [hbr], lambda e: e.activation(out=HB[:nr, h * 64:(h + 1) * 64], in_=pY[:nr, 0:64], func=AF.Copy, scale=SM[:nr, 20 + h:21 + h]))
                        self.ps_release(pYr)

                LOOK = 2
                step = max(1, len(items) // 8)
                for it in items[:LOOK]:
                    stageA(it)
                for i, it in enumerate(items):
                    if i + LOOK < len(items):
                        stageA(items[i + LOOK])
                    stageB(it)
                    if bg is not None and i % step == step - 1:
                        next(bg, None)
                if bg is not None:
                    for _ in bg:
                        pass
                self.to_ht(1, HB, hbr, nr, col0)

            for _ in prologue(0):
                pass
            for qi in range(len(newtiles)):
                attend(qi, prologue(qi + 1) if qi + 1 < len(newtiles) else None)

    def fcum_step(self, kt, nr, FC, FT, LF, fr):
        S = self.S
        pt, pr = self.ps()

        def f(e):
            e.matmul(pt[:, 0:4], self.ONESF[:nr, :], LF[:nr, kt, :], start=True, stop=True)
            return e.matmul(pt[:nr, 4:8], self.UTRI[:nr, :nr], LF[:nr, kt, :], start=True, stop=True)
        S.op("pe", [fr, self.CONSTR], [pr], f)
        if kt == 0:
            S.op("dve", [pr], [fr], lambda e: e.tensor_copy(out=FT[:, kt, :], in_=pt[:, 0:4]))
            S.op("dve", [pr], [fr], lambda e: e.tensor_copy(out=FC[:nr, kt, :], in_=pt[:nr, 4:8]))
        else:
            S.op("dve", [pr, fr], [fr], lambda e: e.tensor_tensor(out=FT[:, kt, :], in0=pt[:, 0:4], in1=FT[:, kt - 1, :], op=ALU.add))
            S.op("dve", [pr, fr], [fr], lambda e: e.tensor_tensor(out=FC[:nr, kt, :], in0=pt[:nr, 4:8], in1=FT[:nr, kt - 1, :], op=ALU.add))

    def mix_conv(self, l):
        S, u, I, O, cfg = self.S, self.u, self.I, self.O, self.cfg
        kind = u["kind"]
        pre = "p" if kind == "p" else "s"
        pan = self.load_panels(l, [(OFF_CONV, 256), (OFF_CONV + 256, 256)])
        S.barrier()
        XP = self.SCR[:, 0:2 * 158].rearrange("p (g c) -> p g c", g=2)
        xpr = Res("xp")
        CO = self.SCR[:, 320:320 + 256].rearrange("p (g c) -> p g c", g=2)
        cor = Res("co")
        CS = self.SCR[:, 576:576 + 256].rearrange("p (g c) -> p g c", g=2)
        csr = Res("cs")
        ST = self.SCR[:, 832:832 + 512]
        str2 = Res("cst")
        for sl, so, newtiles, npast in self.seq_iter():
            if kind == "p":
                S.op("dve", [], [xpr], lambda e: e.memset(XP[:, :, 0:30], 0.0))
            else:
                for g in range(2):
                    S.dma("sp", XP[:, g, 0:30], I["sconv"][l, sl][:, g * 128:(g + 1) * 128].rearrange("t c -> c t"), [], [xpr], xpr, allow_slow_non_contiguous=True)
                S.dma("sp", O["s_conv"][l, so, 0:30 - cfg.TS, :], I["sconv"][l, sl, cfg.TS:30, :], [], [], Res("d2d"), is_out=True)
            for qi, (k, t0, nr) in enumerate(newtiles):
                col0 = k * u["rows"]
                ((pA, pAr),) = self.proj_tok(col0, nr, pan)
                G32 = self.TMPA[:, 0:256]
                S.op("act", [pAr], [self.TMPAR], lambda e, pA=pA, nr=nr: e.activation(out=G32[:nr, :], in_=pA[:nr, 256:512], func=AF.Sigmoid))
                S.op("dve", [pAr, self.TMPAR], [self.TMPAR], lambda e, pA=pA, nr=nr: e.tensor_tensor(out=G32[:nr, :], in0=pA[:nr, 0:256], in1=G32[:nr, :], op=ALU.mult))
                if kind == "p":
                    lo = max(t0, cfg.T - 30)
                    if t0 + nr > lo:
                        S.dma("sp", O["p_conv"][l, so, lo - (cfg.T - 30):30, :], G32[lo - t0:nr, :], [self.TMPAR], [], self.TMPAR, is_out=True)
                else:
                    S.dma("sp", O["s_conv"][l, so, 30 - cfg.TS:30, :], G32[:nr, :], [self.TMPAR], [], self.TMPAR, is_out=True)
                pt, pr = self.ps()

                def f(e, pt=pt, nr=nr):
                    ins = None
                    for g in range(2):
                        ins = e.transpose(out=pt[:, g * 128:g * 128 + nr], in_=G32[:nr, g * 128:(g + 1) * 128], identity=self.IDF[:nr, :nr])
                    return ins
                S.op("pe", [self.TMPAR, self.CONSTR], [pr], f)
                if qi > 0:
                    pnr = newtiles[qi - 1][2]
                    S.op("dve", [xpr], [self.TMPCR], lambda e, pnr=pnr: e.tensor_copy(out=self.TMPC[:, 0:60].rearrange("p (g c) -> p g c", g=2), in_=XP[:, :, pnr:pnr + 30]))
                    S.op("dve", [self.TMPCR], [xpr], lambda e: e.tensor_copy(out=XP[:, :, 0:30], in_=self.TMPC[:, 0:60].rearrange("p (g c) -> p g c", g=2)))
                S.op("act", [pr], [xpr], lambda e, pt=pt, nr=nr: e.copy(out=XP[:, :, 30:30 + nr], in_=pt[:, 0:256].rearrange("p (g c) -> p g c", g=2)[:, :, 0:nr]))
                for g in range(2):
                    S.op("dve", [xpr, self.CONSTR], [cor],
                         lambda e, g=g, nr=nr: e.tensor_scalar(out=CO[:, g, 0:nr], in0=XP[:, g, 0:nr], scalar1=self.WCV[:, l, g, 0:1], scalar2=self.CVP[:, l, 0, g:g + 1], op0=ALU.mult, op1=ALU.add))
                    for w in range(1, CONV_W):
                        S.op("dve", [xpr, self.CONSTR, cor], [cor],
                             lambda e, g=g, nr=nr, w=w: e.scalar_tensor_tensor(out=CO[:, g, 0:nr], in0=XP[:, g, w:w + nr], scalar=self.WCV[:, l, g, w:w + 1], in1=CO[:, g, 0:nr],
                                                                                op0=ALU.mult, op1=ALU.add))
                S.op("dve", [cor], [csr], lambda e, nr=nr: e.tensor_tensor(out=CS[:, :, 0:nr], in0=CO[:, :, 0:nr], in1=CO[:, :, 0:nr], op=ALU.mult))
                ps1, ps1r = self.ps()

                def fs(e, ps1=ps1, nr=nr):
                    e.matmul(ps1[:, 0:nr], self.ONESF[:, :], CO[:, 0, 0:nr], start=True, stop=False)
                    e.matmul(ps1[:, 0:nr], self.ONESF[:, :], CO[:, 1, 0:nr], start=False, stop=True)
                    e.matmul(ps1[:, 128:128 + nr], self.ONESF[:, :], CS[:, 0, 0:nr], start=True, stop=False)
                    return e.matmul(ps1[:, 128:128 + nr], self.ONESF[:, :], CS[:, 1, 0:nr], start=False, stop=True)
                S.op("pe", [cor, csr, self.CONSTR], [ps1r], fs)
                MEAN, MSQ, VAR, RSTD = ST[:, 0:128], ST[:, 128:256], ST[:, 256:384], ST[:, 384:512]
                S.op("dve", [ps1r], [str2], lambda e, ps1=ps1, nr=nr: e.tensor_scalar(out=MEAN[:, 0:nr], in0=ps1[:, 0:nr], scalar1=1.0 / 256, scalar2=None, op0=ALU.mult))
                S.op("dve", [str2], [str2], lambda e, nr=nr: e.tensor_tensor(out=MSQ[:, 0:nr], in0=MEAN[:, 0:nr], in1=MEAN[:, 0:nr], op=ALU.mult))
                S.op("dve", [ps1r, str2], [str2], lambda e, ps1=ps1, nr=nr: e.scalar_tensor_tensor(out=VAR[:, 0:nr], in0=ps1[:, 128:128 + nr], scalar=1.0 / 256, in1=MSQ[:, 0:nr], op0=ALU.mult, op1=ALU.subtract))
                S.op("act", [str2], [str2], lambda e, nr=nr: e.activation(out=RSTD[:, 0:nr], in_=VAR[:, 0:nr], func=AF.Sqrt, bias=self.EPSC[:, :], scale=1.0))
                S.op("dve", [str2], [str2], lambda e, nr=nr: e.reciprocal(out=RSTD[:, 0:nr], in_=RSTD[:, 0:nr]))
                for g in range(2):
                    S.op("dve", [cor, str2], [csr], lambda e, g=g, nr=nr: e.tensor_tensor(out=CS[:, g, 0:nr], in0=CO[:, g, 0:nr], in1=MEAN[:, 0:nr], op=ALU.subtract))
                    S.op("dve", [csr, str2], [csr], lambda e, g=g, nr=nr: e.tensor_tensor(out=CS[:, g, 0:nr], in0=CS[:, g, 0:nr], in1=RSTD[:, 0:nr], op=ALU.mult))
                    S.op("act", [csr, self.CONSTR], [self.HTR[2]],
                         lambda e, g=g, nr=nr, col0=col0: e.activation(out=self.HT[:, 4 + g, col0:col0 + nr], in_=CS[:, g, 0:nr], func=AF.Silu,
                                                                      scale=self.CVP[:, l, 1, g:g + 1], bias=self.CVP[:, l, 2, g:g + 1]))

    def mix_diff(self, l):
        S, u, I, O, cfg = self.S, self.u, self.I, self.O, self.cfg
        kind = u["kind"]
        pre = "p" if kind == "p" else "s"
        lam_init = 0.8 - 0.6 * math.exp(-0.3 * l)
        pan = self.load_panels(l, [(OFF_DIFF, 256), (OFF_DIFF + 256, 256), (OFF_DIFF + 512, 256)])
        S.barrier()
        KT, VA, FC, FT, LF = self.attn_buffers()
        ktr, var = Res("kt"), Res("va")
        SM = self.SM
        for sl, so, newtiles, npast in self.seq_iter():
            S.op("dve", [], [var], lambda e: e.memset(VA[:, :, :, 64:65], 1.0))
            for kt in range(npast):
                KTOK = self.TMPB[:, 0:256]
                S.dma("pool", KTOK, I["cdk"][l, sl, kt * 128:(kt + 1) * 128, :], [], [self.TMPBR], self.TMPBR)
                self.k_transpose(KTOK, self.TMPBR, 128, KT, ktr, kt * 128)
                S.dma("pool", VA[:, kt, :, 0:64], I["cdv"][l, sl, kt * 128:(kt + 1) * 128, :].rearrange("t (h x) -> t h x", h=4), [], [var], var)
            if not hasattr(self, "_qtsr2"):
                self._qtsr2 = [Res("qts0"), Res("qts1")]
            qtsrs = self._qtsr2
            QTSs = [self.SCR[:, 4608 + 256 * i:4608 + 256 * (i + 1)].bitcast(BF16).rearrange("p (v g c) -> p v g c", v=2, g=2) for i in range(2)]
            RT = self.TMPA[:, 0:128].rearrange("p (a c) -> p a c", a=2)
            R32 = self.TMPA[:, 128:640]
            Y32 = self.TMPA[:, 640:896]
            QA = self.TMPB[:, 0:256]
            QB = self.TMPB[:, 256:512]
            KB = self.TMPB[:, 512:768]

            def prologue(qi):
                k, t0, nr = newtiles[qi]
                kt = npast + qi
                qb = kt % 2
                col0 = k * u["rows"]
                tab = (t0 // 128) if kind == "p" else self.NT
                (pA, pAr), (pB, pBr) = self.proj_tok(col0, nr, pan, hold=True)
                S.dma("sp", RT, I["diff_rope"][tab], [], [self.TMPAR], self.TMPAR)
                yield
                A3 = pA[:nr, :].rearrange("p (n x) -> p n x", n=16)
                R3 = R32[:nr, :].rearrange("p (n x) -> p n x", n=16)
                C3 = RT[:nr, 0, :].rearrange("p (n x) -> p n x", n=16)
                S3 = RT[:nr, 1, :].rearrange("p (n x) -> p n x", n=16)
                T1 = self.TMPC[:nr, 0:64].rearrange("p (n x) -> p n x", n=16)
                T2 = self.TMPC[:nr, 64:128].rearrange("p (n x) -> p n x", n=16)
                rd1 = [pAr, self.TMPAR]
                S.op("act", [pAr], [self.TMPAR], lambda e: e.copy(out=R32[:nr, :], in_=pA[:nr, :]))
                S.op("dve", rd1, [self.TMPCR], lambda e: e.tensor_tensor(out=T1, in0=A3[:, :, 0:4], in1=C3, op=ALU.mult))
                S.op("dve", rd1, [self.TMPCR], lambda e: e.tensor_tensor(out=T2, in0=A3[:, :, 4:8], in1=S3, op=ALU.mult))
                S.op("dve", [self.TMPCR, self.TMPAR], [self.TMPAR], lambda e: e.tensor_tensor(out=R3[:, :, 0:4], in0=T1, in1=T2, op=ALU.subtract))
                S.op("dve", rd1, [self.TMPCR], lambda e: e.tensor_tensor(out=T1, in0=A3[:, :, 4:8], in1=C3, op=ALU.mult))
                S.op("dve", rd1, [self.TMPCR], lambda e: e.tensor_tensor(out=T2, in0=A3[:, :, 0:4], in1=S3, op=ALU.mult))
                S.op("dve", [self.TMPCR, self.TMPAR], [self.TMPAR], lambda e: e.tensor_tensor(out=R3[:, :, 4:8], in0=T1, in1=T2, op=ALU.add))
                st, str_ = self.stg()
                S.op("act", [pBr], [str_], lambda e: e.copy(out=st[:nr, 0:256], in_=pB[:nr, 0:256]))
                S.dma("sp", O[pre + "_diff_k"][l, so, t0:t0 + nr, :], R32[:nr, 256:512], [self.TMPAR], [], self.TMPAR, is_out=True)
                S.dma("sp", O[pre + "_diff_v"][l, so, t0:t0 + nr, :], st[:nr, 0:256], [str_], [], str_, is_out=True)
                S.op("dve", [], [self.TMPBR], lambda e: e.memset(self.TMPB[:, 0:512], 0.0))
                R8 = R32[:nr, 0:256].rearrange("p (n t x) -> p n t x", n=4, t=2)
                S.op("dve", [self.TMPAR], [self.TMPBR], lambda e: e.tensor_copy(out=QA[:nr, :].rearrange("p (n t x) -> p n t x", n=4, t=2)[:, :, 0, :], in_=R8[:, :, 0, :]))
                S.op("dve", [self.TMPAR], [self.TMPBR], lambda e: e.tensor_copy(out=QB[:nr, :].rearrange("p (n t x) -> p n t x", n=4, t=2)[:, :, 1, :], in_=R8[:, :, 1, :]))
                S.op("dve", [self.TMPAR], [self.TMPBR], lambda e: e.tensor_copy(out=KB[:nr, :], in_=R32[:nr, 256:512]))
                S.op("dve", [pBr], [var], lambda e: e.tensor_copy(out=VA[:nr, kt, :, 0:64], in_=pB[:nr, 0:256].rearrange("p (h x) -> p h x", h=4)))
                self.ps_release(pAr)
                self.ps_release(pBr)
                yield
                self.k_transpose(KB, self.TMPBR, nr, KT, ktr, kt * 128)
                QTS, qtsr = QTSs[qb], qtsrs[qb]
                for v_, QX in enumerate((QA, QB)):
                    pt, pr = self.ps()
                    pv = pt[:, :].bitcast(BF16)

                    def fq(e, pv=pv, QX=QX):
                        ins = None
                        for g in range(2):
                            ins = e.transpose(out=pv[:, g * 128:g * 128 + nr], in_=QX[:nr, g * 128:(g + 1) * 128], identity=self.IDB[:nr, :nr])
                        return ins
                    S.op("pe", [self.TMPBR, self.CONSTR], [pr], fq)
                    S.op("act", [pr], [qtsr], lambda e, pv=pv, v_=v_: e.copy(out=QTS[:, v_, :, 0:nr], in_=pv[:, 0:256].rearrange("p (g c) -> p g c", g=2)[:, :, 0:nr]))

            def attend(qi, bg):
                k, t0, nr = newtiles[qi]
                kt = npast + qi
                qb = kt % 2
                col0 = k * u["rows"]
                QTS, qtsr = QTSs[qb], qtsrs[qb]
                items = [(h, sub, kk) for h in range(4) for sub in range(2) for kk in range(kt + 1)]
                pSs, pYs = {}, {}

                def stageA(it):
                    h, sub, kk = it
                    n = 2 * h + sub
                    g = n // 4
                    base = ((n % 4) // 2) * 64
                    nk = 128 if kk < kt else nr
                    pS, pSr = self.ps(hold=True)
                    pSs[it] = (pS, pSr)
                    S.op("pe", [ktr, qtsr], [pSr],
                         lambda e: e.matmul(pS[:nk, 0:nr], KT[base:base + 64, g, kk * 128:kk * 128 + nk], QTS[base:base + 64, sub, g, 0:nr], start=True, stop=True))

                def stageB(it):
                    h, sub, kk = it
                    nk = 128 if kk < kt else nr
                    pS, pSr = pSs.pop(it)
                    if kk == 0:
                        pYs[(h, sub)] = self.ps(hold=True)
                    pY, pYr = pYs[(h, sub)]
                    PT, ptr = self.pt_next()
                    S.op("act", [pSr], [ptr], lambda e: e.activation(out=PT[:nk, 0:nr], in_=pS[:nk, 0:nr], func=AF.Exp, scale=32.0 ** -0.5))
                    self.ps_release(pSr)
                    if kk == kt:
                        S.op("dve", [ptr, self.CONSTR], [ptr], lambda e: e.tensor_tensor(out=PT[:nk, 0:nr], in0=PT[:nk, 0:nr], in1=self.DMASK[:nk, 0:nr], op=ALU.mult))
                    S.op("pe", [ptr, var], [pYr],
                         lambda e: e.matmul(pY[:nr, 0:65], PT[:nk, 0:nr], VA[:nk, kk, h, :], start=(kk == 0), stop=(kk == kt)))
                    if kk == kt and sub == 1:
                        (p0, p0r), (p1, p1r) = pYs[(h, 0)], pYs[(h, 1)]
                        S.op("dve", [p0r], [self.SMR], lambda e: e.reciprocal(out=SM[:nr, 20:21], in_=p0[:nr, 64:65]))
                        S.op("dve", [p1r], [self.SMR], lambda e: e.reciprocal(out=SM[:nr, 21:22], in_=p1[:nr, 64:65]))
                        S.op("dve", [self.SMR, self.CONSTR], [self.SMR], lambda e: e.tensor_tensor(out=SM[:nr, 22:23], in0=SM[:nr, 21:22], in1=self.LAM[:nr, l, 1:2], op=ALU.mult))
                        T64 = self.TMPC[:, 256:320]
                        S.op("act", [p0r, self.SMR], [self.TMPCR], lambda e: e.activation(out=T64[:nr, :], in_=p0[:nr, 0:64], func=AF.Copy, scale=SM[:nr, 20:21]))
                        S.op("dve", [p1r, self.SMR, self.TMPCR], [self.TMPAR],
                             lambda e: e.scalar_tensor_tensor(out=Y32[:nr, h * 64:(h + 1) * 64], in0=p1[:nr, 0:64], scalar=SM[:nr, 22:23], in1=T64[:nr, :], op0=ALU.mult, op1=ALU.add))
                        self.ps_release(p0r)
                        self.ps_release(p1r)

                LOOK = 2
                step = max(1, len(items) // 6)
                for it in items[:LOOK]:
                    stageA(it)
                for i, it in enumerate(items):
                    if i + LOOK < len(items):
                        stageA(items[i + LOOK])
                    stageB(it)
                    if bg is not None and i % step == step - 1:
                        next(bg, None)
                if bg is not None:
                    for _ in bg:
                        pass
                HB = self.TMPB[:, 0:256]
                gbc = self.SUBG[:nr, l, :].unsqueeze(1).to_broadcast([nr, 4, 64])
                self.group_norm(Y32, self.TMPAR, nr, 4, 64, False, None, HB, self.TMPBR, gbc, 1.0 - lam_init)
                self.to_ht(3, HB, self.TMPBR, nr, col0)

            for _ in prologue(0):
                pass
            for qi in range(len(newtiles)):
                attend(qi, prologue(qi + 1) if qi + 1 < len(newtiles) else None)


def _shard_inputs(cfg, ncores, inp):
    NP, NS = cfg.NP, cfg.NS
    hc = host_consts(cfg)
    f32 = lambda a: np.ascontiguousarray(np.asarray(a, dtype=np.float32))
    shared = {}
    shared["w_in"] = f32(inp["w_in"])
    shared["b_fox_f"] = f32(inp["b_fox_f"])
    wc = f32(inp["w_conv"])
    shared["wconvT"] = np.ascontiguousarray(wc.reshape(2, 31, 2, 128).transpose(0, 3, 2, 1))
    cp = np.stack([f32(inp["b_conv"]), f32(inp["conv_ln_g"]), f32(inp["conv_ln_b"])], axis=1)
    shared["convp"] = np.ascontiguousarray(cp.reshape(2, 3, 2, 128).transpose(0, 3, 1, 2))
    shared["diff_lambda"] = f32(inp["diff_lambda"]).reshape(2, 128)
    shared["diff_subln_g"] = f32(inp["diff_subln_g"])
    shared["w_branch"] = f32(inp["w_branch"])
    shared["w_out"] = f32(inp["w_out"])
    shared["w_ada"] = f32(inp["w_ada"])
    ba = f32(inp["b_ada"])
    shared["b_ada"] = ba
    shared["b_adaT"] = np.ascontiguousarray(ba.reshape(4, 24, 128).transpose(2, 0, 1))
    shared["ln_g"] = f32(inp["ln_g"])
    shared["ln_b"] = f32(inp["ln_b"])
    shared["w_ffn_in"] = f32(inp["w_ffn_in"])
    shared["w_ffn_out"] = f32(inp["w_ffn_out"])
    shared["w_router"] = np.ascontiguousarray(f32(inp["w_router"])[0].reshape(8, 128, 8).transpose(1, 0, 2))
    shared["b_router"] = f32(inp["b_router"])
    shared["w_exp_in"] = f32(inp["w_exp_in"])
    shared["w_exp_out"] = f32(inp["w_exp_out"])
    for k, v in hc.items():
        shared[k] = v
    maps = []
    for c in range(ncores):
        m = dict(shared)
        m["xp"] = f32(inp["x_prompt"][c * NP:(c + 1) * NP])
        m["xs"] = f32(inp["x_sample"][c * NS:(c + 1) * NS])
        call = np.concatenate([f32(inp["c_prompt"][c * NP:(c + 1) * NP]), f32(inp["c_sample"][c * NS:(c + 1) * NS])], axis=0)
        m["cT"] = np.ascontiguousarray(call.reshape(cfg.NSEQ, 8, 128).transpose(2, 1, 0))
        sl = slice(c * NS, (c + 1) * NS)
        m["cfk"] = f32(np.asarray(inp["cache_fox_k"])[:, sl]).reshape(2, NS, cfg.PAST, 256)
        m["cfv"] = f32(np.asarray(inp["cache_fox_v"])[:, sl]).reshape(2, NS, cfg.PAST, 256)
        m["cfl"] = f32(np.asarray(inp["cache_fox_logf"])[:, sl])
        m["cdk"] = f32(np.asarray(inp["cache_diff_k"])[:, sl]).reshape(2, NS, cfg.PAST, 256)
        m["cdv"] = f32(np.asarray(inp["cache_diff_v"])[:, sl]).reshape(2, NS, cfg.PAST, 256)
        m["sret"] = f32(np.asarray(inp["state_ret"])[:, sl])
        m["sconv"] = f32(np.asarray(inp["state_conv"])[:, sl])
        maps.append(m)
    return maps


def _gather(cfg, ncores, results):
    def cat(name, axis):
        return np.concatenate([np.asarray(r[name]) for r in results], axis=axis)
    B, T, TS = cfg.NP * ncores, cfg.T, cfg.TS
    BS = cfg.NS * ncores
    outs = [cat("yp", 0), cat("ys", 0)]
    for pre, nb, t in (("p", B, T), ("s", BS, TS)):
        outs += [cat(pre + "_fox_k", 1).reshape(2, nb, t, 4, 64), cat(pre + "_fox_v", 1).reshape(2, nb, t, 4, 64),
                 cat(pre + "_fox_logf", 1), cat(pre + "_diff_k", 1).reshape(2, nb, t, 8, 32),
                 cat(pre + "_diff_v", 1).reshape(2, nb, t, 4, 64), cat(pre + "_ret", 1), cat(pre + "_conv", 1)]
    return tuple(np.ascontiguousarray(o, dtype=np.float32) for o in outs)


def run(cfg, ncores, inp):
    b = Builder(cfg)
    nc = b.build()
    maps = _shard_inputs(cfg, ncores, inp)
    res = run_bass_kernel_spmd(nc, maps, core_ids=list(range(ncores)))
    return _gather(cfg, ncores, res.results)


def kernel(**inputs):
    ncores = 8
    B, T = inputs["x_prompt"].shape[0], inputs["x_prompt"].shape[1]
    BS, TS = inputs["x_sample"].shape[0], inputs["x_sample"].shape[1]
    PAST = inputs["cache_fox_k"].shape[2]
    cfg = Cfg(B // ncores, T, BS // ncores, TS, PAST)
    return run(cfg, ncores, inputs)
```
